# Optimizing a Trainium2 kernel written in Bass

```python
import jax, jax.numpy as jnp
from jax import lax
import numpy as np

D_MODEL = 1024
BATCH = 2
SEQ = 8192
DEPTH = 1

GRID_W = 64
CTX_LEN = 256
M_HEADS = 4
M_DH = 128
M_W = M_HEADS * M_DH
M_CHUNK = 128
CONV_W = 3
N_GATES = 4 * M_HEADS
A_HEADS = 8
A_KV = 2
A_REP = A_HEADS // A_KV
A_DH = 64
A_W = A_HEADS * A_DH
A_KVW = A_KV * A_DH
Q_BLOCK = 128
ROPE_THETA = 10000.0
ATTN_SCALE = A_DH ** -0.5
MIX_W = M_W + A_W
G_END = 4 * M_W + N_GATES
PROJ_W = G_END + A_W + 2 * A_KVW
P_HEADS = 8
N_KEYS = 128
N_EXPERTS = N_KEYS * N_KEYS
P_QDIM = 256
P_HALF = P_QDIM // 2
P_TOPK = 16
P_BLOCK = 128
N_MOD = 6
DEEPNORM_ALPHA = (2.0 * DEPTH) ** 0.25
DEEPNORM_BETA = (8.0 * DEPTH) ** -0.25
LN_EPS = 1e-5
RMS_EPS = 1e-6

kernel_name = 'hybrid_mlstm_gqa_peer_dit_block'


def _layer_norm(x, g, b):
    xf = x.astype(jnp.float32)
    mu = jnp.mean(xf, axis=-1, keepdims=True)
    var = jnp.mean(jnp.square(xf - mu), axis=-1, keepdims=True)
    y = (xf - mu) * lax.rsqrt(var + LN_EPS) * g.astype(jnp.float32) + b.astype(jnp.float32)
    return y.astype(x.dtype)


def _rms_norm(x, w):
    xf = x.astype(jnp.float32)
    y = xf * lax.rsqrt(jnp.mean(jnp.square(xf), axis=-1, keepdims=True) + RMS_EPS) * w.astype(jnp.float32)
    return y.astype(x.dtype)


def _dwconv_centred(x, w, b):
    y = lax.conv_general_dilated(
        x, w[:, None, :].astype(x.dtype), window_strides=(1,),
        padding=[((CONV_W - 1) // 2, CONV_W // 2)],
        dimension_numbers=('NWC', 'WIO', 'NWC'), feature_group_count=x.shape[-1])
    return y + b.astype(x.dtype)


def _rope_axis(x, pos):
    half = x.shape[-1] // 2
    freq = ROPE_THETA ** (-jnp.arange(half, dtype=jnp.float32) / half)
    ang = pos[:, None] * freq[None, :]
    cos, sin = jnp.cos(ang), jnp.sin(ang)
    x1, x2 = x[..., :half], x[..., half:]
    return jnp.concatenate([x1 * cos - x2 * sin, x1 * sin + x2 * cos], axis=-1)


def _rope_2d(x, row, col):
    h = x.shape[-1] // 2
    y = jnp.concatenate([_rope_axis(x[..., :h], row), _rope_axis(x[..., h:], col)], axis=-1)
    return y.astype(x.dtype)


def _project(h, w_in, b_gates, conv_w, conv_b, qn_w, kn_w):
    B_, L_, _ = h.shape
    z = h @ w_in
    qk_m = jax.nn.silu(_dwconv_centred(z[..., :2 * M_W], conv_w, conv_b))

    def heads(t):
        return t.reshape(B_, L_, M_HEADS, M_DH).transpose(0, 2, 1, 3).astype(jnp.float32)

    q_m = heads(qk_m[..., :M_W]) * (M_DH ** -0.5)
    k_m = heads(qk_m[..., M_W:])
    v_m = heads(z[..., 2 * M_W:3 * M_W])
    o_m = z[..., 3 * M_W:4 * M_W]
    gates = (z[..., 4 * M_W:G_END].astype(jnp.float32) + b_gates.astype(jnp.float32))
    gates = gates.reshape(B_, L_, 4, M_HEADS).transpose(2, 0, 3, 1)
    a0 = G_END
    q_a = _rms_norm(z[..., a0:a0 + A_W].reshape(B_, L_, A_HEADS, A_DH), qn_w)
    q_a = q_a.transpose(0, 2, 1, 3).reshape(B_, A_KV, A_REP, L_, A_DH)
    k_a = _rms_norm(z[..., a0 + A_W:a0 + A_W + A_KVW].reshape(B_, L_, A_KV, A_DH), kn_w).transpose(0, 2, 1, 3)
    v_a = z[..., a0 + A_W + A_KVW:].reshape(B_, L_, A_KV, A_DH).transpose(0, 2, 1, 3)
    return q_m, k_m, v_m, o_m, gates, q_a, k_a, v_a


def _zero_state(b):
    return (jnp.zeros((b, M_HEADS, M_DH, M_DH), jnp.float32),
            jnp.zeros((b, M_HEADS, M_DH), jnp.float32),
            jnp.zeros((b, M_HEADS), jnp.float32))


def _mlstm_scan(q, k, v, li, lf, state):
    B_, H_, L_, DH_ = q.shape
    nc = L_ // M_CHUNK

    def chunks(t):
        return jnp.moveaxis(t.reshape((B_, H_, nc, M_CHUNK) + t.shape[3:]), 2, 0)

    mask = jnp.tril(jnp.ones((M_CHUNK, M_CHUNK), dtype=bool))

    def step(carry, inp):
        C, n, m = carry
        qc, kc, vc, lic, lfc = inp
        b = jnp.cumsum(lfc, axis=-1)
        logd = jnp.where(mask, b[..., :, None] - b[..., None, :] + lic[..., None, :], -jnp.inf)
        inter = b + m[..., None]
        m_s = jnp.maximum(inter, jnp.max(logd, axis=-1))
        dmat = jnp.exp(logd - m_s[..., None])
        s = jnp.einsum('bhsd,bhrd->bhsr', qc, kc) * dmat
        w_inter = jnp.exp(inter - m_s)
        num = jnp.einsum('bhsr,bhrd->bhsd', s, vc) + w_inter[..., None] * jnp.einsum('bhvk,bhsk->bhsv', C, qc)
        den = jnp.sum(s, axis=-1) + w_inter * jnp.einsum('bhk,bhsk->bhs', n, qc)
        h = num / jnp.maximum(jnp.abs(den), jnp.exp(-m_s))[..., None]
        log_w = b[..., -1:] - b + lic
        m_new = jnp.maximum(b[..., -1] + m, jnp.max(log_w, axis=-1))
        w = jnp.exp(log_w - m_new[..., None])
        decay = jnp.exp(b[..., -1] + m - m_new)
        C_new = decay[..., None, None] * C + jnp.einsum('bhr,bhrv,bhrk->bhvk', w, vc, kc)
        n_new = decay[..., None] * n + jnp.einsum('bhr,bhrk->bhk', w, kc)
        return (C_new, n_new, m_new), h

    state, hs = lax.scan(step, state, (chunks(q), chunks(k), chunks(v), chunks(li), chunks(lf)))
    h = jnp.moveaxis(hs, 0, 2).reshape(B_, H_, L_, DH_)
    return h, state


def _mlstm_bidir(q, k, v, gates, state_f, state_b):
    i_f, f_f, i_b, f_b = gates[0], gates[1], gates[2], gates[3]
    h_f, st_f = _mlstm_scan(q, k, v, i_f, jax.nn.log_sigmoid(f_f), state_f)

    def fl(t):
        return jnp.flip(t, axis=2)

    h_b, st_b = _mlstm_scan(fl(q), fl(k), fl(v), fl(i_b), fl(jax.nn.log_sigmoid(f_b)), state_b)
    return h_f + fl(h_b), st_f, st_b


def _mlstm_out(h, o, w):
    B_, H_, L_, DH_ = h.shape
    mu = jnp.mean(h, axis=-1, keepdims=True)
    var = jnp.mean(jnp.square(h - mu), axis=-1, keepdims=True)
    hn = (h - mu) * lax.rsqrt(var + LN_EPS) * w.astype(jnp.float32).reshape(M_HEADS, 1, M_DH)
    hn = hn.transpose(0, 2, 1, 3).reshape(B_, L_, M_W)
    return (jax.nn.sigmoid(o.astype(jnp.float32)) * hn).astype(o.dtype)


def _attn_latent(q, k_lat, v_lat, k_ctx, v_ctx):
    B_, G_, R_, L_, DH_ = q.shape
    k = jnp.concatenate([k_lat, k_ctx], axis=2)
    v = jnp.concatenate([v_lat, v_ctx], axis=2)
    nb = L_ // Q_BLOCK
    qb = jnp.moveaxis(q.reshape(B_, G_, R_, nb, Q_BLOCK, DH_), 3, 0)

    def block(qi):
        s = jnp.einsum('bgrqd,bgkd->bgrqk', qi, k) * ATTN_SCALE
        p = jax.nn.softmax(s.astype(jnp.float32), axis=-1).astype(v.dtype)
        return jnp.einsum('bgrqk,bgkd->bgrqd', p, v)

    o = lax.map(block, qb)
    return o.transpose(1, 0, 4, 2, 3, 5).reshape(B_, L_, A_W)


def _attn_context(q, k, v):
    B_, G_, R_, L_, DH_ = q.shape
    s = jnp.einsum('bgrqd,bgkd->bgrqk', q, k) * ATTN_SCALE
    p = jax.nn.softmax(s.astype(jnp.float32), axis=-1).astype(v.dtype)
    o = jnp.einsum('bgrqk,bgkd->bgrqd', p, v)
    return o.transpose(0, 3, 1, 2, 4).reshape(B_, L_, A_W)


def _peer(h, wq, keys, u, v):
    B_, L_, D_ = h.shape
    xb = h.reshape(-1, P_BLOCK, D_)

    def block(xt):
        q = (xt @ wq).reshape(P_BLOCK, P_HEADS, 2, P_HALF)
        s = jnp.einsum('nhpd,hpkd->nhpk', q, keys)
        s1, i1 = lax.top_k(s[:, :, 0], P_TOPK)
        s2, i2 = lax.top_k(s[:, :, 1], P_TOPK)
        cand_s = (s1[..., :, None] + s2[..., None, :]).reshape(P_BLOCK, P_HEADS, P_TOPK * P_TOPK)
        cand_i = (i1[..., :, None] * N_KEYS + i2[..., None, :]).reshape(P_BLOCK, P_HEADS, P_TOPK * P_TOPK)
        top_s, pos = lax.top_k(cand_s, P_TOPK)
        idx = jnp.take_along_axis(cand_i, pos, axis=-1).reshape(P_BLOCK, P_HEADS * P_TOPK)
        g = jax.nn.softmax(top_s.astype(jnp.float32), axis=-1).reshape(P_BLOCK, P_HEADS * P_TOPK).astype(xt.dtype)
        act = jax.nn.gelu(jnp.einsum('nd,ned->ne', xt, u[idx]), approximate=False)
        return jnp.einsum('ne,ned->nd', g * act, v[idx])

    return lax.map(block, xb).reshape(B_, L_, D_)


def setup_inputs(seed: int = 0) -> dict:
    key = jax.random.key(seed)
    ks = jax.random.split(key, 24)
    f32 = jnp.float32

    def nrm(k, shape, scale):
        return jax.random.normal(k, shape, f32) * scale

    f_bias = jnp.linspace(3.0, 6.0, M_HEADS, dtype=f32)
    gate_sel = jnp.array([0.0, 1.0, 0.0, 1.0], dtype=f32)
    b_gates = (nrm(ks[9], (DEPTH, 4, M_HEADS), 0.1) + gate_sel[None, :, None] * f_bias[None, None, :]).reshape(DEPTH, N_GATES)
    return {
        'x': nrm(ks[0], (BATCH, SEQ, D_MODEL), 1.0),
        'c': nrm(ks[1], (BATCH, D_MODEL), 1.0),
        'ctx': nrm(ks[2], (BATCH, CTX_LEN, D_MODEL), 1.0),
        'c_ctx': nrm(ks[3], (D_MODEL,), 1.0),
        'w_mod': nrm(ks[4], (DEPTH, D_MODEL, N_MOD * D_MODEL), 0.5 * D_MODEL ** -0.5),
        'b_mod': nrm(ks[5], (DEPTH, N_MOD * D_MODEL), 0.01),
        'w_in': nrm(ks[6], (DEPTH, D_MODEL, PROJ_W), D_MODEL ** -0.5),
        'conv_w': nrm(ks[7], (DEPTH, CONV_W, 2 * M_W), CONV_W ** -0.5),
        'conv_b': nrm(ks[8], (DEPTH, 2 * M_W), 0.01),
        'b_gates': b_gates,
        'mh_norm_w': 1.0 + nrm(ks[10], (DEPTH, M_W), 0.01),
        'q_norm_w': 1.0 + nrm(ks[11], (DEPTH, A_DH), 0.01),
        'k_norm_w': 1.0 + nrm(ks[12], (DEPTH, A_DH), 0.01),
        'w_out': nrm(ks[13], (DEPTH, MIX_W, D_MODEL), DEEPNORM_BETA * MIX_W ** -0.5),
        'ln1_g': 1.0 + nrm(ks[14], (DEPTH, D_MODEL), 0.01),
        'ln1_b': nrm(ks[15], (DEPTH, D_MODEL), 0.01),
        'peer_wq': nrm(ks[16], (DEPTH, D_MODEL, P_HEADS * P_QDIM), D_MODEL ** -0.5),
        'peer_keys': nrm(ks[17], (DEPTH, P_HEADS, 2, N_KEYS, P_HALF), P_HALF ** -0.5),
        'peer_u': nrm(ks[18], (DEPTH, N_EXPERTS, D_MODEL), D_MODEL ** -0.5),
        'peer_v': nrm(ks[19], (DEPTH, N_EXPERTS, D_MODEL), DEEPNORM_BETA),
        'ln2_g': 1.0 + nrm(ks[20], (DEPTH, D_MODEL), 0.01),
        'ln2_b': nrm(ks[21], (DEPTH, D_MODEL), 0.01),
    }


def reference(x, c, ctx, c_ctx, w_mod, b_mod, w_in, conv_w, conv_b, b_gates, mh_norm_w,
              q_norm_w, k_norm_w, w_out, ln1_g, ln1_b, peer_wq, peer_keys, peer_u, peer_v,
              ln2_g, ln2_b):
    n_tok = x.shape[1]
    ROWS = n_tok // GRID_W
    row = jnp.broadcast_to(jnp.arange(ROWS, dtype=jnp.float32)[:, None], (ROWS, GRID_W)).reshape(-1)
    col = jnp.broadcast_to(jnp.arange(GRID_W, dtype=jnp.float32)[None, :], (ROWS, GRID_W)).reshape(-1)
    for l in range(DEPTH):
        last = l == DEPTH - 1
        mod = jax.nn.silu(c) @ w_mod[l] + b_mod[l]
        mod_c = jax.nn.silu(c_ctx) @ w_mod[l] + b_mod[l]
        sh1, sc1, g1, sh2, sc2, g2 = [m[:, None, :] for m in jnp.split(mod, N_MOD, axis=-1)]
        csh1, csc1, cg1, csh2, csc2, cg2 = jnp.split(mod_c, N_MOD, axis=-1)
        proj_args = (w_in[l], b_gates[l], conv_w[l], conv_b[l], q_norm_w[l], k_norm_w[l])

        h_ctx = ctx * (1 + csc1) + csh1
        qm_c, km_c, vm_c, om_c, gt_c, qa_c, ka_c, va_c = _project(h_ctx, *proj_args)
        zero = _zero_state(ctx.shape[0])
        hm_c, st_f, st_b = _mlstm_bidir(qm_c, km_c, vm_c, gt_c, zero, zero)

        h_lat = x * (1 + sc1) + sh1
        qm, km, vm, om, gt, qa, ka, va = _project(h_lat, *proj_args)
        hm, _, _ = _mlstm_bidir(qm, km, vm, gt, st_f, st_b)
        qa = _rope_2d(qa, row, col)
        ka = _rope_2d(ka, row, col)
        att = _attn_latent(qa, ka, va, ka_c, va_c)
        mix = jnp.concatenate([_mlstm_out(hm, om, mh_norm_w[l]), att], axis=-1) @ w_out[l]
        x = _layer_norm(DEEPNORM_ALPHA * x + g1 * mix, ln1_g[l], ln1_b[l])
        y = _peer(x * (1 + sc2) + sh2, peer_wq[l], peer_keys[l], peer_u[l], peer_v[l])
        x = _layer_norm(DEEPNORM_ALPHA * x + g2 * y, ln2_g[l], ln2_b[l])

        if not last:
            att_c = _attn_context(qa_c, ka_c, va_c)
            mix_c = jnp.concatenate([_mlstm_out(hm_c, om_c, mh_norm_w[l]), att_c], axis=-1) @ w_out[l]
            ctx = _layer_norm(DEEPNORM_ALPHA * ctx + cg1 * mix_c, ln1_g[l], ln1_b[l])
            y_c = _peer((ctx * (1 + csc2) + csh2)[None], peer_wq[l], peer_keys[l], peer_u[l], peer_v[l])[0] if ctx.ndim == 2 else _peer(ctx * (1 + csc2) + csh2, peer_wq[l], peer_keys[l], peer_u[l], peer_v[l])
            ctx = _layer_norm(DEEPNORM_ALPHA * ctx + cg2 * y_c, ln2_g[l], ln2_b[l])
    return x
```

```python
import os
import types
import numpy as np
import concourse.bass as bass
import concourse.mybir as mybir
from concourse.bass_utils import run_bass_kernel_spmd

F32 = mybir.dt.float32
BF16 = mybir.dt.bfloat16
I32 = mybir.dt.int32
U32 = mybir.dt.uint32
ALU = mybir.AluOpType
AF = mybir.ActivationFunctionType
AX = mybir.AxisListType

NEG = -30000.0
LN_EPS = 1e-5
RMS_EPS = 1e-6
ALPHA = 2.0 ** 0.25
NGRP = 20
DBG = os.environ.get("KDBG", "")


class Buf:
    __slots__ = ("name", "lw", "rd")

    def __init__(self, name):
        self.name = name
        self.lw = None
        self.rd = []


class Op:
    __slots__ = ("eng", "fn", "reads", "writes", "dma", "deps", "signal", "sem", "val", "slot_prev", "bar")

    def __init__(self, eng, fn, reads, writes, dma):
        self.eng = eng
        self.fn = fn
        self.reads = reads
        self.writes = writes
        self.dma = dma
        self.deps = set()
        self.signal = False
        self.sem = None
        self.val = 0
        self.slot_prev = None
        self.bar = 0


def _freeze(fn):
    if fn is None or fn.__closure__ is None:
        return fn
    cells = []
    for c in fn.__closure__:
        try:
            cells.append(types.CellType(c.cell_contents))
        except ValueError:
            cells.append(c)
    return types.FunctionType(fn.__code__, fn.__globals__, fn.__name__, fn.__defaults__, tuple(cells))


class Sched:
    ENGS = ("pe", "act", "dve", "pool", "sp")

    def __init__(self, nc, n_dma_sems=32):
        self.nc = nc
        self.ops = []
        self.n_dma_sems = n_dma_sems
        self.nbar = 0
        self.bank = {}

    def op(self, eng, fn, reads=(), writes=(), dma=False):
        reads = [b for b in reads if b is not None]
        writes = [b for b in writes if b is not None]
        banks = set()
        for b in reads + writes:
            n = b.name
            if n.startswith("pso") and n[3:4].isdigit():
                banks.add(4 + int(n[3]))
            elif n.startswith("ps") and n[2:3].isdigit():
                banks.add(int(n[2]))
        for k in sorted(banks):
            if k not in self.bank:
                self.bank[k] = Buf("BANK%d" % k)
            writes.append(self.bank[k])
        o = Op(eng, _freeze(fn), reads, writes, dma)
        o.bar = self.nbar
        self.ops.append(o)
        return o

    def pe(self, fn, reads=(), writes=()):
        return self.op("pe", fn, reads, writes)

    def act(self, fn, reads=(), writes=()):
        return self.op("act", fn, reads, writes)

    def dve(self, fn, reads=(), writes=()):
        return self.op("dve", fn, reads, writes)

    def pool(self, fn, reads=(), writes=()):
        return self.op("pool", fn, reads, writes)

    def dma(self, fn, reads=(), writes=(), q="sp"):
        return self.op(q, fn, reads, writes, dma=True)

    def barrier(self):
        self.nbar += 1

    def _resolve(self):
        ops = self.ops
        last_eng = {}
        dma_all = []
        bar_deps = {}
        seen_bar = {e: 0 for e in self.ENGS}
        cur_bar = 0
        for i, o in enumerate(ops):
            if o.bar != cur_bar:
                cur_bar = o.bar
                bar_deps[cur_bar] = list(last_eng.values()) + list(dma_all)
            deps = set()
            raw = set()
            for b in o.reads:
                if b.lw is not None:
                    deps.add(b.lw)
                    raw.add(b.lw)
            for b in o.writes:
                if b.lw is not None:
                    deps.add(b.lw)
                for r in b.rd:
                    deps.add(r)
            for b in o.reads:
                b.rd.append(i)
            for b in o.writes:
                b.lw = i
                b.rd = []
            deps.discard(i)
            for j in deps:
                p = ops[j]
                if (not p.dma) and (not o.dma) and p.eng == o.eng:
                    if o.eng == "pe" or j not in raw:
                        continue
                o.deps.add(j)
                p.signal = True
            if seen_bar[o.eng] != cur_bar:
                seen_bar[o.eng] = cur_bar
                for j in bar_deps[cur_bar]:
                    p = ops[j]
                    if (not p.dma) and (not o.dma) and p.eng == o.eng:
                        continue
                    o.deps.add(j)
                    p.signal = True
            if o.dma:
                dma_all.append(i)
            elif o.fn is not None:
                last_eng[o.eng] = i

    def emit(self):
        nc = self.nc
        self._resolve()
        ops = self.ops
        esem = {e: nc.alloc_semaphore("s_" + e) for e in self.ENGS}
        dsem = [nc.alloc_semaphore("s_dma%d" % k) for k in range(self.n_dma_sems)]
        ecount = {e: 0 for e in self.ENGS}
        dcount = [0] * self.n_dma_sems
        dlast = [None] * self.n_dma_sems
        nd = 0
        for i, o in enumerate(ops):
            if o.dma:
                k = nd % self.n_dma_sems
                nd += 1
                o.signal = True
                o.sem = ("d", k)
                dcount[k] += 16
                o.val = dcount[k]
                o.slot_prev = dlast[k]
                dlast[k] = i
            elif o.signal:
                ecount[o.eng] += 1
                o.sem = ("e", o.eng)
                o.val = ecount[o.eng]
        self.stats = dict(ecount)
        self.stats["ndma"] = nd
        self.stats["nops"] = len(ops)

        def semh(s):
            return esem[s[1]] if s[0] == "e" else dsem[s[1]]

        per_eng = {e: [] for e in self.ENGS}
        for i, o in enumerate(ops):
            per_eng[o.eng].append(i)

        def emit_stream(ename, eng):
            waited = {}
            for i in per_eng[ename]:
                o = ops[i]
                need = {}
                for j in o.deps:
                    p = ops[j]
                    if need.get(p.sem, 0) < p.val:
                        need[p.sem] = p.val
                if o.dma and o.slot_prev is not None:
                    p = ops[o.slot_prev]
                    if need.get(p.sem, 0) < p.val:
                        need[p.sem] = p.val
                for s, v in need.items():
                    if waited.get(s, 0) >= v:
                        continue
                    eng.wait_ge(semh(s), v)
                    waited[s] = v
                if o.fn is None:
                    continue
                ins = o.fn(eng)
                if o.signal:
                    ins.then_inc(semh(o.sem), 16 if o.dma else 1)

        with nc.Block() as block:
            @block.tensor
            def _(e):
                emit_stream("pe", e)

            @block.scalar
            def _(e):
                emit_stream("act", e)

            @block.vector
            def _(e):
                emit_stream("dve", e)

            @block.gpsimd
            def _(e):
                emit_stream("pool", e)

            @block.sync
            def _(e):
                emit_stream("sp", e)


class Arena:
    def __init__(self, nc, nbytes):
        self.t = nc.alloc_sbuf_tensor("arena", [128, nbytes // 4], F32)
        self.off = 0
        self.cap = nbytes
        self.hi = 0

    def alloc(self, shape, dtype=F32):
        esz = 2 if dtype == BF16 else 4
        n = 1
        for s in shape[1:]:
            n *= s
        nb = (n * esz + 31) // 32 * 32
        o = self.off
        self.off += nb
        self.hi = max(self.hi, self.off)
        assert self.off <= self.cap, ("arena overflow", self.off, self.cap)
        v = self.t[:, o // 4:(o + nb) // 4]
        if dtype != F32:
            v = v.bitcast(dtype)
        v = v[:, 0:n]
        if len(shape) == 3:
            v = v.rearrange("p (a b) -> p a b", a=shape[1])
        elif len(shape) == 4:
            v = v.rearrange("p (a b c) -> p a b c", a=shape[1], b=shape[2])
        if shape[0] != 128:
            v = v[0:shape[0]]
        return v


class _Stop(Exception):
    pass


STOP = int(os.environ.get("KSTOP", "9"))
NCORES = int(os.environ.get("KCORES", "8"))


def build_program():
    st = {}
    try:
        _body(st)
    except _Stop:
        pass
    S = st["S"]
    B = st["B"]
    S.op("sp", None, reads=[B("out%d" % i) for i in range(16)] + [B("dbgo"), B("dbgo2"), B("dbgo3")] + [B("dbgx%d" % i) for i in range(16)] + [B("dbgy%d" % i) for i in range(16)])
    S.emit()
    print("sched stats", S.stats)
    return st["nc"], list(st["dbg_out"].keys())


def _body(st):
    nc = bass.Bass("TRN2", target_bir_lowering=False)
    S = Sched(nc)
    bufs = {}
    st["nc"] = nc
    st["S"] = S

    def B(name):
        b = bufs.get(name)
        if b is None:
            b = bufs[name] = Buf(name)
        return b

    def din(name, shape, dt=F32):
        return nc.dram_tensor(name, list(shape), dt, kind="ExternalInput").ap()

    d_cwin = din("cwin", [2, 128, 8, 258])
    d_xwin = din("xwin", [NGRP, 128, 8, 514])
    d_xown = din("xown", [2048, 1024])
    d_wmod = din("wmod", [12, 128, 8, 512])
    d_bmod = din("bmod", [12, 2, 512])
    d_cvec = din("cvec", [128, 16])
    d_win = din("win", [8, 128, 2832])
    d_ktaps = din("ktaps", [128, 22 * 12])
    d_qtaps = din("qtaps", [128, 2 * 12])
    d_cb = din("convb", [128, 8])
    d_flags = din("flags", [128, 44])
    d_gmask = din("gmask", [128, 48])
    d_bg = din("bg", [128, 16])
    d_mhw = din("mhw", [128, 512])
    d_nw = din("nw", [128, 2])
    d_wout = din("wout", [8, 128, 1024])
    d_wqT = din("wqT", [16, 128, 1024])
    d_keysT = din("keysT", [128, 16 * 128])
    d_lnp = din("lnp", [128, 4 * 1024])
    d_pu = din("pu", [16384, 1024])
    d_pv = din("pv", [16384, 1024])
    d_cst = din("cst", [128, 9 * 128])
    d_iota = din("iota16", [128, 256])
    d_rope = din("rope", [16, 2, 128, 512])
    d_out = nc.dram_tensor("out", [2048, 1024], F32, kind="ExternalOutput").ap()
    dbg_out = {}
    st["B"] = B
    st["dbg_out"] = dbg_out

    def dbg(name, shape, dt=F32):
        if name in DBG.split(","):
            dbg_out[name] = nc.dram_tensor("dbg_" + name, list(shape), dt, kind="ExternalOutput").ap()
            return dbg_out[name]
        return None

    AR = Arena(nc, 207 * 1024)
    PS = [nc.alloc_psum_tensor("ps%d" % i, [128, 512], F32) for i in range(8)]

    CST = AR.alloc([128, 9 * 128])
    ident = CST[:, 0:128]
    ones = CST[:, 128:256]
    tri = CST[:, 256:384]
    mask_sr = CST[:, 384:512]
    mask_rs = CST[:, 512:640]
    jmat = CST[:, 640:768]
    blk64 = CST[:, 768:896]
    rTm = CST[:, 896:1024]
    sel0 = CST[:, 1024:1152]
    CSTB = AR.alloc([128, 2 * 128], BF16)
    identb = CSTB[:, 0:128]
    jb = CSTB[:, 128:256]
    COLS = AR.alloc([128, 48, 2])
    SC1P = AR.alloc([128, 8, 2])
    MIX = AR.alloc([128, 16, 1024], BF16)
    MHW = AR.alloc([128, 512])
    KTAPS = AR.alloc([128, 22 * 12])
    QTAPS = AR.alloc([128, 24])
    CONVB = AR.alloc([128, 8])
    FLAGS = AR.alloc([128, 44])
    GMASK = AR.alloc([128, 48])
    BG = AR.alloc([128, 16])
    NW = AR.alloc([128, 2])
    MST = [AR.alloc([128, 4]) for _ in range(2)]
    CT = [AR.alloc([128, 4, 129]) for _ in range(2)]
    CTB = [AR.alloc([128, 4, 129], BF16) for _ in range(2)]
    SMALL = AR.alloc([128, 512])
    smo = [0]

    def small(n):
        o = smo[0]
        smo[0] += n
        assert smo[0] <= 512
        return SMALL[:, o:o + n]

    bC = B("cst")
    S.dma(lambda e: e.dma_start(out=CST, in_=d_cst), writes=[bC])
    S.act(lambda e: e.activation(out=identb, in_=ident, func=AF.Copy), reads=[bC], writes=[B("cstb")])
    S.act(lambda e: e.activation(out=jb, in_=jmat, func=AF.Copy), reads=[bC], writes=[B("cstb")])
    bCB = B("cstb")
    for (t, d, n) in ((MHW, d_mhw, "mhw"), (KTAPS, d_ktaps, "ktaps"), (QTAPS, d_qtaps, "qtaps"), (CONVB, d_cb, "convb"),
                      (FLAGS, d_flags, "flags"), (GMASK, d_gmask, "gmask"), (BG, d_bg, "bg"), (NW, d_nw, "nw")):
        S.dma(lambda e, t=t, d=d: e.dma_start(out=t, in_=d), writes=[B(n)], q="pool")
    for d in range(2):
        S.pool(lambda e, d=d: e.memset(MST[d], 0.0), writes=[B("m%d" % d)])
        S.pool(lambda e, d=d: e.memset(CT[d], 0.0), writes=[B("ct%d" % d)])
        S.pool(lambda e, d=d: e.memset(CTB[d], 0.0), writes=[B("ctb%d" % d)])

    mark_persist = AR.off

    CV = AR.alloc([128, 16])
    SIL = AR.alloc([128, 8, 2])
    WM = [AR.alloc([128, 8, 512]) for _ in range(2)]
    BM = [AR.alloc([2, 512]) for _ in range(2)]
    MROW = [AR.alloc([2, 512]) for _ in range(2)]
    S.dma(lambda e: e.dma_start(out=CV, in_=d_cvec), writes=[B("cv")])
    S.act(lambda e: e.activation(out=SIL.rearrange("p a b -> p (a b)"), in_=CV, func=AF.Silu), reads=[B("cv")], writes=[B("sil")])
    colps = PS[1][:, 0:96]
    for g in range(12):
        wm = WM[g % 2]
        bw = B("wm%d" % (g % 2))
        S.dma(lambda e, wm=wm, g=g: e.dma_start(out=wm, in_=d_wmod[g]), writes=[bw])
        S.dma(lambda e, g=g: e.dma_start(out=BM[g % 2], in_=d_bmod[g]), writes=[B("bm%d" % (g % 2))], q="pool")
        for kc in range(8):
            S.pe(lambda e, wm=wm, kc=kc: e.matmul(PS[0][0:2, :], lhsT=SIL[:, kc, :], rhs=wm[:, kc, :], start=(kc == 0), stop=(kc == 7)),
                 reads=[B("sil"), bw], writes=[B("ps0")])
        mr = MROW[g % 2]
        S.dve(lambda e, g=g, mr=mr: e.tensor_tensor(out=mr, in0=PS[0][0:2, :], in1=BM[g % 2], op=ALU.add),
              reads=[B("ps0"), B("bm%d" % (g % 2))], writes=[B("mrow%d" % (g % 2))])
        for fc in range(4):
            idx = g * 4 + fc
            S.pe(lambda e, mr=mr, fc=fc, idx=idx: e.matmul(colps[:, idx * 2:idx * 2 + 2], lhsT=mr[:, fc * 128:(fc + 1) * 128], rhs=ident[0:2, 0:2],
                                                           start=True, stop=True),
                 reads=[B("mrow%d" % (g % 2)), bC], writes=[B("ps1")])
    S.dve(lambda e: e.tensor_copy(out=COLS.rearrange("p a b -> p (a b)"), in_=colps), reads=[B("ps1")], writes=[B("cols")])
    S.dve(lambda e: e.tensor_scalar(out=SC1P, in0=COLS[:, 8:16, :], scalar1=1.0, scalar2=None, op0=ALU.add), reads=[B("cols")], writes=[B("sc1p")])
    dd = dbg("cols", [128, 96])
    if dd is not None:
        S.dma(lambda e, dd=dd: e.dma_start(out=dd, in_=COLS.rearrange("p a b -> p (a b)")), reads=[B("cols")], writes=[B("dbgo")])

    if STOP == 0:
        raise _Stop()
    AR.off = mark_persist
    S.barrier()

    WB = AR.alloc([128, 8, 2832], BF16)
    QAT = AR.alloc([128, 4, 2048], BF16)
    KAT = AR.alloc([128, 66 * 128], BF16)
    VA = AR.alloc([128, 66, 2, 65], BF16)
    XW = AR.alloc([128, 8, 514])
    HT = AR.alloc([128, 8, 514], BF16)
    KT = AR.alloc([128, 4, 512], BF16)
    QT = AR.alloc([128, 4, 512], BF16)
    ZB = [AR.alloc([128, 514]) for _ in range(2)]
    TB = [AR.alloc([128, 512]) for _ in range(2)]
    SQ = AR.alloc([128, 512])
    SD = AR.alloc([128, 512])
    KN = AR.alloc([128, 512])
    ROPE = AR.alloc([128, 2, 512])
    KTOK = AR.alloc([128, 4, 128], BF16)
    VAUG = AR.alloc([128, 4, 129], BF16)
    VW = [AR.alloc([128, 129], BF16) for _ in range(2)]
    DA = AR.alloc([128, 128])
    DM = AR.alloc([128, 128])
    DTT = AR.alloc([128, 128])
    PT = AR.alloc([128, 128], BF16)
    TT1 = AR.alloc([128, 129])
    TOT = AR.alloc([128, 129])
    HF = AR.alloc([128, 512], BF16)
    HN = AR.alloc([128, 512])
    OSG = AR.alloc([128, 512])
    GP = AR.alloc([128, 16])
    WST = [QAT.rearrange("p a b -> p (a b)").bitcast(F32)[:, 0:2832], KAT.bitcast(F32)[:, 0:2832]]
    print("phase1 arena bytes", AR.off)

    S.pool(lambda e: e.memset(VAUG[:, :, 128:129], 1.0), writes=[B("vaug")])
    S.pool(lambda e: e.memset(VA[:, :, :, 64:65], 1.0), writes=[B("va")])

    for kc in range(8):
        st = WST[kc % 2]
        bs = B("qat" if kc % 2 == 0 else "kat")
        S.dma(lambda e, st=st, kc=kc: e.dma_start(out=st, in_=d_win[kc]), writes=[bs])
        if kc % 2 == 0:
            S.act(lambda e, st=st, kc=kc: e.activation(out=WB[:, kc, :], in_=st, func=AF.Copy), reads=[bs], writes=[B("wb")])
        else:
            S.pool(lambda e, st=st, kc=kc: e.tensor_copy(out=WB[:, kc, :], in_=st), reads=[bs], writes=[B("wb")])

    C_KM, C_QM, C_KA, C_QA, C_VM, C_VA, C_G, C_O = 0, 512, 1024, 1152, 1664, 2176, 2304, 2320

    def mk_small():
        return dict(ef=small(4), nlf=small(4), li=small(4), a=small(4), amx=small(1), d4=small(4), ml=small(4), dm=small(4),
                    dec=small(4), aw=small(4), w=small(4), cm=small(1), mv=small(1), nmv=small(1), dwi=small(1), wi=small(1),
                    dn0=small(1), nrm=small(1), ad=small(1), dn=small(1), rd=small(1))
    SM = [mk_small(), mk_small()]
    S1 = small(4)
    S2 = small(4)
    MEAN = small(4)
    MSQ = small(4)
    VAR = small(4)
    RSTD = small(4)

    def fm_project(c0, N, halo, psb):
        for kc in range(8):
            S.pe(lambda e, kc=kc: e.matmul(PS[psb][:, 0:N], lhsT=WB[:, kc, c0:c0 + 128], rhs=HT[:, kc, 1:N + 1], start=(kc == 0), stop=(kc == 7)),
                 reads=[B("wb"), B("ht")], writes=[B("ps%d" % psb)])
        if halo:
            for kc in range(8):
                S.pe(lambda e, kc=kc: e.matmul(PS[2][:, 0:2], lhsT=WB[:, kc, c0:c0 + 128], rhs=HT[:, kc, 0:N + 2:N + 1], start=(kc == 0), stop=(kc == 7)),
                     reads=[B("wb"), B("ht")], writes=[B("ps2")])

    def conv_block(N, psb, zi, taps, cbias, fl, dst, qscale):
        Z = ZB[zi]
        T = TB[zi]
        bz = B("z%d" % zi)
        bt = B("t%d" % zi)
        S.act(lambda e: e.activation(out=Z[:, 1:N + 1], in_=PS[psb][:, 0:N], func=AF.Copy), reads=[B("ps%d" % psb)], writes=[bz])
        S.dve(lambda e: e.tensor_tensor(out=Z[:, 0:N + 2:N + 1], in0=PS[2][:, 0:2], in1=fl, op=ALU.mult), reads=[B("ps2"), B("flags")], writes=[bz])
        S.dve(lambda e: e.tensor_scalar(out=T[:, 0:N], in0=Z[:, 0:N], scalar1=taps[:, 0:1], scalar2=None, op0=ALU.mult),
              reads=[bz, B("ktaps"), B("qtaps")], writes=[bt])
        S.dve(lambda e: e.scalar_tensor_tensor(out=T[:, 0:N], in0=Z[:, 1:N + 1], scalar=taps[:, 1:2], in1=T[:, 0:N], op0=ALU.mult, op1=ALU.add),
              reads=[bz, bt], writes=[bt])
        S.dve(lambda e: e.scalar_tensor_tensor(out=T[:, 0:N], in0=Z[:, 2:N + 2], scalar=taps[:, 2:3], in1=T[:, 0:N], op0=ALU.mult, op1=ALU.add),
              reads=[bz, bt], writes=[bt])
        if qscale:
            S.act(lambda e: e.activation(out=T[:, 0:N], in_=T[:, 0:N], func=AF.Silu, bias=cbias), reads=[bt, B("convb")], writes=[bt])
            S.pool(lambda e: e.tensor_scalar(out=dst, in0=T[:, 0:N], scalar1=128.0 ** -0.5, scalar2=None, op0=ALU.mult), reads=[bt], writes=[B("qt")])
        else:
            S.act(lambda e: e.activation(out=dst, in_=T[:, 0:N], func=AF.Silu, bias=cbias), reads=[bt, B("convb")], writes=[B("kt")])

    def normrope(N, psb, nwcol, rope, dst, bdst):
        ps = PS[psb][:, 0:N]
        bp = B("ps%d" % psb)
        S.act(lambda e: e.activation(out=SQ[:, 0:N], in_=ps, func=AF.Square), reads=[bp], writes=[B("sq")])
        S.pe(lambda e: e.matmul(PS[6][:, 0:N], lhsT=blk64, rhs=SQ[:, 0:N], start=True, stop=True), reads=[bC, B("sq")], writes=[B("ps6")])
        S.act(lambda e: e.activation(out=SD[:, 0:N], in_=PS[6][:, 0:N], func=AF.Sqrt, scale=1.0 / 64, bias=RMS_EPS), reads=[B("ps6")], writes=[B("sd")])
        S.dve(lambda e: e.reciprocal(out=SD[:, 0:N], in_=SD[:, 0:N]), reads=[B("sd")], writes=[B("sd")])
        S.dve(lambda e: e.scalar_tensor_tensor(out=KN[:, 0:N], in0=ps, scalar=NW[:, nwcol:nwcol + 1], in1=SD[:, 0:N], op0=ALU.mult, op1=ALU.mult),
              reads=[bp, B("nw"), B("sd")], writes=[B("kn")])
        if rope:
            S.pe(lambda e: e.matmul(PS[5][:, 0:N], lhsT=rTm, rhs=KN[:, 0:N], start=True, stop=True), reads=[bC, B("kn")], writes=[B("ps5")])
            S.pool(lambda e: e.tensor_tensor(out=SQ[:, 0:N], in0=KN[:, 0:N], in1=ROPE[:, 0, 0:N], op=ALU.mult), reads=[B("kn"), B("rope")], writes=[B("sq")])
            S.dve(lambda e: e.tensor_tensor(out=KN[:, 0:N], in0=PS[5][:, 0:N], in1=ROPE[:, 1, 0:N], op=ALU.mult), reads=[B("ps5"), B("rope"), B("kn")], writes=[B("kn")])
            S.dve(lambda e: e.tensor_tensor(out=dst, in0=KN[:, 0:N], in1=SQ[:, 0:N], op=ALU.add), reads=[B("kn"), B("sq")], writes=[bdst])
        else:
            S.act(lambda e: e.activation(out=dst, in_=KN[:, 0:N], func=AF.Copy), reads=[B("kn")], writes=[bdst])

    def chunk_state(d, masked_g, t):
        sm = SM[d]
        ic = 0 if d == 0 else 8
        fc = ic + 4
        bn = lambda n: B("sm%d_%s" % (d, n))
        bm = B("m%d" % d)
        S.act(lambda e: e.activation(out=sm["ef"], in_=GP[:, fc:fc + 4], func=AF.Exp, scale=-1.0), reads=[B("gp")], writes=[bn("ef")])
        S.act(lambda e: e.activation(out=sm["nlf"], in_=sm["ef"], func=AF.Ln, bias=1.0), reads=[bn("ef")], writes=[bn("nlf")])
        if masked_g is not None:
            kcol = GMASK[:, masked_g * 4 + 2 * d:masked_g * 4 + 2 * d + 1]
            acol = GMASK[:, masked_g * 4 + 2 * d + 1:masked_g * 4 + 2 * d + 2]
            S.dve(lambda e: e.tensor_scalar(out=sm["nlf"], in0=sm["nlf"], scalar1=kcol, scalar2=None, op0=ALU.mult), reads=[bn("nlf"), B("gmask")], writes=[bn("nlf")])
            S.dve(lambda e: e.tensor_scalar(out=sm["li"], in0=GP[:, ic:ic + 4], scalar1=kcol, scalar2=acol, op0=ALU.mult, op1=ALU.add),
                  reads=[B("gp"), B("gmask")], writes=[bn("li")])
        else:
            S.dve(lambda e: e.tensor_copy(out=sm["li"], in_=GP[:, ic:ic + 4]), reads=[B("gp")], writes=[bn("li")])
        o = 16 + d * 32
        nbps = PS[2][:, o:o + 4]
        nBps = PS[2][:, o + 4:o + 8]
        amBps = PS[2][:, o + 8:o + 12]
        aTps = PS[2][0:4, 128 + d * 128:256 + d * 128]
        bps = B("ps2s%d" % d)
        S.pe(lambda e: e.matmul(nbps, lhsT=tri, rhs=sm["nlf"], start=True, stop=True), reads=[bC, bn("nlf")], writes=[bps])
        S.pe(lambda e: e.matmul(nBps, lhsT=ones, rhs=sm["nlf"], start=True, stop=True), reads=[bC, bn("nlf")], writes=[bps])
        S.dve(lambda e: e.tensor_tensor(out=sm["a"], in0=nbps, in1=sm["li"], op=ALU.add), reads=[bps, bn("li")], writes=[bn("a")])
        S.pe(lambda e: e.matmul(aTps, lhsT=sm["a"], rhs=ident, start=True, stop=True), reads=[bC, bn("a")], writes=[B("ps2t%d" % d)])
        S.dve(lambda e: e.tensor_reduce(out=sm["amx"][0:4], in_=aTps, axis=AX.X, op=ALU.max), reads=[B("ps2t%d" % d)], writes=[bn("amx")])
        S.dve(lambda e: e.tensor_scalar(out=sm["d4"][0:4], in0=ident[0:4, 0:4], scalar1=sm["amx"][0:4], scalar2=None, op0=ALU.mult),
              reads=[bC, bn("amx")], writes=[bn("d4")])
        S.pe(lambda e: e.matmul(amBps, lhsT=ones[0:4, :], rhs=sm["d4"][0:4], start=True, stop=True), reads=[bC, bn("d4")], writes=[bps])
        S.dve(lambda e: e.tensor_tensor(out=sm["ml"], in0=amBps, in1=MST[d], op=ALU.max), reads=[bps, bm], writes=[bn("ml")])
        S.dve(lambda e: e.tensor_tensor(out=sm["dm"], in0=MST[d], in1=sm["ml"], op=ALU.subtract), reads=[bm, bn("ml")], writes=[bn("dm")])
        S.act(lambda e: e.activation(out=sm["dec"], in_=sm["dm"], func=AF.Exp), reads=[bn("dm")], writes=[bn("dec")])
        S.dve(lambda e: e.tensor_tensor(out=sm["aw"], in0=sm["a"], in1=sm["ml"], op=ALU.subtract), reads=[bn("a"), bn("ml")], writes=[bn("aw")])
        S.act(lambda e: e.activation(out=sm["w"], in_=sm["aw"], func=AF.Exp), reads=[bn("aw")], writes=[bn("w")])
        return nbps, nBps, bps

    def chunk_update(d, nBps, bps):
        sm = SM[d]
        bn = lambda n: B("sm%d_%s" % (d, n))
        for h in range(4):
            vw = VW[h % 2]
            bvw = B("vw%d" % (h % 2))
            S.dve(lambda e, h=h, vw=vw: e.tensor_scalar(out=vw, in0=VAUG[:, h, :], scalar1=sm["w"][:, h:h + 1], scalar2=None, op0=ALU.mult),
                  reads=[B("vaug"), bn("w")], writes=[bvw])
            up = PS[7][:, 256:385]
            S.pe(lambda e, h=h, vw=vw, up=up: e.matmul(up, lhsT=KTOK[:, h, :], rhs=vw, start=True, stop=True), reads=[B("ktok"), bvw], writes=[B("ps7u")])
            S.dve(lambda e, h=h, up=up: e.scalar_tensor_tensor(out=CT[d][:, h, :], in0=CT[d][:, h, :], scalar=sm["dec"][:, h:h + 1], in1=up,
                                                               op0=ALU.mult, op1=ALU.add),
                  reads=[B("ct%d" % d), bn("dec"), B("ps7u")], writes=[B("ct%d" % d)])
        S.pool(lambda e: e.tensor_copy(out=CTB[d], in_=CT[d]), reads=[B("ct%d" % d)], writes=[B("ctb%d" % d)])
        S.dve(lambda e: e.tensor_tensor(out=MST[d], in0=sm["ml"], in1=nBps, op=ALU.subtract), reads=[bn("ml"), bps], writes=[B("m%d" % d)])

    def chunk_full(d, t, nbps, bps, hdst, bh):
        sm = SM[d]
        bn = lambda n: B("sm%d_%s" % (d, n))
        bm = B("m%d" % d)
        cs = slice(t * 128, (t + 1) * 128)
        for h in range(4):
            S.dve(lambda e, h=h: e.tensor_scalar(out=DA, in0=ident, scalar1=sm["a"][:, h:h + 1], scalar2=None, op0=ALU.mult), reads=[bC, bn("a")], writes=[B("da")])
            S.pe(lambda e: e.matmul(PS[7][:, 0:128], lhsT=ones, rhs=DA, start=True, stop=False), reads=[bC, B("da")], writes=[B("ps7e")])
            S.pe(lambda e: e.matmul(PS[7][:, 0:128], lhsT=ident, rhs=mask_sr, start=False, stop=True), reads=[bC], writes=[B("ps7e")])
            S.dve(lambda e: e.tensor_reduce(out=sm["cm"], in_=PS[7][:, 0:128], axis=AX.X, op=ALU.max), reads=[B("ps7e")], writes=[bn("cm")])
            S.dve(lambda e, h=h: e.tensor_tensor(out=sm["mv"], in0=sm["cm"], in1=MST[d][:, h:h + 1], op=ALU.max), reads=[bn("cm"), bm], writes=[bn("mv")])
            S.dve(lambda e: e.tensor_scalar(out=sm["nmv"], in0=sm["mv"], scalar1=-1.0, scalar2=None, op0=ALU.mult), reads=[bn("mv")], writes=[bn("nmv")])
            S.dve(lambda e: e.tensor_scalar(out=DM, in0=ident, scalar1=sm["nmv"], scalar2=None, op0=ALU.mult), reads=[bC, bn("nmv")], writes=[B("dmm")])
            S.pe(lambda e: e.matmul(PS[7][:, 128:256], lhsT=ones, rhs=DM, start=True, stop=False), reads=[bC, B("dmm")], writes=[B("ps7x")])
            S.pe(lambda e: e.matmul(PS[7][:, 128:256], lhsT=ident, rhs=mask_rs, start=False, stop=True), reads=[bC], writes=[B("ps7x")])
            S.act(lambda e, h=h: e.activation(out=DTT, in_=PS[7][:, 128:256], func=AF.Exp, bias=sm["a"][:, h:h + 1]), reads=[B("ps7x"), bn("a")], writes=[B("dtt")])
            S.pe(lambda e, h=h: e.matmul(PS[4][:, 0:128], lhsT=KT[:, h, cs], rhs=QT[:, h, cs], start=True, stop=True), reads=[B("kt"), B("qt")], writes=[B("ps4s")])
            S.dve(lambda e: e.tensor_tensor(out=PT, in0=PS[4][:, 0:128], in1=DTT, op=ALU.mult), reads=[B("ps4s"), B("dtt")], writes=[B("pt")])
            S.pe(lambda e, h=h: e.matmul(PS[4][:, 128:257], lhsT=PT, rhs=VAUG[:, h, :], start=True, stop=True), reads=[B("pt"), B("vaug")], writes=[B("ps4n")])
            S.pe(lambda e, h=h: e.matmul(PS[4][:, 257:386], lhsT=QT[:, h, cs], rhs=CTB[d][:, h, :], start=True, stop=True), reads=[B("qt"), B("ctb%d" % d)], writes=[B("ps4i")])
            S.dve(lambda e, h=h: e.tensor_tensor(out=sm["dwi"], in0=MST[d][:, h:h + 1], in1=sm["mv"], op=ALU.subtract), reads=[bm, bn("mv")], writes=[bn("dwi")])
            S.act(lambda e: e.activation(out=sm["wi"], in_=sm["dwi"], func=AF.Exp), reads=[bn("dwi")], writes=[bn("wi")])
            S.act(lambda e: e.activation(out=TT1, in_=PS[4][:, 257:386], func=AF.Identity, scale=sm["wi"]), reads=[B("ps4i"), bn("wi")], writes=[B("tt1")])
            S.dve(lambda e: e.tensor_tensor(out=TOT, in0=PS[4][:, 128:257], in1=TT1, op=ALU.add), reads=[B("ps4n"), B("tt1")], writes=[B("tot")])
            S.dve(lambda e, h=h: e.tensor_tensor(out=sm["dn0"], in0=nbps[:, h:h + 1], in1=sm["mv"], op=ALU.subtract), reads=[bps, bn("mv")], writes=[bn("dn0")])
            S.act(lambda e: e.activation(out=sm["nrm"], in_=sm["dn0"], func=AF.Exp), reads=[bn("dn0")], writes=[bn("nrm")])
            S.act(lambda e: e.activation(out=sm["ad"], in_=TOT[:, 128:129], func=AF.Abs), reads=[B("tot")], writes=[bn("ad")])
            S.dve(lambda e: e.tensor_tensor(out=sm["dn"], in0=sm["ad"], in1=sm["nrm"], op=ALU.max), reads=[bn("ad"), bn("nrm")], writes=[bn("dn")])
            S.dve(lambda e: e.reciprocal(out=sm["rd"], in_=sm["dn"]), reads=[bn("dn")], writes=[bn("rd")])
            S.act(lambda e, h=h: e.activation(out=hdst[:, h * 128:(h + 1) * 128], in_=TOT[:, 0:128], func=AF.Identity, scale=sm["rd"]),
                  reads=[B("tot"), bn("rd")], writes=[bh])

    steps = [("ctxF", 0, 256, 0, 0), ("ctxB", 1, 256, 1, None)]
    for g in range(12):
        steps.append(("oth", g, 512, 2 + g, 2 + 4 * g))
    for g in range(4):
        steps.append(("ownB", 12 + g, 512, 14 + g, None))
    for g in range(4):
        steps.append(("ownF", 16 + g, 512, 18 + g, 50 + 4 * g))

    KG = int(os.environ.get("KG", "99"))
    KSUB = int(os.environ.get("KSUB", "99"))
    if KG == 0:
        raise _Stop()
    for gstep, (kind, src, N, tg, ktb) in enumerate(steps):
        if gstep >= KG:
            raise _Stop()
        last = gstep == KG - 1
        isctx = kind.startswith("ctx")
        mc = 1 if isctx else 0
        if isctx:
            S.dma(lambda e, src=src: e.dma_start(out=XW[:, :, 0:258], in_=d_cwin[src]), writes=[B("xw")])
        else:
            S.dma(lambda e, src=src: e.dma_start(out=XW, in_=d_xwin[src]), writes=[B("xw")])
        for kc in range(8):
            if kc % 2 == 0:
                S.act(lambda e, kc=kc: e.activation(out=HT[:, kc, 0:N + 2], in_=XW[:, kc, 0:N + 2], func=AF.Identity,
                                                    bias=COLS[:, kc, mc:mc + 1], scale=SC1P[:, kc, mc:mc + 1]),
                      reads=[B("xw"), B("cols"), B("sc1p")], writes=[B("ht")])
            else:
                S.pool(lambda e, kc=kc: e.tensor_scalar(out=HT[:, kc, 0:N + 2], in0=XW[:, kc, 0:N + 2], scalar1=SC1P[:, kc, mc:mc + 1],
                                                        scalar2=COLS[:, kc, mc:mc + 1], op0=ALU.mult, op1=ALU.add),
                       reads=[B("xw"), B("cols"), B("sc1p")], writes=[B("ht")])
        if last and KSUB == 0:
            raise _Stop()
        fl = FLAGS[:, tg * 2:tg * 2 + 2]
        for hb in range(4):
            fm_project(C_KM + hb * 128, N, True, hb % 2)
            conv_block(N, hb % 2, hb % 2, KTAPS[:, (tg * 4 + hb) * 3:(tg * 4 + hb) * 3 + 3], CONVB[:, 4 + hb:5 + hb], fl, KT[:, hb, 0:N], False)
        if kind in ("ownB", "ownF"):
            qd = 1 if kind == "ownB" else 0
            for hb in range(4):
                fm_project(C_QM + hb * 128, N, True, hb % 2)
                conv_block(N, hb % 2, hb % 2, QTAPS[:, (qd * 4 + hb) * 3:(qd * 4 + hb) * 3 + 3], CONVB[:, hb:hb + 1], fl, QT[:, hb, 0:N], True)
        if last and KSUB == 1:
            raise _Stop()
        if ktb is not None:
            if not isctx:
                ri = src if kind == "oth" else 12 + (src - 16)
                S.dma(lambda e, ri=ri: e.dma_start(out=ROPE, in_=d_rope[ri].rearrange("a p n -> p a n")), writes=[B("rope")], q="pool")
            fm_project(C_KA, N, False, 0)
            normrope(N, 0, 1, not isctx, KAT[:, ktb * 128:ktb * 128 + N], B("kat"))
            if kind == "ownF":
                gi = src - 16
                for mcq in range(4):
                    fm_project(C_QA + mcq * 128, N, False, 1)
                    normrope(N, 1, 0, True, QAT[:, mcq, gi * 512:(gi + 1) * 512], B("qat"))
        if last and KSUB == 2:
            raise _Stop()
        for t in range(N // 128):
            ts = slice(1 + t * 128, 1 + (t + 1) * 128)
            for kc in range(8):
                S.pe(lambda e, kc=kc, ts=ts: e.matmul(PS[3][:, 0:512], lhsT=HT[:, kc, ts], rhs=WB[:, kc, C_VM:C_VM + 512], start=(kc == 0), stop=(kc == 7)),
                     reads=[B("ht"), B("wb")], writes=[B("ps3")])
            for kc in range(8):
                S.pe(lambda e, kc=kc, ts=ts: e.matmul(PS[6][:, 0:144], lhsT=HT[:, kc, ts], rhs=WB[:, kc, C_VA:C_VA + 144], start=(kc == 0), stop=(kc == 7)),
                     reads=[B("ht"), B("wb")], writes=[B("ps6")])
            S.act(lambda e: e.activation(out=VAUG[:, :, 0:128], in_=PS[3][:, 0:512].rearrange("p (a b) -> p a b", a=4), func=AF.Copy),
                  reads=[B("ps3")], writes=[B("vaug")])
            if ktb is not None:
                S.act(lambda e, kt_=ktb + t: e.activation(out=VA[:, kt_, :, 0:64], in_=PS[6][:, 0:128].rearrange("p (a b) -> p a b", a=2), func=AF.Copy),
                      reads=[B("ps6")], writes=[B("va")])
            S.dve(lambda e: e.tensor_tensor(out=GP, in0=PS[6][:, 128:144], in1=BG, op=ALU.add), reads=[B("ps6"), B("bg")], writes=[B("gp")])
            for h in range(4):
                S.pe(lambda e, h=h, t=t: e.matmul(PS[5][:, h * 128:(h + 1) * 128], lhsT=KT[:, h, t * 128:(t + 1) * 128], rhs=identb, start=True, stop=True),
                     reads=[B("kt"), bCB], writes=[B("ps5")])
            S.act(lambda e: e.activation(out=KTOK.rearrange("p a b -> p (a b)"), in_=PS[5][:, 0:512], func=AF.Copy), reads=[B("ps5")], writes=[B("ktok")])
            if kind == "ownF":
                for kc in range(8):
                    S.pe(lambda e, kc=kc, ts=ts: e.matmul(PS[3][:, 0:512], lhsT=HT[:, kc, ts], rhs=WB[:, kc, C_O:C_O + 512], start=(kc == 0), stop=(kc == 7)),
                         reads=[B("ht"), B("wb")], writes=[B("ps3")])
                S.act(lambda e: e.activation(out=OSG, in_=PS[3][:, 0:512], func=AF.Sigmoid), reads=[B("ps3")], writes=[B("osg")])
            if last and KSUB == 3:
                raise _Stop()
            dirs = {"ctxF": [0], "ctxB": [1], "oth": [0, 1], "ownB": [1], "ownF": [0]}[kind]
            for d in dirs:
                nbps, nBps, bps = chunk_state(d, src if kind == "oth" else None, t)
                if kind == "ownB":
                    cb = (src - 12) * 4 + t
                    chunk_full(d, t, nbps, bps, MIX[:, cb, 512:1024], B("mixb%d" % cb))
                elif kind == "ownF":
                    chunk_full(d, t, nbps, bps, HF, B("hf"))
                chunk_update(d, nBps, bps)
            if last and KSUB == 4:
                raise _Stop()
            if kind == "ownF":
                oc = (src - 16) * 4 + t
                hm = PS[3][:, 0:512]
                S.pe(lambda e: e.matmul(hm, lhsT=identb, rhs=HF, start=True, stop=False), reads=[bCB, B("hf")], writes=[B("ps3")])
                S.pe(lambda e, oc=oc: e.matmul(hm, lhsT=jb, rhs=MIX[:, 15 - oc, 512:1024], start=False, stop=True), reads=[bCB, B("mixb%d" % (15 - oc))], writes=[B("ps3")])
                hm3 = hm.rearrange("p (a b) -> p a b", a=4)
                S.dve(lambda e: e.tensor_reduce(out=S1, in_=hm3, axis=AX.X, op=ALU.add), reads=[B("ps3")], writes=[B("s1")])
                S.act(lambda e: e.activation(out=HN, in_=hm, func=AF.Square), reads=[B("ps3")], writes=[B("hn")])
                S.dve(lambda e: e.tensor_reduce(out=S2, in_=HN.rearrange("p (a b) -> p a b", a=4), axis=AX.X, op=ALU.add), reads=[B("hn")], writes=[B("s2")])
                if last and KSUB == 5:
                    raise _Stop()
                S.dve(lambda e: e.tensor_scalar(out=MEAN, in0=S1, scalar1=1.0 / 128, scalar2=None, op0=ALU.mult), reads=[B("s1")], writes=[B("mean")])
                S.dve(lambda e: e.tensor_tensor(out=MSQ, in0=MEAN, in1=MEAN, op=ALU.mult), reads=[B("mean")], writes=[B("msq")])
                S.dve(lambda e: e.scalar_tensor_tensor(out=VAR, in0=S2, scalar=1.0 / 128, in1=MSQ, op0=ALU.mult, op1=ALU.subtract), reads=[B("s2"), B("msq")], writes=[B("var")])
                S.act(lambda e: e.activation(out=RSTD, in_=VAR, func=AF.Sqrt, bias=LN_EPS), reads=[B("var")], writes=[B("rstd")])
                S.dve(lambda e: e.reciprocal(out=RSTD, in_=RSTD), reads=[B("rstd")], writes=[B("rstd")])
                if last and KSUB == 6:
                    raise _Stop()
                for h in range(4):
                    S.dve(lambda e, h=h: e.tensor_scalar(out=HN[:, h * 128:(h + 1) * 128], in0=hm[:, h * 128:(h + 1) * 128], scalar1=MEAN[:, h:h + 1],
                                                         scalar2=RSTD[:, h:h + 1], op0=ALU.subtract, op1=ALU.mult),
                          reads=[B("ps3"), B("mean"), B("rstd"), B("s2")], writes=[B("hn")])
                S.pool(lambda e: e.tensor_tensor(out=HN, in0=HN, in1=MHW, op=ALU.mult), reads=[B("hn"), B("mhw")], writes=[B("hn")])
                S.dve(lambda e, oc=oc: e.tensor_tensor(out=MIX[:, oc, 0:512], in0=HN, in1=OSG, op=ALU.mult), reads=[B("hn"), B("osg")], writes=[B("mixa%d" % oc)])

    dd = dbg("mixa", [128, 16, 512], BF16)
    if dd is not None:
        S.dma(lambda e, dd=dd: e.dma_start(out=dd, in_=MIX[:, :, 0:512]), reads=[B("mixa%d" % i) for i in range(16)], writes=[B("dbgo3")])

    if STOP == 1:
        raise _Stop()
    PB = [AR.alloc([128, 512], BF16) for _ in range(2)]
    RDEN = AR.alloc([128, 4])
    allmixb = [B("mixb%d" % i) for i in range(16)]
    for qg in range(4):
        for mcq in range(4):
            for half in range(2):
                hq = half * 4 + mcq
                r0 = half * 64
                for kt in range(66):
                    sb = kt % 2
                    S.pe(lambda e, kt=kt, sb=sb: e.matmul(PS[sb][:, 0:512], lhsT=KAT[r0:r0 + 64, kt * 128:(kt + 1) * 128],
                                                          rhs=QAT[r0:r0 + 64, mcq, qg * 512:(qg + 1) * 512], start=True, stop=True),
                         reads=[B("kat"), B("qat")], writes=[B("ps%d" % sb)])
                    S.act(lambda e, sb=sb: e.activation(out=PB[sb], in_=PS[sb][:, 0:512], func=AF.Exp, scale=0.125), reads=[B("ps%d" % sb)], writes=[B("pb%d" % sb)])
                    for qt in range(4):
                        S.pe(lambda e, kt=kt, sb=sb, qt=qt: e.matmul(PS[4 + qt][:, 0:65], lhsT=PB[sb][:, qt * 128:(qt + 1) * 128], rhs=VA[:, kt, half, :],
                                                                     start=(kt == 0), stop=(kt == 65)),
                             reads=[B("pb%d" % sb), B("va")], writes=[B("pso%d" % qt)])
                for qt in range(4):
                    oc = qg * 4 + qt
                    S.dve(lambda e, qt=qt: e.reciprocal(out=RDEN[:, qt:qt + 1], in_=PS[4 + qt][:, 64:65]), reads=[B("pso%d" % qt)], writes=[B("rden%d" % qt)])
                    S.act(lambda e, qt=qt, oc=oc: e.activation(out=MIX[:, oc, 512 + hq * 64:512 + (hq + 1) * 64], in_=PS[4 + qt][:, 0:64], func=AF.Identity,
                                                               scale=RDEN[:, qt:qt + 1]),
                          reads=[B("pso%d" % qt), B("rden%d" % qt)], writes=[B("att%d" % oc), B("mixb%d" % oc)])

    dd = dbg("att", [128, 16, 512], BF16)
    if dd is not None:
        S.dma(lambda e, dd=dd: e.dma_start(out=dd, in_=MIX[:, :, 512:1024]), reads=[B("att%d" % i) for i in range(16)], writes=[B("dbgo2")])

    if STOP == 2:
        raise _Stop()
    AR.off = mark_persist
    S.barrier()

    WKB = AR.alloc([128, 8, 2048], BF16)
    WOB = AR.alloc([128, 8, 1024], BF16)
    GB = AR.alloc([128, 4, 1024])
    LNP = AR.alloc([128, 4, 1024])
    UG = [AR.alloc([128, 2, 1024]) for _ in range(2)]
    XT = AR.alloc([128, 1024])
    R1 = AR.alloc([128, 1024])
    X1 = AR.alloc([128, 1024])
    XM = AR.alloc([128, 1024])
    SC = AR.alloc([128, 2048])
    MIXT = AR.alloc([128, 8, 128], BF16)
    XMT = AR.alloc([128, 8, 128], BF16)
    TP1 = AR.alloc([128, 16, 16])
    IP1 = AR.alloc([128, 16, 16], U32)
    IP1F = AR.alloc([128, 16, 16])
    TMP = AR.alloc([128, 256])
    CAND = AR.alloc([128, 256])
    TP2 = AR.alloc([128, 8, 16])
    PP2 = AR.alloc([128, 8, 16], U32)
    PJ = AR.alloc([128, 2, 128], U32)
    PJF = AR.alloc([128, 2, 128])
    OH = AR.alloc([128, 2048])
    IDXF = AR.alloc([128, 2, 128])
    EIF = AR.alloc([128, 128])
    EI = AR.alloc([128, 128], I32)
    GEX = AR.alloc([128, 8, 16])
    GSUM = AR.alloc([128, 8])
    DOT = AR.alloc([128, 128])
    COEF = AR.alloc([128, 128])
    DOT2 = AR.alloc([128, 128])
    IOTA = AR.alloc([128, 256])
    JUNK = AR.alloc([128, 1024])
    ST = AR.alloc([128, 8])
    print("phase3 arena bytes", AR.off)

    S.dma(lambda e: e.dma_start(out=LNP.rearrange("p a b -> p (a b)"), in_=d_lnp), writes=[B("lnp")])
    S.dma(lambda e: e.dma_start(out=IOTA, in_=d_iota), writes=[B("iota")], q="pool")
    for kc in range(8):
        st = UG[kc % 2][:, 0, :]
        bs = B("ug%d" % (kc % 2))
        S.dma(lambda e, st=st, kc=kc: e.dma_start(out=st, in_=d_wout[kc]), writes=[bs])
        S.act(lambda e, st=st, kc=kc: e.activation(out=WOB[:, kc, :], in_=st, func=AF.Copy), reads=[bs], writes=[B("wob")])
    KEYB = OH.bitcast(BF16)[:, 0:2048]
    S.dma(lambda e: e.dma_start(out=SC, in_=d_keysT), writes=[B("sc")])
    S.act(lambda e: e.activation(out=KEYB, in_=SC, func=AF.Copy), reads=[B("sc")], writes=[B("oh")])
    WQB = [XT.bitcast(BF16)[:, 0:1024], R1.bitcast(BF16)[:, 0:1024]]
    for blk in range(16):
        st = UG[blk % 2][:, 1, :]
        bs = B("ug%d" % (blk % 2))
        S.dma(lambda e, st=st, blk=blk: e.dma_start(out=st, in_=d_wqT[blk]), writes=[bs])
        S.act(lambda e, st=st, blk=blk: e.activation(out=WQB[blk % 2], in_=st, func=AF.Copy), reads=[bs], writes=[B(("xt", "r1")[blk % 2])])
        for kc in range(8):
            S.pe(lambda e, blk=blk, kc=kc: e.matmul(PS[kc % 2][:, 0:128], lhsT=WQB[blk % 2][:, kc * 128:(kc + 1) * 128], rhs=KEYB[:, blk * 128:(blk + 1) * 128],
                                                    start=True, stop=True),
                 reads=[B(("xt", "r1")[blk % 2]), B("oh")], writes=[B("ps%d" % (kc % 2))])
            if kc % 2 == 0:
                S.dve(lambda e, blk=blk, kc=kc: e.tensor_copy(out=WKB[:, kc, blk * 128:(blk + 1) * 128], in_=PS[kc % 2][:, 0:128]), reads=[B("ps%d" % (kc % 2))], writes=[B("wkb")])
            else:
                S.act(lambda e, blk=blk, kc=kc: e.activation(out=WKB[:, kc, blk * 128:(blk + 1) * 128], in_=PS[kc % 2][:, 0:128], func=AF.Copy),
                      reads=[B("ps%d" % (kc % 2))], writes=[B("wkb")])
    for vi, c0 in enumerate((16, 24, 32, 40)):
        for kc in range(8):
            S.dve(lambda e, c0=c0, kc=kc: e.tensor_scalar(out=JUNK[:, 0:128], in0=ident, scalar1=COLS[:, c0 + kc, 0:1], scalar2=None, op0=ALU.mult),
                  reads=[bC, B("cols")], writes=[B("junk")])
            S.pe(lambda e, kc=kc: e.matmul(PS[2 + kc % 2][:, 0:128], lhsT=ones, rhs=JUNK[:, 0:128], start=True, stop=True), reads=[bC, B("junk")], writes=[B("ps%d" % (2 + kc % 2))])
            if vi == 2:
                S.act(lambda e, vi=vi, kc=kc: e.activation(out=GB[:, vi, kc * 128:(kc + 1) * 128], in_=PS[2 + kc % 2][:, 0:128], func=AF.Identity, bias=1.0),
                      reads=[B("ps%d" % (2 + kc % 2))], writes=[B("gb")])
            else:
                S.act(lambda e, vi=vi, kc=kc: e.activation(out=GB[:, vi, kc * 128:(kc + 1) * 128], in_=PS[2 + kc % 2][:, 0:128], func=AF.Copy),
                      reads=[B("ps%d" % (2 + kc % 2))], writes=[B("gb")])

    def layer_norm(src, dst, gi, bi, bsrc, bdst):
        S.act(lambda e: e.activation(out=JUNK, in_=src, func=AF.Copy, accum_out=ST[:, 0:1]), reads=[bsrc], writes=[B("junk"), B("st")])
        S.act(lambda e: e.activation(out=JUNK, in_=src, func=AF.Square, accum_out=ST[:, 1:2]), reads=[bsrc], writes=[B("junk"), B("st")])
        S.dve(lambda e: e.tensor_scalar(out=ST[:, 2:3], in0=ST[:, 0:1], scalar1=1.0 / 1024, scalar2=None, op0=ALU.mult), reads=[B("st")], writes=[B("st")])
        S.dve(lambda e: e.tensor_tensor(out=ST[:, 3:4], in0=ST[:, 2:3], in1=ST[:, 2:3], op=ALU.mult), reads=[B("st")], writes=[B("st")])
        S.dve(lambda e: e.scalar_tensor_tensor(out=ST[:, 4:5], in0=ST[:, 1:2], scalar=1.0 / 1024, in1=ST[:, 3:4], op0=ALU.mult, op1=ALU.subtract), reads=[B("st")], writes=[B("st")])
        S.act(lambda e: e.activation(out=ST[:, 5:6], in_=ST[:, 4:5], func=AF.Sqrt, bias=LN_EPS), reads=[B("st")], writes=[B("st")])
        S.dve(lambda e: e.reciprocal(out=ST[:, 6:7], in_=ST[:, 5:6]), reads=[B("st")], writes=[B("st")])
        S.dve(lambda e: e.tensor_scalar(out=dst, in0=src, scalar1=ST[:, 2:3], scalar2=ST[:, 6:7], op0=ALU.subtract, op1=ALU.mult), reads=[bsrc, B("st")], writes=[bdst])
        S.pool(lambda e: e.tensor_tensor(out=dst, in0=dst, in1=LNP[:, gi, :], op=ALU.mult), reads=[bdst, B("lnp")], writes=[bdst])
        S.pool(lambda e: e.tensor_tensor(out=dst, in0=dst, in1=LNP[:, bi, :], op=ALU.add), reads=[bdst, B("lnp")], writes=[bdst])

    dd_x1 = dbg("x1", [2048, 1024])
    dd_y = dbg("y", [2048, 1024])
    XMP = [PS[0], PS[1]]
    YP = [PS[2], PS[3]]
    for tt in range(16):
        S.dma(lambda e, tt=tt: e.dma_start(out=XT, in_=d_xown[tt * 128:(tt + 1) * 128, :]), writes=[B("xt")])
        for kc in range(8):
            S.pe(lambda e, kc=kc, tt=tt: e.matmul(PS[6 + kc // 4][:, (kc % 4) * 128:(kc % 4 + 1) * 128], lhsT=MIX[:, tt, kc * 128:(kc + 1) * 128], rhs=identb,
                                                  start=True, stop=True),
                 reads=[B("mixa%d" % tt), B("att%d" % tt), bCB], writes=[B("ps%d" % (6 + kc // 4))])
        for hf in range(2):
            S.act(lambda e, hf=hf: e.activation(out=MIXT[:, hf * 4:(hf + 1) * 4, :].rearrange("p a b -> p (a b)"), in_=PS[6 + hf][:, 0:512], func=AF.Copy),
                  reads=[B("ps%d" % (6 + hf))], writes=[B("mixt")])
        for hf in range(2):
            for kc in range(8):
                S.pe(lambda e, hf=hf, kc=kc: e.matmul(PS[4 + hf][:, 0:512], lhsT=MIXT[:, kc, :], rhs=WOB[:, kc, hf * 512:(hf + 1) * 512], start=(kc == 0), stop=(kc == 7)),
                     reads=[B("mixt"), B("wob")], writes=[B("ps%d" % (4 + hf))])
        for hf in range(2):
            S.dve(lambda e, hf=hf: e.tensor_tensor(out=R1[:, hf * 512:(hf + 1) * 512], in0=PS[4 + hf][:, 0:512], in1=GB[:, 0, hf * 512:(hf + 1) * 512], op=ALU.mult),
                  reads=[B("ps%d" % (4 + hf)), B("gb")], writes=[B("r1")])
        S.dve(lambda e: e.scalar_tensor_tensor(out=R1, in0=XT, scalar=ALPHA, in1=R1, op0=ALU.mult, op1=ALU.add), reads=[B("xt"), B("r1")], writes=[B("r1")])
        layer_norm(R1, X1, 0, 1, B("r1"), B("x1"))
        if dd_x1 is not None:
            S.dma(lambda e, tt=tt: e.dma_start(out=dd_x1[tt * 128:(tt + 1) * 128, :], in_=X1), reads=[B("x1")], writes=[B("dbgx%d" % tt)])
        S.dve(lambda e: e.tensor_tensor(out=XM, in0=X1, in1=GB[:, 2, :], op=ALU.mult), reads=[B("x1"), B("gb")], writes=[B("xm")])
        S.dve(lambda e: e.tensor_tensor(out=XM, in0=XM, in1=GB[:, 1, :], op=ALU.add), reads=[B("xm"), B("gb")], writes=[B("xm")])
        for hf in range(2):
            S.act(lambda e, hf=hf: e.activation(out=XMP[hf][:, 0:512], in_=XM[:, hf * 512:(hf + 1) * 512], func=AF.Copy), reads=[B("xm")], writes=[B("ps%d" % hf)])
        for kc in range(8):
            S.pe(lambda e, kc=kc: e.matmul(PS[6 + kc // 4][:, (kc % 4) * 128:(kc % 4 + 1) * 128], lhsT=XM[:, kc * 128:(kc + 1) * 128], rhs=ident, start=True, stop=True),
                 reads=[B("xm"), bC], writes=[B("ps%d" % (6 + kc // 4))])
        for hf in range(2):
            S.act(lambda e, hf=hf: e.activation(out=XMT[:, hf * 4:(hf + 1) * 4, :].rearrange("p a b -> p (a b)"), in_=PS[6 + hf][:, 0:512], func=AF.Copy),
                  reads=[B("ps%d" % (6 + hf))], writes=[B("xmt")])
        for nb in range(4):
            for kc in range(8):
                S.pe(lambda e, nb=nb, kc=kc: e.matmul(PS[4 + nb][:, 0:512], lhsT=XMT[:, kc, :], rhs=WKB[:, kc, nb * 512:(nb + 1) * 512], start=(kc == 0), stop=(kc == 7)),
                     reads=[B("xmt"), B("wkb")], writes=[B("ps%d" % (4 + nb))])
            S.act(lambda e, nb=nb: e.activation(out=SC[:, nb * 512:(nb + 1) * 512], in_=PS[4 + nb][:, 0:512], func=AF.Copy), reads=[B("ps%d" % (4 + nb))], writes=[B("sc")])
        for blk in range(16):
            sc = SC[:, blk * 128:(blk + 1) * 128]
            S.dve(lambda e, blk=blk, sc=sc: e.max(out=TP1[:, blk, 0:8], in_=sc), reads=[B("sc")], writes=[B("tp1")])
            S.dve(lambda e, blk=blk, sc=sc: e.max_index(out=IP1[:, blk, 0:8], in_max=TP1[:, blk, 0:8], in_values=sc), reads=[B("sc"), B("tp1")], writes=[B("ip1")])
            S.dve(lambda e, blk=blk, sc=sc: e.match_replace(out=TMP[:, 0:128], in_to_replace=TP1[:, blk, 0:8], in_values=sc, imm_value=-1e30),
                  reads=[B("sc"), B("tp1")], writes=[B("tmp")])
            S.dve(lambda e, blk=blk: e.max(out=TP1[:, blk, 8:16], in_=TMP[:, 0:128]), reads=[B("tmp")], writes=[B("tp1")])
            S.dve(lambda e, blk=blk: e.max_index(out=IP1[:, blk, 8:16], in_max=TP1[:, blk, 8:16], in_values=TMP[:, 0:128]), reads=[B("tmp"), B("tp1")], writes=[B("ip1")])
        S.dve(lambda e: e.tensor_copy(out=IP1F, in_=IP1), reads=[B("ip1")], writes=[B("ip1f")])
        for h in range(8):
            S.dve(lambda e, h=h: e.tensor_tensor(out=CAND.rearrange("p (a b) -> p a b", a=16), in0=TP1[:, 2 * h, :].unsqueeze(2).to_broadcast([128, 16, 16]),
                                                 in1=TP1[:, 2 * h + 1, :].unsqueeze(1).to_broadcast([128, 16, 16]), op=ALU.add),
                  reads=[B("tp1")], writes=[B("cand")])
            S.dve(lambda e, h=h: e.max(out=TP2[:, h, 0:8], in_=CAND), reads=[B("cand")], writes=[B("tp2")])
            S.dve(lambda e, h=h: e.max_index(out=PP2[:, h, 0:8], in_max=TP2[:, h, 0:8], in_values=CAND), reads=[B("cand"), B("tp2")], writes=[B("pp2")])
            S.dve(lambda e, h=h: e.match_replace(out=TMP, in_to_replace=TP2[:, h, 0:8], in_values=CAND, imm_value=-1e30), reads=[B("cand"), B("tp2")], writes=[B("tmp")])
            S.dve(lambda e, h=h: e.max(out=TP2[:, h, 8:16], in_=TMP), reads=[B("tmp")], writes=[B("tp2")])
            S.dve(lambda e, h=h: e.max_index(out=PP2[:, h, 8:16], in_max=TP2[:, h, 8:16], in_values=TMP), reads=[B("tmp"), B("tp2")], writes=[B("pp2")])
        pp2f = PP2.rearrange("p a b -> p (a b)")
        S.dve(lambda e: e.tensor_single_scalar(out=PJ[:, 0, :], in_=pp2f, scalar=4, op=ALU.logical_shift_right), reads=[B("pp2")], writes=[B("pj")])
        S.dve(lambda e: e.tensor_single_scalar(out=PJ[:, 1, :], in_=pp2f, scalar=15, op=ALU.bitwise_and), reads=[B("pp2")], writes=[B("pj")])
        S.dve(lambda e: e.tensor_copy(out=PJF, in_=PJ), reads=[B("pj")], writes=[B("pjf")])
        for p in range(2):
            for h in range(8):
                oh = OH[:, h * 256:(h + 1) * 256].rearrange("p (a b) -> p a b", a=16)
                S.dve(lambda e, p=p, h=h, oh=oh: e.tensor_tensor(out=oh, in0=IOTA.rearrange("p (a b) -> p a b", a=16),
                                                                 in1=PJF[:, p, h * 16:(h + 1) * 16].unsqueeze(2).to_broadcast([128, 16, 16]), op=ALU.is_equal),
                      reads=[B("iota"), B("pjf")], writes=[B("oh")])
                S.dve(lambda e, p=p, h=h, oh=oh: e.tensor_tensor(out=oh, in0=oh, in1=IP1F[:, 2 * h + p, :].unsqueeze(1).to_broadcast([128, 16, 16]), op=ALU.mult),
                      reads=[B("oh"), B("ip1f")], writes=[B("oh")])
            S.dve(lambda e, p=p: e.tensor_reduce(out=IDXF[:, p, :], in_=OH.rearrange("p (a b) -> p a b", a=128), axis=AX.X, op=ALU.add), reads=[B("oh")], writes=[B("idxf")])
        S.dve(lambda e: e.scalar_tensor_tensor(out=EIF, in0=IDXF[:, 0, :], scalar=128.0, in1=IDXF[:, 1, :], op0=ALU.mult, op1=ALU.add), reads=[B("idxf")], writes=[B("eif")])
        S.dve(lambda e: e.tensor_copy(out=EI, in_=EIF), reads=[B("eif")], writes=[B("ei")])
        S.dve(lambda e: e.tensor_tensor(out=GEX, in0=TP2, in1=TP2[:, :, 0:1].to_broadcast([128, 8, 16]), op=ALU.subtract), reads=[B("tp2")], writes=[B("gex")])
        S.act(lambda e: e.activation(out=GEX, in_=GEX, func=AF.Exp), reads=[B("gex")], writes=[B("gex")])
        S.dve(lambda e: e.tensor_reduce(out=GSUM, in_=GEX, axis=AX.X, op=ALU.add), reads=[B("gex")], writes=[B("gsum")])
        S.dve(lambda e: e.reciprocal(out=GSUM, in_=GSUM), reads=[B("gsum")], writes=[B("gsum")])
        S.dve(lambda e: e.tensor_tensor(out=GEX, in0=GEX, in1=GSUM.unsqueeze(2).to_broadcast([128, 8, 16]), op=ALU.mult), reads=[B("gex"), B("gsum")], writes=[B("gex")])
        for s0 in range(0, 128, 2):
            ug = UG[(s0 // 2) % 2]
            bu = B("ug%d" % ((s0 // 2) % 2))
            for i in range(2):
                S.dma(lambda e, ug=ug, i=i, s=s0 + i: e.indirect_dma_start(out=ug[:, i, :], out_offset=None, in_=d_pu,
                                                                           in_offset=bass.IndirectOffsetOnAxis(ap=EI[:, s:s + 1], axis=0)),
                      reads=[B("ei")], writes=[bu], q="pool")
            for i in range(2):
                for hf in range(2):
                    S.dve(lambda e, ug=ug, i=i, hf=hf, s=s0 + i: e.scalar_tensor_tensor(out=JUNK[:, hf * 512:(hf + 1) * 512], in0=ug[:, i, hf * 512:(hf + 1) * 512], scalar=1.0,
                                                                                       in1=XMP[hf][:, 0:512], op0=ALU.mult, op1=ALU.mult,
                                                                                       accum_out=DOT[:, s:s + 1] if hf == 0 else DOT2[:, s:s + 1]),
                          reads=[bu, B("ps%d" % hf)], writes=[B("junk"), B("dot")])
        S.dve(lambda e: e.tensor_tensor(out=DOT, in0=DOT, in1=DOT2, op=ALU.add), reads=[B("dot")], writes=[B("dot")])
        S.act(lambda e: e.activation(out=DOT, in_=DOT, func=AF.Gelu), reads=[B("dot")], writes=[B("dot")])
        S.dve(lambda e: e.tensor_tensor(out=COEF, in0=DOT, in1=GEX.rearrange("p a b -> p (a b)"), op=ALU.mult), reads=[B("dot"), B("gex")], writes=[B("coef")])
        for hf in range(2):
            S.dve(lambda e, hf=hf: e.memset(YP[hf][:, 0:512], 0.0), writes=[B("ps%d" % (2 + hf))])
        for s0 in range(0, 128, 2):
            ug = UG[(s0 // 2) % 2]
            bu = B("ug%d" % ((s0 // 2) % 2))
            for i in range(2):
                S.dma(lambda e, ug=ug, i=i, s=s0 + i: e.indirect_dma_start(out=ug[:, i, :], out_offset=None, in_=d_pv,
                                                                           in_offset=bass.IndirectOffsetOnAxis(ap=EI[:, s:s + 1], axis=0)),
                      reads=[B("ei")], writes=[bu], q="pool")
            for i in range(2):
                for hf in range(2):
                    S.dve(lambda e, ug=ug, i=i, hf=hf, s=s0 + i: e.scalar_tensor_tensor(out=YP[hf][:, 0:512], in0=ug[:, i, hf * 512:(hf + 1) * 512], scalar=COEF[:, s:s + 1],
                                                                                       in1=YP[hf][:, 0:512], op0=ALU.mult, op1=ALU.add),
                          reads=[bu, B("coef"), B("ps%d" % (2 + hf))], writes=[B("ps%d" % (2 + hf))])
        if dd_y is not None:
            for hf in range(2):
                S.act(lambda e, hf=hf: e.activation(out=JUNK[:, hf * 512:(hf + 1) * 512], in_=YP[hf][:, 0:512], func=AF.Copy), reads=[B("ps%d" % (2 + hf))], writes=[B("junk")])
            S.dma(lambda e, tt=tt: e.dma_start(out=dd_y[tt * 128:(tt + 1) * 128, :], in_=JUNK), reads=[B("junk")], writes=[B("dbgy%d" % tt)])
        for hf in range(2):
            S.dve(lambda e, hf=hf: e.tensor_tensor(out=R1[:, hf * 512:(hf + 1) * 512], in0=YP[hf][:, 0:512], in1=GB[:, 3, hf * 512:(hf + 1) * 512], op=ALU.mult),
                  reads=[B("ps%d" % (2 + hf)), B("gb")], writes=[B("r1")])
        S.dve(lambda e: e.scalar_tensor_tensor(out=R1, in0=X1, scalar=ALPHA, in1=R1, op0=ALU.mult, op1=ALU.add), reads=[B("x1"), B("r1")], writes=[B("r1")])
        layer_norm(R1, XM, 2, 3, B("r1"), B("xm"))
        S.dma(lambda e, tt=tt: e.dma_start(out=d_out[tt * 128:(tt + 1) * 128, :], in_=XM), reads=[B("xm")], writes=[B("out%d" % tt)])


def _win(xb, lo, n, rev):
    L = xb.shape[0]
    w = np.zeros((n + 2, 1024), np.float32)
    a, b = lo - 1, lo + n + 1
    sa, sb = max(a, 0), min(b, L)
    w[sa - a:sb - a] = xb[sa:sb]
    if rev:
        w = w[::-1]
    return np.ascontiguousarray(w.reshape(n + 2, 8, 128).transpose(2, 1, 0))


def _rope_tables(tok):
    half = 16
    freq = (10000.0 ** (-np.arange(half, dtype=np.float32) / half)).astype(np.float32)
    row = (tok // 64).astype(np.float32)
    col = (tok % 64).astype(np.float32)
    cos = np.zeros((64, len(tok)), np.float32)
    sin = np.zeros((64, len(tok)), np.float32)
    for dd in range(64):
        pos = row if dd < 32 else col
        ang = pos * freq[dd % 16]
        cos[dd] = np.cos(ang)
        sin[dd] = np.sin(ang)
    return np.concatenate([cos, cos], 0), np.concatenate([sin, sin], 0)


_CACHE = {}


def kernel(x, c, ctx, c_ctx, w_mod, b_mod, w_in, conv_w, conv_b, b_gates, mh_norm_w, q_norm_w, k_norm_w, w_out,
           ln1_g, ln1_b, peer_wq, peer_keys, peer_u, peer_v, ln2_g, ln2_b):
    f32 = np.float32
    x = np.asarray(x, f32); c = np.asarray(c, f32); ctx = np.asarray(ctx, f32); c_ctx = np.asarray(c_ctx, f32)
    w_mod = np.asarray(w_mod, f32)[0]; b_mod = np.asarray(b_mod, f32)[0]; w_in = np.asarray(w_in, f32)[0]
    conv_w = np.asarray(conv_w, f32)[0]; conv_b = np.asarray(conv_b, f32)[0]; b_gates = np.asarray(b_gates, f32)[0]
    mh_norm_w = np.asarray(mh_norm_w, f32)[0]; q_norm_w = np.asarray(q_norm_w, f32)[0]; k_norm_w = np.asarray(k_norm_w, f32)[0]
    w_out = np.asarray(w_out, f32)[0]; ln1_g = np.asarray(ln1_g, f32)[0]; ln1_b = np.asarray(ln1_b, f32)[0]
    peer_wq = np.asarray(peer_wq, f32)[0]; peer_keys = np.asarray(peer_keys, f32)[0]
    peer_u = np.asarray(peer_u, f32)[0]; peer_v = np.asarray(peer_v, f32)[0]
    ln2_g = np.asarray(ln2_g, f32)[0]; ln2_b = np.asarray(ln2_b, f32)[0]

    if "nc" not in _CACHE:
        _CACHE["nc"] = build_program()
    nc, dbg_names = _CACHE["nc"]

    rep = lambda v: np.ascontiguousarray(np.broadcast_to(np.asarray(v, f32).reshape(1, -1), (128, np.asarray(v).size)))
    wmod_l = np.ascontiguousarray(w_mod.reshape(8, 128, 12, 512).transpose(2, 1, 0, 3))
    bmod_l = np.ascontiguousarray(np.broadcast_to(b_mod.reshape(12, 1, 512), (12, 2, 512)))
    qa_cols = []
    for mc in range(4):
        qa_cols += list(range(2064 + mc * 64, 2064 + (mc + 1) * 64)) + list(range(2064 + (4 + mc) * 64, 2064 + (5 + mc) * 64))
    perm = (list(range(512, 1024)) + list(range(0, 512)) + list(range(2576, 2704)) + qa_cols + list(range(1024, 1536))
            + list(range(2704, 2832)) + list(range(2048, 2064)) + list(range(1536, 2048)))
    win_l = np.ascontiguousarray(w_in[:, perm].reshape(8, 128, 2832))
    convb_l = np.ascontiguousarray(conv_b.reshape(8, 128).T)
    cw = conv_w.reshape(3, 8, 128)
    taps_nat = np.ascontiguousarray(cw.transpose(2, 1, 0))
    taps_rev = np.ascontiguousarray(taps_nat[:, :, ::-1])
    qtaps_l = np.ascontiguousarray(np.stack([taps_nat[:, 0:4], taps_rev[:, 0:4]], 1).reshape(128, 24))
    bg_l = rep(b_gates)
    mhw_l = rep(mh_norm_w)
    nw_l = np.ascontiguousarray(np.stack([np.tile(q_norm_w, 2), np.tile(k_norm_w, 2)], 1))
    wout_l = np.ascontiguousarray(w_out.reshape(8, 128, 1024))
    wqT_l = np.ascontiguousarray(peer_wq.T.reshape(16, 128, 1024))
    keysT_l = np.ascontiguousarray(peer_keys.reshape(16, 128, 128).transpose(2, 0, 1).reshape(128, 2048))
    lnp_l = np.ascontiguousarray(np.concatenate([rep(ln1_g), rep(ln1_b), rep(ln2_g), rep(ln2_b)], 1))
    ii = np.arange(128)
    ident = np.eye(128, dtype=f32)
    tri = (ii[:, None] <= ii[None, :]).astype(f32)
    mask_sr = np.where(ii[None, :] <= ii[:, None], 0.0, NEG).astype(f32)
    mask_rs = np.where(ii[:, None] <= ii[None, :], 0.0, NEG).astype(f32)
    jm = ident[::-1].copy()
    blk64 = (ii[:, None] // 64 == ii[None, :] // 64).astype(f32)
    R = np.zeros((128, 128), f32)
    for i in range(128):
        if (i % 32) < 16:
            R[i, i + 16] = -1.0
        else:
            R[i, i - 16] = 1.0
    sel0 = np.zeros((128, 128), f32)
    sel0[0, :] = 1.0
    cst_l = np.ascontiguousarray(np.concatenate([ident, np.ones((128, 128), f32), tri, mask_sr, mask_rs, jm, blk64, R.T.copy(), sel0], 1))
    iota_l = np.ascontiguousarray(np.broadcast_to(np.tile(np.arange(16, dtype=f32), 16).reshape(1, 256), (128, 256)))

    in_maps = []
    for core in range(NCORES):
        b, j = divmod(core, 4)
        xb = x[b]
        cwin = np.stack([_win(ctx[b], 0, 256, False), _win(ctx[b], 0, 256, True)], 0)
        oth = [(G, False) for G in range(0, 4 * j)] + [(G, True) for G in range(15, 4 * j + 3, -1)]
        ownB = [(G, True) for G in range(4 * j + 3, 4 * j - 1, -1)]
        ownF = [(G, False) for G in range(4 * j, 4 * j + 4)]
        srcs = oth + ownB + ownF
        assert len(oth) == 12 and len(srcs) == NGRP
        xwin = np.stack([_win(xb, 512 * G, 512, rv) for (G, rv) in srcs], 0)
        ktaps = np.zeros((128, 22, 4, 3), f32)
        flags = np.zeros((128, 22, 2), f32)
        ktaps[:, 0] = taps_nat[:, 4:8]
        ktaps[:, 1] = taps_rev[:, 4:8]
        for gi, (G, rv) in enumerate(srcs):
            ktaps[:, 2 + gi] = (taps_rev if rv else taps_nat)[:, 4:8]
            fl = [0.0 if G == 0 else 1.0, 0.0 if G == 15 else 1.0]
            flags[:, 2 + gi] = fl[::-1] if rv else fl
        gmask = np.zeros((128, 12, 4), f32)
        for gi, (G, rv) in enumerate(oth):
            fwd_real = not rv
            gmask[:, gi] = [1.0, 0.0, 0.0, NEG] if fwd_real else [0.0, NEG, 1.0, 0.0]
        rope = np.zeros((16, 2, 128, 512), f32)
        for ri, (G, rv) in enumerate(oth + ownF):
            tok = np.arange(512 * G, 512 * G + 512)
            if rv:
                tok = tok[::-1]
            cs, sn = _rope_tables(tok)
            rope[ri, 0] = cs
            rope[ri, 1] = sn
        cvec = np.ascontiguousarray(np.stack([c[b].reshape(8, 128).T, c_ctx.reshape(8, 128).T], 2).reshape(128, 16))
        in_maps.append(dict(
            cwin=cwin, xwin=xwin, xown=np.ascontiguousarray(xb[2048 * j:2048 * (j + 1)]), wmod=wmod_l, bmod=bmod_l, cvec=cvec,
            win=win_l, ktaps=np.ascontiguousarray(ktaps.reshape(128, 264)), qtaps=qtaps_l, convb=convb_l,
            flags=np.ascontiguousarray(flags.reshape(128, 44)), gmask=np.ascontiguousarray(gmask.reshape(128, 48)), bg=bg_l, mhw=mhw_l, nw=nw_l,
            wout=wout_l, wqT=wqT_l, keysT=keysT_l, lnp=lnp_l, pu=peer_u, pv=peer_v, cst=cst_l, iota16=iota_l, rope=rope))
    res = run_bass_kernel_spmd(nc, in_maps[:NCORES], core_ids=list(range(NCORES)))
    out = np.zeros((2, 8192, 1024), f32)
    for core in range(NCORES):
        b, j = divmod(core, 4)
        out[b, 2048 * j:2048 * (j + 1)] = res.results[core]["out"]
    if dbg_names:
        _CACHE["dbg"] = [{n: res.results[core]["dbg_" + n] for n in dbg_names} for core in range(NCORES)]
    return out
```

```python
import os
import types
import numpy as np
import concourse.bass as bass
import concourse.mybir as mybir
from concourse.bass_utils import run_bass_kernel_spmd

F32 = mybir.dt.float32
BF16 = mybir.dt.bfloat16
I32 = mybir.dt.int32
U32 = mybir.dt.uint32
ALU = mybir.AluOpType
AF = mybir.ActivationFunctionType
AX = mybir.AxisListType

NEG = -30000.0
LN_EPS = 1e-5
RMS_EPS = 1e-6
ALPHA = 2.0 ** 0.25
NGRP = 20
DBG = os.environ.get("KDBG", "")


class Buf:
    __slots__ = ("name", "lw", "rd")

    def __init__(self, name):
        self.name = name
        self.lw = None
        self.rd = []


class Op:
    __slots__ = ("eng", "fn", "reads", "writes", "dma", "deps", "signal", "sem", "val", "slot_prev", "bar")

    def __init__(self, eng, fn, reads, writes, dma):
        self.eng = eng
        self.fn = fn
        self.reads = reads
        self.writes = writes
        self.dma = dma
        self.deps = set()
        self.signal = False
        self.sem = None
        self.val = 0
        self.slot_prev = None
        self.bar = 0


def _freeze(fn):
    if fn is None or fn.__closure__ is None:
        return fn
    cells = []
    for c in fn.__closure__:
        try:
            cells.append(types.CellType(c.cell_contents))
        except ValueError:
            cells.append(c)
    return types.FunctionType(fn.__code__, fn.__globals__, fn.__name__, fn.__defaults__, tuple(cells))


class Sched:
    ENGS = ("pe", "act", "dve", "pool", "sp")

    def __init__(self, nc, n_dma_sems=32):
        self.nc = nc
        self.ops = []
        self.n_dma_sems = n_dma_sems
        self.nbar = 0
        self.bank = {}

    def op(self, eng, fn, reads=(), writes=(), dma=False):
        reads = [b for b in reads if b is not None]
        writes = [b for b in writes if b is not None]
        banks = set()
        for b in reads + writes:
            n = b.name
            if n.startswith("pso") and n[3:4].isdigit():
                banks.add(4 + int(n[3]))
            elif n.startswith("ps") and n[2:3].isdigit():
                banks.add(int(n[2]))
        for k in sorted(banks):
            if k not in self.bank:
                self.bank[k] = Buf("BANK%d" % k)
            writes.append(self.bank[k])
        o = Op(eng, _freeze(fn), reads, writes, dma)
        o.bar = self.nbar
        self.ops.append(o)
        return o

    def pe(self, fn, reads=(), writes=()):
        return self.op("pe", fn, reads, writes)

    def act(self, fn, reads=(), writes=()):
        return self.op("act", fn, reads, writes)

    def dve(self, fn, reads=(), writes=()):
        return self.op("dve", fn, reads, writes)

    def pool(self, fn, reads=(), writes=()):
        return self.op("pool", fn, reads, writes)

    def dma(self, fn, reads=(), writes=(), q="sp"):
        return self.op(q, fn, reads, writes, dma=True)

    def barrier(self):
        self.nbar += 1

    def _resolve(self):
        ops = self.ops
        last_eng = {}
        dma_all = []
        bar_deps = {}
        seen_bar = {e: 0 for e in self.ENGS}
        cur_bar = 0
        for i, o in enumerate(ops):
            if o.bar != cur_bar:
                cur_bar = o.bar
                bar_deps[cur_bar] = list(last_eng.values()) + list(dma_all)
            deps = set()
            raw = set()
            for b in o.reads:
                if b.lw is not None:
                    deps.add(b.lw)
                    raw.add(b.lw)
            for b in o.writes:
                if b.lw is not None:
                    deps.add(b.lw)
                for r in b.rd:
                    deps.add(r)
            for b in o.reads:
                b.rd.append(i)
            for b in o.writes:
                b.lw = i
                b.rd = []
            deps.discard(i)
            for j in deps:
                p = ops[j]
                if (not p.dma) and (not o.dma) and p.eng == o.eng:
                    if o.eng == "pe" or j not in raw:
                        continue
                o.deps.add(j)
                p.signal = True
            if seen_bar[o.eng] != cur_bar:
                seen_bar[o.eng] = cur_bar
                for j in bar_deps[cur_bar]:
                    p = ops[j]
                    if (not p.dma) and (not o.dma) and p.eng == o.eng:
                        continue
                    o.deps.add(j)
                    p.signal = True
            if o.dma:
                dma_all.append(i)
            elif o.fn is not None:
                last_eng[o.eng] = i

    def emit(self):
        nc = self.nc
        self._resolve()
        ops = self.ops
        esem = {e: nc.alloc_semaphore("s_" + e) for e in self.ENGS}
        nds = self.n_dma_sems
        dsem = [nc.alloc_semaphore("s_dma%d" % k) for k in range(2 * nds)]
        ecount = {e: 0 for e in self.ENGS}
        dcount = [0] * (2 * nds)
        dlast = [None] * (2 * nds)
        nd = 0
        ndq = {"sp": 0, "pool": 0, "act": 0}
        for i, o in enumerate(ops):
            if o.dma:
                base = nds if o.eng == "pool" else 0
                k = base + ndq[o.eng] % nds
                ndq[o.eng] += 1
                nd += 1
                o.signal = True
                o.sem = ("d", k)
                dcount[k] += 16
                o.val = dcount[k]
                o.slot_prev = dlast[k]
                dlast[k] = i
            elif o.signal:
                ecount[o.eng] += 1
                o.sem = ("e", o.eng)
                o.val = ecount[o.eng]
        self.stats = dict(ecount)
        self.stats["ndma"] = nd
        self.stats["nops"] = len(ops)

        def semh(s):
            return esem[s[1]] if s[0] == "e" else dsem[s[1]]

        per_eng = {e: [] for e in self.ENGS}
        for i, o in enumerate(ops):
            per_eng[o.eng].append(i)

        def emit_stream(ename, eng):
            waited = {}
            for i in per_eng[ename]:
                o = ops[i]
                need = {}
                for j in o.deps:
                    p = ops[j]
                    if need.get(p.sem, 0) < p.val:
                        need[p.sem] = p.val
                if o.dma and o.slot_prev is not None:
                    p = ops[o.slot_prev]
                    if need.get(p.sem, 0) < p.val:
                        need[p.sem] = p.val
                for s, v in need.items():
                    if waited.get(s, 0) >= v:
                        continue
                    eng.wait_ge(semh(s), v)
                    waited[s] = v
                if o.fn is None:
                    continue
                ins = o.fn(eng)
                if o.signal:
                    ins.then_inc(semh(o.sem), 16 if o.dma else 1)

        with nc.Block() as block:
            @block.tensor
            def _(e):
                emit_stream("pe", e)

            @block.scalar
            def _(e):
                emit_stream("act", e)

            @block.vector
            def _(e):
                emit_stream("dve", e)

            @block.gpsimd
            def _(e):
                emit_stream("pool", e)

            @block.sync
            def _(e):
                emit_stream("sp", e)


class Arena:
    def __init__(self, nc, nbytes):
        self.t = nc.alloc_sbuf_tensor("arena", [128, nbytes // 4], F32)
        self.off = 0
        self.cap = nbytes
        self.hi = 0

    def alloc(self, shape, dtype=F32):
        esz = 2 if dtype == BF16 else 4
        n = 1
        for s in shape[1:]:
            n *= s
        nb = (n * esz + 31) // 32 * 32
        o = self.off
        self.off += nb
        self.hi = max(self.hi, self.off)
        assert self.off <= self.cap, ("arena overflow", self.off, self.cap)
        v = self.t[:, o // 4:(o + nb) // 4]
        if dtype != F32:
            v = v.bitcast(dtype)
        v = v[:, 0:n]
        if len(shape) == 3:
            v = v.rearrange("p (a b) -> p a b", a=shape[1])
        elif len(shape) == 4:
            v = v.rearrange("p (a b c) -> p a b c", a=shape[1], b=shape[2])
        if shape[0] != 128:
            v = v[0:shape[0]]
        return v


class _Stop(Exception):
    pass


STOP = int(os.environ.get("KSTOP", "9"))
NCORES = int(os.environ.get("KCORES", "8"))


def build_program():
    st = {}
    try:
        _body(st)
    except _Stop:
        pass
    S = st["S"]
    B = st["B"]
    S.op("sp", None, reads=[B("out%d" % i) for i in range(16)] + [B("dbgo"), B("dbgo2"), B("dbgo3")] + [B("dbgx%d" % i) for i in range(16)] + [B("dbgy%d" % i) for i in range(16)])
    S.emit()
    print("sched stats", S.stats)
    return st["nc"], list(st["dbg_out"].keys())


def _body(st):
    nc = bass.Bass("TRN2", target_bir_lowering=False)
    S = Sched(nc)
    bufs = {}
    st["nc"] = nc
    st["S"] = S

    def B(name):
        b = bufs.get(name)
        if b is None:
            b = bufs[name] = Buf(name)
        return b

    def din(name, shape, dt=F32):
        return nc.dram_tensor(name, list(shape), dt, kind="ExternalInput").ap()

    d_cwin = din("cwin", [2, 128, 8, 258])
    d_xwin = din("xwin", [NGRP, 128, 8, 514])
    d_xown = din("xown", [2048, 1024])
    d_wmod = din("wmod", [12, 128, 8, 512])
    d_bmod = din("bmod", [12, 2, 512])
    d_cvec = din("cvec", [128, 16])
    d_win = din("win", [8, 128, 2832])
    d_ktaps = din("ktaps", [128, 22 * 12])
    d_qtaps = din("qtaps", [128, 2 * 12])
    d_cb = din("convb", [128, 8])
    d_flags = din("flags", [128, 44])
    d_gmask = din("gmask", [128, 48])
    d_bg = din("bg", [128, 16])
    d_mhw = din("mhw", [128, 512])
    d_nw = din("nw", [128, 2])
    d_wout = din("wout", [8, 128, 1024])
    d_wqT = din("wqT", [16, 128, 1024])
    d_keysT = din("keysT", [128, 16 * 128])
    d_lnp = din("lnp", [128, 4 * 1024])
    d_pu = din("pu", [16384, 1024])
    d_pv = din("pv", [16384, 1024])
    d_cst = din("cst", [128, 9 * 128])
    d_iota = din("iota16", [128, 256])
    d_rope = din("rope", [16, 2, 128, 512])
    d_out = nc.dram_tensor("out", [2048, 1024], F32, kind="ExternalOutput").ap()
    d_pub = nc.dram_tensor("pub", [16384, 1024], BF16).ap()
    d_pvb = nc.dram_tensor("pvb", [16384, 1024], BF16).ap()
    dbg_out = {}
    st["B"] = B
    st["dbg_out"] = dbg_out

    def dbg(name, shape, dt=F32):
        if name in DBG.split(","):
            dbg_out[name] = nc.dram_tensor("dbg_" + name, list(shape), dt, kind="ExternalOutput").ap()
            return dbg_out[name]
        return None

    AR = Arena(nc, 207 * 1024)
    PS = [nc.alloc_psum_tensor("ps%d" % i, [128, 512], F32) for i in range(8)]

    CST = AR.alloc([128, 9 * 128])
    ident = CST[:, 0:128]
    ones = CST[:, 128:256]
    tri = CST[:, 256:384]
    mask_sr = CST[:, 384:512]
    mask_rs = CST[:, 512:640]
    jmat = CST[:, 640:768]
    blk64 = CST[:, 768:896]
    rTm = CST[:, 896:1024]
    sel0 = CST[:, 1024:1152]
    CSTB = AR.alloc([128, 2 * 128], BF16)
    identb = CSTB[:, 0:128]
    jb = CSTB[:, 128:256]
    COLS = AR.alloc([128, 48, 2])
    SC1P = AR.alloc([128, 8, 2])
    MIX = AR.alloc([128, 16, 1024], BF16)
    MHW = AR.alloc([128, 512])
    KTAPS = AR.alloc([128, 22 * 12])
    QTAPS = AR.alloc([128, 24])
    CONVB = AR.alloc([128, 8])
    FLAGS = AR.alloc([128, 44])
    GMASK = AR.alloc([128, 48])
    BG = AR.alloc([128, 16])
    NW = AR.alloc([128, 2])
    MST = [AR.alloc([128, 4]) for _ in range(2)]
    CT = [AR.alloc([128, 4, 129]) for _ in range(2)]
    CTB = [AR.alloc([128, 4, 129], BF16) for _ in range(2)]
    SMALL = AR.alloc([128, 512])
    smo = [0]

    def small(n):
        o = smo[0]
        smo[0] += n
        assert smo[0] <= 512
        return SMALL[:, o:o + n]

    bC = B("cst")
    S.dma(lambda e: e.dma_start(out=CST, in_=d_cst), writes=[bC])
    S.act(lambda e: e.activation(out=identb, in_=ident, func=AF.Copy), reads=[bC], writes=[B("cstb")])
    S.act(lambda e: e.activation(out=jb, in_=jmat, func=AF.Copy), reads=[bC], writes=[B("cstb")])
    bCB = B("cstb")
    for (t, d, n) in ((MHW, d_mhw, "mhw"), (KTAPS, d_ktaps, "ktaps"), (QTAPS, d_qtaps, "qtaps"), (CONVB, d_cb, "convb"),
                      (FLAGS, d_flags, "flags"), (GMASK, d_gmask, "gmask"), (BG, d_bg, "bg"), (NW, d_nw, "nw")):
        S.dma(lambda e, t=t, d=d: e.dma_start(out=t, in_=d), writes=[B(n)], q="pool")
    for d in range(2):
        S.pool(lambda e, d=d: e.memset(MST[d], 0.0), writes=[B("m%d" % d)])
        S.pool(lambda e, d=d: e.memset(CT[d], 0.0), writes=[B("ct%d" % d)])
        S.pool(lambda e, d=d: e.memset(CTB[d], 0.0), writes=[B("ctb%d" % d)])

    mark_persist = AR.off
    for (src_, dst_, nm) in ((d_pu, d_pub, "pub"), (d_pv, d_pvb, "pvb")):
        for r in range(0, 16384, 2048):
            S.dma(lambda e, src_=src_, dst_=dst_, r=r: e.dma_start(out=dst_[r:r + 2048, :], in_=src_[r:r + 2048, :]), writes=[B("%s%d" % (nm, r))], q="pool")

    CV = AR.alloc([128, 16])
    SIL = AR.alloc([128, 8, 2])
    WM = [AR.alloc([128, 8, 512]) for _ in range(2)]
    BM = [AR.alloc([2, 512]) for _ in range(2)]
    MROW = [AR.alloc([2, 512]) for _ in range(2)]
    S.dma(lambda e: e.dma_start(out=CV, in_=d_cvec), writes=[B("cv")])
    S.act(lambda e: e.activation(out=SIL.rearrange("p a b -> p (a b)"), in_=CV, func=AF.Silu), reads=[B("cv")], writes=[B("sil")])
    colps = PS[1][:, 0:96]
    for g in range(12):
        wm = WM[g % 2]
        bw = B("wm%d" % (g % 2))
        S.dma(lambda e, wm=wm, g=g: e.dma_start(out=wm, in_=d_wmod[g]), writes=[bw])
        S.dma(lambda e, g=g: e.dma_start(out=BM[g % 2], in_=d_bmod[g]), writes=[B("bm%d" % (g % 2))], q="pool")
        for kc in range(8):
            S.pe(lambda e, wm=wm, kc=kc: e.matmul(PS[0][0:2, :], lhsT=SIL[:, kc, :], rhs=wm[:, kc, :], start=(kc == 0), stop=(kc == 7)),
                 reads=[B("sil"), bw], writes=[B("ps0")])
        mr = MROW[g % 2]
        S.dve(lambda e, g=g, mr=mr: e.tensor_tensor(out=mr, in0=PS[0][0:2, :], in1=BM[g % 2], op=ALU.add),
              reads=[B("ps0"), B("bm%d" % (g % 2))], writes=[B("mrow%d" % (g % 2))])
        for fc in range(4):
            idx = g * 4 + fc
            S.pe(lambda e, mr=mr, fc=fc, idx=idx: e.matmul(colps[:, idx * 2:idx * 2 + 2], lhsT=mr[:, fc * 128:(fc + 1) * 128], rhs=ident[0:2, 0:2],
                                                           start=True, stop=True),
                 reads=[B("mrow%d" % (g % 2)), bC], writes=[B("ps1")])
    S.dve(lambda e: e.tensor_copy(out=COLS.rearrange("p a b -> p (a b)"), in_=colps), reads=[B("ps1")], writes=[B("cols")])
    S.dve(lambda e: e.tensor_scalar(out=SC1P, in0=COLS[:, 8:16, :], scalar1=1.0, scalar2=None, op0=ALU.add), reads=[B("cols")], writes=[B("sc1p")])
    dd = dbg("cols", [128, 96])
    if dd is not None:
        S.dma(lambda e, dd=dd: e.dma_start(out=dd, in_=COLS.rearrange("p a b -> p (a b)")), reads=[B("cols")], writes=[B("dbgo")])

    if STOP == 0:
        raise _Stop()
    AR.off = mark_persist
    S.barrier()

    WB = AR.alloc([128, 8, 2832], BF16)
    QAT = AR.alloc([128, 4, 2048], BF16)
    KAT = AR.alloc([128, 66 * 128], BF16)
    VA = AR.alloc([128, 66, 2, 65], BF16)
    XW = AR.alloc([128, 8, 514])
    HT = AR.alloc([128, 8, 514], BF16)
    KT = AR.alloc([128, 4, 512], BF16)
    QT = AR.alloc([128, 4, 512], BF16)
    ZB = [AR.alloc([128, 514]) for _ in range(2)]
    TB = [AR.alloc([128, 512]) for _ in range(2)]
    SQ = AR.alloc([128, 512])
    SD = AR.alloc([128, 512])
    KN = AR.alloc([128, 512])
    ROPE = AR.alloc([128, 2, 512])
    KTOK = AR.alloc([128, 4, 128], BF16)
    VAUG = AR.alloc([128, 4, 129], BF16)
    VW = [AR.alloc([128, 129], BF16) for _ in range(2)]
    DA = AR.alloc([128, 128])
    DM = AR.alloc([128, 128])
    DTT = AR.alloc([128, 128])
    PT = AR.alloc([128, 128], BF16)
    TT1 = AR.alloc([128, 129])
    TOT = AR.alloc([128, 129])
    HF = AR.alloc([128, 512], BF16)
    HN = AR.alloc([128, 512])
    OSG = AR.alloc([128, 512])
    GP = AR.alloc([128, 16])
    WST = [QAT.rearrange("p a b -> p (a b)").bitcast(F32)[:, 0:2832], KAT.bitcast(F32)[:, 0:2832]]
    print("phase1 arena bytes", AR.off)

    S.pool(lambda e: e.memset(VAUG[:, :, 128:129], 1.0), writes=[B("vaug")])
    S.pool(lambda e: e.memset(VA[:, :, :, 64:65], 1.0), writes=[B("va")])

    for kc in range(8):
        st = WST[kc % 2]
        bs = B("qat" if kc % 2 == 0 else "kat")
        S.dma(lambda e, st=st, kc=kc: e.dma_start(out=st, in_=d_win[kc]), writes=[bs])
        if kc % 2 == 0:
            S.act(lambda e, st=st, kc=kc: e.activation(out=WB[:, kc, :], in_=st, func=AF.Copy), reads=[bs], writes=[B("wb")])
        else:
            S.pool(lambda e, st=st, kc=kc: e.tensor_copy(out=WB[:, kc, :], in_=st), reads=[bs], writes=[B("wb")])

    C_KM, C_QM, C_KA, C_QA, C_VM, C_VA, C_G, C_O = 0, 512, 1024, 1152, 1664, 2176, 2304, 2320

    def mk_small():
        return dict(ef=small(4), nlf=small(4), li=small(4), a=small(4), amx=small(1), d4=small(4), ml=small(4), dm=small(4),
                    dec=small(4), aw=small(4), w=small(4), cm=small(1), mv=small(1), nmv=small(1), dwi=small(1), wi=small(1),
                    dn0=small(1), nrm=small(1), ad=small(1), dn=small(1), rd=small(1))
    SM = [mk_small(), mk_small()]
    S1 = small(4)
    S2 = small(4)
    MEAN = small(4)
    MSQ = small(4)
    VAR = small(4)
    RSTD = small(4)

    def fm_project(c0, N, halo, psb):
        for kc in range(8):
            S.pe(lambda e, kc=kc: e.matmul(PS[psb][:, 0:N], lhsT=WB[:, kc, c0:c0 + 128], rhs=HT[:, kc, 1:N + 1], start=(kc == 0), stop=(kc == 7)),
                 reads=[B("wb"), B("ht")], writes=[B("ps%d" % psb)])
        if halo:
            for kc in range(8):
                S.pe(lambda e, kc=kc: e.matmul(PS[2][:, 0:2], lhsT=WB[:, kc, c0:c0 + 128], rhs=HT[:, kc, 0:N + 2:N + 1], start=(kc == 0), stop=(kc == 7)),
                     reads=[B("wb"), B("ht")], writes=[B("ps2")])

    def conv_block(N, psb, zi, taps, cbias, fl, dst, qscale):
        Z = ZB[zi]
        T = TB[zi]
        bz = B("z%d" % zi)
        bt = B("t%d" % zi)
        S.act(lambda e: e.activation(out=Z[:, 1:N + 1], in_=PS[psb][:, 0:N], func=AF.Copy), reads=[B("ps%d" % psb)], writes=[bz])
        S.dve(lambda e: e.tensor_tensor(out=Z[:, 0:N + 2:N + 1], in0=PS[2][:, 0:2], in1=fl, op=ALU.mult), reads=[B("ps2"), B("flags")], writes=[bz])
        S.dve(lambda e: e.tensor_scalar(out=T[:, 0:N], in0=Z[:, 0:N], scalar1=taps[:, 0:1], scalar2=None, op0=ALU.mult),
              reads=[bz, B("ktaps"), B("qtaps")], writes=[bt])
        S.dve(lambda e: e.scalar_tensor_tensor(out=T[:, 0:N], in0=Z[:, 1:N + 1], scalar=taps[:, 1:2], in1=T[:, 0:N], op0=ALU.mult, op1=ALU.add),
              reads=[bz, bt], writes=[bt])
        S.dve(lambda e: e.scalar_tensor_tensor(out=T[:, 0:N], in0=Z[:, 2:N + 2], scalar=taps[:, 2:3], in1=T[:, 0:N], op0=ALU.mult, op1=ALU.add),
              reads=[bz, bt], writes=[bt])
        if qscale:
            S.act(lambda e: e.activation(out=T[:, 0:N], in_=T[:, 0:N], func=AF.Silu, bias=cbias), reads=[bt, B("convb")], writes=[bt])
            S.pool(lambda e: e.tensor_scalar(out=dst, in0=T[:, 0:N], scalar1=128.0 ** -0.5, scalar2=None, op0=ALU.mult), reads=[bt], writes=[B("qt")])
        else:
            S.act(lambda e: e.activation(out=dst, in_=T[:, 0:N], func=AF.Silu, bias=cbias), reads=[bt, B("convb")], writes=[B("kt")])

    def normrope(N, psb, nwcol, rope, dst, bdst):
        ps = PS[psb][:, 0:N]
        bp = B("ps%d" % psb)
        S.act(lambda e: e.activation(out=SQ[:, 0:N], in_=ps, func=AF.Square), reads=[bp], writes=[B("sq")])
        S.pe(lambda e: e.matmul(PS[6][:, 0:N], lhsT=blk64, rhs=SQ[:, 0:N], start=True, stop=True), reads=[bC, B("sq")], writes=[B("ps6")])
        S.act(lambda e: e.activation(out=SD[:, 0:N], in_=PS[6][:, 0:N], func=AF.Sqrt, scale=1.0 / 64, bias=RMS_EPS), reads=[B("ps6")], writes=[B("sd")])
        S.dve(lambda e: e.reciprocal(out=SD[:, 0:N], in_=SD[:, 0:N]), reads=[B("sd")], writes=[B("sd")])
        S.dve(lambda e: e.scalar_tensor_tensor(out=KN[:, 0:N], in0=ps, scalar=NW[:, nwcol:nwcol + 1], in1=SD[:, 0:N], op0=ALU.mult, op1=ALU.mult),
              reads=[bp, B("nw"), B("sd")], writes=[B("kn")])
        if rope:
            S.pe(lambda e: e.matmul(PS[5][:, 0:N], lhsT=rTm, rhs=KN[:, 0:N], start=True, stop=True), reads=[bC, B("kn")], writes=[B("ps5")])
            S.pool(lambda e: e.tensor_tensor(out=SQ[:, 0:N], in0=KN[:, 0:N], in1=ROPE[:, 0, 0:N], op=ALU.mult), reads=[B("kn"), B("rope")], writes=[B("sq")])
            S.dve(lambda e: e.tensor_tensor(out=KN[:, 0:N], in0=PS[5][:, 0:N], in1=ROPE[:, 1, 0:N], op=ALU.mult), reads=[B("ps5"), B("rope"), B("kn")], writes=[B("kn")])
            S.dve(lambda e: e.tensor_tensor(out=dst, in0=KN[:, 0:N], in1=SQ[:, 0:N], op=ALU.add), reads=[B("kn"), B("sq")], writes=[bdst])
        else:
            S.act(lambda e: e.activation(out=dst, in_=KN[:, 0:N], func=AF.Copy), reads=[B("kn")], writes=[bdst])

    def chunk_state(d, masked_g, t):
        sm = SM[d]
        ic = 0 if d == 0 else 8
        fc = ic + 4
        bn = lambda n: B("sm%d_%s" % (d, n))
        bm = B("m%d" % d)
        S.act(lambda e: e.activation(out=sm["ef"], in_=GP[:, fc:fc + 4], func=AF.Exp, scale=-1.0), reads=[B("gp")], writes=[bn("ef")])
        S.act(lambda e: e.activation(out=sm["nlf"], in_=sm["ef"], func=AF.Ln, bias=1.0), reads=[bn("ef")], writes=[bn("nlf")])
        if masked_g is not None:
            kcol = GMASK[:, masked_g * 4 + 2 * d:masked_g * 4 + 2 * d + 1]
            acol = GMASK[:, masked_g * 4 + 2 * d + 1:masked_g * 4 + 2 * d + 2]
            S.dve(lambda e: e.tensor_scalar(out=sm["nlf"], in0=sm["nlf"], scalar1=kcol, scalar2=None, op0=ALU.mult), reads=[bn("nlf"), B("gmask")], writes=[bn("nlf")])
            S.dve(lambda e: e.tensor_scalar(out=sm["li"], in0=GP[:, ic:ic + 4], scalar1=kcol, scalar2=acol, op0=ALU.mult, op1=ALU.add),
                  reads=[B("gp"), B("gmask")], writes=[bn("li")])
        else:
            S.dve(lambda e: e.tensor_copy(out=sm["li"], in_=GP[:, ic:ic + 4]), reads=[B("gp")], writes=[bn("li")])
        o = 16 + d * 32
        nbps = PS[2][:, o:o + 4]
        nBps = PS[2][:, o + 4:o + 8]
        amBps = PS[2][:, o + 8:o + 12]
        aTps = PS[2][0:4, 128 + d * 128:256 + d * 128]
        bps = B("ps2s%d" % d)
        S.pe(lambda e: e.matmul(nbps, lhsT=tri, rhs=sm["nlf"], start=True, stop=True), reads=[bC, bn("nlf")], writes=[bps])
        S.pe(lambda e: e.matmul(nBps, lhsT=ones, rhs=sm["nlf"], start=True, stop=True), reads=[bC, bn("nlf")], writes=[bps])
        S.dve(lambda e: e.tensor_tensor(out=sm["a"], in0=nbps, in1=sm["li"], op=ALU.add), reads=[bps, bn("li")], writes=[bn("a")])
        S.pe(lambda e: e.matmul(aTps, lhsT=sm["a"], rhs=ident, start=True, stop=True), reads=[bC, bn("a")], writes=[B("ps2t%d" % d)])
        S.dve(lambda e: e.tensor_reduce(out=sm["amx"][0:4], in_=aTps, axis=AX.X, op=ALU.max), reads=[B("ps2t%d" % d)], writes=[bn("amx")])
        S.dve(lambda e: e.tensor_scalar(out=sm["d4"][0:4], in0=ident[0:4, 0:4], scalar1=sm["amx"][0:4], scalar2=None, op0=ALU.mult),
              reads=[bC, bn("amx")], writes=[bn("d4")])
        S.pe(lambda e: e.matmul(amBps, lhsT=ones[0:4, :], rhs=sm["d4"][0:4], start=True, stop=True), reads=[bC, bn("d4")], writes=[bps])
        S.dve(lambda e: e.tensor_tensor(out=sm["ml"], in0=amBps, in1=MST[d], op=ALU.max), reads=[bps, bm], writes=[bn("ml")])
        S.dve(lambda e: e.tensor_tensor(out=sm["dm"], in0=MST[d], in1=sm["ml"], op=ALU.subtract), reads=[bm, bn("ml")], writes=[bn("dm")])
        S.act(lambda e: e.activation(out=sm["dec"], in_=sm["dm"], func=AF.Exp), reads=[bn("dm")], writes=[bn("dec")])
        S.dve(lambda e: e.tensor_tensor(out=sm["aw"], in0=sm["a"], in1=sm["ml"], op=ALU.subtract), reads=[bn("a"), bn("ml")], writes=[bn("aw")])
        S.act(lambda e: e.activation(out=sm["w"], in_=sm["aw"], func=AF.Exp), reads=[bn("aw")], writes=[bn("w")])
        return nbps, nBps, bps

    def chunk_update(d, nBps, bps):
        sm = SM[d]
        bn = lambda n: B("sm%d_%s" % (d, n))
        for h in range(4):
            vw = VW[h % 2]
            bvw = B("vw%d" % (h % 2))
            S.dve(lambda e, h=h, vw=vw: e.tensor_scalar(out=vw, in0=VAUG[:, h, :], scalar1=sm["w"][:, h:h + 1], scalar2=None, op0=ALU.mult),
                  reads=[B("vaug"), bn("w")], writes=[bvw])
            up = PS[7][:, 256:385]
            S.pe(lambda e, h=h, vw=vw, up=up: e.matmul(up, lhsT=KTOK[:, h, :], rhs=vw, start=True, stop=True), reads=[B("ktok"), bvw], writes=[B("ps7u")])
            S.dve(lambda e, h=h, up=up: e.scalar_tensor_tensor(out=CT[d][:, h, :], in0=CT[d][:, h, :], scalar=sm["dec"][:, h:h + 1], in1=up,
                                                               op0=ALU.mult, op1=ALU.add),
                  reads=[B("ct%d" % d), bn("dec"), B("ps7u")], writes=[B("ct%d" % d)])
        S.pool(lambda e: e.tensor_copy(out=CTB[d], in_=CT[d]), reads=[B("ct%d" % d)], writes=[B("ctb%d" % d)])
        S.dve(lambda e: e.tensor_tensor(out=MST[d], in0=sm["ml"], in1=nBps, op=ALU.subtract), reads=[bn("ml"), bps], writes=[B("m%d" % d)])

    def chunk_full(d, t, nbps, bps, hdst, bh):
        sm = SM[d]
        bn = lambda n: B("sm%d_%s" % (d, n))
        bm = B("m%d" % d)
        cs = slice(t * 128, (t + 1) * 128)
        for h in range(4):
            S.dve(lambda e, h=h: e.tensor_scalar(out=DA, in0=ident, scalar1=sm["a"][:, h:h + 1], scalar2=None, op0=ALU.mult), reads=[bC, bn("a")], writes=[B("da")])
            S.pe(lambda e: e.matmul(PS[7][:, 0:128], lhsT=ones, rhs=DA, start=True, stop=False), reads=[bC, B("da")], writes=[B("ps7e")])
            S.pe(lambda e: e.matmul(PS[7][:, 0:128], lhsT=ident, rhs=mask_sr, start=False, stop=True), reads=[bC], writes=[B("ps7e")])
            S.dve(lambda e: e.tensor_reduce(out=sm["cm"], in_=PS[7][:, 0:128], axis=AX.X, op=ALU.max), reads=[B("ps7e")], writes=[bn("cm")])
            S.dve(lambda e, h=h: e.tensor_tensor(out=sm["mv"], in0=sm["cm"], in1=MST[d][:, h:h + 1], op=ALU.max), reads=[bn("cm"), bm], writes=[bn("mv")])
            S.dve(lambda e: e.tensor_scalar(out=sm["nmv"], in0=sm["mv"], scalar1=-1.0, scalar2=None, op0=ALU.mult), reads=[bn("mv")], writes=[bn("nmv")])
            S.dve(lambda e: e.tensor_scalar(out=DM, in0=ident, scalar1=sm["nmv"], scalar2=None, op0=ALU.mult), reads=[bC, bn("nmv")], writes=[B("dmm")])
            S.pe(lambda e: e.matmul(PS[7][:, 128:256], lhsT=ones, rhs=DM, start=True, stop=False), reads=[bC, B("dmm")], writes=[B("ps7x")])
            S.pe(lambda e: e.matmul(PS[7][:, 128:256], lhsT=ident, rhs=mask_rs, start=False, stop=True), reads=[bC], writes=[B("ps7x")])
            S.act(lambda e, h=h: e.activation(out=DTT, in_=PS[7][:, 128:256], func=AF.Exp, bias=sm["a"][:, h:h + 1]), reads=[B("ps7x"), bn("a")], writes=[B("dtt")])
            S.pe(lambda e, h=h: e.matmul(PS[4][:, 0:128], lhsT=KT[:, h, cs], rhs=QT[:, h, cs], start=True, stop=True), reads=[B("kt"), B("qt")], writes=[B("ps4s")])
            S.dve(lambda e: e.tensor_tensor(out=PT, in0=PS[4][:, 0:128], in1=DTT, op=ALU.mult), reads=[B("ps4s"), B("dtt")], writes=[B("pt")])
            S.pe(lambda e, h=h: e.matmul(PS[4][:, 128:257], lhsT=PT, rhs=VAUG[:, h, :], start=True, stop=True), reads=[B("pt"), B("vaug")], writes=[B("ps4n")])
            S.pe(lambda e, h=h: e.matmul(PS[4][:, 257:386], lhsT=QT[:, h, cs], rhs=CTB[d][:, h, :], start=True, stop=True), reads=[B("qt"), B("ctb%d" % d)], writes=[B("ps4i")])
            S.dve(lambda e, h=h: e.tensor_tensor(out=sm["dwi"], in0=MST[d][:, h:h + 1], in1=sm["mv"], op=ALU.subtract), reads=[bm, bn("mv")], writes=[bn("dwi")])
            S.act(lambda e: e.activation(out=sm["wi"], in_=sm["dwi"], func=AF.Exp), reads=[bn("dwi")], writes=[bn("wi")])
            S.act(lambda e: e.activation(out=TT1, in_=PS[4][:, 257:386], func=AF.Identity, scale=sm["wi"]), reads=[B("ps4i"), bn("wi")], writes=[B("tt1")])
            S.dve(lambda e: e.tensor_tensor(out=TOT, in0=PS[4][:, 128:257], in1=TT1, op=ALU.add), reads=[B("ps4n"), B("tt1")], writes=[B("tot")])
            S.dve(lambda e, h=h: e.tensor_tensor(out=sm["dn0"], in0=nbps[:, h:h + 1], in1=sm["mv"], op=ALU.subtract), reads=[bps, bn("mv")], writes=[bn("dn0")])
            S.act(lambda e: e.activation(out=sm["nrm"], in_=sm["dn0"], func=AF.Exp), reads=[bn("dn0")], writes=[bn("nrm")])
            S.act(lambda e: e.activation(out=sm["ad"], in_=TOT[:, 128:129], func=AF.Abs), reads=[B("tot")], writes=[bn("ad")])
            S.dve(lambda e: e.tensor_tensor(out=sm["dn"], in0=sm["ad"], in1=sm["nrm"], op=ALU.max), reads=[bn("ad"), bn("nrm")], writes=[bn("dn")])
            S.dve(lambda e: e.reciprocal(out=sm["rd"], in_=sm["dn"]), reads=[bn("dn")], writes=[bn("rd")])
            S.act(lambda e, h=h: e.activation(out=hdst[:, h * 128:(h + 1) * 128], in_=TOT[:, 0:128], func=AF.Identity, scale=sm["rd"]),
                  reads=[B("tot"), bn("rd")], writes=[bh])

    steps = [("ctxF", 0, 256, 0, 0), ("ctxB", 1, 256, 1, None)]
    for g in range(12):
        steps.append(("oth", g, 512, 2 + g, 2 + 4 * g))
    for g in range(4):
        steps.append(("ownB", 12 + g, 512, 14 + g, None))
    for g in range(4):
        steps.append(("ownF", 16 + g, 512, 18 + g, 50 + 4 * g))

    KG = int(os.environ.get("KG", "99"))
    KSUB = int(os.environ.get("KSUB", "99"))
    if KG == 0:
        raise _Stop()
    for gstep, (kind, src, N, tg, ktb) in enumerate(steps):
        if gstep >= KG:
            raise _Stop()
        last = gstep == KG - 1
        isctx = kind.startswith("ctx")
        mc = 1 if isctx else 0
        if isctx:
            S.dma(lambda e, src=src: e.dma_start(out=XW[:, :, 0:258], in_=d_cwin[src]), writes=[B("xw")])
        else:
            S.dma(lambda e, src=src: e.dma_start(out=XW, in_=d_xwin[src]), writes=[B("xw")])
        for kc in range(8):
            if kc % 2 == 0:
                S.act(lambda e, kc=kc: e.activation(out=HT[:, kc, 0:N + 2], in_=XW[:, kc, 0:N + 2], func=AF.Identity,
                                                    bias=COLS[:, kc, mc:mc + 1], scale=SC1P[:, kc, mc:mc + 1]),
                      reads=[B("xw"), B("cols"), B("sc1p")], writes=[B("ht")])
            else:
                S.pool(lambda e, kc=kc: e.tensor_scalar(out=HT[:, kc, 0:N + 2], in0=XW[:, kc, 0:N + 2], scalar1=SC1P[:, kc, mc:mc + 1],
                                                        scalar2=COLS[:, kc, mc:mc + 1], op0=ALU.mult, op1=ALU.add),
                       reads=[B("xw"), B("cols"), B("sc1p")], writes=[B("ht")])
        if last and KSUB == 0:
            raise _Stop()
        fl = FLAGS[:, tg * 2:tg * 2 + 2]
        for hb in range(4):
            fm_project(C_KM + hb * 128, N, True, hb % 2)
            conv_block(N, hb % 2, hb % 2, KTAPS[:, (tg * 4 + hb) * 3:(tg * 4 + hb) * 3 + 3], CONVB[:, 4 + hb:5 + hb], fl, KT[:, hb, 0:N], False)
        if kind in ("ownB", "ownF"):
            qd = 1 if kind == "ownB" else 0
            for hb in range(4):
                fm_project(C_QM + hb * 128, N, True, hb % 2)
                conv_block(N, hb % 2, hb % 2, QTAPS[:, (qd * 4 + hb) * 3:(qd * 4 + hb) * 3 + 3], CONVB[:, hb:hb + 1], fl, QT[:, hb, 0:N], True)
        if last and KSUB == 1:
            raise _Stop()
        if ktb is not None:
            if not isctx:
                ri = src if kind == "oth" else 12 + (src - 16)
                S.dma(lambda e, ri=ri: e.dma_start(out=ROPE, in_=d_rope[ri].rearrange("a p n -> p a n")), writes=[B("rope")])
            fm_project(C_KA, N, False, 0)
            normrope(N, 0, 1, not isctx, KAT[:, ktb * 128:ktb * 128 + N], B("kat"))
            if kind == "ownF":
                gi = src - 16
                for mcq in range(4):
                    fm_project(C_QA + mcq * 128, N, False, 1)
                    normrope(N, 1, 0, True, QAT[:, mcq, gi * 512:(gi + 1) * 512], B("qat"))
        if last and KSUB == 2:
            raise _Stop()
        for t in range(N // 128):
            ts = slice(1 + t * 128, 1 + (t + 1) * 128)
            for kc in range(8):
                S.pe(lambda e, kc=kc, ts=ts: e.matmul(PS[3][:, 0:512], lhsT=HT[:, kc, ts], rhs=WB[:, kc, C_VM:C_VM + 512], start=(kc == 0), stop=(kc == 7)),
                     reads=[B("ht"), B("wb")], writes=[B("ps3")])
            for kc in range(8):
                S.pe(lambda e, kc=kc, ts=ts: e.matmul(PS[6][:, 0:144], lhsT=HT[:, kc, ts], rhs=WB[:, kc, C_VA:C_VA + 144], start=(kc == 0), stop=(kc == 7)),
                     reads=[B("ht"), B("wb")], writes=[B("ps6")])
            S.act(lambda e: e.activation(out=VAUG[:, :, 0:128], in_=PS[3][:, 0:512].rearrange("p (a b) -> p a b", a=4), func=AF.Copy),
                  reads=[B("ps3")], writes=[B("vaug")])
            if ktb is not None:
                S.act(lambda e, kt_=ktb + t: e.activation(out=VA[:, kt_, :, 0:64], in_=PS[6][:, 0:128].rearrange("p (a b) -> p a b", a=2), func=AF.Copy),
                      reads=[B("ps6")], writes=[B("va")])
            S.dve(lambda e: e.tensor_tensor(out=GP, in0=PS[6][:, 128:144], in1=BG, op=ALU.add), reads=[B("ps6"), B("bg")], writes=[B("gp")])
            for h in range(4):
                S.pe(lambda e, h=h, t=t: e.matmul(PS[5][:, h * 128:(h + 1) * 128], lhsT=KT[:, h, t * 128:(t + 1) * 128], rhs=identb, start=True, stop=True),
                     reads=[B("kt"), bCB], writes=[B("ps5")])
            S.act(lambda e: e.activation(out=KTOK.rearrange("p a b -> p (a b)"), in_=PS[5][:, 0:512], func=AF.Copy), reads=[B("ps5")], writes=[B("ktok")])
            if kind == "ownF":
                for kc in range(8):
                    S.pe(lambda e, kc=kc, ts=ts: e.matmul(PS[3][:, 0:512], lhsT=HT[:, kc, ts], rhs=WB[:, kc, C_O:C_O + 512], start=(kc == 0), stop=(kc == 7)),
                         reads=[B("ht"), B("wb")], writes=[B("ps3")])
                S.act(lambda e: e.activation(out=OSG, in_=PS[3][:, 0:512], func=AF.Sigmoid), reads=[B("ps3")], writes=[B("osg")])
            if last and KSUB == 3:
                raise _Stop()
            dirs = {"ctxF": [0], "ctxB": [1], "oth": [0, 1], "ownB": [1], "ownF": [0]}[kind]
            for d in dirs:
                nbps, nBps, bps = chunk_state(d, src if kind == "oth" else None, t)
                if kind == "ownB":
                    cb = (src - 12) * 4 + t
                    chunk_full(d, t, nbps, bps, MIX[:, cb, 512:1024], B("mixb%d" % cb))
                elif kind == "ownF":
                    chunk_full(d, t, nbps, bps, HF, B("hf"))
                chunk_update(d, nBps, bps)
            if last and KSUB == 4:
                raise _Stop()
            if kind == "ownF":
                oc = (src - 16) * 4 + t
                hm = PS[3][:, 0:512]
                S.pe(lambda e: e.matmul(hm, lhsT=identb, rhs=HF, start=True, stop=False), reads=[bCB, B("hf")], writes=[B("ps3")])
                S.pe(lambda e, oc=oc: e.matmul(hm, lhsT=jb, rhs=MIX[:, 15 - oc, 512:1024], start=False, stop=True), reads=[bCB, B("mixb%d" % (15 - oc))], writes=[B("ps3")])
                hm3 = hm.rearrange("p (a b) -> p a b", a=4)
                S.dve(lambda e: e.tensor_reduce(out=S1, in_=hm3, axis=AX.X, op=ALU.add), reads=[B("ps3")], writes=[B("s1")])
                S.act(lambda e: e.activation(out=HN, in_=hm, func=AF.Square), reads=[B("ps3")], writes=[B("hn")])
                S.dve(lambda e: e.tensor_reduce(out=S2, in_=HN.rearrange("p (a b) -> p a b", a=4), axis=AX.X, op=ALU.add), reads=[B("hn")], writes=[B("s2")])
                if last and KSUB == 5:
                    raise _Stop()
                S.dve(lambda e: e.tensor_scalar(out=MEAN, in0=S1, scalar1=1.0 / 128, scalar2=None, op0=ALU.mult), reads=[B("s1")], writes=[B("mean")])
                S.dve(lambda e: e.tensor_tensor(out=MSQ, in0=MEAN, in1=MEAN, op=ALU.mult), reads=[B("mean")], writes=[B("msq")])
                S.dve(lambda e: e.scalar_tensor_tensor(out=VAR, in0=S2, scalar=1.0 / 128, in1=MSQ, op0=ALU.mult, op1=ALU.subtract), reads=[B("s2"), B("msq")], writes=[B("var")])
                S.act(lambda e: e.activation(out=RSTD, in_=VAR, func=AF.Sqrt, bias=LN_EPS), reads=[B("var")], writes=[B("rstd")])
                S.dve(lambda e: e.reciprocal(out=RSTD, in_=RSTD), reads=[B("rstd")], writes=[B("rstd")])
                if last and KSUB == 6:
                    raise _Stop()
                for h in range(4):
                    S.dve(lambda e, h=h: e.tensor_scalar(out=HN[:, h * 128:(h + 1) * 128], in0=hm[:, h * 128:(h + 1) * 128], scalar1=MEAN[:, h:h + 1],
                                                         scalar2=RSTD[:, h:h + 1], op0=ALU.subtract, op1=ALU.mult),
                          reads=[B("ps3"), B("mean"), B("rstd"), B("s2")], writes=[B("hn")])
                S.pool(lambda e: e.tensor_tensor(out=HN, in0=HN, in1=MHW, op=ALU.mult), reads=[B("hn"), B("mhw")], writes=[B("hn")])
                S.dve(lambda e, oc=oc: e.tensor_tensor(out=MIX[:, oc, 0:512], in0=HN, in1=OSG, op=ALU.mult), reads=[B("hn"), B("osg")], writes=[B("mixa%d" % oc)])

    dd = dbg("mixa", [128, 16, 512], BF16)
    if dd is not None:
        S.dma(lambda e, dd=dd: e.dma_start(out=dd, in_=MIX[:, :, 0:512]), reads=[B("mixa%d" % i) for i in range(16)], writes=[B("dbgo3")])

    if STOP == 1:
        raise _Stop()
    PB = [AR.alloc([128, 512], BF16) for _ in range(2)]
    RDEN = AR.alloc([128, 4])
    allmixb = [B("mixb%d" % i) for i in range(16)]
    for qg in range(4):
        for mcq in range(4):
            for half in range(2):
                hq = half * 4 + mcq
                r0 = half * 64
                for kt in range(66):
                    sb = kt % 2
                    S.pe(lambda e, kt=kt, sb=sb: e.matmul(PS[sb][:, 0:512], lhsT=KAT[r0:r0 + 64, kt * 128:(kt + 1) * 128],
                                                          rhs=QAT[r0:r0 + 64, mcq, qg * 512:(qg + 1) * 512], start=True, stop=True),
                         reads=[B("kat"), B("qat")], writes=[B("ps%d" % sb)])
                    S.act(lambda e, sb=sb: e.activation(out=PB[sb], in_=PS[sb][:, 0:512], func=AF.Exp, scale=0.125), reads=[B("ps%d" % sb)], writes=[B("pb%d" % sb)])
                    for qt in range(4):
                        S.pe(lambda e, kt=kt, sb=sb, qt=qt: e.matmul(PS[4 + qt][:, 0:65], lhsT=PB[sb][:, qt * 128:(qt + 1) * 128], rhs=VA[:, kt, half, :],
                                                                     start=(kt == 0), stop=(kt == 65)),
                             reads=[B("pb%d" % sb), B("va")], writes=[B("pso%d" % qt)])
                for qt in range(4):
                    oc = qg * 4 + qt
                    S.dve(lambda e, qt=qt: e.reciprocal(out=RDEN[:, qt:qt + 1], in_=PS[4 + qt][:, 64:65]), reads=[B("pso%d" % qt)], writes=[B("rden%d" % qt)])
                    S.act(lambda e, qt=qt, oc=oc: e.activation(out=MIX[:, oc, 512 + hq * 64:512 + (hq + 1) * 64], in_=PS[4 + qt][:, 0:64], func=AF.Identity,
                                                               scale=RDEN[:, qt:qt + 1]),
                          reads=[B("pso%d" % qt), B("rden%d" % qt)], writes=[B("att%d" % oc), B("mixb%d" % oc)])

    dd = dbg("att", [128, 16, 512], BF16)
    if dd is not None:
        S.dma(lambda e, dd=dd: e.dma_start(out=dd, in_=MIX[:, :, 512:1024]), reads=[B("att%d" % i) for i in range(16)], writes=[B("dbgo2")])

    if STOP == 2:
        raise _Stop()
    AR.off = mark_persist
    S.barrier()

    WKB = AR.alloc([128, 8, 2048], BF16)
    WOB = AR.alloc([128, 8, 1024], BF16)
    GB = AR.alloc([128, 4, 1024])
    LNP = AR.alloc([128, 4, 1024])
    NSLOT = 12
    UGB = [AR.alloc([128, 1024], BF16) for _ in range(NSLOT)]
    XT = AR.alloc([128, 1024])
    R1 = AR.alloc([128, 1024])
    X1 = AR.alloc([128, 1024])
    XM = AR.alloc([128, 1024])
    SC = AR.alloc([128, 2048])
    MIXT = AR.alloc([128, 8, 128], BF16)
    XMT = AR.alloc([128, 8, 128], BF16)
    TP1 = AR.alloc([128, 16, 16])
    IP1 = AR.alloc([128, 16, 16], U32)
    IP1F = AR.alloc([128, 16, 16])
    TMP = AR.alloc([128, 256])
    CAND = AR.alloc([128, 256])
    TP2 = AR.alloc([128, 8, 16])
    PP2 = AR.alloc([128, 8, 16], U32)
    PJ = AR.alloc([128, 2, 128], U32)
    PJF = AR.alloc([128, 2, 128])
    OH = AR.alloc([128, 2048])
    IDXF = AR.alloc([128, 2, 128])
    EIF = AR.alloc([128, 128])
    EI = AR.alloc([128, 128], I32)
    GEX = AR.alloc([128, 8, 16])
    GSUM = AR.alloc([128, 8])
    DOT = AR.alloc([128, 128])
    COEF = AR.alloc([128, 128])
    DOT2 = AR.alloc([128, 128])
    IOTA = AR.alloc([128, 256])
    JUNK = AR.alloc([128, 1024])
    ST = AR.alloc([128, 8])
    print("phase3 arena bytes", AR.off)

    S.dma(lambda e: e.dma_start(out=LNP.rearrange("p a b -> p (a b)"), in_=d_lnp), writes=[B("lnp")])
    S.dma(lambda e: e.dma_start(out=IOTA, in_=d_iota), writes=[B("iota")], q="pool")
    for kc in range(8):
        st = (X1, XM)[kc % 2]
        bs = B(("x1", "xm")[kc % 2])
        S.dma(lambda e, st=st, kc=kc: e.dma_start(out=st, in_=d_wout[kc]), writes=[bs])
        S.act(lambda e, st=st, kc=kc: e.activation(out=WOB[:, kc, :], in_=st, func=AF.Copy), reads=[bs], writes=[B("wob")])
    KEYB = OH.bitcast(BF16)[:, 0:2048]
    S.dma(lambda e: e.dma_start(out=SC, in_=d_keysT), writes=[B("sc")])
    S.act(lambda e: e.activation(out=KEYB, in_=SC, func=AF.Copy), reads=[B("sc")], writes=[B("oh")])
    WQB = [XT.bitcast(BF16)[:, 0:1024], R1.bitcast(BF16)[:, 0:1024]]
    for blk in range(16):
        st = (X1, XM)[blk % 2]
        bs = B(("x1", "xm")[blk % 2])
        S.dma(lambda e, st=st, blk=blk: e.dma_start(out=st, in_=d_wqT[blk]), writes=[bs])
        S.act(lambda e, st=st, blk=blk: e.activation(out=WQB[blk % 2], in_=st, func=AF.Copy), reads=[bs], writes=[B(("xt", "r1")[blk % 2])])
        for kc in range(8):
            S.pe(lambda e, blk=blk, kc=kc: e.matmul(PS[kc % 2][:, 0:128], lhsT=WQB[blk % 2][:, kc * 128:(kc + 1) * 128], rhs=KEYB[:, blk * 128:(blk + 1) * 128],
                                                    start=True, stop=True),
                 reads=[B(("xt", "r1")[blk % 2]), B("oh")], writes=[B("ps%d" % (kc % 2))])
            if kc % 2 == 0:
                S.dve(lambda e, blk=blk, kc=kc: e.tensor_copy(out=WKB[:, kc, blk * 128:(blk + 1) * 128], in_=PS[kc % 2][:, 0:128]), reads=[B("ps%d" % (kc % 2))], writes=[B("wkb")])
            else:
                S.act(lambda e, blk=blk, kc=kc: e.activation(out=WKB[:, kc, blk * 128:(blk + 1) * 128], in_=PS[kc % 2][:, 0:128], func=AF.Copy),
                      reads=[B("ps%d" % (kc % 2))], writes=[B("wkb")])
    for vi, c0 in enumerate((16, 24, 32, 40)):
        for kc in range(8):
            S.dve(lambda e, c0=c0, kc=kc: e.tensor_scalar(out=JUNK[:, 0:128], in0=ident, scalar1=COLS[:, c0 + kc, 0:1], scalar2=None, op0=ALU.mult),
                  reads=[bC, B("cols")], writes=[B("junk")])
            S.pe(lambda e, kc=kc: e.matmul(PS[2 + kc % 2][:, 0:128], lhsT=ones, rhs=JUNK[:, 0:128], start=True, stop=True), reads=[bC, B("junk")], writes=[B("ps%d" % (2 + kc % 2))])
            if vi == 2:
                S.act(lambda e, vi=vi, kc=kc: e.activation(out=GB[:, vi, kc * 128:(kc + 1) * 128], in_=PS[2 + kc % 2][:, 0:128], func=AF.Identity, bias=1.0),
                      reads=[B("ps%d" % (2 + kc % 2))], writes=[B("gb")])
            else:
                S.act(lambda e, vi=vi, kc=kc: e.activation(out=GB[:, vi, kc * 128:(kc + 1) * 128], in_=PS[2 + kc % 2][:, 0:128], func=AF.Copy),
                      reads=[B("ps%d" % (2 + kc % 2))], writes=[B("gb")])

    def layer_norm(src, dst, gi, bi, bsrc, bdst):
        S.act(lambda e: e.activation(out=JUNK, in_=src, func=AF.Copy, accum_out=ST[:, 0:1]), reads=[bsrc], writes=[B("junk"), B("st")])
        S.act(lambda e: e.activation(out=JUNK, in_=src, func=AF.Square, accum_out=ST[:, 1:2]), reads=[bsrc], writes=[B("junk"), B("st")])
        S.dve(lambda e: e.tensor_scalar(out=ST[:, 2:3], in0=ST[:, 0:1], scalar1=1.0 / 1024, scalar2=None, op0=ALU.mult), reads=[B("st")], writes=[B("st")])
        S.dve(lambda e: e.tensor_tensor(out=ST[:, 3:4], in0=ST[:, 2:3], in1=ST[:, 2:3], op=ALU.mult), reads=[B("st")], writes=[B("st")])
        S.dve(lambda e: e.scalar_tensor_tensor(out=ST[:, 4:5], in0=ST[:, 1:2], scalar=1.0 / 1024, in1=ST[:, 3:4], op0=ALU.mult, op1=ALU.subtract), reads=[B("st")], writes=[B("st")])
        S.act(lambda e: e.activation(out=ST[:, 5:6], in_=ST[:, 4:5], func=AF.Sqrt, bias=LN_EPS), reads=[B("st")], writes=[B("st")])
        S.dve(lambda e: e.reciprocal(out=ST[:, 6:7], in_=ST[:, 5:6]), reads=[B("st")], writes=[B("st")])
        S.dve(lambda e: e.tensor_scalar(out=dst, in0=src, scalar1=ST[:, 2:3], scalar2=ST[:, 6:7], op0=ALU.subtract, op1=ALU.mult), reads=[bsrc, B("st")], writes=[bdst])
        S.pool(lambda e: e.tensor_tensor(out=dst, in0=dst, in1=LNP[:, gi, :], op=ALU.mult), reads=[bdst, B("lnp")], writes=[bdst])
        S.pool(lambda e: e.tensor_tensor(out=dst, in0=dst, in1=LNP[:, bi, :], op=ALU.add), reads=[bdst, B("lnp")], writes=[bdst])

    dd_x1 = dbg("x1", [2048, 1024])
    dd_y = dbg("y", [2048, 1024])
    gcount = [0]
    XMP = [PS[0], PS[1]]
    YP = [PS[2], PS[3]]
    for tt in range(16):
        S.dma(lambda e, tt=tt: e.dma_start(out=XT, in_=d_xown[tt * 128:(tt + 1) * 128, :]), writes=[B("xt")])
        for kc in range(8):
            S.pe(lambda e, kc=kc, tt=tt: e.matmul(PS[6 + kc // 4][:, (kc % 4) * 128:(kc % 4 + 1) * 128], lhsT=MIX[:, tt, kc * 128:(kc + 1) * 128], rhs=identb,
                                                  start=True, stop=True),
                 reads=[B("mixa%d" % tt), B("att%d" % tt), bCB], writes=[B("ps%d" % (6 + kc // 4))])
        for hf in range(2):
            S.act(lambda e, hf=hf: e.activation(out=MIXT[:, hf * 4:(hf + 1) * 4, :].rearrange("p a b -> p (a b)"), in_=PS[6 + hf][:, 0:512], func=AF.Copy),
                  reads=[B("ps%d" % (6 + hf))], writes=[B("mixt")])
        for hf in range(2):
            for kc in range(8):
                S.pe(lambda e, hf=hf, kc=kc: e.matmul(PS[4 + hf][:, 0:512], lhsT=MIXT[:, kc, :], rhs=WOB[:, kc, hf * 512:(hf + 1) * 512], start=(kc == 0), stop=(kc == 7)),
                     reads=[B("mixt"), B("wob")], writes=[B("ps%d" % (4 + hf))])
        for hf in range(2):
            S.dve(lambda e, hf=hf: e.tensor_tensor(out=R1[:, hf * 512:(hf + 1) * 512], in0=PS[4 + hf][:, 0:512], in1=GB[:, 0, hf * 512:(hf + 1) * 512], op=ALU.mult),
                  reads=[B("ps%d" % (4 + hf)), B("gb")], writes=[B("r1")])
        S.dve(lambda e: e.scalar_tensor_tensor(out=R1, in0=XT, scalar=ALPHA, in1=R1, op0=ALU.mult, op1=ALU.add), reads=[B("xt"), B("r1")], writes=[B("r1")])
        layer_norm(R1, X1, 0, 1, B("r1"), B("x1"))
        if dd_x1 is not None:
            S.dma(lambda e, tt=tt: e.dma_start(out=dd_x1[tt * 128:(tt + 1) * 128, :], in_=X1), reads=[B("x1")], writes=[B("dbgx%d" % tt)])
        S.dve(lambda e: e.tensor_tensor(out=XM, in0=X1, in1=GB[:, 2, :], op=ALU.mult), reads=[B("x1"), B("gb")], writes=[B("xm")])
        S.dve(lambda e: e.tensor_tensor(out=XM, in0=XM, in1=GB[:, 1, :], op=ALU.add), reads=[B("xm"), B("gb")], writes=[B("xm")])
        for hf in range(2):
            S.act(lambda e, hf=hf: e.activation(out=XMP[hf][:, 0:512], in_=XM[:, hf * 512:(hf + 1) * 512], func=AF.Copy), reads=[B("xm")], writes=[B("ps%d" % hf)])
        for kc in range(8):
            S.pe(lambda e, kc=kc: e.matmul(PS[6 + kc // 4][:, (kc % 4) * 128:(kc % 4 + 1) * 128], lhsT=XM[:, kc * 128:(kc + 1) * 128], rhs=ident, start=True, stop=True),
                 reads=[B("xm"), bC], writes=[B("ps%d" % (6 + kc // 4))])
        for hf in range(2):
            S.act(lambda e, hf=hf: e.activation(out=XMT[:, hf * 4:(hf + 1) * 4, :].rearrange("p a b -> p (a b)"), in_=PS[6 + hf][:, 0:512], func=AF.Copy),
                  reads=[B("ps%d" % (6 + hf))], writes=[B("xmt")])
        for nb in range(4):
            for kc in range(8):
                S.pe(lambda e, nb=nb, kc=kc: e.matmul(PS[4 + nb][:, 0:512], lhsT=XMT[:, kc, :], rhs=WKB[:, kc, nb * 512:(nb + 1) * 512], start=(kc == 0), stop=(kc == 7)),
                     reads=[B("xmt"), B("wkb")], writes=[B("ps%d" % (4 + nb))])
            S.act(lambda e, nb=nb: e.activation(out=SC[:, nb * 512:(nb + 1) * 512], in_=PS[4 + nb][:, 0:512], func=AF.Copy), reads=[B("ps%d" % (4 + nb))], writes=[B("sc")])
        for blk in range(16):
            sc = SC[:, blk * 128:(blk + 1) * 128]
            S.dve(lambda e, blk=blk, sc=sc: e.max(out=TP1[:, blk, 0:8], in_=sc), reads=[B("sc")], writes=[B("tp1")])
            S.dve(lambda e, blk=blk, sc=sc: e.max_index(out=IP1[:, blk, 0:8], in_max=TP1[:, blk, 0:8], in_values=sc), reads=[B("sc"), B("tp1")], writes=[B("ip1")])
            S.dve(lambda e, blk=blk, sc=sc: e.match_replace(out=TMP[:, 0:128], in_to_replace=TP1[:, blk, 0:8], in_values=sc, imm_value=-1e30),
                  reads=[B("sc"), B("tp1")], writes=[B("tmp")])
            S.dve(lambda e, blk=blk: e.max(out=TP1[:, blk, 8:16], in_=TMP[:, 0:128]), reads=[B("tmp")], writes=[B("tp1")])
            S.dve(lambda e, blk=blk: e.max_index(out=IP1[:, blk, 8:16], in_max=TP1[:, blk, 8:16], in_values=TMP[:, 0:128]), reads=[B("tmp"), B("tp1")], writes=[B("ip1")])
        S.dve(lambda e: e.tensor_copy(out=IP1F, in_=IP1), reads=[B("ip1")], writes=[B("ip1f")])
        for h in range(8):
            S.dve(lambda e, h=h: e.tensor_tensor(out=CAND.rearrange("p (a b) -> p a b", a=16), in0=TP1[:, 2 * h, :].unsqueeze(2).to_broadcast([128, 16, 16]),
                                                 in1=TP1[:, 2 * h + 1, :].unsqueeze(1).to_broadcast([128, 16, 16]), op=ALU.add),
                  reads=[B("tp1")], writes=[B("cand")])
            S.dve(lambda e, h=h: e.max(out=TP2[:, h, 0:8], in_=CAND), reads=[B("cand")], writes=[B("tp2")])
            S.dve(lambda e, h=h: e.max_index(out=PP2[:, h, 0:8], in_max=TP2[:, h, 0:8], in_values=CAND), reads=[B("cand"), B("tp2")], writes=[B("pp2")])
            S.dve(lambda e, h=h: e.match_replace(out=TMP, in_to_replace=TP2[:, h, 0:8], in_values=CAND, imm_value=-1e30), reads=[B("cand"), B("tp2")], writes=[B("tmp")])
            S.dve(lambda e, h=h: e.max(out=TP2[:, h, 8:16], in_=TMP), reads=[B("tmp")], writes=[B("tp2")])
            S.dve(lambda e, h=h: e.max_index(out=PP2[:, h, 8:16], in_max=TP2[:, h, 8:16], in_values=TMP), reads=[B("tmp"), B("tp2")], writes=[B("pp2")])
        pp2f = PP2.rearrange("p a b -> p (a b)")
        S.dve(lambda e: e.tensor_single_scalar(out=PJ[:, 0, :], in_=pp2f, scalar=4, op=ALU.logical_shift_right), reads=[B("pp2")], writes=[B("pj")])
        S.dve(lambda e: e.tensor_single_scalar(out=PJ[:, 1, :], in_=pp2f, scalar=15, op=ALU.bitwise_and), reads=[B("pp2")], writes=[B("pj")])
        S.dve(lambda e: e.tensor_copy(out=PJF, in_=PJ), reads=[B("pj")], writes=[B("pjf")])
        for p in range(2):
            for h in range(8):
                oh = OH[:, h * 256:(h + 1) * 256].rearrange("p (a b) -> p a b", a=16)
                S.dve(lambda e, p=p, h=h, oh=oh: e.tensor_tensor(out=oh, in0=IOTA.rearrange("p (a b) -> p a b", a=16),
                                                                 in1=PJF[:, p, h * 16:(h + 1) * 16].unsqueeze(2).to_broadcast([128, 16, 16]), op=ALU.is_equal),
                      reads=[B("iota"), B("pjf")], writes=[B("oh")])
                S.dve(lambda e, p=p, h=h, oh=oh: e.tensor_tensor(out=oh, in0=oh, in1=IP1F[:, 2 * h + p, :].unsqueeze(1).to_broadcast([128, 16, 16]), op=ALU.mult),
                      reads=[B("oh"), B("ip1f")], writes=[B("oh")])
            S.dve(lambda e, p=p: e.tensor_reduce(out=IDXF[:, p, :], in_=OH.rearrange("p (a b) -> p a b", a=128), axis=AX.X, op=ALU.add), reads=[B("oh")], writes=[B("idxf")])
        S.dve(lambda e: e.scalar_tensor_tensor(out=EIF, in0=IDXF[:, 0, :], scalar=128.0, in1=IDXF[:, 1, :], op0=ALU.mult, op1=ALU.add), reads=[B("idxf")], writes=[B("eif")])
        S.dve(lambda e: e.tensor_copy(out=EI, in_=EIF), reads=[B("eif")], writes=[B("ei")])
        S.dve(lambda e: e.tensor_tensor(out=GEX, in0=TP2, in1=TP2[:, :, 0:1].to_broadcast([128, 8, 16]), op=ALU.subtract), reads=[B("tp2")], writes=[B("gex")])
        S.act(lambda e: e.activation(out=GEX, in_=GEX, func=AF.Exp), reads=[B("gex")], writes=[B("gex")])
        S.dve(lambda e: e.tensor_reduce(out=GSUM, in_=GEX, axis=AX.X, op=ALU.add), reads=[B("gex")], writes=[B("gsum")])
        S.dve(lambda e: e.reciprocal(out=GSUM, in_=GSUM), reads=[B("gsum")], writes=[B("gsum")])
        S.dve(lambda e: e.tensor_tensor(out=GEX, in0=GEX, in1=GSUM.unsqueeze(2).to_broadcast([128, 8, 16]), op=ALU.mult), reads=[B("gex"), B("gsum")], writes=[B("gex")])
        allpub = [B("pub%d" % r) for r in range(0, 16384, 2048)]
        allpvb = [B("pvb%d" % r) for r in range(0, 16384, 2048)]
        for sl in range(128):
            ug = UGB[gcount[0] % NSLOT]
            bu = B("ugb%d" % (gcount[0] % NSLOT))
            gcount[0] += 1
            S.dma(lambda e, ug=ug, sl=sl: e.indirect_dma_start(out=ug, out_offset=None, in_=d_pub,
                                                                in_offset=bass.IndirectOffsetOnAxis(ap=EI[:, sl:sl + 1], axis=0)),
                  reads=[B("ei")] + allpub, writes=[bu], q="pool")
            for hf in range(2):
                S.dve(lambda e, ug=ug, hf=hf, sl=sl: e.scalar_tensor_tensor(out=JUNK[:, hf * 512:(hf + 1) * 512], in0=ug[:, hf * 512:(hf + 1) * 512], scalar=1.0,
                                                                            in1=XMP[hf][:, 0:512], op0=ALU.mult, op1=ALU.mult,
                                                                            accum_out=DOT[:, sl:sl + 1] if hf == 0 else DOT2[:, sl:sl + 1]),
                      reads=[bu, B("ps%d" % hf)], writes=[B("junk"), B("dot")])
        S.dve(lambda e: e.tensor_tensor(out=DOT, in0=DOT, in1=DOT2, op=ALU.add), reads=[B("dot")], writes=[B("dot")])
        S.act(lambda e: e.activation(out=DOT, in_=DOT, func=AF.Gelu), reads=[B("dot")], writes=[B("dot")])
        S.dve(lambda e: e.tensor_tensor(out=COEF, in0=DOT, in1=GEX.rearrange("p a b -> p (a b)"), op=ALU.mult), reads=[B("dot"), B("gex")], writes=[B("coef")])
        for hf in range(2):
            S.dve(lambda e, hf=hf: e.memset(YP[hf][:, 0:512], 0.0), writes=[B("ps%d" % (2 + hf))])
        for sl in range(128):
            ug = UGB[gcount[0] % NSLOT]
            bu = B("ugb%d" % (gcount[0] % NSLOT))
            gcount[0] += 1
            S.dma(lambda e, ug=ug, sl=sl: e.indirect_dma_start(out=ug, out_offset=None, in_=d_pvb,
                                                                in_offset=bass.IndirectOffsetOnAxis(ap=EI[:, sl:sl + 1], axis=0)),
                  reads=[B("ei")] + allpvb, writes=[bu], q="pool")
            for hf in range(2):
                S.dve(lambda e, ug=ug, hf=hf, sl=sl: e.scalar_tensor_tensor(out=YP[hf][:, 0:512], in0=ug[:, hf * 512:(hf + 1) * 512], scalar=COEF[:, sl:sl + 1],
                                                                            in1=YP[hf][:, 0:512], op0=ALU.mult, op1=ALU.add),
                      reads=[bu, B("coef"), B("ps%d" % (2 + hf))], writes=[B("ps%d" % (2 + hf))])
        if dd_y is not None:
            for hf in range(2):
                S.act(lambda e, hf=hf: e.activation(out=JUNK[:, hf * 512:(hf + 1) * 512], in_=YP[hf][:, 0:512], func=AF.Copy), reads=[B("ps%d" % (2 + hf))], writes=[B("junk")])
            S.dma(lambda e, tt=tt: e.dma_start(out=dd_y[tt * 128:(tt + 1) * 128, :], in_=JUNK), reads=[B("junk")], writes=[B("dbgy%d" % tt)])
        for hf in range(2):
            S.dve(lambda e, hf=hf: e.tensor_tensor(out=R1[:, hf * 512:(hf + 1) * 512], in0=YP[hf][:, 0:512], in1=GB[:, 3, hf * 512:(hf + 1) * 512], op=ALU.mult),
                  reads=[B("ps%d" % (2 + hf)), B("gb")], writes=[B("r1")])
        S.dve(lambda e: e.scalar_tensor_tensor(out=R1, in0=X1, scalar=ALPHA, in1=R1, op0=ALU.mult, op1=ALU.add), reads=[B("x1"), B("r1")], writes=[B("r1")])
        layer_norm(R1, XM, 2, 3, B("r1"), B("xm"))
        S.dma(lambda e, tt=tt: e.dma_start(out=d_out[tt * 128:(tt + 1) * 128, :], in_=XM), reads=[B("xm")], writes=[B("out%d" % tt)])


def _win(xb, lo, n, rev):
    L = xb.shape[0]
    w = np.zeros((n + 2, 1024), np.float32)
    a, b = lo - 1, lo + n + 1
    sa, sb = max(a, 0), min(b, L)
    w[sa - a:sb - a] = xb[sa:sb]
    if rev:
        w = w[::-1]
    return np.ascontiguousarray(w.reshape(n + 2, 8, 128).transpose(2, 1, 0))


def _rope_tables(tok):
    half = 16
    freq = (10000.0 ** (-np.arange(half, dtype=np.float32) / half)).astype(np.float32)
    row = (tok // 64).astype(np.float32)
    col = (tok % 64).astype(np.float32)
    cos = np.zeros((64, len(tok)), np.float32)
    sin = np.zeros((64, len(tok)), np.float32)
    for dd in range(64):
        pos = row if dd < 32 else col
        ang = pos * freq[dd % 16]
        cos[dd] = np.cos(ang)
        sin[dd] = np.sin(ang)
    return np.concatenate([cos, cos], 0), np.concatenate([sin, sin], 0)


_CACHE = {}


def kernel(x, c, ctx, c_ctx, w_mod, b_mod, w_in, conv_w, conv_b, b_gates, mh_norm_w, q_norm_w, k_norm_w, w_out,
           ln1_g, ln1_b, peer_wq, peer_keys, peer_u, peer_v, ln2_g, ln2_b):
    f32 = np.float32
    x = np.asarray(x, f32); c = np.asarray(c, f32); ctx = np.asarray(ctx, f32); c_ctx = np.asarray(c_ctx, f32)
    w_mod = np.asarray(w_mod, f32)[0]; b_mod = np.asarray(b_mod, f32)[0]; w_in = np.asarray(w_in, f32)[0]
    conv_w = np.asarray(conv_w, f32)[0]; conv_b = np.asarray(conv_b, f32)[0]; b_gates = np.asarray(b_gates, f32)[0]
    mh_norm_w = np.asarray(mh_norm_w, f32)[0]; q_norm_w = np.asarray(q_norm_w, f32)[0]; k_norm_w = np.asarray(k_norm_w, f32)[0]
    w_out = np.asarray(w_out, f32)[0]; ln1_g = np.asarray(ln1_g, f32)[0]; ln1_b = np.asarray(ln1_b, f32)[0]
    peer_wq = np.asarray(peer_wq, f32)[0]; peer_keys = np.asarray(peer_keys, f32)[0]
    peer_u = np.asarray(peer_u, f32)[0]; peer_v = np.asarray(peer_v, f32)[0]
    ln2_g = np.asarray(ln2_g, f32)[0]; ln2_b = np.asarray(ln2_b, f32)[0]

    if "nc" not in _CACHE:
        _CACHE["nc"] = build_program()
    nc, dbg_names = _CACHE["nc"]

    rep = lambda v: np.ascontiguousarray(np.broadcast_to(np.asarray(v, f32).reshape(1, -1), (128, np.asarray(v).size)))
    wmod_l = np.ascontiguousarray(w_mod.reshape(8, 128, 12, 512).transpose(2, 1, 0, 3))
    bmod_l = np.ascontiguousarray(np.broadcast_to(b_mod.reshape(12, 1, 512), (12, 2, 512)))
    qa_cols = []
    for mc in range(4):
        qa_cols += list(range(2064 + mc * 64, 2064 + (mc + 1) * 64)) + list(range(2064 + (4 + mc) * 64, 2064 + (5 + mc) * 64))
    perm = (list(range(512, 1024)) + list(range(0, 512)) + list(range(2576, 2704)) + qa_cols + list(range(1024, 1536))
            + list(range(2704, 2832)) + list(range(2048, 2064)) + list(range(1536, 2048)))
    win_l = np.ascontiguousarray(w_in[:, perm].reshape(8, 128, 2832))
    convb_l = np.ascontiguousarray(conv_b.reshape(8, 128).T)
    cw = conv_w.reshape(3, 8, 128)
    taps_nat = np.ascontiguousarray(cw.transpose(2, 1, 0))
    taps_rev = np.ascontiguousarray(taps_nat[:, :, ::-1])
    qtaps_l = np.ascontiguousarray(np.stack([taps_nat[:, 0:4], taps_rev[:, 0:4]], 1).reshape(128, 24))
    bg_l = rep(b_gates)
    mhw_l = rep(mh_norm_w)
    nw_l = np.ascontiguousarray(np.stack([np.tile(q_norm_w, 2), np.tile(k_norm_w, 2)], 1))
    wout_l = np.ascontiguousarray(w_out.reshape(8, 128, 1024))
    wqT_l = np.ascontiguousarray(peer_wq.T.reshape(16, 128, 1024))
    keysT_l = np.ascontiguousarray(peer_keys.reshape(16, 128, 128).transpose(2, 0, 1).reshape(128, 2048))
    lnp_l = np.ascontiguousarray(np.concatenate([rep(ln1_g), rep(ln1_b), rep(ln2_g), rep(ln2_b)], 1))
    ii = np.arange(128)
    ident = np.eye(128, dtype=f32)
    tri = (ii[:, None] <= ii[None, :]).astype(f32)
    mask_sr = np.where(ii[None, :] <= ii[:, None], 0.0, NEG).astype(f32)
    mask_rs = np.where(ii[:, None] <= ii[None, :], 0.0, NEG).astype(f32)
    jm = ident[::-1].copy()
    blk64 = (ii[:, None] // 64 == ii[None, :] // 64).astype(f32)
    R = np.zeros((128, 128), f32)
    for i in range(128):
        if (i % 32) < 16:
            R[i, i + 16] = -1.0
        else:
            R[i, i - 16] = 1.0
    sel0 = np.zeros((128, 128), f32)
    sel0[0, :] = 1.0
    cst_l = np.ascontiguousarray(np.concatenate([ident, np.ones((128, 128), f32), tri, mask_sr, mask_rs, jm, blk64, R.T.copy(), sel0], 1))
    iota_l = np.ascontiguousarray(np.broadcast_to(np.tile(np.arange(16, dtype=f32), 16).reshape(1, 256), (128, 256)))

    in_maps = []
    for core in range(NCORES):
        b, j = divmod(core, 4)
        xb = x[b]
        cwin = np.stack([_win(ctx[b], 0, 256, False), _win(ctx[b], 0, 256, True)], 0)
        oth = [(G, False) for G in range(0, 4 * j)] + [(G, True) for G in range(15, 4 * j + 3, -1)]
        ownB = [(G, True) for G in range(4 * j + 3, 4 * j - 1, -1)]
        ownF = [(G, False) for G in range(4 * j, 4 * j + 4)]
        srcs = oth + ownB + ownF
        assert len(oth) == 12 and len(srcs) == NGRP
        xwin = np.stack([_win(xb, 512 * G, 512, rv) for (G, rv) in srcs], 0)
        ktaps = np.zeros((128, 22, 4, 3), f32)
        flags = np.zeros((128, 22, 2), f32)
        ktaps[:, 0] = taps_nat[:, 4:8]
        ktaps[:, 1] = taps_rev[:, 4:8]
        for gi, (G, rv) in enumerate(srcs):
            ktaps[:, 2 + gi] = (taps_rev if rv else taps_nat)[:, 4:8]
            fl = [0.0 if G == 0 else 1.0, 0.0 if G == 15 else 1.0]
            flags[:, 2 + gi] = fl[::-1] if rv else fl
        gmask = np.zeros((128, 12, 4), f32)
        for gi, (G, rv) in enumerate(oth):
            fwd_real = not rv
            gmask[:, gi] = [1.0, 0.0, 0.0, NEG] if fwd_real else [0.0, NEG, 1.0, 0.0]
        rope = np.zeros((16, 2, 128, 512), f32)
        for ri, (G, rv) in enumerate(oth + ownF):
            tok = np.arange(512 * G, 512 * G + 512)
            if rv:
                tok = tok[::-1]
            cs, sn = _rope_tables(tok)
            rope[ri, 0] = cs
            rope[ri, 1] = sn
        cvec = np.ascontiguousarray(np.stack([c[b].reshape(8, 128).T, c_ctx.reshape(8, 128).T], 2).reshape(128, 16))
        in_maps.append(dict(
            cwin=cwin, xwin=xwin, xown=np.ascontiguousarray(xb[2048 * j:2048 * (j + 1)]), wmod=wmod_l, bmod=bmod_l, cvec=cvec,
            win=win_l, ktaps=np.ascontiguousarray(ktaps.reshape(128, 264)), qtaps=qtaps_l, convb=convb_l,
            flags=np.ascontiguousarray(flags.reshape(128, 44)), gmask=np.ascontiguousarray(gmask.reshape(128, 48)), bg=bg_l, mhw=mhw_l, nw=nw_l,
            wout=wout_l, wqT=wqT_l, keysT=keysT_l, lnp=lnp_l, pu=peer_u, pv=peer_v, cst=cst_l, iota16=iota_l, rope=rope))
    res = run_bass_kernel_spmd(nc, in_maps[:NCORES], core_ids=list(range(NCORES)))
    out = np.zeros((2, 8192, 1024), f32)
    for core in range(NCORES):
        b, j = divmod(core, 4)
        out[b, 2048 * j:2048 * (j + 1)] = res.results[core]["out"]
    if dbg_names:
        _CACHE["dbg"] = [{n: res.results[core]["dbg_" + n] for n in dbg_names} for core in range(NCORES)]
    return out
```

```python
import os
import types
import numpy as np
import concourse.bass as bass
import concourse.mybir as mybir
from concourse.bass_utils import run_bass_kernel_spmd

F32 = mybir.dt.float32
BF16 = mybir.dt.bfloat16
I32 = mybir.dt.int32
U32 = mybir.dt.uint32
ALU = mybir.AluOpType
AF = mybir.ActivationFunctionType
AX = mybir.AxisListType

NEG = -30000.0
LN_EPS = 1e-5
RMS_EPS = 1e-6
ALPHA = 2.0 ** 0.25
NGRP = 20
DBG = os.environ.get("KDBG", "")


class Buf:
    __slots__ = ("name", "lw", "rd")

    def __init__(self, name):
        self.name = name
        self.lw = None
        self.rd = []


class Op:
    __slots__ = ("eng", "fn", "reads", "writes", "dma", "deps", "signal", "sem", "val", "slot_prev", "bar")

    def __init__(self, eng, fn, reads, writes, dma):
        self.eng = eng
        self.fn = fn
        self.reads = reads
        self.writes = writes
        self.dma = dma
        self.deps = set()
        self.signal = False
        self.sem = None
        self.val = 0
        self.slot_prev = None
        self.bar = 0


def _freeze(fn):
    if fn is None or fn.__closure__ is None:
        return fn
    cells = []
    for c in fn.__closure__:
        try:
            cells.append(types.CellType(c.cell_contents))
        except ValueError:
            cells.append(c)
    return types.FunctionType(fn.__code__, fn.__globals__, fn.__name__, fn.__defaults__, tuple(cells))


class Sched:
    ENGS = ("pe", "act", "dve", "pool", "sp")

    def __init__(self, nc, n_dma_sems=32):
        self.nc = nc
        self.ops = []
        self.n_dma_sems = n_dma_sems
        self.nbar = 0
        self.bank = {}

    def op(self, eng, fn, reads=(), writes=(), dma=False):
        reads = [b for b in reads if b is not None]
        writes = [b for b in writes if b is not None]
        banks = set()
        for b in reads + writes:
            n = b.name
            if n.startswith("pso") and n[3:4].isdigit():
                banks.add(4 + int(n[3]))
            elif n.startswith("ps") and n[2:3].isdigit():
                banks.add(int(n[2]))
        for k in sorted(banks):
            if k not in self.bank:
                self.bank[k] = Buf("BANK%d" % k)
            writes.append(self.bank[k])
        o = Op(eng, _freeze(fn), reads, writes, dma)
        o.bar = self.nbar
        self.ops.append(o)
        return o

    def pe(self, fn, reads=(), writes=()):
        return self.op("pe", fn, reads, writes)

    def act(self, fn, reads=(), writes=()):
        return self.op("act", fn, reads, writes)

    def dve(self, fn, reads=(), writes=()):
        return self.op("dve", fn, reads, writes)

    def pool(self, fn, reads=(), writes=()):
        return self.op("pool", fn, reads, writes)

    def dma(self, fn, reads=(), writes=(), q="sp"):
        return self.op(q, fn, reads, writes, dma=True)

    def barrier(self):
        self.nbar += 1

    def _resolve(self):
        ops = self.ops
        last_eng = {}
        dma_all = []
        bar_deps = {}
        seen_bar = {e: 0 for e in self.ENGS}
        cur_bar = 0
        for i, o in enumerate(ops):
            if o.bar != cur_bar:
                cur_bar = o.bar
                bar_deps[cur_bar] = list(last_eng.values()) + list(dma_all)
            deps = set()
            raw = set()
            for b in o.reads:
                if b.lw is not None:
                    deps.add(b.lw)
                    raw.add(b.lw)
            for b in o.writes:
                if b.lw is not None:
                    deps.add(b.lw)
                for r in b.rd:
                    deps.add(r)
            for b in o.reads:
                b.rd.append(i)
            for b in o.writes:
                b.lw = i
                b.rd = []
            deps.discard(i)
            for j in deps:
                p = ops[j]
                if (not p.dma) and (not o.dma) and p.eng == o.eng:
                    if o.eng == "pe" or j not in raw:
                        continue
                o.deps.add(j)
                p.signal = True
            if seen_bar[o.eng] != cur_bar:
                seen_bar[o.eng] = cur_bar
                for j in bar_deps[cur_bar]:
                    p = ops[j]
                    if (not p.dma) and (not o.dma) and p.eng == o.eng:
                        continue
                    o.deps.add(j)
                    p.signal = True
            if o.dma:
                dma_all.append(i)
            elif o.fn is not None:
                last_eng[o.eng] = i

    def emit(self):
        nc = self.nc
        self._resolve()
        ops = self.ops
        esem = {e: nc.alloc_semaphore("s_" + e) for e in self.ENGS}
        nds = self.n_dma_sems
        dsem = [nc.alloc_semaphore("s_dma%d" % k) for k in range(2 * nds)]
        ecount = {e: 0 for e in self.ENGS}
        dcount = [0] * (2 * nds)
        dlast = [None] * (2 * nds)
        nd = 0
        ndq = {"sp": 0, "pool": 0, "act": 0}
        for i, o in enumerate(ops):
            if o.dma:
                base = nds if o.eng == "pool" else 0
                k = base + ndq[o.eng] % nds
                ndq[o.eng] += 1
                nd += 1
                o.signal = True
                o.sem = ("d", k)
                dcount[k] += 16
                o.val = dcount[k]
                o.slot_prev = dlast[k]
                dlast[k] = i
            elif o.signal:
                ecount[o.eng] += 1
                o.sem = ("e", o.eng)
                o.val = ecount[o.eng]
        self.stats = dict(ecount)
        self.stats["ndma"] = nd
        self.stats["nops"] = len(ops)

        def semh(s):
            return esem[s[1]] if s[0] == "e" else dsem[s[1]]

        per_eng = {e: [] for e in self.ENGS}
        for i, o in enumerate(ops):
            per_eng[o.eng].append(i)

        def emit_stream(ename, eng):
            waited = {}
            for i in per_eng[ename]:
                o = ops[i]
                need = {}
                for j in o.deps:
                    p = ops[j]
                    if need.get(p.sem, 0) < p.val:
                        need[p.sem] = p.val
                if o.dma and o.slot_prev is not None:
                    p = ops[o.slot_prev]
                    if need.get(p.sem, 0) < p.val:
                        need[p.sem] = p.val
                for s, v in need.items():
                    if waited.get(s, 0) >= v:
                        continue
                    eng.wait_ge(semh(s), v)
                    waited[s] = v
                if o.fn is None:
                    continue
                ins = o.fn(eng)
                if o.signal:
                    ins.then_inc(semh(o.sem), 16 if o.dma else 1)

        with nc.Block() as block:
            @block.tensor
            def _(e):
                emit_stream("pe", e)

            @block.scalar
            def _(e):
                emit_stream("act", e)

            @block.vector
            def _(e):
                emit_stream("dve", e)

            @block.gpsimd
            def _(e):
                emit_stream("pool", e)

            @block.sync
            def _(e):
                emit_stream("sp", e)


class Arena:
    def __init__(self, nc, nbytes):
        self.t = nc.alloc_sbuf_tensor("arena", [128, nbytes // 4], F32)
        self.off = 0
        self.cap = nbytes
        self.hi = 0

    def alloc(self, shape, dtype=F32):
        esz = 2 if dtype == BF16 else 4
        n = 1
        for s in shape[1:]:
            n *= s
        nb = (n * esz + 31) // 32 * 32
        o = self.off
        self.off += nb
        self.hi = max(self.hi, self.off)
        assert self.off <= self.cap, ("arena overflow", self.off, self.cap)
        v = self.t[:, o // 4:(o + nb) // 4]
        if dtype != F32:
            v = v.bitcast(dtype)
        v = v[:, 0:n]
        if len(shape) == 3:
            v = v.rearrange("p (a b) -> p a b", a=shape[1])
        elif len(shape) == 4:
            v = v.rearrange("p (a b c) -> p a b c", a=shape[1], b=shape[2])
        if shape[0] != 128:
            v = v[0:shape[0]]
        return v


class _Stop(Exception):
    pass


STOP = int(os.environ.get("KSTOP", "9"))
NCORES = int(os.environ.get("KCORES", "8"))


def build_program():
    st = {}
    try:
        _body(st)
    except _Stop:
        pass
    S = st["S"]
    B = st["B"]
    S.op("sp", None, reads=[B("out%d" % i) for i in range(16)] + [B("dbgo"), B("dbgo2"), B("dbgo3")] + [B("dbgx%d" % i) for i in range(16)] + [B("dbgy%d" % i) for i in range(16)])
    S.emit()
    print("sched stats", S.stats)
    return st["nc"], list(st["dbg_out"].keys())


def _body(st):
    nc = bass.Bass("TRN2", target_bir_lowering=False)
    S = Sched(nc)
    bufs = {}
    st["nc"] = nc
    st["S"] = S

    def B(name):
        b = bufs.get(name)
        if b is None:
            b = bufs[name] = Buf(name)
        return b

    def din(name, shape, dt=F32):
        return nc.dram_tensor(name, list(shape), dt, kind="ExternalInput").ap()

    d_cwin = din("cwin", [2, 128, 8, 258])
    d_xwin = din("xwin", [NGRP, 128, 8, 514])
    d_xown = din("xown", [2048, 1024])
    d_wmod = din("wmod", [12, 128, 8, 512])
    d_bmod = din("bmod", [12, 2, 512])
    d_cvec = din("cvec", [128, 16])
    d_win = din("win", [8, 128, 2832])
    d_ktaps = din("ktaps", [128, 22 * 12])
    d_qtaps = din("qtaps", [128, 2 * 12])
    d_cb = din("convb", [128, 8])
    d_flags = din("flags", [128, 44])
    d_gmask = din("gmask", [128, 48])
    d_bg = din("bg", [128, 16])
    d_mhw = din("mhw", [128, 512])
    d_nw = din("nw", [128, 2])
    d_wout = din("wout", [8, 128, 1024])
    d_wqT = din("wqT", [16, 128, 1024])
    d_keysT = din("keysT", [128, 16 * 128])
    d_lnp = din("lnp", [128, 4 * 1024])
    d_pu = din("pu", [16384, 1024])
    d_pv = din("pv", [16384, 1024])
    d_cst = din("cst", [128, 9 * 128])
    d_iota = din("iota16", [128, 256])
    d_rope = din("rope", [16, 2, 128, 512])
    d_out = nc.dram_tensor("out", [2048, 1024], F32, kind="ExternalOutput").ap()
    d_pub = nc.dram_tensor("pub", [16384, 1024], BF16).ap()
    d_pvb = nc.dram_tensor("pvb", [16384, 1024], BF16).ap()
    dbg_out = {}
    st["B"] = B
    st["dbg_out"] = dbg_out

    def dbg(name, shape, dt=F32):
        if name in DBG.split(","):
            dbg_out[name] = nc.dram_tensor("dbg_" + name, list(shape), dt, kind="ExternalOutput").ap()
            return dbg_out[name]
        return None

    AR = Arena(nc, 212800)
    PS = [nc.alloc_psum_tensor("ps%d" % i, [128, 512], F32) for i in range(8)]

    CST = AR.alloc([128, 9 * 128])
    ident = CST[:, 0:128]
    ones = CST[:, 128:256]
    tri = CST[:, 256:384]
    mask_sr = CST[:, 384:512]
    mask_rs = CST[:, 512:640]
    jmat = CST[:, 640:768]
    blk64 = CST[:, 768:896]
    rTm = CST[:, 896:1024]
    sel0 = CST[:, 1024:1152]
    CSTB = AR.alloc([128, 2 * 128], BF16)
    identb = CSTB[:, 0:128]
    jb = CSTB[:, 128:256]
    COLS = AR.alloc([128, 48, 2])
    SC1P = AR.alloc([128, 8, 2])
    MIX = AR.alloc([128, 16, 1024], BF16)
    MHW = AR.alloc([128, 512])
    KTAPS = AR.alloc([128, 22 * 12])
    QTAPS = AR.alloc([128, 24])
    CONVB = AR.alloc([128, 8])
    FLAGS = AR.alloc([128, 44])
    GMASK = AR.alloc([128, 48])
    BG = AR.alloc([128, 16])
    NW = AR.alloc([128, 2])
    MST = [AR.alloc([128, 4]) for _ in range(2)]
    CT = [AR.alloc([128, 4, 129]) for _ in range(2)]
    CTB = [AR.alloc([128, 4, 129], BF16) for _ in range(2)]
    SMALL = AR.alloc([128, 512])
    smo = [0]

    def small(n):
        o = smo[0]
        smo[0] += n
        assert smo[0] <= 512
        return SMALL[:, o:o + n]

    bC = B("cst")
    S.dma(lambda e: e.dma_start(out=CST, in_=d_cst), writes=[bC])
    S.act(lambda e: e.activation(out=identb, in_=ident, func=AF.Copy), reads=[bC], writes=[B("cstb")])
    S.act(lambda e: e.activation(out=jb, in_=jmat, func=AF.Copy), reads=[bC], writes=[B("cstb")])
    bCB = B("cstb")
    for (t, d, n) in ((MHW, d_mhw, "mhw"), (KTAPS, d_ktaps, "ktaps"), (QTAPS, d_qtaps, "qtaps"), (CONVB, d_cb, "convb"),
                      (FLAGS, d_flags, "flags"), (GMASK, d_gmask, "gmask"), (BG, d_bg, "bg"), (NW, d_nw, "nw")):
        S.dma(lambda e, t=t, d=d: e.dma_start(out=t, in_=d), writes=[B(n)], q="pool")
    for d in range(2):
        S.pool(lambda e, d=d: e.memset(MST[d], 0.0), writes=[B("m%d" % d)])
        S.pool(lambda e, d=d: e.memset(CT[d], 0.0), writes=[B("ct%d" % d)])
        S.pool(lambda e, d=d: e.memset(CTB[d], 0.0), writes=[B("ctb%d" % d)])

    mark_persist = AR.off
    for (src_, dst_, nm) in ((d_pu, d_pub, "pub"), (d_pv, d_pvb, "pvb")):
        for r in range(0, 16384, 2048):
            S.dma(lambda e, src_=src_, dst_=dst_, r=r: e.dma_start(out=dst_[r:r + 2048, :], in_=src_[r:r + 2048, :]), writes=[B("%s%d" % (nm, r))], q="pool")

    CV = AR.alloc([128, 16])
    SIL = AR.alloc([128, 8, 2])
    WM = [AR.alloc([128, 8, 512]) for _ in range(2)]
    BM = [AR.alloc([2, 512]) for _ in range(2)]
    MROW = [AR.alloc([2, 512]) for _ in range(2)]
    S.dma(lambda e: e.dma_start(out=CV, in_=d_cvec), writes=[B("cv")])
    S.act(lambda e: e.activation(out=SIL.rearrange("p a b -> p (a b)"), in_=CV, func=AF.Silu), reads=[B("cv")], writes=[B("sil")])
    colps = PS[1][:, 0:96]
    for g in range(12):
        wm = WM[g % 2]
        bw = B("wm%d" % (g % 2))
        S.dma(lambda e, wm=wm, g=g: e.dma_start(out=wm, in_=d_wmod[g]), writes=[bw])
        S.dma(lambda e, g=g: e.dma_start(out=BM[g % 2], in_=d_bmod[g]), writes=[B("bm%d" % (g % 2))], q="pool")
        for kc in range(8):
            S.pe(lambda e, wm=wm, kc=kc: e.matmul(PS[0][0:2, :], lhsT=SIL[:, kc, :], rhs=wm[:, kc, :], start=(kc == 0), stop=(kc == 7)),
                 reads=[B("sil"), bw], writes=[B("ps0")])
        mr = MROW[g % 2]
        S.dve(lambda e, g=g, mr=mr: e.tensor_tensor(out=mr, in0=PS[0][0:2, :], in1=BM[g % 2], op=ALU.add),
              reads=[B("ps0"), B("bm%d" % (g % 2))], writes=[B("mrow%d" % (g % 2))])
        for fc in range(4):
            idx = g * 4 + fc
            S.pe(lambda e, mr=mr, fc=fc, idx=idx: e.matmul(colps[:, idx * 2:idx * 2 + 2], lhsT=mr[:, fc * 128:(fc + 1) * 128], rhs=ident[0:2, 0:2],
                                                           start=True, stop=True),
                 reads=[B("mrow%d" % (g % 2)), bC], writes=[B("ps1")])
    S.dve(lambda e: e.tensor_copy(out=COLS.rearrange("p a b -> p (a b)"), in_=colps), reads=[B("ps1")], writes=[B("cols")])
    S.dve(lambda e: e.tensor_scalar(out=SC1P, in0=COLS[:, 8:16, :], scalar1=1.0, scalar2=None, op0=ALU.add), reads=[B("cols")], writes=[B("sc1p")])
    dd = dbg("cols", [128, 96])
    if dd is not None:
        S.dma(lambda e, dd=dd: e.dma_start(out=dd, in_=COLS.rearrange("p a b -> p (a b)")), reads=[B("cols")], writes=[B("dbgo")])

    if STOP == 0:
        raise _Stop()
    AR.off = mark_persist
    S.barrier()

    WB = AR.alloc([128, 8, 2832], BF16)
    QAT = AR.alloc([128, 4, 2048], BF16)
    KAT = AR.alloc([128, 66 * 128], BF16)
    VA = AR.alloc([128, 66, 2, 65], BF16)
    XW = AR.alloc([128, 8, 514])
    HT = AR.alloc([128, 8, 514], BF16)
    KT = AR.alloc([128, 4, 512], BF16)
    QT = AR.alloc([128, 4, 512], BF16)
    ZB = [AR.alloc([128, 514]) for _ in range(2)]
    TB = [AR.alloc([128, 512]) for _ in range(2)]
    SQ = AR.alloc([128, 512])
    SD = AR.alloc([128, 512])
    KN = AR.alloc([128, 512])
    ROPE = AR.alloc([128, 2, 512])
    KTOK2 = [AR.alloc([128, 4, 128], BF16) for _ in range(2)]
    VAUG2 = [AR.alloc([128, 4, 129], BF16) for _ in range(2)]
    VW = [AR.alloc([128, 129], BF16) for _ in range(2)]
    DA = AR.alloc([128, 128])
    DM = AR.alloc([128, 128])
    DTT = AR.alloc([128, 128])
    PT = AR.alloc([128, 128], BF16)
    TT1 = AR.alloc([128, 129])
    TOT = AR.alloc([128, 129])
    HF = AR.alloc([128, 512], BF16)
    HN = AR.alloc([128, 512])
    OSG = AR.alloc([128, 512])
    GP2 = [AR.alloc([128, 16]) for _ in range(2)]
    WST = [QAT.rearrange("p a b -> p (a b)").bitcast(F32)[:, 0:2832], KAT.bitcast(F32)[:, 0:2832]]
    print("phase1 arena bytes", AR.off)

    for par in range(2):
        S.pool(lambda e: e.memset(VAUG2[par][:, :, 128:129], 1.0), writes=[B("vaug%d" % par)])
    S.pool(lambda e: e.memset(VA[:, :, :, 64:65], 1.0), writes=[B("va")])

    for kc in range(8):
        st = WST[kc % 2]
        bs = B("qat" if kc % 2 == 0 else "kat")
        S.dma(lambda e, st=st, kc=kc: e.dma_start(out=st, in_=d_win[kc]), writes=[bs])
        if kc % 2 == 0:
            S.act(lambda e, st=st, kc=kc: e.activation(out=WB[:, kc, :], in_=st, func=AF.Copy), reads=[bs], writes=[B("wb")])
        else:
            S.pool(lambda e, st=st, kc=kc: e.tensor_copy(out=WB[:, kc, :], in_=st), reads=[bs], writes=[B("wb")])

    C_KM, C_QM, C_KA, C_QA, C_VM, C_VA, C_G, C_O = 0, 512, 1024, 1152, 1664, 2176, 2304, 2320

    def mk_small():
        return dict(ef=small(4), nlf=small(4), li=small(4), a=small(4), amx=small(1), d4=small(4), ml=small(4), dm=small(4),
                    dec=small(4), aw=small(4), w=small(4), cm=small(1), mv=small(1), nmv=small(1), dwi=small(1), wi=small(1),
                    dn0=small(1), nrm=small(1), ad=small(1), dn=small(1), rd=small(1))
    SM = [mk_small(), mk_small()]
    S1 = small(4)
    S2 = small(4)
    MEAN = small(4)
    MSQ = small(4)
    VAR = small(4)
    RSTD = small(4)

    def fm_project(c0, N, halo, psb):
        for kc in range(8):
            S.pe(lambda e, kc=kc: e.matmul(PS[psb][:, 0:N], lhsT=WB[:, kc, c0:c0 + 128], rhs=HT[:, kc, 1:N + 1], start=(kc == 0), stop=(kc == 7)),
                 reads=[B("wb"), B("ht")], writes=[B("ps%d" % psb)])
        if halo:
            for kc in range(8):
                S.pe(lambda e, kc=kc: e.matmul(PS[2][:, 0:2], lhsT=WB[:, kc, c0:c0 + 128], rhs=HT[:, kc, 0:N + 2:N + 1], start=(kc == 0), stop=(kc == 7)),
                     reads=[B("wb"), B("ht")], writes=[B("ps2")])

    def conv_block(N, psb, zi, taps, cbias, fl, dst, qscale):
        Z = ZB[zi]
        T = TB[zi]
        bz = B("z%d" % zi)
        bt = B("t%d" % zi)
        S.act(lambda e: e.activation(out=Z[:, 1:N + 1], in_=PS[psb][:, 0:N], func=AF.Copy), reads=[B("ps%d" % psb)], writes=[bz])
        S.dve(lambda e: e.tensor_tensor(out=Z[:, 0:N + 2:N + 1], in0=PS[2][:, 0:2], in1=fl, op=ALU.mult), reads=[B("ps2"), B("flags")], writes=[bz])
        S.dve(lambda e: e.tensor_scalar(out=T[:, 0:N], in0=Z[:, 0:N], scalar1=taps[:, 0:1], scalar2=None, op0=ALU.mult),
              reads=[bz, B("ktaps"), B("qtaps")], writes=[bt])
        S.dve(lambda e: e.scalar_tensor_tensor(out=T[:, 0:N], in0=Z[:, 1:N + 1], scalar=taps[:, 1:2], in1=T[:, 0:N], op0=ALU.mult, op1=ALU.add),
              reads=[bz, bt], writes=[bt])
        S.dve(lambda e: e.scalar_tensor_tensor(out=T[:, 0:N], in0=Z[:, 2:N + 2], scalar=taps[:, 2:3], in1=T[:, 0:N], op0=ALU.mult, op1=ALU.add),
              reads=[bz, bt], writes=[bt])
        if qscale:
            S.act(lambda e: e.activation(out=T[:, 0:N], in_=T[:, 0:N], func=AF.Silu, bias=cbias), reads=[bt, B("convb")], writes=[bt])
            S.pool(lambda e: e.tensor_scalar(out=dst, in0=T[:, 0:N], scalar1=128.0 ** -0.5, scalar2=None, op0=ALU.mult), reads=[bt], writes=[B("qt")])
        else:
            S.act(lambda e: e.activation(out=dst, in_=T[:, 0:N], func=AF.Silu, bias=cbias), reads=[bt, B("convb")], writes=[B("kt")])

    def normrope(N, psb, nwcol, rope, dst, bdst):
        ps = PS[psb][:, 0:N]
        bp = B("ps%d" % psb)
        S.act(lambda e: e.activation(out=SQ[:, 0:N], in_=ps, func=AF.Square), reads=[bp], writes=[B("sq")])
        S.pe(lambda e: e.matmul(PS[6][:, 0:N], lhsT=blk64, rhs=SQ[:, 0:N], start=True, stop=True), reads=[bC, B("sq")], writes=[B("ps6")])
        S.act(lambda e: e.activation(out=SD[:, 0:N], in_=PS[6][:, 0:N], func=AF.Sqrt, scale=1.0 / 64, bias=RMS_EPS), reads=[B("ps6")], writes=[B("sd")])
        S.dve(lambda e: e.reciprocal(out=SD[:, 0:N], in_=SD[:, 0:N]), reads=[B("sd")], writes=[B("sd")])
        S.dve(lambda e: e.scalar_tensor_tensor(out=KN[:, 0:N], in0=ps, scalar=NW[:, nwcol:nwcol + 1], in1=SD[:, 0:N], op0=ALU.mult, op1=ALU.mult),
              reads=[bp, B("nw"), B("sd")], writes=[B("kn")])
        if rope:
            S.pe(lambda e: e.matmul(PS[5][:, 0:N], lhsT=rTm, rhs=KN[:, 0:N], start=True, stop=True), reads=[bC, B("kn")], writes=[B("ps5")])
            S.pool(lambda e: e.tensor_tensor(out=SQ[:, 0:N], in0=KN[:, 0:N], in1=ROPE[:, 0, 0:N], op=ALU.mult), reads=[B("kn"), B("rope")], writes=[B("sq")])
            S.dve(lambda e: e.tensor_tensor(out=KN[:, 0:N], in0=PS[5][:, 0:N], in1=ROPE[:, 1, 0:N], op=ALU.mult), reads=[B("ps5"), B("rope"), B("kn")], writes=[B("kn")])
            S.dve(lambda e: e.tensor_tensor(out=dst, in0=KN[:, 0:N], in1=SQ[:, 0:N], op=ALU.add), reads=[B("kn"), B("sq")], writes=[bdst])
        else:
            S.act(lambda e: e.activation(out=dst, in_=KN[:, 0:N], func=AF.Copy), reads=[B("kn")], writes=[bdst])

    def chunk_state(d, masked_g, t, par):
        GP = GP2[par]
        bgp = B("gp%d" % par)
        sm = SM[d]
        ic = 0 if d == 0 else 8
        fc = ic + 4
        bn = lambda n: B("sm%d_%s" % (d, n))
        bm = B("m%d" % d)
        S.act(lambda e: e.activation(out=sm["ef"], in_=GP[:, fc:fc + 4], func=AF.Exp, scale=-1.0), reads=[bgp], writes=[bn("ef")])
        S.act(lambda e: e.activation(out=sm["nlf"], in_=sm["ef"], func=AF.Ln, bias=1.0), reads=[bn("ef")], writes=[bn("nlf")])
        if masked_g is not None:
            kcol = GMASK[:, masked_g * 4 + 2 * d:masked_g * 4 + 2 * d + 1]
            acol = GMASK[:, masked_g * 4 + 2 * d + 1:masked_g * 4 + 2 * d + 2]
            S.dve(lambda e: e.tensor_scalar(out=sm["nlf"], in0=sm["nlf"], scalar1=kcol, scalar2=None, op0=ALU.mult), reads=[bn("nlf"), B("gmask")], writes=[bn("nlf")])
            S.dve(lambda e: e.tensor_scalar(out=sm["li"], in0=GP[:, ic:ic + 4], scalar1=kcol, scalar2=acol, op0=ALU.mult, op1=ALU.add),
                  reads=[bgp, B("gmask")], writes=[bn("li")])
        else:
            S.dve(lambda e: e.tensor_copy(out=sm["li"], in_=GP[:, ic:ic + 4]), reads=[bgp], writes=[bn("li")])
        o = 16 + d * 32
        nbps = PS[2][:, o:o + 4]
        nBps = PS[2][:, o + 4:o + 8]
        amBps = PS[2][:, o + 8:o + 12]
        aTps = PS[2][0:4, 128 + d * 128:256 + d * 128]
        bps = B("ps2s%d" % d)
        S.pe(lambda e: e.matmul(nbps, lhsT=tri, rhs=sm["nlf"], start=True, stop=True), reads=[bC, bn("nlf")], writes=[bps])
        S.pe(lambda e: e.matmul(nBps, lhsT=ones, rhs=sm["nlf"], start=True, stop=True), reads=[bC, bn("nlf")], writes=[bps])
        S.dve(lambda e: e.tensor_tensor(out=sm["a"], in0=nbps, in1=sm["li"], op=ALU.add), reads=[bps, bn("li")], writes=[bn("a")])
        S.pe(lambda e: e.matmul(aTps, lhsT=sm["a"], rhs=ident, start=True, stop=True), reads=[bC, bn("a")], writes=[B("ps2t%d" % d)])
        S.dve(lambda e: e.tensor_reduce(out=sm["amx"][0:4], in_=aTps, axis=AX.X, op=ALU.max), reads=[B("ps2t%d" % d)], writes=[bn("amx")])
        S.dve(lambda e: e.tensor_scalar(out=sm["d4"][0:4], in0=ident[0:4, 0:4], scalar1=sm["amx"][0:4], scalar2=None, op0=ALU.mult),
              reads=[bC, bn("amx")], writes=[bn("d4")])
        S.pe(lambda e: e.matmul(amBps, lhsT=ones[0:4, :], rhs=sm["d4"][0:4], start=True, stop=True), reads=[bC, bn("d4")], writes=[bps])
        S.dve(lambda e: e.tensor_tensor(out=sm["ml"], in0=amBps, in1=MST[d], op=ALU.max), reads=[bps, bm], writes=[bn("ml")])
        S.dve(lambda e: e.tensor_tensor(out=sm["dm"], in0=MST[d], in1=sm["ml"], op=ALU.subtract), reads=[bm, bn("ml")], writes=[bn("dm")])
        S.act(lambda e: e.activation(out=sm["dec"], in_=sm["dm"], func=AF.Exp), reads=[bn("dm")], writes=[bn("dec")])
        S.dve(lambda e: e.tensor_tensor(out=sm["aw"], in0=sm["a"], in1=sm["ml"], op=ALU.subtract), reads=[bn("a"), bn("ml")], writes=[bn("aw")])
        S.act(lambda e: e.activation(out=sm["w"], in_=sm["aw"], func=AF.Exp), reads=[bn("aw")], writes=[bn("w")])
        return nbps, nBps, bps

    def chunk_update(d, nBps, bps, par):
        VAUG = VAUG2[par]
        KTOK = KTOK2[par]
        bva = B("vaug%d" % par)
        bkt = B("ktok%d" % par)
        sm = SM[d]
        bn = lambda n: B("sm%d_%s" % (d, n))
        for h in range(4):
            vw = VW[h % 2]
            bvw = B("vw%d" % (h % 2))
            S.dve(lambda e, h=h, vw=vw: e.tensor_scalar(out=vw, in0=VAUG[:, h, :], scalar1=sm["w"][:, h:h + 1], scalar2=None, op0=ALU.mult),
                  reads=[bva, bn("w")], writes=[bvw])
            up = PS[7][:, 256:385]
            S.pe(lambda e, h=h, vw=vw, up=up: e.matmul(up, lhsT=KTOK[:, h, :], rhs=vw, start=True, stop=True), reads=[bkt, bvw], writes=[B("ps7u")])
            S.dve(lambda e, h=h, up=up: e.scalar_tensor_tensor(out=CT[d][:, h, :], in0=CT[d][:, h, :], scalar=sm["dec"][:, h:h + 1], in1=up,
                                                               op0=ALU.mult, op1=ALU.add),
                  reads=[B("ct%d" % d), bn("dec"), B("ps7u")], writes=[B("ct%d" % d)])
        S.pool(lambda e: e.tensor_copy(out=CTB[d], in_=CT[d]), reads=[B("ct%d" % d)], writes=[B("ctb%d" % d)])
        S.dve(lambda e: e.tensor_tensor(out=MST[d], in0=sm["ml"], in1=nBps, op=ALU.subtract), reads=[bn("ml"), bps], writes=[B("m%d" % d)])

    def chunk_full(d, t, nbps, bps, hdst, bh, par):
        VAUG = VAUG2[par]
        bva = B("vaug%d" % par)
        sm = SM[d]
        bn = lambda n: B("sm%d_%s" % (d, n))
        bm = B("m%d" % d)
        cs = slice(t * 128, (t + 1) * 128)
        for h in range(4):
            S.dve(lambda e, h=h: e.tensor_scalar(out=DA, in0=ident, scalar1=sm["a"][:, h:h + 1], scalar2=None, op0=ALU.mult), reads=[bC, bn("a")], writes=[B("da")])
            S.pe(lambda e: e.matmul(PS[7][:, 0:128], lhsT=ones, rhs=DA, start=True, stop=False), reads=[bC, B("da")], writes=[B("ps7e")])
            S.pe(lambda e: e.matmul(PS[7][:, 0:128], lhsT=ident, rhs=mask_sr, start=False, stop=True), reads=[bC], writes=[B("ps7e")])
            S.dve(lambda e: e.tensor_reduce(out=sm["cm"], in_=PS[7][:, 0:128], axis=AX.X, op=ALU.max), reads=[B("ps7e")], writes=[bn("cm")])
            S.dve(lambda e, h=h: e.tensor_tensor(out=sm["mv"], in0=sm["cm"], in1=MST[d][:, h:h + 1], op=ALU.max), reads=[bn("cm"), bm], writes=[bn("mv")])
            S.dve(lambda e: e.tensor_scalar(out=sm["nmv"], in0=sm["mv"], scalar1=-1.0, scalar2=None, op0=ALU.mult), reads=[bn("mv")], writes=[bn("nmv")])
            S.dve(lambda e: e.tensor_scalar(out=DM, in0=ident, scalar1=sm["nmv"], scalar2=None, op0=ALU.mult), reads=[bC, bn("nmv")], writes=[B("dmm")])
            S.pe(lambda e: e.matmul(PS[7][:, 128:256], lhsT=ones, rhs=DM, start=True, stop=False), reads=[bC, B("dmm")], writes=[B("ps7x")])
            S.pe(lambda e: e.matmul(PS[7][:, 128:256], lhsT=ident, rhs=mask_rs, start=False, stop=True), reads=[bC], writes=[B("ps7x")])
            S.act(lambda e, h=h: e.activation(out=DTT, in_=PS[7][:, 128:256], func=AF.Exp, bias=sm["a"][:, h:h + 1]), reads=[B("ps7x"), bn("a")], writes=[B("dtt")])
            S.pe(lambda e, h=h: e.matmul(PS[4][:, 0:128], lhsT=KT[:, h, cs], rhs=QT[:, h, cs], start=True, stop=True), reads=[B("kt"), B("qt")], writes=[B("ps4s")])
            S.dve(lambda e: e.tensor_tensor(out=PT, in0=PS[4][:, 0:128], in1=DTT, op=ALU.mult), reads=[B("ps4s"), B("dtt")], writes=[B("pt")])
            S.pe(lambda e, h=h: e.matmul(PS[4][:, 128:257], lhsT=PT, rhs=VAUG[:, h, :], start=True, stop=True), reads=[B("pt"), bva], writes=[B("ps4n")])
            S.pe(lambda e, h=h: e.matmul(PS[4][:, 257:386], lhsT=QT[:, h, cs], rhs=CTB[d][:, h, :], start=True, stop=True), reads=[B("qt"), B("ctb%d" % d)], writes=[B("ps4i")])
            S.dve(lambda e, h=h: e.tensor_tensor(out=sm["dwi"], in0=MST[d][:, h:h + 1], in1=sm["mv"], op=ALU.subtract), reads=[bm, bn("mv")], writes=[bn("dwi")])
            S.act(lambda e: e.activation(out=sm["wi"], in_=sm["dwi"], func=AF.Exp), reads=[bn("dwi")], writes=[bn("wi")])
            S.act(lambda e: e.activation(out=TT1, in_=PS[4][:, 257:386], func=AF.Identity, scale=sm["wi"]), reads=[B("ps4i"), bn("wi")], writes=[B("tt1")])
            S.dve(lambda e: e.tensor_tensor(out=TOT, in0=PS[4][:, 128:257], in1=TT1, op=ALU.add), reads=[B("ps4n"), B("tt1")], writes=[B("tot")])
            S.dve(lambda e, h=h: e.tensor_tensor(out=sm["dn0"], in0=nbps[:, h:h + 1], in1=sm["mv"], op=ALU.subtract), reads=[bps, bn("mv")], writes=[bn("dn0")])
            S.act(lambda e: e.activation(out=sm["nrm"], in_=sm["dn0"], func=AF.Exp), reads=[bn("dn0")], writes=[bn("nrm")])
            S.act(lambda e: e.activation(out=sm["ad"], in_=TOT[:, 128:129], func=AF.Abs), reads=[B("tot")], writes=[bn("ad")])
            S.dve(lambda e: e.tensor_tensor(out=sm["dn"], in0=sm["ad"], in1=sm["nrm"], op=ALU.max), reads=[bn("ad"), bn("nrm")], writes=[bn("dn")])
            S.dve(lambda e: e.reciprocal(out=sm["rd"], in_=sm["dn"]), reads=[bn("dn")], writes=[bn("rd")])
            S.act(lambda e, h=h: e.activation(out=hdst[:, h * 128:(h + 1) * 128], in_=TOT[:, 0:128], func=AF.Identity, scale=sm["rd"]),
                  reads=[B("tot"), bn("rd")], writes=[bh])

    steps = [("ctxF", 0, 256, 0, 0), ("ctxB", 1, 256, 1, None)]
    for g in range(12):
        steps.append(("oth", g, 512, 2 + g, 2 + 4 * g))
    for g in range(4):
        steps.append(("ownB", 12 + g, 512, 14 + g, None))
    for g in range(4):
        steps.append(("ownF", 16 + g, 512, 18 + g, 50 + 4 * g))

    KG = int(os.environ.get("KG", "99"))
    KSUB = int(os.environ.get("KSUB", "99"))
    if KG == 0:
        raise _Stop()
    for gstep, (kind, src, N, tg, ktb) in enumerate(steps):
        if gstep >= KG:
            raise _Stop()
        last = gstep == KG - 1
        isctx = kind.startswith("ctx")
        mc = 1 if isctx else 0
        if isctx:
            S.dma(lambda e, src=src: e.dma_start(out=XW[:, :, 0:258], in_=d_cwin[src]), writes=[B("xw")])
        else:
            S.dma(lambda e, src=src: e.dma_start(out=XW, in_=d_xwin[src]), writes=[B("xw")])
        for kc in range(8):
            if kc % 2 == 0:
                S.act(lambda e, kc=kc: e.activation(out=HT[:, kc, 0:N + 2], in_=XW[:, kc, 0:N + 2], func=AF.Identity,
                                                    bias=COLS[:, kc, mc:mc + 1], scale=SC1P[:, kc, mc:mc + 1]),
                      reads=[B("xw"), B("cols"), B("sc1p")], writes=[B("ht")])
            else:
                S.pool(lambda e, kc=kc: e.tensor_scalar(out=HT[:, kc, 0:N + 2], in0=XW[:, kc, 0:N + 2], scalar1=SC1P[:, kc, mc:mc + 1],
                                                        scalar2=COLS[:, kc, mc:mc + 1], op0=ALU.mult, op1=ALU.add),
                       reads=[B("xw"), B("cols"), B("sc1p")], writes=[B("ht")])
        if last and KSUB == 0:
            raise _Stop()
        fl = FLAGS[:, tg * 2:tg * 2 + 2]
        for hb in range(4):
            fm_project(C_KM + hb * 128, N, True, hb % 2)
            conv_block(N, hb % 2, hb % 2, KTAPS[:, (tg * 4 + hb) * 3:(tg * 4 + hb) * 3 + 3], CONVB[:, 4 + hb:5 + hb], fl, KT[:, hb, 0:N], False)
        if kind in ("ownB", "ownF"):
            qd = 1 if kind == "ownB" else 0
            for hb in range(4):
                fm_project(C_QM + hb * 128, N, True, hb % 2)
                conv_block(N, hb % 2, hb % 2, QTAPS[:, (qd * 4 + hb) * 3:(qd * 4 + hb) * 3 + 3], CONVB[:, hb:hb + 1], fl, QT[:, hb, 0:N], True)
        if last and KSUB == 1:
            raise _Stop()
        if ktb is not None:
            if not isctx:
                ri = src if kind == "oth" else 12 + (src - 16)
                S.dma(lambda e, ri=ri: e.dma_start(out=ROPE, in_=d_rope[ri].rearrange("a p n -> p a n")), writes=[B("rope")])
            fm_project(C_KA, N, False, 0)
            normrope(N, 0, 1, not isctx, KAT[:, ktb * 128:ktb * 128 + N], B("kat"))
            if kind == "ownF":
                gi = src - 16
                for mcq in range(4):
                    fm_project(C_QA + mcq * 128, N, False, 1)
                    normrope(N, 1, 0, True, QAT[:, mcq, gi * 512:(gi + 1) * 512], B("qat"))
        if last and KSUB == 2:
            raise _Stop()
        for t in range(N // 128):
            par = t % 2
            VAUG = VAUG2[par]
            KTOK = KTOK2[par]
            GP = GP2[par]
            ts = slice(1 + t * 128, 1 + (t + 1) * 128)
            for kc in range(8):
                S.pe(lambda e, kc=kc, ts=ts: e.matmul(PS[3][:, 0:512], lhsT=HT[:, kc, ts], rhs=WB[:, kc, C_VM:C_VM + 512], start=(kc == 0), stop=(kc == 7)),
                     reads=[B("ht"), B("wb")], writes=[B("ps3")])
            for kc in range(8):
                S.pe(lambda e, kc=kc, ts=ts: e.matmul(PS[6][:, 0:144], lhsT=HT[:, kc, ts], rhs=WB[:, kc, C_VA:C_VA + 144], start=(kc == 0), stop=(kc == 7)),
                     reads=[B("ht"), B("wb")], writes=[B("ps6")])
            S.act(lambda e: e.activation(out=VAUG[:, :, 0:128], in_=PS[3][:, 0:512].rearrange("p (a b) -> p a b", a=4), func=AF.Copy),
                  reads=[B("ps3")], writes=[B("vaug%d" % par)])
            if ktb is not None:
                S.act(lambda e, kt_=ktb + t: e.activation(out=VA[:, kt_, :, 0:64], in_=PS[6][:, 0:128].rearrange("p (a b) -> p a b", a=2), func=AF.Copy),
                      reads=[B("ps6")], writes=[B("va")])
            S.dve(lambda e: e.tensor_tensor(out=GP, in0=PS[6][:, 128:144], in1=BG, op=ALU.add), reads=[B("ps6"), B("bg")], writes=[B("gp%d" % par)])
            for h in range(4):
                S.pe(lambda e, h=h, t=t: e.matmul(PS[5][:, h * 128:(h + 1) * 128], lhsT=KT[:, h, t * 128:(t + 1) * 128], rhs=identb, start=True, stop=True),
                     reads=[B("kt"), bCB], writes=[B("ps5")])
            S.act(lambda e: e.activation(out=KTOK.rearrange("p a b -> p (a b)"), in_=PS[5][:, 0:512], func=AF.Copy), reads=[B("ps5")], writes=[B("ktok%d" % par)])
            if kind == "ownF":
                for kc in range(8):
                    S.pe(lambda e, kc=kc, ts=ts: e.matmul(PS[3][:, 0:512], lhsT=HT[:, kc, ts], rhs=WB[:, kc, C_O:C_O + 512], start=(kc == 0), stop=(kc == 7)),
                         reads=[B("ht"), B("wb")], writes=[B("ps3")])
                S.act(lambda e: e.activation(out=OSG, in_=PS[3][:, 0:512], func=AF.Sigmoid), reads=[B("ps3")], writes=[B("osg")])
            if last and KSUB == 3:
                raise _Stop()
            dirs = {"ctxF": [0], "ctxB": [1], "oth": [0, 1], "ownB": [1], "ownF": [0]}[kind]
            for d in dirs:
                nbps, nBps, bps = chunk_state(d, src if kind == "oth" else None, t, par)
                if kind == "ownB":
                    cb = (src - 12) * 4 + t
                    chunk_full(d, t, nbps, bps, MIX[:, cb, 512:1024], B("mixb%d" % cb), par)
                elif kind == "ownF":
                    chunk_full(d, t, nbps, bps, HF, B("hf"), par)
                chunk_update(d, nBps, bps, par)
            if last and KSUB == 4:
                raise _Stop()
            if kind == "ownF":
                oc = (src - 16) * 4 + t
                hm = PS[3][:, 0:512]
                S.pe(lambda e: e.matmul(hm, lhsT=identb, rhs=HF, start=True, stop=False), reads=[bCB, B("hf")], writes=[B("ps3")])
                S.pe(lambda e, oc=oc: e.matmul(hm, lhsT=jb, rhs=MIX[:, 15 - oc, 512:1024], start=False, stop=True), reads=[bCB, B("mixb%d" % (15 - oc))], writes=[B("ps3")])
                hm3 = hm.rearrange("p (a b) -> p a b", a=4)
                S.dve(lambda e: e.tensor_reduce(out=S1, in_=hm3, axis=AX.X, op=ALU.add), reads=[B("ps3")], writes=[B("s1")])
                S.act(lambda e: e.activation(out=HN, in_=hm, func=AF.Square), reads=[B("ps3")], writes=[B("hn")])
                S.dve(lambda e: e.tensor_reduce(out=S2, in_=HN.rearrange("p (a b) -> p a b", a=4), axis=AX.X, op=ALU.add), reads=[B("hn")], writes=[B("s2")])
                if last and KSUB == 5:
                    raise _Stop()
                S.dve(lambda e: e.tensor_scalar(out=MEAN, in0=S1, scalar1=1.0 / 128, scalar2=None, op0=ALU.mult), reads=[B("s1")], writes=[B("mean")])
                S.dve(lambda e: e.tensor_tensor(out=MSQ, in0=MEAN, in1=MEAN, op=ALU.mult), reads=[B("mean")], writes=[B("msq")])
                S.dve(lambda e: e.scalar_tensor_tensor(out=VAR, in0=S2, scalar=1.0 / 128, in1=MSQ, op0=ALU.mult, op1=ALU.subtract), reads=[B("s2"), B("msq")], writes=[B("var")])
                S.act(lambda e: e.activation(out=RSTD, in_=VAR, func=AF.Sqrt, bias=LN_EPS), reads=[B("var")], writes=[B("rstd")])
                S.dve(lambda e: e.reciprocal(out=RSTD, in_=RSTD), reads=[B("rstd")], writes=[B("rstd")])
                if last and KSUB == 6:
                    raise _Stop()
                for h in range(4):
                    S.dve(lambda e, h=h: e.tensor_scalar(out=HN[:, h * 128:(h + 1) * 128], in0=hm[:, h * 128:(h + 1) * 128], scalar1=MEAN[:, h:h + 1],
                                                         scalar2=RSTD[:, h:h + 1], op0=ALU.subtract, op1=ALU.mult),
                          reads=[B("ps3"), B("mean"), B("rstd"), B("s2")], writes=[B("hn")])
                S.pool(lambda e: e.tensor_tensor(out=HN, in0=HN, in1=MHW, op=ALU.mult), reads=[B("hn"), B("mhw")], writes=[B("hn")])
                S.dve(lambda e, oc=oc: e.tensor_tensor(out=MIX[:, oc, 0:512], in0=HN, in1=OSG, op=ALU.mult), reads=[B("hn"), B("osg")], writes=[B("mixa%d" % oc)])

    dd = dbg("mixa", [128, 16, 512], BF16)
    if dd is not None:
        S.dma(lambda e, dd=dd: e.dma_start(out=dd, in_=MIX[:, :, 0:512]), reads=[B("mixa%d" % i) for i in range(16)], writes=[B("dbgo3")])

    if STOP == 1:
        raise _Stop()
    PB = [AR.alloc([128, 512], BF16) for _ in range(2)]
    RDEN = [AR.alloc([128, 4]) for _ in range(2)]
    OTS = [HN, OSG]
    combo = 0
    for qg in range(4):
        for mcq in range(4):
            for half in range(2):
                hq = half * 4 + mcq
                r0 = half * 64
                ob = 4 + 2 * (combo % 2)
                eb = ob + 1
                def s_mm(kt):
                    sb = kt % 2
                    S.pe(lambda e: e.matmul(PS[sb][:, 0:512], lhsT=KAT[r0:r0 + 64, kt * 128:(kt + 1) * 128],
                                            rhs=QAT[r0:r0 + 64, mcq, qg * 512:(qg + 1) * 512], start=True, stop=True),
                         reads=[B("kat"), B("qat")], writes=[B("ps%d" % sb)])
                s_mm(0)
                for kt in range(66):
                    sb = kt % 2
                    if kt + 1 < 66:
                        s_mm(kt + 1)
                    S.act(lambda e: e.activation(out=PB[sb], in_=PS[sb][:, 0:512], func=AF.Exp, scale=0.125), reads=[B("ps%d" % sb)], writes=[B("pb%d" % sb)])
                    S.pe(lambda e: e.matmul(PS[ob][0:65, 0:512], lhsT=VA[:, kt, half, :], rhs=PB[sb], start=(kt == 0), stop=(kt == 65)),
                         reads=[B("pb%d" % sb), B("va")], writes=[B("ps%d" % ob)])
                ots = OTS[combo % 2]
                bo = B(("hn", "osg")[combo % 2])
                S.act(lambda e: e.activation(out=ots[0:65, :], in_=PS[ob][0:65, 0:512], func=AF.Copy), reads=[B("ps%d" % ob)], writes=[bo])
                for qt in range(4):
                    S.pe(lambda e: e.matmul(PS[eb][:, qt * 65:(qt + 1) * 65], lhsT=ots[0:65, qt * 128:(qt + 1) * 128], rhs=ident[0:65, 0:65], start=True, stop=True),
                         reads=[bo, bC], writes=[B("ps%d" % eb)])
                rd = RDEN[combo % 2]
                brd = B("rden%d" % (combo % 2))
                S.dve(lambda e: e.reciprocal(out=rd, in_=PS[eb][:, 64:260:65]), reads=[B("ps%d" % eb)], writes=[brd])
                for qt in range(4):
                    oc = qg * 4 + qt
                    S.act(lambda e: e.activation(out=MIX[:, oc, 512 + hq * 64:512 + (hq + 1) * 64], in_=PS[eb][:, qt * 65:qt * 65 + 64], func=AF.Identity,
                                                 scale=rd[:, qt:qt + 1]),
                          reads=[B("ps%d" % eb), brd], writes=[B("att%d" % oc), B("mixb%d" % oc)])
                combo += 1

    dd = dbg("att", [128, 16, 512], BF16)
    if dd is not None:
        S.dma(lambda e, dd=dd: e.dma_start(out=dd, in_=MIX[:, :, 512:1024]), reads=[B("att%d" % i) for i in range(16)], writes=[B("dbgo2")])

    if STOP == 2:
        raise _Stop()
    AR.off = mark_persist
    S.barrier()

    WKB = AR.alloc([128, 8, 2048], BF16)
    WOB = AR.alloc([128, 8, 1024], BF16)
    GB = AR.alloc([128, 4, 1024])
    LNP = AR.alloc([128, 4, 1024])
    NSLOT = 12
    UGB = [AR.alloc([128, 1024], BF16) for _ in range(NSLOT)]
    XT = AR.alloc([128, 1024])
    R1 = AR.alloc([128, 1024])
    X1 = AR.alloc([128, 1024])
    XM = AR.alloc([128, 1024])
    SC = AR.alloc([128, 2048])
    MIXT = AR.alloc([128, 8, 128], BF16)
    XMT = AR.alloc([128, 8, 128], BF16)
    TP1 = AR.alloc([128, 16, 16])
    IP1 = AR.alloc([128, 16, 16], U32)
    IP1F = AR.alloc([128, 16, 16])
    TMP = AR.alloc([128, 256])
    CAND = AR.alloc([128, 256])
    TP2 = AR.alloc([128, 8, 16])
    PP2 = AR.alloc([128, 8, 16], U32)
    PJ = AR.alloc([128, 2, 128], U32)
    PJF = AR.alloc([128, 2, 128])
    OH = AR.alloc([128, 2048])
    IDXF = AR.alloc([128, 2, 128])
    EIF = AR.alloc([128, 128])
    EI = AR.alloc([128, 128], I32)
    GEX = AR.alloc([128, 8, 16])
    GSUM = AR.alloc([128, 8])
    DOT = AR.alloc([128, 128])
    COEF = AR.alloc([128, 128])
    DOT2 = AR.alloc([128, 128])
    IOTA = AR.alloc([128, 256])
    JUNK = AR.alloc([128, 1024])
    ST = AR.alloc([128, 8])
    print("phase3 arena bytes", AR.off)

    S.dma(lambda e: e.dma_start(out=LNP.rearrange("p a b -> p (a b)"), in_=d_lnp), writes=[B("lnp")])
    S.dma(lambda e: e.dma_start(out=IOTA, in_=d_iota), writes=[B("iota")], q="pool")
    for kc in range(8):
        st = (X1, XM)[kc % 2]
        bs = B(("x1", "xm")[kc % 2])
        S.dma(lambda e, st=st, kc=kc: e.dma_start(out=st, in_=d_wout[kc]), writes=[bs])
        S.act(lambda e, st=st, kc=kc: e.activation(out=WOB[:, kc, :], in_=st, func=AF.Copy), reads=[bs], writes=[B("wob")])
    KEYB = OH.bitcast(BF16)[:, 0:2048]
    S.dma(lambda e: e.dma_start(out=SC, in_=d_keysT), writes=[B("sc")])
    S.act(lambda e: e.activation(out=KEYB, in_=SC, func=AF.Copy), reads=[B("sc")], writes=[B("oh")])
    WQB = [XT.bitcast(BF16)[:, 0:1024], R1.bitcast(BF16)[:, 0:1024]]
    for blk in range(16):
        st = (X1, XM)[blk % 2]
        bs = B(("x1", "xm")[blk % 2])
        S.dma(lambda e, st=st, blk=blk: e.dma_start(out=st, in_=d_wqT[blk]), writes=[bs])
        S.act(lambda e, st=st, blk=blk: e.activation(out=WQB[blk % 2], in_=st, func=AF.Copy), reads=[bs], writes=[B(("xt", "r1")[blk % 2])])
        for kc in range(8):
            S.pe(lambda e, blk=blk, kc=kc: e.matmul(PS[kc % 2][:, 0:128], lhsT=WQB[blk % 2][:, kc * 128:(kc + 1) * 128], rhs=KEYB[:, blk * 128:(blk + 1) * 128],
                                                    start=True, stop=True),
                 reads=[B(("xt", "r1")[blk % 2]), B("oh")], writes=[B("ps%d" % (kc % 2))])
            if kc % 2 == 0:
                S.dve(lambda e, blk=blk, kc=kc: e.tensor_copy(out=WKB[:, kc, blk * 128:(blk + 1) * 128], in_=PS[kc % 2][:, 0:128]), reads=[B("ps%d" % (kc % 2))], writes=[B("wkb")])
            else:
                S.act(lambda e, blk=blk, kc=kc: e.activation(out=WKB[:, kc, blk * 128:(blk + 1) * 128], in_=PS[kc % 2][:, 0:128], func=AF.Copy),
                      reads=[B("ps%d" % (kc % 2))], writes=[B("wkb")])
    for vi, c0 in enumerate((16, 24, 32, 40)):
        for kc in range(8):
            S.dve(lambda e, c0=c0, kc=kc: e.tensor_scalar(out=JUNK[:, 0:128], in0=ident, scalar1=COLS[:, c0 + kc, 0:1], scalar2=None, op0=ALU.mult),
                  reads=[bC, B("cols")], writes=[B("junk")])
            S.pe(lambda e, kc=kc: e.matmul(PS[2 + kc % 2][:, 0:128], lhsT=ones, rhs=JUNK[:, 0:128], start=True, stop=True), reads=[bC, B("junk")], writes=[B("ps%d" % (2 + kc % 2))])
            if vi == 2:
                S.act(lambda e, vi=vi, kc=kc: e.activation(out=GB[:, vi, kc * 128:(kc + 1) * 128], in_=PS[2 + kc % 2][:, 0:128], func=AF.Identity, bias=1.0),
                      reads=[B("ps%d" % (2 + kc % 2))], writes=[B("gb")])
            else:
                S.act(lambda e, vi=vi, kc=kc: e.activation(out=GB[:, vi, kc * 128:(kc + 1) * 128], in_=PS[2 + kc % 2][:, 0:128], func=AF.Copy),
                      reads=[B("ps%d" % (2 + kc % 2))], writes=[B("gb")])

    def layer_norm(src, dst, gi, bi, bsrc, bdst):
        S.act(lambda e: e.activation(out=JUNK, in_=src, func=AF.Copy, accum_out=ST[:, 0:1]), reads=[bsrc], writes=[B("junk"), B("st")])
        S.act(lambda e: e.activation(out=JUNK, in_=src, func=AF.Square, accum_out=ST[:, 1:2]), reads=[bsrc], writes=[B("junk"), B("st")])
        S.dve(lambda e: e.tensor_scalar(out=ST[:, 2:3], in0=ST[:, 0:1], scalar1=1.0 / 1024, scalar2=None, op0=ALU.mult), reads=[B("st")], writes=[B("st")])
        S.dve(lambda e: e.tensor_tensor(out=ST[:, 3:4], in0=ST[:, 2:3], in1=ST[:, 2:3], op=ALU.mult), reads=[B("st")], writes=[B("st")])
        S.dve(lambda e: e.scalar_tensor_tensor(out=ST[:, 4:5], in0=ST[:, 1:2], scalar=1.0 / 1024, in1=ST[:, 3:4], op0=ALU.mult, op1=ALU.subtract), reads=[B("st")], writes=[B("st")])
        S.act(lambda e: e.activation(out=ST[:, 5:6], in_=ST[:, 4:5], func=AF.Sqrt, bias=LN_EPS), reads=[B("st")], writes=[B("st")])
        S.dve(lambda e: e.reciprocal(out=ST[:, 6:7], in_=ST[:, 5:6]), reads=[B("st")], writes=[B("st")])
        S.dve(lambda e: e.tensor_scalar(out=dst, in0=src, scalar1=ST[:, 2:3], scalar2=ST[:, 6:7], op0=ALU.subtract, op1=ALU.mult), reads=[bsrc, B("st")], writes=[bdst])
        S.pool(lambda e: e.tensor_tensor(out=dst, in0=dst, in1=LNP[:, gi, :], op=ALU.mult), reads=[bdst, B("lnp")], writes=[bdst])
        S.pool(lambda e: e.tensor_tensor(out=dst, in0=dst, in1=LNP[:, bi, :], op=ALU.add), reads=[bdst, B("lnp")], writes=[bdst])

    dd_x1 = dbg("x1", [2048, 1024])
    dd_y = dbg("y", [2048, 1024])
    gcount = [0]
    XMP = [PS[0], PS[1]]
    YP = [PS[2], PS[3]]
    for tt in range(16):
        S.dma(lambda e, tt=tt: e.dma_start(out=XT, in_=d_xown[tt * 128:(tt + 1) * 128, :]), writes=[B("xt")])
        for kc in range(8):
            S.pe(lambda e, kc=kc, tt=tt: e.matmul(PS[6 + kc // 4][:, (kc % 4) * 128:(kc % 4 + 1) * 128], lhsT=MIX[:, tt, kc * 128:(kc + 1) * 128], rhs=identb,
                                                  start=True, stop=True),
                 reads=[B("mixa%d" % tt), B("att%d" % tt), bCB], writes=[B("ps%d" % (6 + kc // 4))])
        for hf in range(2):
            S.act(lambda e, hf=hf: e.activation(out=MIXT[:, hf * 4:(hf + 1) * 4, :].rearrange("p a b -> p (a b)"), in_=PS[6 + hf][:, 0:512], func=AF.Copy),
                  reads=[B("ps%d" % (6 + hf))], writes=[B("mixt")])
        for hf in range(2):
            for kc in range(8):
                S.pe(lambda e, hf=hf, kc=kc: e.matmul(PS[4 + hf][:, 0:512], lhsT=MIXT[:, kc, :], rhs=WOB[:, kc, hf * 512:(hf + 1) * 512], start=(kc == 0), stop=(kc == 7)),
                     reads=[B("mixt"), B("wob")], writes=[B("ps%d" % (4 + hf))])
        for hf in range(2):
            S.dve(lambda e, hf=hf: e.tensor_tensor(out=R1[:, hf * 512:(hf + 1) * 512], in0=PS[4 + hf][:, 0:512], in1=GB[:, 0, hf * 512:(hf + 1) * 512], op=ALU.mult),
                  reads=[B("ps%d" % (4 + hf)), B("gb")], writes=[B("r1")])
        S.dve(lambda e: e.scalar_tensor_tensor(out=R1, in0=XT, scalar=ALPHA, in1=R1, op0=ALU.mult, op1=ALU.add), reads=[B("xt"), B("r1")], writes=[B("r1")])
        layer_norm(R1, X1, 0, 1, B("r1"), B("x1"))
        if dd_x1 is not None:
            S.dma(lambda e, tt=tt: e.dma_start(out=dd_x1[tt * 128:(tt + 1) * 128, :], in_=X1), reads=[B("x1")], writes=[B("dbgx%d" % tt)])
        S.dve(lambda e: e.tensor_tensor(out=XM, in0=X1, in1=GB[:, 2, :], op=ALU.mult), reads=[B("x1"), B("gb")], writes=[B("xm")])
        S.dve(lambda e: e.tensor_tensor(out=XM, in0=XM, in1=GB[:, 1, :], op=ALU.add), reads=[B("xm"), B("gb")], writes=[B("xm")])
        for hf in range(2):
            S.act(lambda e, hf=hf: e.activation(out=XMP[hf][:, 0:512], in_=XM[:, hf * 512:(hf + 1) * 512], func=AF.Copy), reads=[B("xm")], writes=[B("ps%d" % hf)])
        for kc in range(8):
            S.pe(lambda e, kc=kc: e.matmul(PS[6 + kc // 4][:, (kc % 4) * 128:(kc % 4 + 1) * 128], lhsT=XM[:, kc * 128:(kc + 1) * 128], rhs=ident, start=True, stop=True),
                 reads=[B("xm"), bC], writes=[B("ps%d" % (6 + kc // 4))])
        for hf in range(2):
            S.act(lambda e, hf=hf: e.activation(out=XMT[:, hf * 4:(hf + 1) * 4, :].rearrange("p a b -> p (a b)"), in_=PS[6 + hf][:, 0:512], func=AF.Copy),
                  reads=[B("ps%d" % (6 + hf))], writes=[B("xmt")])
        for nb in range(4):
            for kc in range(8):
                S.pe(lambda e, nb=nb, kc=kc: e.matmul(PS[4 + nb][:, 0:512], lhsT=XMT[:, kc, :], rhs=WKB[:, kc, nb * 512:(nb + 1) * 512], start=(kc == 0), stop=(kc == 7)),
                     reads=[B("xmt"), B("wkb")], writes=[B("ps%d" % (4 + nb))])
            S.act(lambda e, nb=nb: e.activation(out=SC[:, nb * 512:(nb + 1) * 512], in_=PS[4 + nb][:, 0:512], func=AF.Copy), reads=[B("ps%d" % (4 + nb))], writes=[B("sc")])
        for blk in range(16):
            sc = SC[:, blk * 128:(blk + 1) * 128]
            S.dve(lambda e, blk=blk, sc=sc: e.max(out=TP1[:, blk, 0:8], in_=sc), reads=[B("sc")], writes=[B("tp1")])
            S.dve(lambda e, blk=blk, sc=sc: e.max_index(out=IP1[:, blk, 0:8], in_max=TP1[:, blk, 0:8], in_values=sc), reads=[B("sc"), B("tp1")], writes=[B("ip1")])
            S.dve(lambda e, blk=blk, sc=sc: e.match_replace(out=TMP[:, 0:128], in_to_replace=TP1[:, blk, 0:8], in_values=sc, imm_value=-1e30),
                  reads=[B("sc"), B("tp1")], writes=[B("tmp")])
            S.dve(lambda e, blk=blk: e.max(out=TP1[:, blk, 8:16], in_=TMP[:, 0:128]), reads=[B("tmp")], writes=[B("tp1")])
            S.dve(lambda e, blk=blk: e.max_index(out=IP1[:, blk, 8:16], in_max=TP1[:, blk, 8:16], in_values=TMP[:, 0:128]), reads=[B("tmp"), B("tp1")], writes=[B("ip1")])
        S.dve(lambda e: e.tensor_copy(out=IP1F, in_=IP1), reads=[B("ip1")], writes=[B("ip1f")])
        for h in range(8):
            S.dve(lambda e, h=h: e.tensor_tensor(out=CAND.rearrange("p (a b) -> p a b", a=16), in0=TP1[:, 2 * h, :].unsqueeze(2).to_broadcast([128, 16, 16]),
                                                 in1=TP1[:, 2 * h + 1, :].unsqueeze(1).to_broadcast([128, 16, 16]), op=ALU.add),
                  reads=[B("tp1")], writes=[B("cand")])
            S.dve(lambda e, h=h: e.max(out=TP2[:, h, 0:8], in_=CAND), reads=[B("cand")], writes=[B("tp2")])
            S.dve(lambda e, h=h: e.max_index(out=PP2[:, h, 0:8], in_max=TP2[:, h, 0:8], in_values=CAND), reads=[B("cand"), B("tp2")], writes=[B("pp2")])
            S.dve(lambda e, h=h: e.match_replace(out=TMP, in_to_replace=TP2[:, h, 0:8], in_values=CAND, imm_value=-1e30), reads=[B("cand"), B("tp2")], writes=[B("tmp")])
            S.dve(lambda e, h=h: e.max(out=TP2[:, h, 8:16], in_=TMP), reads=[B("tmp")], writes=[B("tp2")])
            S.dve(lambda e, h=h: e.max_index(out=PP2[:, h, 8:16], in_max=TP2[:, h, 8:16], in_values=TMP), reads=[B("tmp"), B("tp2")], writes=[B("pp2")])
        pp2f = PP2.rearrange("p a b -> p (a b)")
        S.dve(lambda e: e.tensor_single_scalar(out=PJ[:, 0, :], in_=pp2f, scalar=4, op=ALU.logical_shift_right), reads=[B("pp2")], writes=[B("pj")])
        S.dve(lambda e: e.tensor_single_scalar(out=PJ[:, 1, :], in_=pp2f, scalar=15, op=ALU.bitwise_and), reads=[B("pp2")], writes=[B("pj")])
        S.dve(lambda e: e.tensor_copy(out=PJF, in_=PJ), reads=[B("pj")], writes=[B("pjf")])
        for p in range(2):
            for h in range(8):
                oh = OH[:, h * 256:(h + 1) * 256].rearrange("p (a b) -> p a b", a=16)
                S.dve(lambda e, p=p, h=h, oh=oh: e.tensor_tensor(out=oh, in0=IOTA.rearrange("p (a b) -> p a b", a=16),
                                                                 in1=PJF[:, p, h * 16:(h + 1) * 16].unsqueeze(2).to_broadcast([128, 16, 16]), op=ALU.is_equal),
                      reads=[B("iota"), B("pjf")], writes=[B("oh")])
                S.dve(lambda e, p=p, h=h, oh=oh: e.tensor_tensor(out=oh, in0=oh, in1=IP1F[:, 2 * h + p, :].unsqueeze(1).to_broadcast([128, 16, 16]), op=ALU.mult),
                      reads=[B("oh"), B("ip1f")], writes=[B("oh")])
            S.dve(lambda e, p=p: e.tensor_reduce(out=IDXF[:, p, :], in_=OH.rearrange("p (a b) -> p a b", a=128), axis=AX.X, op=ALU.add), reads=[B("oh")], writes=[B("idxf")])
        S.dve(lambda e: e.scalar_tensor_tensor(out=EIF, in0=IDXF[:, 0, :], scalar=128.0, in1=IDXF[:, 1, :], op0=ALU.mult, op1=ALU.add), reads=[B("idxf")], writes=[B("eif")])
        S.dve(lambda e: e.tensor_copy(out=EI, in_=EIF), reads=[B("eif")], writes=[B("ei")])
        S.dve(lambda e: e.tensor_tensor(out=GEX, in0=TP2, in1=TP2[:, :, 0:1].to_broadcast([128, 8, 16]), op=ALU.subtract), reads=[B("tp2")], writes=[B("gex")])
        S.act(lambda e: e.activation(out=GEX, in_=GEX, func=AF.Exp), reads=[B("gex")], writes=[B("gex")])
        S.dve(lambda e: e.tensor_reduce(out=GSUM, in_=GEX, axis=AX.X, op=ALU.add), reads=[B("gex")], writes=[B("gsum")])
        S.dve(lambda e: e.reciprocal(out=GSUM, in_=GSUM), reads=[B("gsum")], writes=[B("gsum")])
        S.dve(lambda e: e.tensor_tensor(out=GEX, in0=GEX, in1=GSUM.unsqueeze(2).to_broadcast([128, 8, 16]), op=ALU.mult), reads=[B("gex"), B("gsum")], writes=[B("gex")])
        allpub = [B("pub%d" % r) for r in range(0, 16384, 2048)]
        allpvb = [B("pvb%d" % r) for r in range(0, 16384, 2048)]
        for sl in range(128):
            ug = UGB[gcount[0] % NSLOT]
            bu = B("ugb%d" % (gcount[0] % NSLOT))
            gcount[0] += 1
            S.dma(lambda e, ug=ug, sl=sl: e.indirect_dma_start(out=ug, out_offset=None, in_=d_pub,
                                                                in_offset=bass.IndirectOffsetOnAxis(ap=EI[:, sl:sl + 1], axis=0)),
                  reads=[B("ei")] + allpub, writes=[bu], q="pool")
            for hf in range(2):
                S.dve(lambda e, ug=ug, hf=hf, sl=sl: e.scalar_tensor_tensor(out=JUNK[:, hf * 512:(hf + 1) * 512], in0=ug[:, hf * 512:(hf + 1) * 512], scalar=1.0,
                                                                            in1=XMP[hf][:, 0:512], op0=ALU.mult, op1=ALU.mult,
                                                                            accum_out=DOT[:, sl:sl + 1] if hf == 0 else DOT2[:, sl:sl + 1]),
                      reads=[bu, B("ps%d" % hf)], writes=[B("junk"), B("dot")])
        S.dve(lambda e: e.tensor_tensor(out=DOT, in0=DOT, in1=DOT2, op=ALU.add), reads=[B("dot")], writes=[B("dot")])
        S.act(lambda e: e.activation(out=DOT, in_=DOT, func=AF.Gelu), reads=[B("dot")], writes=[B("dot")])
        S.dve(lambda e: e.tensor_tensor(out=COEF, in0=DOT, in1=GEX.rearrange("p a b -> p (a b)"), op=ALU.mult), reads=[B("dot"), B("gex")], writes=[B("coef")])
        for hf in range(2):
            S.dve(lambda e, hf=hf: e.memset(YP[hf][:, 0:512], 0.0), writes=[B("ps%d" % (2 + hf))])
        for sl in range(128):
            ug = UGB[gcount[0] % NSLOT]
            bu = B("ugb%d" % (gcount[0] % NSLOT))
            gcount[0] += 1
            S.dma(lambda e, ug=ug, sl=sl: e.indirect_dma_start(out=ug, out_offset=None, in_=d_pvb,
                                                                in_offset=bass.IndirectOffsetOnAxis(ap=EI[:, sl:sl + 1], axis=0)),
                  reads=[B("ei")] + allpvb, writes=[bu], q="pool")
            for hf in range(2):
                S.dve(lambda e, ug=ug, hf=hf, sl=sl: e.scalar_tensor_tensor(out=YP[hf][:, 0:512], in0=ug[:, hf * 512:(hf + 1) * 512], scalar=COEF[:, sl:sl + 1],
                                                                            in1=YP[hf][:, 0:512], op0=ALU.mult, op1=ALU.add),
                      reads=[bu, B("coef"), B("ps%d" % (2 + hf))], writes=[B("ps%d" % (2 + hf))])
        if dd_y is not None:
            for hf in range(2):
                S.act(lambda e, hf=hf: e.activation(out=JUNK[:, hf * 512:(hf + 1) * 512], in_=YP[hf][:, 0:512], func=AF.Copy), reads=[B("ps%d" % (2 + hf))], writes=[B("junk")])
            S.dma(lambda e, tt=tt: e.dma_start(out=dd_y[tt * 128:(tt + 1) * 128, :], in_=JUNK), reads=[B("junk")], writes=[B("dbgy%d" % tt)])
        for hf in range(2):
            S.dve(lambda e, hf=hf: e.tensor_tensor(out=R1[:, hf * 512:(hf + 1) * 512], in0=YP[hf][:, 0:512], in1=GB[:, 3, hf * 512:(hf + 1) * 512], op=ALU.mult),
                  reads=[B("ps%d" % (2 + hf)), B("gb")], writes=[B("r1")])
        S.dve(lambda e: e.scalar_tensor_tensor(out=R1, in0=X1, scalar=ALPHA, in1=R1, op0=ALU.mult, op1=ALU.add), reads=[B("x1"), B("r1")], writes=[B("r1")])
        layer_norm(R1, XM, 2, 3, B("r1"), B("xm"))
        S.dma(lambda e, tt=tt: e.dma_start(out=d_out[tt * 128:(tt + 1) * 128, :], in_=XM), reads=[B("xm")], writes=[B("out%d" % tt)])


def _win(xb, lo, n, rev):
    L = xb.shape[0]
    w = np.zeros((n + 2, 1024), np.float32)
    a, b = lo - 1, lo + n + 1
    sa, sb = max(a, 0), min(b, L)
    w[sa - a:sb - a] = xb[sa:sb]
    if rev:
        w = w[::-1]
    return np.ascontiguousarray(w.reshape(n + 2, 8, 128).transpose(2, 1, 0))


def _rope_tables(tok):
    half = 16
    freq = (10000.0 ** (-np.arange(half, dtype=np.float32) / half)).astype(np.float32)
    row = (tok // 64).astype(np.float32)
    col = (tok % 64).astype(np.float32)
    cos = np.zeros((64, len(tok)), np.float32)
    sin = np.zeros((64, len(tok)), np.float32)
    for dd in range(64):
        pos = row if dd < 32 else col
        ang = pos * freq[dd % 16]
        cos[dd] = np.cos(ang)
        sin[dd] = np.sin(ang)
    return np.concatenate([cos, cos], 0), np.concatenate([sin, sin], 0)


_CACHE = {}


def kernel(x, c, ctx, c_ctx, w_mod, b_mod, w_in, conv_w, conv_b, b_gates, mh_norm_w, q_norm_w, k_norm_w, w_out,
           ln1_g, ln1_b, peer_wq, peer_keys, peer_u, peer_v, ln2_g, ln2_b):
    f32 = np.float32
    x = np.asarray(x, f32); c = np.asarray(c, f32); ctx = np.asarray(ctx, f32); c_ctx = np.asarray(c_ctx, f32)
    w_mod = np.asarray(w_mod, f32)[0]; b_mod = np.asarray(b_mod, f32)[0]; w_in = np.asarray(w_in, f32)[0]
    conv_w = np.asarray(conv_w, f32)[0]; conv_b = np.asarray(conv_b, f32)[0]; b_gates = np.asarray(b_gates, f32)[0]
    mh_norm_w = np.asarray(mh_norm_w, f32)[0]; q_norm_w = np.asarray(q_norm_w, f32)[0]; k_norm_w = np.asarray(k_norm_w, f32)[0]
    w_out = np.asarray(w_out, f32)[0]; ln1_g = np.asarray(ln1_g, f32)[0]; ln1_b = np.asarray(ln1_b, f32)[0]
    peer_wq = np.asarray(peer_wq, f32)[0]; peer_keys = np.asarray(peer_keys, f32)[0]
    peer_u = np.asarray(peer_u, f32)[0]; peer_v = np.asarray(peer_v, f32)[0]
    ln2_g = np.asarray(ln2_g, f32)[0]; ln2_b = np.asarray(ln2_b, f32)[0]

    if "nc" not in _CACHE:
        _CACHE["nc"] = build_program()
    nc, dbg_names = _CACHE["nc"]

    rep = lambda v: np.ascontiguousarray(np.broadcast_to(np.asarray(v, f32).reshape(1, -1), (128, np.asarray(v).size)))
    wmod_l = np.ascontiguousarray(w_mod.reshape(8, 128, 12, 512).transpose(2, 1, 0, 3))
    bmod_l = np.ascontiguousarray(np.broadcast_to(b_mod.reshape(12, 1, 512), (12, 2, 512)))
    qa_cols = []
    for mc in range(4):
        qa_cols += list(range(2064 + mc * 64, 2064 + (mc + 1) * 64)) + list(range(2064 + (4 + mc) * 64, 2064 + (5 + mc) * 64))
    perm = (list(range(512, 1024)) + list(range(0, 512)) + list(range(2576, 2704)) + qa_cols + list(range(1024, 1536))
            + list(range(2704, 2832)) + list(range(2048, 2064)) + list(range(1536, 2048)))
    win_l = np.ascontiguousarray(w_in[:, perm].reshape(8, 128, 2832))
    convb_l = np.ascontiguousarray(conv_b.reshape(8, 128).T)
    cw = conv_w.reshape(3, 8, 128)
    taps_nat = np.ascontiguousarray(cw.transpose(2, 1, 0))
    taps_rev = np.ascontiguousarray(taps_nat[:, :, ::-1])
    qtaps_l = np.ascontiguousarray(np.stack([taps_nat[:, 0:4], taps_rev[:, 0:4]], 1).reshape(128, 24))
    bg_l = rep(b_gates)
    mhw_l = rep(mh_norm_w)
    nw_l = np.ascontiguousarray(np.stack([np.tile(q_norm_w, 2), np.tile(k_norm_w, 2)], 1))
    wout_l = np.ascontiguousarray(w_out.reshape(8, 128, 1024))
    wqT_l = np.ascontiguousarray(peer_wq.T.reshape(16, 128, 1024))
    keysT_l = np.ascontiguousarray(peer_keys.reshape(16, 128, 128).transpose(2, 0, 1).reshape(128, 2048))
    lnp_l = np.ascontiguousarray(np.concatenate([rep(ln1_g), rep(ln1_b), rep(ln2_g), rep(ln2_b)], 1))
    ii = np.arange(128)
    ident = np.eye(128, dtype=f32)
    tri = (ii[:, None] <= ii[None, :]).astype(f32)
    mask_sr = np.where(ii[None, :] <= ii[:, None], 0.0, NEG).astype(f32)
    mask_rs = np.where(ii[:, None] <= ii[None, :], 0.0, NEG).astype(f32)
    jm = ident[::-1].copy()
    blk64 = (ii[:, None] // 64 == ii[None, :] // 64).astype(f32)
    R = np.zeros((128, 128), f32)
    for i in range(128):
        if (i % 32) < 16:
            R[i, i + 16] = -1.0
        else:
            R[i, i - 16] = 1.0
    sel0 = np.zeros((128, 128), f32)
    sel0[0, :] = 1.0
    cst_l = np.ascontiguousarray(np.concatenate([ident, np.ones((128, 128), f32), tri, mask_sr, mask_rs, jm, blk64, R.T.copy(), sel0], 1))
    iota_l = np.ascontiguousarray(np.broadcast_to(np.tile(np.arange(16, dtype=f32), 16).reshape(1, 256), (128, 256)))

    in_maps = []
    for core in range(NCORES):
        b, j = divmod(core, 4)
        xb = x[b]
        cwin = np.stack([_win(ctx[b], 0, 256, False), _win(ctx[b], 0, 256, True)], 0)
        oth = [(G, False) for G in range(0, 4 * j)] + [(G, True) for G in range(15, 4 * j + 3, -1)]
        ownB = [(G, True) for G in range(4 * j + 3, 4 * j - 1, -1)]
        ownF = [(G, False) for G in range(4 * j, 4 * j + 4)]
        srcs = oth + ownB + ownF
        assert len(oth) == 12 and len(srcs) == NGRP
        xwin = np.stack([_win(xb, 512 * G, 512, rv) for (G, rv) in srcs], 0)
        ktaps = np.zeros((128, 22, 4, 3), f32)
        flags = np.zeros((128, 22, 2), f32)
        ktaps[:, 0] = taps_nat[:, 4:8]
        ktaps[:, 1] = taps_rev[:, 4:8]
        for gi, (G, rv) in enumerate(srcs):
            ktaps[:, 2 + gi] = (taps_rev if rv else taps_nat)[:, 4:8]
            fl = [0.0 if G == 0 else 1.0, 0.0 if G == 15 else 1.0]
            flags[:, 2 + gi] = fl[::-1] if rv else fl
        gmask = np.zeros((128, 12, 4), f32)
        for gi, (G, rv) in enumerate(oth):
            fwd_real = not rv
            gmask[:, gi] = [1.0, 0.0, 0.0, NEG] if fwd_real else [0.0, NEG, 1.0, 0.0]
        rope = np.zeros((16, 2, 128, 512), f32)
        for ri, (G, rv) in enumerate(oth + ownF):
            tok = np.arange(512 * G, 512 * G + 512)
            if rv:
                tok = tok[::-1]
            cs, sn = _rope_tables(tok)
            rope[ri, 0] = cs
            rope[ri, 1] = sn
        cvec = np.ascontiguousarray(np.stack([c[b].reshape(8, 128).T, c_ctx.reshape(8, 128).T], 2).reshape(128, 16))
        in_maps.append(dict(
            cwin=cwin, xwin=xwin, xown=np.ascontiguousarray(xb[2048 * j:2048 * (j + 1)]), wmod=wmod_l, bmod=bmod_l, cvec=cvec,
            win=win_l, ktaps=np.ascontiguousarray(ktaps.reshape(128, 264)), qtaps=qtaps_l, convb=convb_l,
            flags=np.ascontiguousarray(flags.reshape(128, 44)), gmask=np.ascontiguousarray(gmask.reshape(128, 48)), bg=bg_l, mhw=mhw_l, nw=nw_l,
            wout=wout_l, wqT=wqT_l, keysT=keysT_l, lnp=lnp_l, pu=peer_u, pv=peer_v, cst=cst_l, iota16=iota_l, rope=rope))
    res = run_bass_kernel_spmd(nc, in_maps[:NCORES], core_ids=list(range(NCORES)))
    out = np.zeros((2, 8192, 1024), f32)
    for core in range(NCORES):
        b, j = divmod(core, 4)
        out[b, 2048 * j:2048 * (j + 1)] = res.results[core]["out"]
    if dbg_names:
        _CACHE["dbg"] = [{n: res.results[core]["dbg_" + n] for n in dbg_names} for core in range(NCORES)]
    return out
```

```python
import os
import types
import numpy as np
import concourse.bass as bass
import concourse.mybir as mybir
from concourse.bass_utils import run_bass_kernel_spmd

F32 = mybir.dt.float32
BF16 = mybir.dt.bfloat16
I32 = mybir.dt.int32
U32 = mybir.dt.uint32
ALU = mybir.AluOpType
AF = mybir.ActivationFunctionType
AX = mybir.AxisListType

NEG = -30000.0
LN_EPS = 1e-5
RMS_EPS = 1e-6
ALPHA = 2.0 ** 0.25
NGRP = 20
DBG = os.environ.get("KDBG", "")


class Buf:
    __slots__ = ("name", "lw", "rd")

    def __init__(self, name):
        self.name = name
        self.lw = None
        self.rd = []


class Op:
    __slots__ = ("eng", "fn", "reads", "writes", "dma", "deps", "signal", "sem", "val", "slot_prev", "bar")

    def __init__(self, eng, fn, reads, writes, dma):
        self.eng = eng
        self.fn = fn
        self.reads = reads
        self.writes = writes
        self.dma = dma
        self.deps = set()
        self.signal = False
        self.sem = None
        self.val = 0
        self.slot_prev = None
        self.bar = 0


def _freeze(fn):
    if fn is None or fn.__closure__ is None:
        return fn
    cells = []
    for c in fn.__closure__:
        try:
            cells.append(types.CellType(c.cell_contents))
        except ValueError:
            cells.append(c)
    return types.FunctionType(fn.__code__, fn.__globals__, fn.__name__, fn.__defaults__, tuple(cells))


class Sched:
    ENGS = ("pe", "act", "dve", "pool", "sp")

    def __init__(self, nc, n_dma_sems=32):
        self.nc = nc
        self.ops = []
        self.n_dma_sems = n_dma_sems
        self.nbar = 0
        self.bank = {}

    def op(self, eng, fn, reads=(), writes=(), dma=False):
        reads = [b for b in reads if b is not None]
        writes = [b for b in writes if b is not None]
        banks = set()
        for b in reads + writes:
            n = b.name
            if n.startswith("pso") and n[3:4].isdigit():
                banks.add(4 + int(n[3]))
            elif n.startswith("ps") and n[2:3].isdigit():
                banks.add(int(n[2]))
        for k in sorted(banks):
            if k not in self.bank:
                self.bank[k] = Buf("BANK%d" % k)
            writes.append(self.bank[k])
        o = Op(eng, _freeze(fn), reads, writes, dma)
        o.bar = self.nbar
        self.ops.append(o)
        return o

    def pe(self, fn, reads=(), writes=()):
        return self.op("pe", fn, reads, writes)

    def act(self, fn, reads=(), writes=()):
        return self.op("act", fn, reads, writes)

    def dve(self, fn, reads=(), writes=()):
        return self.op("dve", fn, reads, writes)

    def pool(self, fn, reads=(), writes=()):
        return self.op("pool", fn, reads, writes)

    def dma(self, fn, reads=(), writes=(), q="sp"):
        return self.op(q, fn, reads, writes, dma=True)

    def barrier(self):
        self.nbar += 1

    def fork(self):
        self._saved = self.ops
        self.ops = []

    def take(self):
        lst = self.ops
        self.ops = []
        return lst

    def join(self, lists):
        merged = []
        n = max(len(l) for l in lists)
        for i in range(n):
            for l in lists:
                if i < len(l):
                    merged.append(l[i])
        self.ops = self._saved + merged

    def _resolve(self):
        ops = self.ops
        last_eng = {}
        dma_all = []
        bar_deps = {}
        seen_bar = {e: 0 for e in self.ENGS}
        cur_bar = 0
        for i, o in enumerate(ops):
            if o.bar != cur_bar:
                cur_bar = o.bar
                bar_deps[cur_bar] = list(last_eng.values()) + list(dma_all)
            deps = set()
            raw = set()
            for b in o.reads:
                if b.lw is not None:
                    deps.add(b.lw)
                    raw.add(b.lw)
            for b in o.writes:
                if b.lw is not None:
                    deps.add(b.lw)
                for r in b.rd:
                    deps.add(r)
            for b in o.reads:
                b.rd.append(i)
            for b in o.writes:
                b.lw = i
                b.rd = []
            deps.discard(i)
            for j in deps:
                p = ops[j]
                if (not p.dma) and (not o.dma) and p.eng == o.eng:
                    if o.eng == "pe" or j not in raw:
                        continue
                o.deps.add(j)
                p.signal = True
            if seen_bar[o.eng] != cur_bar:
                seen_bar[o.eng] = cur_bar
                for j in bar_deps[cur_bar]:
                    p = ops[j]
                    if (not p.dma) and (not o.dma) and p.eng == o.eng:
                        continue
                    o.deps.add(j)
                    p.signal = True
            if o.dma:
                dma_all.append(i)
            elif o.fn is not None:
                last_eng[o.eng] = i

    def emit(self):
        nc = self.nc
        self._resolve()
        ops = self.ops
        esem = {e: nc.alloc_semaphore("s_" + e) for e in self.ENGS}
        nds = self.n_dma_sems
        dsem = [nc.alloc_semaphore("s_dma%d" % k) for k in range(2 * nds)]
        ecount = {e: 0 for e in self.ENGS}
        dcount = [0] * (2 * nds)
        dlast = [None] * (2 * nds)
        nd = 0
        ndq = {"sp": 0, "pool": 0, "act": 0}
        for i, o in enumerate(ops):
            if o.dma:
                base = nds if o.eng == "pool" else 0
                k = base + ndq[o.eng] % nds
                ndq[o.eng] += 1
                nd += 1
                o.signal = True
                o.sem = ("d", k)
                dcount[k] += 16
                o.val = dcount[k]
                o.slot_prev = dlast[k]
                dlast[k] = i
            elif o.signal:
                ecount[o.eng] += 1
                o.sem = ("e", o.eng)
                o.val = ecount[o.eng]
        self.stats = dict(ecount)
        self.stats["ndma"] = nd
        self.stats["nops"] = len(ops)

        def semh(s):
            return esem[s[1]] if s[0] == "e" else dsem[s[1]]

        per_eng = {e: [] for e in self.ENGS}
        for i, o in enumerate(ops):
            per_eng[o.eng].append(i)

        def emit_stream(ename, eng):
            waited = {}
            for i in per_eng[ename]:
                o = ops[i]
                need = {}
                for j in o.deps:
                    p = ops[j]
                    if need.get(p.sem, 0) < p.val:
                        need[p.sem] = p.val
                if o.dma and o.slot_prev is not None:
                    p = ops[o.slot_prev]
                    if need.get(p.sem, 0) < p.val:
                        need[p.sem] = p.val
                for s, v in need.items():
                    if waited.get(s, 0) >= v:
                        continue
                    eng.wait_ge(semh(s), v)
                    waited[s] = v
                if o.fn is None:
                    continue
                ins = o.fn(eng)
                if o.signal:
                    ins.then_inc(semh(o.sem), 16 if o.dma else 1)

        with nc.Block() as block:
            @block.tensor
            def _(e):
                emit_stream("pe", e)

            @block.scalar
            def _(e):
                emit_stream("act", e)

            @block.vector
            def _(e):
                emit_stream("dve", e)

            @block.gpsimd
            def _(e):
                emit_stream("pool", e)

            @block.sync
            def _(e):
                emit_stream("sp", e)


class Arena:
    def __init__(self, nc, nbytes):
        self.t = nc.alloc_sbuf_tensor("arena", [128, nbytes // 4], F32)
        self.off = 0
        self.cap = nbytes
        self.hi = 0

    def alloc(self, shape, dtype=F32):
        esz = 2 if dtype == BF16 else 4
        n = 1
        for s in shape[1:]:
            n *= s
        nb = (n * esz + 31) // 32 * 32
        o = self.off
        self.off += nb
        self.hi = max(self.hi, self.off)
        assert self.off <= self.cap, ("arena overflow", self.off, self.cap)
        v = self.t[:, o // 4:(o + nb) // 4]
        if dtype != F32:
            v = v.bitcast(dtype)
        v = v[:, 0:n]
        if len(shape) == 3:
            v = v.rearrange("p (a b) -> p a b", a=shape[1])
        elif len(shape) == 4:
            v = v.rearrange("p (a b c) -> p a b c", a=shape[1], b=shape[2])
        if shape[0] != 128:
            v = v[0:shape[0]]
        return v


class _Stop(Exception):
    pass


STOP = int(os.environ.get("KSTOP", "9"))
NCORES = int(os.environ.get("KCORES", "8"))


def build_program():
    st = {}
    try:
        _body(st)
    except _Stop:
        pass
    S = st["S"]
    B = st["B"]
    S.op("sp", None, reads=[B("out%d" % i) for i in range(16)] + [B("dbgo"), B("dbgo2"), B("dbgo3")] + [B("dbgx%d" % i) for i in range(16)] + [B("dbgy%d" % i) for i in range(16)])
    S.emit()
    print("sched stats", S.stats)
    return st["nc"], list(st["dbg_out"].keys())


def _body(st):
    nc = bass.Bass("TRN2", target_bir_lowering=False)
    S = Sched(nc)
    bufs = {}
    st["nc"] = nc
    st["S"] = S

    def B(name):
        b = bufs.get(name)
        if b is None:
            b = bufs[name] = Buf(name)
        return b

    def din(name, shape, dt=F32):
        return nc.dram_tensor(name, list(shape), dt, kind="ExternalInput").ap()

    d_cwin = din("cwin", [2, 128, 8, 258])
    d_xwin = din("xwin", [NGRP, 128, 8, 514])
    d_xown = din("xown", [2048, 1024])
    d_wmod = din("wmod", [12, 128, 8, 512])
    d_bmod = din("bmod", [12, 2, 512])
    d_cvec = din("cvec", [128, 16])
    d_win = din("win", [8, 128, 2832])
    d_ktaps = din("ktaps", [128, 22 * 12])
    d_qtaps = din("qtaps", [128, 2 * 12])
    d_cb = din("convb", [128, 8])
    d_flags = din("flags", [128, 44])
    d_gmask = din("gmask", [128, 48])
    d_bg = din("bg", [128, 16])
    d_mhw = din("mhw", [128, 512])
    d_nw = din("nw", [128, 2])
    d_wout = din("wout", [8, 128, 1024])
    d_wqT = din("wqT", [16, 128, 1024])
    d_keysT = din("keysT", [128, 16 * 128])
    d_lnp = din("lnp", [128, 4 * 1024])
    d_pu = din("pu", [16384, 1024])
    d_pv = din("pv", [16384, 1024])
    d_cst = din("cst", [128, 9 * 128])
    d_iota = din("iota16", [128, 256])
    d_rope = din("rope", [16, 2, 128, 512])
    d_out = nc.dram_tensor("out", [2048, 1024], F32, kind="ExternalOutput").ap()
    d_pub = nc.dram_tensor("pub", [16384, 1024], BF16).ap()
    d_pvb = nc.dram_tensor("pvb", [16384, 1024], BF16).ap()
    dbg_out = {}
    st["B"] = B
    st["dbg_out"] = dbg_out

    def dbg(name, shape, dt=F32):
        if name in DBG.split(","):
            dbg_out[name] = nc.dram_tensor("dbg_" + name, list(shape), dt, kind="ExternalOutput").ap()
            return dbg_out[name]
        return None

    AR = Arena(nc, 212800)
    PS = [nc.alloc_psum_tensor("ps%d" % i, [128, 512], F32) for i in range(8)]

    CST = AR.alloc([128, 9 * 128])
    ident = CST[:, 0:128]
    ones = CST[:, 128:256]
    tri = CST[:, 256:384]
    mask_sr = CST[:, 384:512]
    mask_rs = CST[:, 512:640]
    jmat = CST[:, 640:768]
    blk64 = CST[:, 768:896]
    rTm = CST[:, 896:1024]
    sel0 = CST[:, 1024:1152]
    CSTB = AR.alloc([128, 2 * 128], BF16)
    identb = CSTB[:, 0:128]
    jb = CSTB[:, 128:256]
    COLS = AR.alloc([128, 48, 2])
    SC1P = AR.alloc([128, 8, 2])
    MIX = AR.alloc([128, 16, 1024], BF16)
    MHW = AR.alloc([128, 512])
    KTAPS = AR.alloc([128, 22 * 12])
    QTAPS = AR.alloc([128, 24])
    CONVB = AR.alloc([128, 8])
    FLAGS = AR.alloc([128, 44])
    GMASK = AR.alloc([128, 48])
    BG = AR.alloc([128, 16])
    NW = AR.alloc([128, 2])
    MST = [AR.alloc([128, 4]) for _ in range(2)]
    CT = [AR.alloc([128, 4, 129]) for _ in range(2)]
    CTB = [AR.alloc([128, 4, 129], BF16) for _ in range(2)]
    SMALL = AR.alloc([128, 512])
    smo = [0]

    def small(n):
        o = smo[0]
        smo[0] += n
        assert smo[0] <= 512
        return SMALL[:, o:o + n]

    bC = B("cst")
    S.dma(lambda e: e.dma_start(out=CST, in_=d_cst), writes=[bC])
    S.act(lambda e: e.activation(out=identb, in_=ident, func=AF.Copy), reads=[bC], writes=[B("cstb")])
    S.act(lambda e: e.activation(out=jb, in_=jmat, func=AF.Copy), reads=[bC], writes=[B("cstb")])
    bCB = B("cstb")
    for (t, d, n) in ((MHW, d_mhw, "mhw"), (KTAPS, d_ktaps, "ktaps"), (QTAPS, d_qtaps, "qtaps"), (CONVB, d_cb, "convb"),
                      (FLAGS, d_flags, "flags"), (GMASK, d_gmask, "gmask"), (BG, d_bg, "bg"), (NW, d_nw, "nw")):
        S.dma(lambda e, t=t, d=d: e.dma_start(out=t, in_=d), writes=[B(n)], q="pool")
    for d in range(2):
        S.pool(lambda e, d=d: e.memset(MST[d], 0.0), writes=[B("m%d" % d)])
        S.pool(lambda e, d=d: e.memset(CT[d], 0.0), writes=[B("ct%d" % d)])
        S.pool(lambda e, d=d: e.memset(CTB[d], 0.0), writes=[B("ctb%d" % d)])

    mark_persist = AR.off
    for (src_, dst_, nm) in ((d_pu, d_pub, "pub"), (d_pv, d_pvb, "pvb")):
        for r in range(0, 16384, 2048):
            S.dma(lambda e, src_=src_, dst_=dst_, r=r: e.dma_start(out=dst_[r:r + 2048, :], in_=src_[r:r + 2048, :]), writes=[B("%s%d" % (nm, r))], q="pool")

    CV = AR.alloc([128, 16])
    SIL = AR.alloc([128, 8, 2])
    WM = [AR.alloc([128, 8, 512]) for _ in range(2)]
    BM = [AR.alloc([2, 512]) for _ in range(2)]
    MROW = [AR.alloc([2, 512]) for _ in range(2)]
    S.dma(lambda e: e.dma_start(out=CV, in_=d_cvec), writes=[B("cv")])
    S.act(lambda e: e.activation(out=SIL.rearrange("p a b -> p (a b)"), in_=CV, func=AF.Silu), reads=[B("cv")], writes=[B("sil")])
    colps = PS[1][:, 0:96]
    for g in range(12):
        wm = WM[g % 2]
        bw = B("wm%d" % (g % 2))
        S.dma(lambda e, wm=wm, g=g: e.dma_start(out=wm, in_=d_wmod[g]), writes=[bw])
        S.dma(lambda e, g=g: e.dma_start(out=BM[g % 2], in_=d_bmod[g]), writes=[B("bm%d" % (g % 2))], q="pool")
        for kc in range(8):
            S.pe(lambda e, wm=wm, kc=kc: e.matmul(PS[0][0:2, :], lhsT=SIL[:, kc, :], rhs=wm[:, kc, :], start=(kc == 0), stop=(kc == 7)),
                 reads=[B("sil"), bw], writes=[B("ps0")])
        mr = MROW[g % 2]
        S.dve(lambda e, g=g, mr=mr: e.tensor_tensor(out=mr, in0=PS[0][0:2, :], in1=BM[g % 2], op=ALU.add),
              reads=[B("ps0"), B("bm%d" % (g % 2))], writes=[B("mrow%d" % (g % 2))])
        for fc in range(4):
            idx = g * 4 + fc
            S.pe(lambda e, mr=mr, fc=fc, idx=idx: e.matmul(colps[:, idx * 2:idx * 2 + 2], lhsT=mr[:, fc * 128:(fc + 1) * 128], rhs=ident[0:2, 0:2],
                                                           start=True, stop=True),
                 reads=[B("mrow%d" % (g % 2)), bC], writes=[B("ps1")])
    S.dve(lambda e: e.tensor_copy(out=COLS.rearrange("p a b -> p (a b)"), in_=colps), reads=[B("ps1")], writes=[B("cols")])
    S.dve(lambda e: e.tensor_scalar(out=SC1P, in0=COLS[:, 8:16, :], scalar1=1.0, scalar2=None, op0=ALU.add), reads=[B("cols")], writes=[B("sc1p")])
    dd = dbg("cols", [128, 96])
    if dd is not None:
        S.dma(lambda e, dd=dd: e.dma_start(out=dd, in_=COLS.rearrange("p a b -> p (a b)")), reads=[B("cols")], writes=[B("dbgo")])

    if STOP == 0:
        raise _Stop()
    AR.off = mark_persist
    S.barrier()

    WB = AR.alloc([128, 8, 2832], BF16)
    QAT = AR.alloc([128, 4, 2048], BF16)
    KAT = AR.alloc([128, 66 * 128], BF16)
    VA = AR.alloc([128, 66, 2, 65], BF16)
    XW = AR.alloc([128, 8, 514])
    HT = AR.alloc([128, 8, 514], BF16)
    KT = AR.alloc([128, 4, 512], BF16)
    QT = AR.alloc([128, 4, 512], BF16)
    ZB = [AR.alloc([128, 514]) for _ in range(2)]
    TB = [AR.alloc([128, 512]) for _ in range(2)]
    SQ = AR.alloc([128, 512])
    SD = AR.alloc([128, 512])
    KN = AR.alloc([128, 512])
    ROPE = AR.alloc([128, 2, 512])
    KTOK2 = [AR.alloc([128, 4, 128], BF16) for _ in range(2)]
    VAUG2 = [AR.alloc([128, 4, 129], BF16) for _ in range(2)]
    VW = [[AR.alloc([128, 129], BF16) for _ in range(2)] for _ in range(2)]
    DA = AR.alloc([128, 128])
    DM = AR.alloc([128, 128])
    DTT = AR.alloc([128, 128])
    PT = AR.alloc([128, 128], BF16)
    TT1 = AR.alloc([128, 129])
    TOT = AR.alloc([128, 129])
    HF = AR.alloc([128, 512], BF16)
    HN = AR.alloc([128, 512])
    OSG = AR.alloc([128, 512])
    GP2 = [AR.alloc([128, 16]) for _ in range(2)]
    WST = [QAT.rearrange("p a b -> p (a b)").bitcast(F32)[:, 0:2832], KAT.bitcast(F32)[:, 0:2832]]
    print("phase1 arena bytes", AR.off)

    for par in range(2):
        S.pool(lambda e: e.memset(VAUG2[par][:, :, 128:129], 1.0), writes=[B("vaug%d" % par)])
    S.pool(lambda e: e.memset(VA[:, :, :, 64:65], 1.0), writes=[B("va")])

    for kc in range(8):
        st = WST[kc % 2]
        bs = B("qat" if kc % 2 == 0 else "kat")
        S.dma(lambda e, st=st, kc=kc: e.dma_start(out=st, in_=d_win[kc]), writes=[bs])
        if kc % 2 == 0:
            S.act(lambda e, st=st, kc=kc: e.activation(out=WB[:, kc, :], in_=st, func=AF.Copy), reads=[bs], writes=[B("wb")])
        else:
            S.pool(lambda e, st=st, kc=kc: e.tensor_copy(out=WB[:, kc, :], in_=st), reads=[bs], writes=[B("wb")])

    C_KM, C_QM, C_KA, C_QA, C_VM, C_VA, C_G, C_O = 0, 512, 1024, 1152, 1664, 2176, 2304, 2320

    def mk_small():
        return dict(ef=small(4), nlf=small(4), li=small(4), a=small(4), amx=small(1), d4=small(4), ml=small(4), dm=small(4),
                    dec=small(4), aw=small(4), w=small(4), cm=small(1), mv=small(1), nmv=small(1), dwi=small(1), wi=small(1),
                    dn0=small(1), nrm=small(1), ad=small(1), dn=small(1), rd=small(1))
    SM = [mk_small(), mk_small()]
    S1 = small(4)
    S2 = small(4)
    MEAN = small(4)
    MSQ = small(4)
    VAR = small(4)
    RSTD = small(4)

    def fm_project(c0, N, halo, psb):
        for kc in range(8):
            S.pe(lambda e, kc=kc: e.matmul(PS[psb][:, 0:N], lhsT=WB[:, kc, c0:c0 + 128], rhs=HT[:, kc, 1:N + 1], start=(kc == 0), stop=(kc == 7)),
                 reads=[B("wb"), B("ht")], writes=[B("ps%d" % psb)])
        if halo:
            for kc in range(8):
                S.pe(lambda e, kc=kc: e.matmul(PS[2][:, 0:2], lhsT=WB[:, kc, c0:c0 + 128], rhs=HT[:, kc, 0:N + 2:N + 1], start=(kc == 0), stop=(kc == 7)),
                     reads=[B("wb"), B("ht")], writes=[B("ps2")])

    def conv_block(N, psb, zi, taps, cbias, fl, dst, qscale):
        Z = ZB[zi]
        T = TB[zi]
        bz = B("z%d" % zi)
        bt = B("t%d" % zi)
        S.act(lambda e: e.activation(out=Z[:, 1:N + 1], in_=PS[psb][:, 0:N], func=AF.Copy), reads=[B("ps%d" % psb)], writes=[bz])
        S.dve(lambda e: e.tensor_tensor(out=Z[:, 0:N + 2:N + 1], in0=PS[2][:, 0:2], in1=fl, op=ALU.mult), reads=[B("ps2"), B("flags")], writes=[bz])
        S.dve(lambda e: e.tensor_scalar(out=T[:, 0:N], in0=Z[:, 0:N], scalar1=taps[:, 0:1], scalar2=None, op0=ALU.mult),
              reads=[bz, B("ktaps"), B("qtaps")], writes=[bt])
        S.dve(lambda e: e.scalar_tensor_tensor(out=T[:, 0:N], in0=Z[:, 1:N + 1], scalar=taps[:, 1:2], in1=T[:, 0:N], op0=ALU.mult, op1=ALU.add),
              reads=[bz, bt], writes=[bt])
        S.dve(lambda e: e.scalar_tensor_tensor(out=T[:, 0:N], in0=Z[:, 2:N + 2], scalar=taps[:, 2:3], in1=T[:, 0:N], op0=ALU.mult, op1=ALU.add),
              reads=[bz, bt], writes=[bt])
        if qscale:
            S.act(lambda e: e.activation(out=T[:, 0:N], in_=T[:, 0:N], func=AF.Silu, bias=cbias), reads=[bt, B("convb")], writes=[bt])
            S.pool(lambda e: e.tensor_scalar(out=dst, in0=T[:, 0:N], scalar1=128.0 ** -0.5, scalar2=None, op0=ALU.mult), reads=[bt], writes=[B("qt")])
        else:
            S.act(lambda e: e.activation(out=dst, in_=T[:, 0:N], func=AF.Silu, bias=cbias), reads=[bt, B("convb")], writes=[B("kt")])

    def normrope(N, psb, nwcol, rope, dst, bdst):
        ps = PS[psb][:, 0:N]
        bp = B("ps%d" % psb)
        S.act(lambda e: e.activation(out=SQ[:, 0:N], in_=ps, func=AF.Square), reads=[bp], writes=[B("sq")])
        S.pe(lambda e: e.matmul(PS[6][:, 0:N], lhsT=blk64, rhs=SQ[:, 0:N], start=True, stop=True), reads=[bC, B("sq")], writes=[B("ps6")])
        S.act(lambda e: e.activation(out=SD[:, 0:N], in_=PS[6][:, 0:N], func=AF.Sqrt, scale=1.0 / 64, bias=RMS_EPS), reads=[B("ps6")], writes=[B("sd")])
        S.dve(lambda e: e.reciprocal(out=SD[:, 0:N], in_=SD[:, 0:N]), reads=[B("sd")], writes=[B("sd")])
        S.dve(lambda e: e.scalar_tensor_tensor(out=KN[:, 0:N], in0=ps, scalar=NW[:, nwcol:nwcol + 1], in1=SD[:, 0:N], op0=ALU.mult, op1=ALU.mult),
              reads=[bp, B("nw"), B("sd")], writes=[B("kn")])
        if rope:
            S.pe(lambda e: e.matmul(PS[5][:, 0:N], lhsT=rTm, rhs=KN[:, 0:N], start=True, stop=True), reads=[bC, B("kn")], writes=[B("ps5")])
            S.pool(lambda e: e.tensor_tensor(out=SQ[:, 0:N], in0=KN[:, 0:N], in1=ROPE[:, 0, 0:N], op=ALU.mult), reads=[B("kn"), B("rope")], writes=[B("sq")])
            S.dve(lambda e: e.tensor_tensor(out=KN[:, 0:N], in0=PS[5][:, 0:N], in1=ROPE[:, 1, 0:N], op=ALU.mult), reads=[B("ps5"), B("rope"), B("kn")], writes=[B("kn")])
            S.dve(lambda e: e.tensor_tensor(out=dst, in0=KN[:, 0:N], in1=SQ[:, 0:N], op=ALU.add), reads=[B("kn"), B("sq")], writes=[bdst])
        else:
            S.act(lambda e: e.activation(out=dst, in_=KN[:, 0:N], func=AF.Copy), reads=[B("kn")], writes=[bdst])

    def chunk_state(d, masked_g, t, par):
        GP = GP2[par]
        bgp = B("gp%d" % par)
        sm = SM[d]
        ic = 0 if d == 0 else 8
        fc = ic + 4
        bn = lambda n: B("sm%d_%s" % (d, n))
        bm = B("m%d" % d)
        S.act(lambda e: e.activation(out=sm["ef"], in_=GP[:, fc:fc + 4], func=AF.Exp, scale=-1.0), reads=[bgp], writes=[bn("ef")])
        S.act(lambda e: e.activation(out=sm["nlf"], in_=sm["ef"], func=AF.Ln, bias=1.0), reads=[bn("ef")], writes=[bn("nlf")])
        if masked_g is not None:
            kcol = GMASK[:, masked_g * 4 + 2 * d:masked_g * 4 + 2 * d + 1]
            acol = GMASK[:, masked_g * 4 + 2 * d + 1:masked_g * 4 + 2 * d + 2]
            S.dve(lambda e: e.tensor_scalar(out=sm["nlf"], in0=sm["nlf"], scalar1=kcol, scalar2=None, op0=ALU.mult), reads=[bn("nlf"), B("gmask")], writes=[bn("nlf")])
            S.dve(lambda e: e.tensor_scalar(out=sm["li"], in0=GP[:, ic:ic + 4], scalar1=kcol, scalar2=acol, op0=ALU.mult, op1=ALU.add),
                  reads=[bgp, B("gmask")], writes=[bn("li")])
        else:
            S.dve(lambda e: e.tensor_copy(out=sm["li"], in_=GP[:, ic:ic + 4]), reads=[bgp], writes=[bn("li")])
        o = 16 + d * 32
        nbps = PS[2][:, o:o + 4]
        nBps = PS[2][:, o + 4:o + 8]
        amBps = PS[2][:, o + 8:o + 12]
        aTps = PS[2][0:4, 128 + d * 128:256 + d * 128]
        bps = B("ps2s%d" % d)
        S.pe(lambda e: e.matmul(nbps, lhsT=tri, rhs=sm["nlf"], start=True, stop=True), reads=[bC, bn("nlf")], writes=[bps])
        S.pe(lambda e: e.matmul(nBps, lhsT=ones, rhs=sm["nlf"], start=True, stop=True), reads=[bC, bn("nlf")], writes=[bps])
        S.dve(lambda e: e.tensor_tensor(out=sm["a"], in0=nbps, in1=sm["li"], op=ALU.add), reads=[bps, bn("li")], writes=[bn("a")])
        S.pe(lambda e: e.matmul(aTps, lhsT=sm["a"], rhs=ident, start=True, stop=True), reads=[bC, bn("a")], writes=[B("ps2t%d" % d)])
        S.dve(lambda e: e.tensor_reduce(out=sm["amx"][0:4], in_=aTps, axis=AX.X, op=ALU.max), reads=[B("ps2t%d" % d)], writes=[bn("amx")])
        S.dve(lambda e: e.tensor_scalar(out=sm["d4"][0:4], in0=ident[0:4, 0:4], scalar1=sm["amx"][0:4], scalar2=None, op0=ALU.mult),
              reads=[bC, bn("amx")], writes=[bn("d4")])
        S.pe(lambda e: e.matmul(amBps, lhsT=ones[0:4, :], rhs=sm["d4"][0:4], start=True, stop=True), reads=[bC, bn("d4")], writes=[bps])
        S.dve(lambda e: e.tensor_tensor(out=sm["ml"], in0=amBps, in1=MST[d], op=ALU.max), reads=[bps, bm], writes=[bn("ml")])
        S.dve(lambda e: e.tensor_tensor(out=sm["dm"], in0=MST[d], in1=sm["ml"], op=ALU.subtract), reads=[bm, bn("ml")], writes=[bn("dm")])
        S.act(lambda e: e.activation(out=sm["dec"], in_=sm["dm"], func=AF.Exp), reads=[bn("dm")], writes=[bn("dec")])
        S.dve(lambda e: e.tensor_tensor(out=sm["aw"], in0=sm["a"], in1=sm["ml"], op=ALU.subtract), reads=[bn("a"), bn("ml")], writes=[bn("aw")])
        S.act(lambda e: e.activation(out=sm["w"], in_=sm["aw"], func=AF.Exp), reads=[bn("aw")], writes=[bn("w")])
        return nbps, nBps, bps

    def chunk_update(d, nBps, bps, par):
        VAUG = VAUG2[par]
        KTOK = KTOK2[par]
        bva = B("vaug%d" % par)
        bkt = B("ktok%d" % par)
        sm = SM[d]
        bn = lambda n: B("sm%d_%s" % (d, n))
        for h in range(4):
            vw = VW[d][h % 2]
            bvw = B("vw%d_%d" % (d, h % 2))
            S.dve(lambda e, h=h, vw=vw: e.tensor_scalar(out=vw, in0=VAUG[:, h, :], scalar1=sm["w"][:, h:h + 1], scalar2=None, op0=ALU.mult),
                  reads=[bva, bn("w")], writes=[bvw])
            up = PS[7][:, 256:385] if d == 0 else PS[1][:, 0:129]
            bup = B("ps7u") if d == 0 else B("ps1")
            S.pe(lambda e, h=h, vw=vw, up=up: e.matmul(up, lhsT=KTOK[:, h, :], rhs=vw, start=True, stop=True), reads=[bkt, bvw], writes=[bup])
            S.dve(lambda e, h=h, up=up: e.scalar_tensor_tensor(out=CT[d][:, h, :], in0=CT[d][:, h, :], scalar=sm["dec"][:, h:h + 1], in1=up,
                                                               op0=ALU.mult, op1=ALU.add),
                  reads=[B("ct%d" % d), bn("dec"), bup], writes=[B("ct%d" % d)])
        S.pool(lambda e: e.tensor_copy(out=CTB[d], in_=CT[d]), reads=[B("ct%d" % d)], writes=[B("ctb%d" % d)])
        S.dve(lambda e: e.tensor_tensor(out=MST[d], in0=sm["ml"], in1=nBps, op=ALU.subtract), reads=[bn("ml"), bps], writes=[B("m%d" % d)])

    def chunk_full(d, t, nbps, bps, hdst, bh, par):
        VAUG = VAUG2[par]
        bva = B("vaug%d" % par)
        sm = SM[d]
        bn = lambda n: B("sm%d_%s" % (d, n))
        bm = B("m%d" % d)
        cs = slice(t * 128, (t + 1) * 128)
        for h in range(4):
            S.dve(lambda e, h=h: e.tensor_scalar(out=DA, in0=ident, scalar1=sm["a"][:, h:h + 1], scalar2=None, op0=ALU.mult), reads=[bC, bn("a")], writes=[B("da")])
            S.pe(lambda e: e.matmul(PS[7][:, 0:128], lhsT=ones, rhs=DA, start=True, stop=False), reads=[bC, B("da")], writes=[B("ps7e")])
            S.pe(lambda e: e.matmul(PS[7][:, 0:128], lhsT=ident, rhs=mask_sr, start=False, stop=True), reads=[bC], writes=[B("ps7e")])
            S.dve(lambda e: e.tensor_reduce(out=sm["cm"], in_=PS[7][:, 0:128], axis=AX.X, op=ALU.max), reads=[B("ps7e")], writes=[bn("cm")])
            S.dve(lambda e, h=h: e.tensor_tensor(out=sm["mv"], in0=sm["cm"], in1=MST[d][:, h:h + 1], op=ALU.max), reads=[bn("cm"), bm], writes=[bn("mv")])
            S.dve(lambda e: e.tensor_scalar(out=sm["nmv"], in0=sm["mv"], scalar1=-1.0, scalar2=None, op0=ALU.mult), reads=[bn("mv")], writes=[bn("nmv")])
            S.dve(lambda e: e.tensor_scalar(out=DM, in0=ident, scalar1=sm["nmv"], scalar2=None, op0=ALU.mult), reads=[bC, bn("nmv")], writes=[B("dmm")])
            S.pe(lambda e: e.matmul(PS[7][:, 128:256], lhsT=ones, rhs=DM, start=True, stop=False), reads=[bC, B("dmm")], writes=[B("ps7x")])
            S.pe(lambda e: e.matmul(PS[7][:, 128:256], lhsT=ident, rhs=mask_rs, start=False, stop=True), reads=[bC], writes=[B("ps7x")])
            S.act(lambda e, h=h: e.activation(out=DTT, in_=PS[7][:, 128:256], func=AF.Exp, bias=sm["a"][:, h:h + 1]), reads=[B("ps7x"), bn("a")], writes=[B("dtt")])
            S.pe(lambda e, h=h: e.matmul(PS[4][:, 0:128], lhsT=KT[:, h, cs], rhs=QT[:, h, cs], start=True, stop=True), reads=[B("kt"), B("qt")], writes=[B("ps4s")])
            S.dve(lambda e: e.tensor_tensor(out=PT, in0=PS[4][:, 0:128], in1=DTT, op=ALU.mult), reads=[B("ps4s"), B("dtt")], writes=[B("pt")])
            S.pe(lambda e, h=h: e.matmul(PS[4][:, 128:257], lhsT=PT, rhs=VAUG[:, h, :], start=True, stop=True), reads=[B("pt"), bva], writes=[B("ps4n")])
            S.pe(lambda e, h=h: e.matmul(PS[4][:, 257:386], lhsT=QT[:, h, cs], rhs=CTB[d][:, h, :], start=True, stop=True), reads=[B("qt"), B("ctb%d" % d)], writes=[B("ps4i")])
            S.dve(lambda e, h=h: e.tensor_tensor(out=sm["dwi"], in0=MST[d][:, h:h + 1], in1=sm["mv"], op=ALU.subtract), reads=[bm, bn("mv")], writes=[bn("dwi")])
            S.act(lambda e: e.activation(out=sm["wi"], in_=sm["dwi"], func=AF.Exp), reads=[bn("dwi")], writes=[bn("wi")])
            S.act(lambda e: e.activation(out=TT1, in_=PS[4][:, 257:386], func=AF.Identity, scale=sm["wi"]), reads=[B("ps4i"), bn("wi")], writes=[B("tt1")])
            S.dve(lambda e: e.tensor_tensor(out=TOT, in0=PS[4][:, 128:257], in1=TT1, op=ALU.add), reads=[B("ps4n"), B("tt1")], writes=[B("tot")])
            S.dve(lambda e, h=h: e.tensor_tensor(out=sm["dn0"], in0=nbps[:, h:h + 1], in1=sm["mv"], op=ALU.subtract), reads=[bps, bn("mv")], writes=[bn("dn0")])
            S.act(lambda e: e.activation(out=sm["nrm"], in_=sm["dn0"], func=AF.Exp), reads=[bn("dn0")], writes=[bn("nrm")])
            S.act(lambda e: e.activation(out=sm["ad"], in_=TOT[:, 128:129], func=AF.Abs), reads=[B("tot")], writes=[bn("ad")])
            S.dve(lambda e: e.tensor_tensor(out=sm["dn"], in0=sm["ad"], in1=sm["nrm"], op=ALU.max), reads=[bn("ad"), bn("nrm")], writes=[bn("dn")])
            S.dve(lambda e: e.reciprocal(out=sm["rd"], in_=sm["dn"]), reads=[bn("dn")], writes=[bn("rd")])
            S.act(lambda e, h=h: e.activation(out=hdst[:, h * 128:(h + 1) * 128], in_=TOT[:, 0:128], func=AF.Identity, scale=sm["rd"]),
                  reads=[B("tot"), bn("rd")], writes=[bh])

    steps = [("ctxF", 0, 256, 0, 0), ("ctxB", 1, 256, 1, None)]
    for g in range(12):
        steps.append(("oth", g, 512, 2 + g, 2 + 4 * g))
    for g in range(4):
        steps.append(("ownB", 12 + g, 512, 14 + g, None))
    for g in range(4):
        steps.append(("ownF", 16 + g, 512, 18 + g, 50 + 4 * g))

    KG = int(os.environ.get("KG", "99"))
    KSUB = int(os.environ.get("KSUB", "99"))
    if KG == 0:
        raise _Stop()
    for gstep, (kind, src, N, tg, ktb) in enumerate(steps):
        if gstep >= KG:
            raise _Stop()
        last = gstep == KG - 1
        isctx = kind.startswith("ctx")
        mc = 1 if isctx else 0
        if isctx:
            S.dma(lambda e, src=src: e.dma_start(out=XW[:, :, 0:258], in_=d_cwin[src]), writes=[B("xw")])
        else:
            S.dma(lambda e, src=src: e.dma_start(out=XW, in_=d_xwin[src]), writes=[B("xw")])
        for kc in range(8):
            if kc % 2 == 0:
                S.act(lambda e, kc=kc: e.activation(out=HT[:, kc, 0:N + 2], in_=XW[:, kc, 0:N + 2], func=AF.Identity,
                                                    bias=COLS[:, kc, mc:mc + 1], scale=SC1P[:, kc, mc:mc + 1]),
                      reads=[B("xw"), B("cols"), B("sc1p")], writes=[B("ht")])
            else:
                S.pool(lambda e, kc=kc: e.tensor_scalar(out=HT[:, kc, 0:N + 2], in0=XW[:, kc, 0:N + 2], scalar1=SC1P[:, kc, mc:mc + 1],
                                                        scalar2=COLS[:, kc, mc:mc + 1], op0=ALU.mult, op1=ALU.add),
                       reads=[B("xw"), B("cols"), B("sc1p")], writes=[B("ht")])
        if last and KSUB == 0:
            raise _Stop()
        fl = FLAGS[:, tg * 2:tg * 2 + 2]
        for hb in range(4):
            fm_project(C_KM + hb * 128, N, True, hb % 2)
            conv_block(N, hb % 2, hb % 2, KTAPS[:, (tg * 4 + hb) * 3:(tg * 4 + hb) * 3 + 3], CONVB[:, 4 + hb:5 + hb], fl, KT[:, hb, 0:N], False)
        if kind in ("ownB", "ownF"):
            qd = 1 if kind == "ownB" else 0
            for hb in range(4):
                fm_project(C_QM + hb * 128, N, True, hb % 2)
                conv_block(N, hb % 2, hb % 2, QTAPS[:, (qd * 4 + hb) * 3:(qd * 4 + hb) * 3 + 3], CONVB[:, hb:hb + 1], fl, QT[:, hb, 0:N], True)
        if last and KSUB == 1:
            raise _Stop()
        if ktb is not None:
            if not isctx:
                ri = src if kind == "oth" else 12 + (src - 16)
                S.dma(lambda e, ri=ri: e.dma_start(out=ROPE, in_=d_rope[ri].rearrange("a p n -> p a n")), writes=[B("rope")])
            fm_project(C_KA, N, False, 0)
            normrope(N, 0, 1, not isctx, KAT[:, ktb * 128:ktb * 128 + N], B("kat"))
            if kind == "ownF":
                gi = src - 16
                for mcq in range(4):
                    fm_project(C_QA + mcq * 128, N, False, 1)
                    normrope(N, 1, 0, True, QAT[:, mcq, gi * 512:(gi + 1) * 512], B("qat"))
        if last and KSUB == 2:
            raise _Stop()
        for t in range(N // 128):
            par = t % 2
            VAUG = VAUG2[par]
            KTOK = KTOK2[par]
            GP = GP2[par]
            ts = slice(1 + t * 128, 1 + (t + 1) * 128)
            for kc in range(8):
                S.pe(lambda e, kc=kc, ts=ts: e.matmul(PS[3][:, 0:512], lhsT=HT[:, kc, ts], rhs=WB[:, kc, C_VM:C_VM + 512], start=(kc == 0), stop=(kc == 7)),
                     reads=[B("ht"), B("wb")], writes=[B("ps3")])
            for kc in range(8):
                S.pe(lambda e, kc=kc, ts=ts: e.matmul(PS[6][:, 0:144], lhsT=HT[:, kc, ts], rhs=WB[:, kc, C_VA:C_VA + 144], start=(kc == 0), stop=(kc == 7)),
                     reads=[B("ht"), B("wb")], writes=[B("ps6")])
            S.act(lambda e: e.activation(out=VAUG[:, :, 0:128], in_=PS[3][:, 0:512].rearrange("p (a b) -> p a b", a=4), func=AF.Copy),
                  reads=[B("ps3")], writes=[B("vaug%d" % par)])
            if ktb is not None:
                S.act(lambda e, kt_=ktb + t: e.activation(out=VA[:, kt_, :, 0:64], in_=PS[6][:, 0:128].rearrange("p (a b) -> p a b", a=2), func=AF.Copy),
                      reads=[B("ps6")], writes=[B("va")])
            S.dve(lambda e: e.tensor_tensor(out=GP, in0=PS[6][:, 128:144], in1=BG, op=ALU.add), reads=[B("ps6"), B("bg")], writes=[B("gp%d" % par)])
            for h in range(4):
                S.pe(lambda e, h=h, t=t: e.matmul(PS[5][:, h * 128:(h + 1) * 128], lhsT=KT[:, h, t * 128:(t + 1) * 128], rhs=identb, start=True, stop=True),
                     reads=[B("kt"), bCB], writes=[B("ps5")])
            S.act(lambda e: e.activation(out=KTOK.rearrange("p a b -> p (a b)"), in_=PS[5][:, 0:512], func=AF.Copy), reads=[B("ps5")], writes=[B("ktok%d" % par)])
            if kind == "ownF":
                for kc in range(8):
                    S.pe(lambda e, kc=kc, ts=ts: e.matmul(PS[3][:, 0:512], lhsT=HT[:, kc, ts], rhs=WB[:, kc, C_O:C_O + 512], start=(kc == 0), stop=(kc == 7)),
                         reads=[B("ht"), B("wb")], writes=[B("ps3")])
                S.act(lambda e: e.activation(out=OSG, in_=PS[3][:, 0:512], func=AF.Sigmoid), reads=[B("ps3")], writes=[B("osg")])
            if last and KSUB == 3:
                raise _Stop()
            dirs = {"ctxF": [0], "ctxB": [1], "oth": [0, 1], "ownB": [1], "ownF": [0]}[kind]
            if kind == "oth":
                S.fork()
                lists = []
                for d in dirs:
                    nbps, nBps, bps = chunk_state(d, src, t, par)
                    chunk_update(d, nBps, bps, par)
                    lists.append(S.take())
                S.join(lists)
                dirs = []
            for d in dirs:
                nbps, nBps, bps = chunk_state(d, src if kind == "oth" else None, t, par)
                if kind == "ownB":
                    cb = (src - 12) * 4 + t
                    chunk_full(d, t, nbps, bps, MIX[:, cb, 512:1024], B("mixb%d" % cb), par)
                elif kind == "ownF":
                    chunk_full(d, t, nbps, bps, HF, B("hf"), par)
                chunk_update(d, nBps, bps, par)
            if last and KSUB == 4:
                raise _Stop()
            if kind == "ownF":
                oc = (src - 16) * 4 + t
                hm = PS[3][:, 0:512]
                S.pe(lambda e: e.matmul(hm, lhsT=identb, rhs=HF, start=True, stop=False), reads=[bCB, B("hf")], writes=[B("ps3")])
                S.pe(lambda e, oc=oc: e.matmul(hm, lhsT=jb, rhs=MIX[:, 15 - oc, 512:1024], start=False, stop=True), reads=[bCB, B("mixb%d" % (15 - oc))], writes=[B("ps3")])
                hm3 = hm.rearrange("p (a b) -> p a b", a=4)
                S.dve(lambda e: e.tensor_reduce(out=S1, in_=hm3, axis=AX.X, op=ALU.add), reads=[B("ps3")], writes=[B("s1")])
                S.act(lambda e: e.activation(out=HN, in_=hm, func=AF.Square), reads=[B("ps3")], writes=[B("hn")])
                S.dve(lambda e: e.tensor_reduce(out=S2, in_=HN.rearrange("p (a b) -> p a b", a=4), axis=AX.X, op=ALU.add), reads=[B("hn")], writes=[B("s2")])
                if last and KSUB == 5:
                    raise _Stop()
                S.dve(lambda e: e.tensor_scalar(out=MEAN, in0=S1, scalar1=1.0 / 128, scalar2=None, op0=ALU.mult), reads=[B("s1")], writes=[B("mean")])
                S.dve(lambda e: e.tensor_tensor(out=MSQ, in0=MEAN, in1=MEAN, op=ALU.mult), reads=[B("mean")], writes=[B("msq")])
                S.dve(lambda e: e.scalar_tensor_tensor(out=VAR, in0=S2, scalar=1.0 / 128, in1=MSQ, op0=ALU.mult, op1=ALU.subtract), reads=[B("s2"), B("msq")], writes=[B("var")])
                S.act(lambda e: e.activation(out=RSTD, in_=VAR, func=AF.Sqrt, bias=LN_EPS), reads=[B("var")], writes=[B("rstd")])
                S.dve(lambda e: e.reciprocal(out=RSTD, in_=RSTD), reads=[B("rstd")], writes=[B("rstd")])
                if last and KSUB == 6:
                    raise _Stop()
                for h in range(4):
                    S.dve(lambda e, h=h: e.tensor_scalar(out=HN[:, h * 128:(h + 1) * 128], in0=hm[:, h * 128:(h + 1) * 128], scalar1=MEAN[:, h:h + 1],
                                                         scalar2=RSTD[:, h:h + 1], op0=ALU.subtract, op1=ALU.mult),
                          reads=[B("ps3"), B("mean"), B("rstd"), B("s2")], writes=[B("hn")])
                S.pool(lambda e: e.tensor_tensor(out=HN, in0=HN, in1=MHW, op=ALU.mult), reads=[B("hn"), B("mhw")], writes=[B("hn")])
                S.dve(lambda e, oc=oc: e.tensor_tensor(out=MIX[:, oc, 0:512], in0=HN, in1=OSG, op=ALU.mult), reads=[B("hn"), B("osg")], writes=[B("mixa%d" % oc)])

    dd = dbg("mixa", [128, 16, 512], BF16)
    if dd is not None:
        S.dma(lambda e, dd=dd: e.dma_start(out=dd, in_=MIX[:, :, 0:512]), reads=[B("mixa%d" % i) for i in range(16)], writes=[B("dbgo3")])

    if STOP == 1:
        raise _Stop()
    PB = [SQ.bitcast(BF16)[:, 0:512], SD.bitcast(BF16)[:, 0:512]]
    RDEN = [AR.alloc([128, 4]) for _ in range(2)]
    OTS = [HN, OSG]
    combo = 0
    for qg in range(4):
        for mcq in range(4):
            for half in range(2):
                hq = half * 4 + mcq
                r0 = half * 64
                ob = 4 + 2 * (combo % 2)
                eb = ob + 1
                def s_mm(kt):
                    sb = kt % 2
                    S.pe(lambda e: e.matmul(PS[sb][:, 0:512], lhsT=KAT[r0:r0 + 64, kt * 128:(kt + 1) * 128],
                                            rhs=QAT[r0:r0 + 64, mcq, qg * 512:(qg + 1) * 512], start=True, stop=True),
                         reads=[B("kat"), B("qat")], writes=[B("ps%d" % sb)])
                s_mm(0)
                for kt in range(66):
                    sb = kt % 2
                    if kt + 1 < 66:
                        s_mm(kt + 1)
                    S.act(lambda e: e.activation(out=PB[sb], in_=PS[sb][:, 0:512], func=AF.Exp, scale=0.125), reads=[B("ps%d" % sb)], writes=[B(("sq", "sd")[sb])])
                    S.pe(lambda e: e.matmul(PS[ob][0:65, 0:512], lhsT=VA[:, kt, half, :], rhs=PB[sb], start=(kt == 0), stop=(kt == 65)),
                         reads=[B(("sq", "sd")[sb]), B("va")], writes=[B("ps%d" % ob)])
                ots = OTS[combo % 2]
                bo = B(("hn", "osg")[combo % 2])
                S.act(lambda e: e.activation(out=ots[0:65, :], in_=PS[ob][0:65, 0:512], func=AF.Copy), reads=[B("ps%d" % ob)], writes=[bo])
                for qt in range(4):
                    S.pe(lambda e: e.matmul(PS[eb][:, qt * 65:(qt + 1) * 65], lhsT=ots[0:65, qt * 128:(qt + 1) * 128], rhs=ident[0:65, 0:65], start=True, stop=True),
                         reads=[bo, bC], writes=[B("ps%d" % eb)])
                rd = RDEN[combo % 2]
                brd = B("rden%d" % (combo % 2))
                S.dve(lambda e: e.reciprocal(out=rd, in_=PS[eb][:, 64:260:65]), reads=[B("ps%d" % eb)], writes=[brd])
                for qt in range(4):
                    oc = qg * 4 + qt
                    S.act(lambda e: e.activation(out=MIX[:, oc, 512 + hq * 64:512 + (hq + 1) * 64], in_=PS[eb][:, qt * 65:qt * 65 + 64], func=AF.Identity,
                                                 scale=rd[:, qt:qt + 1]),
                          reads=[B("ps%d" % eb), brd], writes=[B("att%d" % oc), B("mixb%d" % oc)])
                combo += 1

    dd = dbg("att", [128, 16, 512], BF16)
    if dd is not None:
        S.dma(lambda e, dd=dd: e.dma_start(out=dd, in_=MIX[:, :, 512:1024]), reads=[B("att%d" % i) for i in range(16)], writes=[B("dbgo2")])

    if STOP == 2:
        raise _Stop()
    AR.off = mark_persist
    S.barrier()

    WKB = AR.alloc([128, 8, 2048], BF16)
    WOB = AR.alloc([128, 8, 1024], BF16)
    GB = AR.alloc([128, 4, 1024])
    LNP = AR.alloc([128, 4, 1024])
    NSLOT = 12
    UGB = [AR.alloc([128, 1024], BF16) for _ in range(NSLOT)]
    XT = AR.alloc([128, 1024])
    R1 = AR.alloc([128, 1024])
    X1 = AR.alloc([128, 1024])
    XM = AR.alloc([128, 1024])
    SC = AR.alloc([128, 2048])
    MIXT = AR.alloc([128, 8, 128], BF16)
    XMT = AR.alloc([128, 8, 128], BF16)
    TP1 = AR.alloc([128, 16, 16])
    IP1 = AR.alloc([128, 16, 16], U32)
    IP1F = AR.alloc([128, 16, 16])
    TMP = AR.alloc([128, 256])
    CAND = AR.alloc([128, 256])
    TP2 = AR.alloc([128, 8, 16])
    PP2 = AR.alloc([128, 8, 16], U32)
    PJ = AR.alloc([128, 2, 128], U32)
    PJF = AR.alloc([128, 2, 128])
    OH = AR.alloc([128, 2048])
    IDXF = AR.alloc([128, 2, 128])
    EIF = AR.alloc([128, 128])
    EI = AR.alloc([128, 128], I32)
    GEX = AR.alloc([128, 8, 16])
    GSUM = AR.alloc([128, 8])
    DOT = AR.alloc([128, 128])
    COEF = AR.alloc([128, 128])
    DOT2 = AR.alloc([128, 128])
    IOTA = AR.alloc([128, 256])
    JUNK = AR.alloc([128, 1024])
    ST = AR.alloc([128, 8])
    print("phase3 arena bytes", AR.off)

    S.dma(lambda e: e.dma_start(out=LNP.rearrange("p a b -> p (a b)"), in_=d_lnp), writes=[B("lnp")])
    S.dma(lambda e: e.dma_start(out=IOTA, in_=d_iota), writes=[B("iota")], q="pool")
    for kc in range(8):
        st = (X1, XM)[kc % 2]
        bs = B(("x1", "xm")[kc % 2])
        S.dma(lambda e, st=st, kc=kc: e.dma_start(out=st, in_=d_wout[kc]), writes=[bs])
        S.act(lambda e, st=st, kc=kc: e.activation(out=WOB[:, kc, :], in_=st, func=AF.Copy), reads=[bs], writes=[B("wob")])
    KEYB = OH.bitcast(BF16)[:, 0:2048]
    S.dma(lambda e: e.dma_start(out=SC, in_=d_keysT), writes=[B("sc")])
    S.act(lambda e: e.activation(out=KEYB, in_=SC, func=AF.Copy), reads=[B("sc")], writes=[B("oh")])
    WQB = [XT.bitcast(BF16)[:, 0:1024], R1.bitcast(BF16)[:, 0:1024]]
    for blk in range(16):
        st = (X1, XM)[blk % 2]
        bs = B(("x1", "xm")[blk % 2])
        S.dma(lambda e, st=st, blk=blk: e.dma_start(out=st, in_=d_wqT[blk]), writes=[bs])
        S.act(lambda e, st=st, blk=blk: e.activation(out=WQB[blk % 2], in_=st, func=AF.Copy), reads=[bs], writes=[B(("xt", "r1")[blk % 2])])
        for kc in range(8):
            S.pe(lambda e, blk=blk, kc=kc: e.matmul(PS[kc % 2][:, 0:128], lhsT=WQB[blk % 2][:, kc * 128:(kc + 1) * 128], rhs=KEYB[:, blk * 128:(blk + 1) * 128],
                                                    start=True, stop=True),
                 reads=[B(("xt", "r1")[blk % 2]), B("oh")], writes=[B("ps%d" % (kc % 2))])
            if kc % 2 == 0:
                S.dve(lambda e, blk=blk, kc=kc: e.tensor_copy(out=WKB[:, kc, blk * 128:(blk + 1) * 128], in_=PS[kc % 2][:, 0:128]), reads=[B("ps%d" % (kc % 2))], writes=[B("wkb")])
            else:
                S.act(lambda e, blk=blk, kc=kc: e.activation(out=WKB[:, kc, blk * 128:(blk + 1) * 128], in_=PS[kc % 2][:, 0:128], func=AF.Copy),
                      reads=[B("ps%d" % (kc % 2))], writes=[B("wkb")])
    for vi, c0 in enumerate((16, 24, 32, 40)):
        for kc in range(8):
            S.dve(lambda e, c0=c0, kc=kc: e.tensor_scalar(out=JUNK[:, 0:128], in0=ident, scalar1=COLS[:, c0 + kc, 0:1], scalar2=None, op0=ALU.mult),
                  reads=[bC, B("cols")], writes=[B("junk")])
            S.pe(lambda e, kc=kc: e.matmul(PS[2 + kc % 2][:, 0:128], lhsT=ones, rhs=JUNK[:, 0:128], start=True, stop=True), reads=[bC, B("junk")], writes=[B("ps%d" % (2 + kc % 2))])
            if vi == 2:
                S.act(lambda e, vi=vi, kc=kc: e.activation(out=GB[:, vi, kc * 128:(kc + 1) * 128], in_=PS[2 + kc % 2][:, 0:128], func=AF.Identity, bias=1.0),
                      reads=[B("ps%d" % (2 + kc % 2))], writes=[B("gb")])
            else:
                S.act(lambda e, vi=vi, kc=kc: e.activation(out=GB[:, vi, kc * 128:(kc + 1) * 128], in_=PS[2 + kc % 2][:, 0:128], func=AF.Copy),
                      reads=[B("ps%d" % (2 + kc % 2))], writes=[B("gb")])

    def layer_norm(src, dst, gi, bi, bsrc, bdst):
        S.act(lambda e: e.activation(out=JUNK, in_=src, func=AF.Copy, accum_out=ST[:, 0:1]), reads=[bsrc], writes=[B("junk"), B("st")])
        S.act(lambda e: e.activation(out=JUNK, in_=src, func=AF.Square, accum_out=ST[:, 1:2]), reads=[bsrc], writes=[B("junk"), B("st")])
        S.dve(lambda e: e.tensor_scalar(out=ST[:, 2:3], in0=ST[:, 0:1], scalar1=1.0 / 1024, scalar2=None, op0=ALU.mult), reads=[B("st")], writes=[B("st")])
        S.dve(lambda e: e.tensor_tensor(out=ST[:, 3:4], in0=ST[:, 2:3], in1=ST[:, 2:3], op=ALU.mult), reads=[B("st")], writes=[B("st")])
        S.dve(lambda e: e.scalar_tensor_tensor(out=ST[:, 4:5], in0=ST[:, 1:2], scalar=1.0 / 1024, in1=ST[:, 3:4], op0=ALU.mult, op1=ALU.subtract), reads=[B("st")], writes=[B("st")])
        S.act(lambda e: e.activation(out=ST[:, 5:6], in_=ST[:, 4:5], func=AF.Sqrt, bias=LN_EPS), reads=[B("st")], writes=[B("st")])
        S.dve(lambda e: e.reciprocal(out=ST[:, 6:7], in_=ST[:, 5:6]), reads=[B("st")], writes=[B("st")])
        S.dve(lambda e: e.tensor_scalar(out=dst, in0=src, scalar1=ST[:, 2:3], scalar2=ST[:, 6:7], op0=ALU.subtract, op1=ALU.mult), reads=[bsrc, B("st")], writes=[bdst])
        S.pool(lambda e: e.tensor_tensor(out=dst, in0=dst, in1=LNP[:, gi, :], op=ALU.mult), reads=[bdst, B("lnp")], writes=[bdst])
        S.pool(lambda e: e.tensor_tensor(out=dst, in0=dst, in1=LNP[:, bi, :], op=ALU.add), reads=[bdst, B("lnp")], writes=[bdst])

    dd_x1 = dbg("x1", [2048, 1024])
    dd_y = dbg("y", [2048, 1024])
    gcount = [0]
    XMP = [PS[0], PS[1]]
    YP = [PS[2], PS[3]]
    for tt in range(16):
        S.dma(lambda e, tt=tt: e.dma_start(out=XT, in_=d_xown[tt * 128:(tt + 1) * 128, :]), writes=[B("xt")])
        for kc in range(8):
            S.pe(lambda e, kc=kc, tt=tt: e.matmul(PS[6 + kc // 4][:, (kc % 4) * 128:(kc % 4 + 1) * 128], lhsT=MIX[:, tt, kc * 128:(kc + 1) * 128], rhs=identb,
                                                  start=True, stop=True),
                 reads=[B("mixa%d" % tt), B("att%d" % tt), bCB], writes=[B("ps%d" % (6 + kc // 4))])
        for hf in range(2):
            S.act(lambda e, hf=hf: e.activation(out=MIXT[:, hf * 4:(hf + 1) * 4, :].rearrange("p a b -> p (a b)"), in_=PS[6 + hf][:, 0:512], func=AF.Copy),
                  reads=[B("ps%d" % (6 + hf))], writes=[B("mixt")])
        for hf in range(2):
            for kc in range(8):
                S.pe(lambda e, hf=hf, kc=kc: e.matmul(PS[4 + hf][:, 0:512], lhsT=MIXT[:, kc, :], rhs=WOB[:, kc, hf * 512:(hf + 1) * 512], start=(kc == 0), stop=(kc == 7)),
                     reads=[B("mixt"), B("wob")], writes=[B("ps%d" % (4 + hf))])
        for hf in range(2):
            S.dve(lambda e, hf=hf: e.tensor_tensor(out=R1[:, hf * 512:(hf + 1) * 512], in0=PS[4 + hf][:, 0:512], in1=GB[:, 0, hf * 512:(hf + 1) * 512], op=ALU.mult),
                  reads=[B("ps%d" % (4 + hf)), B("gb")], writes=[B("r1")])
        S.dve(lambda e: e.scalar_tensor_tensor(out=R1, in0=XT, scalar=ALPHA, in1=R1, op0=ALU.mult, op1=ALU.add), reads=[B("xt"), B("r1")], writes=[B("r1")])
        layer_norm(R1, X1, 0, 1, B("r1"), B("x1"))
        if dd_x1 is not None:
            S.dma(lambda e, tt=tt: e.dma_start(out=dd_x1[tt * 128:(tt + 1) * 128, :], in_=X1), reads=[B("x1")], writes=[B("dbgx%d" % tt)])
        S.dve(lambda e: e.tensor_tensor(out=XM, in0=X1, in1=GB[:, 2, :], op=ALU.mult), reads=[B("x1"), B("gb")], writes=[B("xm")])
        S.dve(lambda e: e.tensor_tensor(out=XM, in0=XM, in1=GB[:, 1, :], op=ALU.add), reads=[B("xm"), B("gb")], writes=[B("xm")])
        for hf in range(2):
            S.act(lambda e, hf=hf: e.activation(out=XMP[hf][:, 0:512], in_=XM[:, hf * 512:(hf + 1) * 512], func=AF.Copy), reads=[B("xm")], writes=[B("ps%d" % hf)])
        for kc in range(8):
            S.pe(lambda e, kc=kc: e.matmul(PS[6 + kc // 4][:, (kc % 4) * 128:(kc % 4 + 1) * 128], lhsT=XM[:, kc * 128:(kc + 1) * 128], rhs=ident, start=True, stop=True),
                 reads=[B("xm"), bC], writes=[B("ps%d" % (6 + kc // 4))])
        for hf in range(2):
            S.act(lambda e, hf=hf: e.activation(out=XMT[:, hf * 4:(hf + 1) * 4, :].rearrange("p a b -> p (a b)"), in_=PS[6 + hf][:, 0:512], func=AF.Copy),
                  reads=[B("ps%d" % (6 + hf))], writes=[B("xmt")])
        for nb in range(4):
            for kc in range(8):
                S.pe(lambda e, nb=nb, kc=kc: e.matmul(PS[4 + nb][:, 0:512], lhsT=XMT[:, kc, :], rhs=WKB[:, kc, nb * 512:(nb + 1) * 512], start=(kc == 0), stop=(kc == 7)),
                     reads=[B("xmt"), B("wkb")], writes=[B("ps%d" % (4 + nb))])
            S.act(lambda e, nb=nb: e.activation(out=SC[:, nb * 512:(nb + 1) * 512], in_=PS[4 + nb][:, 0:512], func=AF.Copy), reads=[B("ps%d" % (4 + nb))], writes=[B("sc")])
        for blk in range(16):
            sc = SC[:, blk * 128:(blk + 1) * 128]
            S.dve(lambda e, blk=blk, sc=sc: e.max(out=TP1[:, blk, 0:8], in_=sc), reads=[B("sc")], writes=[B("tp1")])
            S.dve(lambda e, blk=blk, sc=sc: e.max_index(out=IP1[:, blk, 0:8], in_max=TP1[:, blk, 0:8], in_values=sc), reads=[B("sc"), B("tp1")], writes=[B("ip1")])
            S.dve(lambda e, blk=blk, sc=sc: e.match_replace(out=TMP[:, 0:128], in_to_replace=TP1[:, blk, 0:8], in_values=sc, imm_value=-1e30),
                  reads=[B("sc"), B("tp1")], writes=[B("tmp")])
            S.dve(lambda e, blk=blk: e.max(out=TP1[:, blk, 8:16], in_=TMP[:, 0:128]), reads=[B("tmp")], writes=[B("tp1")])
            S.dve(lambda e, blk=blk: e.max_index(out=IP1[:, blk, 8:16], in_max=TP1[:, blk, 8:16], in_values=TMP[:, 0:128]), reads=[B("tmp"), B("tp1")], writes=[B("ip1")])
        S.dve(lambda e: e.tensor_copy(out=IP1F, in_=IP1), reads=[B("ip1")], writes=[B("ip1f")])
        for h in range(8):
            S.dve(lambda e, h=h: e.tensor_tensor(out=CAND.rearrange("p (a b) -> p a b", a=16), in0=TP1[:, 2 * h, :].unsqueeze(2).to_broadcast([128, 16, 16]),
                                                 in1=TP1[:, 2 * h + 1, :].unsqueeze(1).to_broadcast([128, 16, 16]), op=ALU.add),
                  reads=[B("tp1")], writes=[B("cand")])
            S.dve(lambda e, h=h: e.max(out=TP2[:, h, 0:8], in_=CAND), reads=[B("cand")], writes=[B("tp2")])
            S.dve(lambda e, h=h: e.max_index(out=PP2[:, h, 0:8], in_max=TP2[:, h, 0:8], in_values=CAND), reads=[B("cand"), B("tp2")], writes=[B("pp2")])
            S.dve(lambda e, h=h: e.match_replace(out=TMP, in_to_replace=TP2[:, h, 0:8], in_values=CAND, imm_value=-1e30), reads=[B("cand"), B("tp2")], writes=[B("tmp")])
            S.dve(lambda e, h=h: e.max(out=TP2[:, h, 8:16], in_=TMP), reads=[B("tmp")], writes=[B("tp2")])
            S.dve(lambda e, h=h: e.max_index(out=PP2[:, h, 8:16], in_max=TP2[:, h, 8:16], in_values=TMP), reads=[B("tmp"), B("tp2")], writes=[B("pp2")])
        pp2f = PP2.rearrange("p a b -> p (a b)")
        S.dve(lambda e: e.tensor_single_scalar(out=PJ[:, 0, :], in_=pp2f, scalar=4, op=ALU.logical_shift_right), reads=[B("pp2")], writes=[B("pj")])
        S.dve(lambda e: e.tensor_single_scalar(out=PJ[:, 1, :], in_=pp2f, scalar=15, op=ALU.bitwise_and), reads=[B("pp2")], writes=[B("pj")])
        S.dve(lambda e: e.tensor_copy(out=PJF, in_=PJ), reads=[B("pj")], writes=[B("pjf")])
        for p in range(2):
            for h in range(8):
                oh = OH[:, h * 256:(h + 1) * 256].rearrange("p (a b) -> p a b", a=16)
                S.dve(lambda e, p=p, h=h, oh=oh: e.tensor_tensor(out=oh, in0=IOTA.rearrange("p (a b) -> p a b", a=16),
                                                                 in1=PJF[:, p, h * 16:(h + 1) * 16].unsqueeze(2).to_broadcast([128, 16, 16]), op=ALU.is_equal),
                      reads=[B("iota"), B("pjf")], writes=[B("oh")])
                S.dve(lambda e, p=p, h=h, oh=oh: e.tensor_tensor(out=oh, in0=oh, in1=IP1F[:, 2 * h + p, :].unsqueeze(1).to_broadcast([128, 16, 16]), op=ALU.mult),
                      reads=[B("oh"), B("ip1f")], writes=[B("oh")])
            S.dve(lambda e, p=p: e.tensor_reduce(out=IDXF[:, p, :], in_=OH.rearrange("p (a b) -> p a b", a=128), axis=AX.X, op=ALU.add), reads=[B("oh")], writes=[B("idxf")])
        S.dve(lambda e: e.scalar_tensor_tensor(out=EIF, in0=IDXF[:, 0, :], scalar=128.0, in1=IDXF[:, 1, :], op0=ALU.mult, op1=ALU.add), reads=[B("idxf")], writes=[B("eif")])
        S.dve(lambda e: e.tensor_copy(out=EI, in_=EIF), reads=[B("eif")], writes=[B("ei")])
        S.dve(lambda e: e.tensor_tensor(out=GEX, in0=TP2, in1=TP2[:, :, 0:1].to_broadcast([128, 8, 16]), op=ALU.subtract), reads=[B("tp2")], writes=[B("gex")])
        S.act(lambda e: e.activation(out=GEX, in_=GEX, func=AF.Exp), reads=[B("gex")], writes=[B("gex")])
        S.dve(lambda e: e.tensor_reduce(out=GSUM, in_=GEX, axis=AX.X, op=ALU.add), reads=[B("gex")], writes=[B("gsum")])
        S.dve(lambda e: e.reciprocal(out=GSUM, in_=GSUM), reads=[B("gsum")], writes=[B("gsum")])
        S.dve(lambda e: e.tensor_tensor(out=GEX, in0=GEX, in1=GSUM.unsqueeze(2).to_broadcast([128, 8, 16]), op=ALU.mult), reads=[B("gex"), B("gsum")], writes=[B("gex")])
        allpub = [B("pub%d" % r) for r in range(0, 16384, 2048)]
        allpvb = [B("pvb%d" % r) for r in range(0, 16384, 2048)]
        for sl in range(128):
            ug = UGB[gcount[0] % NSLOT]
            bu = B("ugb%d" % (gcount[0] % NSLOT))
            gcount[0] += 1
            S.dma(lambda e, ug=ug, sl=sl: e.indirect_dma_start(out=ug, out_offset=None, in_=d_pub,
                                                                in_offset=bass.IndirectOffsetOnAxis(ap=EI[:, sl:sl + 1], axis=0)),
                  reads=[B("ei")] + allpub, writes=[bu], q="pool")
            for hf in range(2):
                S.dve(lambda e, ug=ug, hf=hf, sl=sl: e.scalar_tensor_tensor(out=JUNK[:, hf * 512:(hf + 1) * 512], in0=ug[:, hf * 512:(hf + 1) * 512], scalar=1.0,
                                                                            in1=XMP[hf][:, 0:512], op0=ALU.mult, op1=ALU.mult,
                                                                            accum_out=DOT[:, sl:sl + 1] if hf == 0 else DOT2[:, sl:sl + 1]),
                      reads=[bu, B("ps%d" % hf)], writes=[B("junk"), B("dot")])
        S.dve(lambda e: e.tensor_tensor(out=DOT, in0=DOT, in1=DOT2, op=ALU.add), reads=[B("dot")], writes=[B("dot")])
        S.act(lambda e: e.activation(out=DOT, in_=DOT, func=AF.Gelu), reads=[B("dot")], writes=[B("dot")])
        S.dve(lambda e: e.tensor_tensor(out=COEF, in0=DOT, in1=GEX.rearrange("p a b -> p (a b)"), op=ALU.mult), reads=[B("dot"), B("gex")], writes=[B("coef")])
        for hf in range(2):
            S.dve(lambda e, hf=hf: e.memset(YP[hf][:, 0:512], 0.0), writes=[B("ps%d" % (2 + hf))])
        for sl in range(128):
            ug = UGB[gcount[0] % NSLOT]
            bu = B("ugb%d" % (gcount[0] % NSLOT))
            gcount[0] += 1
            S.dma(lambda e, ug=ug, sl=sl: e.indirect_dma_start(out=ug, out_offset=None, in_=d_pvb,
                                                                in_offset=bass.IndirectOffsetOnAxis(ap=EI[:, sl:sl + 1], axis=0)),
                  reads=[B("ei")] + allpvb, writes=[bu], q="pool")
            for hf in range(2):
                S.dve(lambda e, ug=ug, hf=hf, sl=sl: e.scalar_tensor_tensor(out=YP[hf][:, 0:512], in0=ug[:, hf * 512:(hf + 1) * 512], scalar=COEF[:, sl:sl + 1],
                                                                            in1=YP[hf][:, 0:512], op0=ALU.mult, op1=ALU.add),
                      reads=[bu, B("coef"), B("ps%d" % (2 + hf))], writes=[B("ps%d" % (2 + hf))])
        if dd_y is not None:
            for hf in range(2):
                S.act(lambda e, hf=hf: e.activation(out=JUNK[:, hf * 512:(hf + 1) * 512], in_=YP[hf][:, 0:512], func=AF.Copy), reads=[B("ps%d" % (2 + hf))], writes=[B("junk")])
            S.dma(lambda e, tt=tt: e.dma_start(out=dd_y[tt * 128:(tt + 1) * 128, :], in_=JUNK), reads=[B("junk")], writes=[B("dbgy%d" % tt)])
        for hf in range(2):
            S.dve(lambda e, hf=hf: e.tensor_tensor(out=R1[:, hf * 512:(hf + 1) * 512], in0=YP[hf][:, 0:512], in1=GB[:, 3, hf * 512:(hf + 1) * 512], op=ALU.mult),
                  reads=[B("ps%d" % (2 + hf)), B("gb")], writes=[B("r1")])
        S.dve(lambda e: e.scalar_tensor_tensor(out=R1, in0=X1, scalar=ALPHA, in1=R1, op0=ALU.mult, op1=ALU.add), reads=[B("x1"), B("r1")], writes=[B("r1")])
        layer_norm(R1, XM, 2, 3, B("r1"), B("xm"))
        S.dma(lambda e, tt=tt: e.dma_start(out=d_out[tt * 128:(tt + 1) * 128, :], in_=XM), reads=[B("xm")], writes=[B("out%d" % tt)])


def _win(xb, lo, n, rev):
    L = xb.shape[0]
    w = np.zeros((n + 2, 1024), np.float32)
    a, b = lo - 1, lo + n + 1
    sa, sb = max(a, 0), min(b, L)
    w[sa - a:sb - a] = xb[sa:sb]
    if rev:
        w = w[::-1]
    return np.ascontiguousarray(w.reshape(n + 2, 8, 128).transpose(2, 1, 0))


def _rope_tables(tok):
    half = 16
    freq = (10000.0 ** (-np.arange(half, dtype=np.float32) / half)).astype(np.float32)
    row = (tok // 64).astype(np.float32)
    col = (tok % 64).astype(np.float32)
    cos = np.zeros((64, len(tok)), np.float32)
    sin = np.zeros((64, len(tok)), np.float32)
    for dd in range(64):
        pos = row if dd < 32 else col
        ang = pos * freq[dd % 16]
        cos[dd] = np.cos(ang)
        sin[dd] = np.sin(ang)
    return np.concatenate([cos, cos], 0), np.concatenate([sin, sin], 0)


_CACHE = {}


def kernel(x, c, ctx, c_ctx, w_mod, b_mod, w_in, conv_w, conv_b, b_gates, mh_norm_w, q_norm_w, k_norm_w, w_out,
           ln1_g, ln1_b, peer_wq, peer_keys, peer_u, peer_v, ln2_g, ln2_b):
    f32 = np.float32
    x = np.asarray(x, f32); c = np.asarray(c, f32); ctx = np.asarray(ctx, f32); c_ctx = np.asarray(c_ctx, f32)
    w_mod = np.asarray(w_mod, f32)[0]; b_mod = np.asarray(b_mod, f32)[0]; w_in = np.asarray(w_in, f32)[0]
    conv_w = np.asarray(conv_w, f32)[0]; conv_b = np.asarray(conv_b, f32)[0]; b_gates = np.asarray(b_gates, f32)[0]
    mh_norm_w = np.asarray(mh_norm_w, f32)[0]; q_norm_w = np.asarray(q_norm_w, f32)[0]; k_norm_w = np.asarray(k_norm_w, f32)[0]
    w_out = np.asarray(w_out, f32)[0]; ln1_g = np.asarray(ln1_g, f32)[0]; ln1_b = np.asarray(ln1_b, f32)[0]
    peer_wq = np.asarray(peer_wq, f32)[0]; peer_keys = np.asarray(peer_keys, f32)[0]
    peer_u = np.asarray(peer_u, f32)[0]; peer_v = np.asarray(peer_v, f32)[0]
    ln2_g = np.asarray(ln2_g, f32)[0]; ln2_b = np.asarray(ln2_b, f32)[0]

    if "nc" not in _CACHE:
        _CACHE["nc"] = build_program()
    nc, dbg_names = _CACHE["nc"]

    rep = lambda v: np.ascontiguousarray(np.broadcast_to(np.asarray(v, f32).reshape(1, -1), (128, np.asarray(v).size)))
    wmod_l = np.ascontiguousarray(w_mod.reshape(8, 128, 12, 512).transpose(2, 1, 0, 3))
    bmod_l = np.ascontiguousarray(np.broadcast_to(b_mod.reshape(12, 1, 512), (12, 2, 512)))
    qa_cols = []
    for mc in range(4):
        qa_cols += list(range(2064 + mc * 64, 2064 + (mc + 1) * 64)) + list(range(2064 + (4 + mc) * 64, 2064 + (5 + mc) * 64))
    perm = (list(range(512, 1024)) + list(range(0, 512)) + list(range(2576, 2704)) + qa_cols + list(range(1024, 1536))
            + list(range(2704, 2832)) + list(range(2048, 2064)) + list(range(1536, 2048)))
    win_l = np.ascontiguousarray(w_in[:, perm].reshape(8, 128, 2832))
    convb_l = np.ascontiguousarray(conv_b.reshape(8, 128).T)
    cw = conv_w.reshape(3, 8, 128)
    taps_nat = np.ascontiguousarray(cw.transpose(2, 1, 0))
    taps_rev = np.ascontiguousarray(taps_nat[:, :, ::-1])
    qtaps_l = np.ascontiguousarray(np.stack([taps_nat[:, 0:4], taps_rev[:, 0:4]], 1).reshape(128, 24))
    bg_l = rep(b_gates)
    mhw_l = rep(mh_norm_w)
    nw_l = np.ascontiguousarray(np.stack([np.tile(q_norm_w, 2), np.tile(k_norm_w, 2)], 1))
    wout_l = np.ascontiguousarray(w_out.reshape(8, 128, 1024))
    wqT_l = np.ascontiguousarray(peer_wq.T.reshape(16, 128, 1024))
    keysT_l = np.ascontiguousarray(peer_keys.reshape(16, 128, 128).transpose(2, 0, 1).reshape(128, 2048))
    lnp_l = np.ascontiguousarray(np.concatenate([rep(ln1_g), rep(ln1_b), rep(ln2_g), rep(ln2_b)], 1))
    ii = np.arange(128)
    ident = np.eye(128, dtype=f32)
    tri = (ii[:, None] <= ii[None, :]).astype(f32)
    mask_sr = np.where(ii[None, :] <= ii[:, None], 0.0, NEG).astype(f32)
    mask_rs = np.where(ii[:, None] <= ii[None, :], 0.0, NEG).astype(f32)
    jm = ident[::-1].copy()
    blk64 = (ii[:, None] // 64 == ii[None, :] // 64).astype(f32)
    R = np.zeros((128, 128), f32)
    for i in range(128):
        if (i % 32) < 16:
            R[i, i + 16] = -1.0
        else:
            R[i, i - 16] = 1.0
    sel0 = np.zeros((128, 128), f32)
    sel0[0, :] = 1.0
    cst_l = np.ascontiguousarray(np.concatenate([ident, np.ones((128, 128), f32), tri, mask_sr, mask_rs, jm, blk64, R.T.copy(), sel0], 1))
    iota_l = np.ascontiguousarray(np.broadcast_to(np.tile(np.arange(16, dtype=f32), 16).reshape(1, 256), (128, 256)))

    in_maps = []
    for core in range(NCORES):
        b, j = divmod(core, 4)
        xb = x[b]
        cwin = np.stack([_win(ctx[b], 0, 256, False), _win(ctx[b], 0, 256, True)], 0)
        oth = [(G, False) for G in range(0, 4 * j)] + [(G, True) for G in range(15, 4 * j + 3, -1)]
        ownB = [(G, True) for G in range(4 * j + 3, 4 * j - 1, -1)]
        ownF = [(G, False) for G in range(4 * j, 4 * j + 4)]
        srcs = oth + ownB + ownF
        assert len(oth) == 12 and len(srcs) == NGRP
        xwin = np.stack([_win(xb, 512 * G, 512, rv) for (G, rv) in srcs], 0)
        ktaps = np.zeros((128, 22, 4, 3), f32)
        flags = np.zeros((128, 22, 2), f32)
        ktaps[:, 0] = taps_nat[:, 4:8]
        ktaps[:, 1] = taps_rev[:, 4:8]
        for gi, (G, rv) in enumerate(srcs):
            ktaps[:, 2 + gi] = (taps_rev if rv else taps_nat)[:, 4:8]
            fl = [0.0 if G == 0 else 1.0, 0.0 if G == 15 else 1.0]
            flags[:, 2 + gi] = fl[::-1] if rv else fl
        gmask = np.zeros((128, 12, 4), f32)
        for gi, (G, rv) in enumerate(oth):
            fwd_real = not rv
            gmask[:, gi] = [1.0, 0.0, 0.0, NEG] if fwd_real else [0.0, NEG, 1.0, 0.0]
        rope = np.zeros((16, 2, 128, 512), f32)
        for ri, (G, rv) in enumerate(oth + ownF):
            tok = np.arange(512 * G, 512 * G + 512)
            if rv:
                tok = tok[::-1]
            cs, sn = _rope_tables(tok)
            rope[ri, 0] = cs
            rope[ri, 1] = sn
        cvec = np.ascontiguousarray(np.stack([c[b].reshape(8, 128).T, c_ctx.reshape(8, 128).T], 2).reshape(128, 16))
        in_maps.append(dict(
            cwin=cwin, xwin=xwin, xown=np.ascontiguousarray(xb[2048 * j:2048 * (j + 1)]), wmod=wmod_l, bmod=bmod_l, cvec=cvec,
            win=win_l, ktaps=np.ascontiguousarray(ktaps.reshape(128, 264)), qtaps=qtaps_l, convb=convb_l,
            flags=np.ascontiguousarray(flags.reshape(128, 44)), gmask=np.ascontiguousarray(gmask.reshape(128, 48)), bg=bg_l, mhw=mhw_l, nw=nw_l,
            wout=wout_l, wqT=wqT_l, keysT=keysT_l, lnp=lnp_l, pu=peer_u, pv=peer_v, cst=cst_l, iota16=iota_l, rope=rope))
    res = run_bass_kernel_spmd(nc, in_maps[:NCORES], core_ids=list(range(NCORES)))
    out = np.zeros((2, 8192, 1024), f32)
    for core in range(NCORES):
        b, j = divmod(core, 4)
        out[b, 2048 * j:2048 * (j + 1)] = res.results[core]["out"]
    if dbg_names:
        _CACHE["dbg"] = [{n: res.results[core]["dbg_" + n] for n in dbg_names} for core in range(NCORES)]
    return out
```

```python
import os
import types
import numpy as np
import concourse.bass as bass
import concourse.mybir as mybir
from concourse.bass_utils import run_bass_kernel_spmd

F32 = mybir.dt.float32
BF16 = mybir.dt.bfloat16
I32 = mybir.dt.int32
U32 = mybir.dt.uint32
ALU = mybir.AluOpType
AF = mybir.ActivationFunctionType
AX = mybir.AxisListType

NEG = -30000.0
LN_EPS = 1e-5
RMS_EPS = 1e-6
ALPHA = 2.0 ** 0.25
NGRP = 20
DBG = os.environ.get("KDBG", "")


class Buf:
    __slots__ = ("name", "lw", "rd")

    def __init__(self, name):
        self.name = name
        self.lw = None
        self.rd = []


class Op:
    __slots__ = ("eng", "fn", "reads", "writes", "dma", "deps", "signal", "sem", "val", "slot_prev", "bar")

    def __init__(self, eng, fn, reads, writes, dma):
        self.eng = eng
        self.fn = fn
        self.reads = reads
        self.writes = writes
        self.dma = dma
        self.deps = set()
        self.signal = False
        self.sem = None
        self.val = 0
        self.slot_prev = None
        self.bar = 0


def _freeze(fn):
    if fn is None or fn.__closure__ is None:
        return fn
    cells = []
    for c in fn.__closure__:
        try:
            cells.append(types.CellType(c.cell_contents))
        except ValueError:
            cells.append(c)
    return types.FunctionType(fn.__code__, fn.__globals__, fn.__name__, fn.__defaults__, tuple(cells))


class Sched:
    ENGS = ("pe", "act", "dve", "pool", "sp")

    def __init__(self, nc, n_dma_sems=32):
        self.nc = nc
        self.ops = []
        self.n_dma_sems = n_dma_sems
        self.nbar = 0
        self.bank = {}

    def op(self, eng, fn, reads=(), writes=(), dma=False):
        reads = [b for b in reads if b is not None]
        writes = [b for b in writes if b is not None]
        banks = set()
        for b in reads + writes:
            n = b.name
            if n.startswith("pso") and n[3:4].isdigit():
                banks.add(4 + int(n[3]))
            elif n.startswith("ps") and n[2:3].isdigit():
                banks.add(int(n[2]))
        for k in sorted(banks):
            if k not in self.bank:
                self.bank[k] = Buf("BANK%d" % k)
            writes.append(self.bank[k])
        o = Op(eng, _freeze(fn), reads, writes, dma)
        o.bar = self.nbar
        self.ops.append(o)
        return o

    def pe(self, fn, reads=(), writes=()):
        return self.op("pe", fn, reads, writes)

    def act(self, fn, reads=(), writes=()):
        return self.op("act", fn, reads, writes)

    def dve(self, fn, reads=(), writes=()):
        return self.op("dve", fn, reads, writes)

    def pool(self, fn, reads=(), writes=()):
        return self.op("pool", fn, reads, writes)

    def dma(self, fn, reads=(), writes=(), q="sp"):
        return self.op(q, fn, reads, writes, dma=True)

    def barrier(self):
        self.nbar += 1

    def fork(self):
        self._saved = self.ops
        self.ops = []

    def take(self):
        lst = self.ops
        self.ops = []
        return lst

    def join(self, lists):
        merged = []
        n = max(len(l) for l in lists)
        for i in range(n):
            for l in lists:
                if i < len(l):
                    merged.append(l[i])
        self.ops = self._saved + merged

    def _resolve(self):
        ops = self.ops
        last_eng = {}
        dma_all = []
        bar_deps = {}
        seen_bar = {e: 0 for e in self.ENGS}
        cur_bar = 0
        for i, o in enumerate(ops):
            if o.bar != cur_bar:
                cur_bar = o.bar
                bar_deps[cur_bar] = list(last_eng.values()) + list(dma_all)
            deps = set()
            raw = set()
            for b in o.reads:
                if b.lw is not None:
                    deps.add(b.lw)
                    raw.add(b.lw)
            for b in o.writes:
                if b.lw is not None:
                    deps.add(b.lw)
                for r in b.rd:
                    deps.add(r)
            for b in o.reads:
                b.rd.append(i)
            for b in o.writes:
                b.lw = i
                b.rd = []
            deps.discard(i)
            for j in deps:
                p = ops[j]
                if (not p.dma) and (not o.dma) and p.eng == o.eng:
                    if o.eng == "pe" or j not in raw:
                        continue
                o.deps.add(j)
                p.signal = True
            if seen_bar[o.eng] != cur_bar:
                seen_bar[o.eng] = cur_bar
                for j in bar_deps[cur_bar]:
                    p = ops[j]
                    if (not p.dma) and (not o.dma) and p.eng == o.eng:
                        continue
                    o.deps.add(j)
                    p.signal = True
            if o.dma:
                dma_all.append(i)
            elif o.fn is not None:
                last_eng[o.eng] = i

    def emit(self):
        nc = self.nc
        self._resolve()
        ops = self.ops
        esem = {e: nc.alloc_semaphore("s_" + e) for e in self.ENGS}
        nds = self.n_dma_sems
        dsem = [nc.alloc_semaphore("s_dma%d" % k) for k in range(2 * nds)]
        ecount = {e: 0 for e in self.ENGS}
        dcount = [0] * (2 * nds)
        dlast = [None] * (2 * nds)
        nd = 0
        ndq = {"sp": 0, "pool": 0, "act": 0}
        for i, o in enumerate(ops):
            if o.dma:
                base = nds if o.eng == "pool" else 0
                k = base + ndq[o.eng] % nds
                ndq[o.eng] += 1
                nd += 1
                o.signal = True
                o.sem = ("d", k)
                dcount[k] += 16
                o.val = dcount[k]
                o.slot_prev = dlast[k]
                dlast[k] = i
            elif o.signal:
                ecount[o.eng] += 1
                o.sem = ("e", o.eng)
                o.val = ecount[o.eng]
        self.stats = dict(ecount)
        self.stats["ndma"] = nd
        self.stats["nops"] = len(ops)

        def semh(s):
            return esem[s[1]] if s[0] == "e" else dsem[s[1]]

        per_eng = {e: [] for e in self.ENGS}
        for i, o in enumerate(ops):
            per_eng[o.eng].append(i)

        def emit_stream(ename, eng):
            waited = {}
            for i in per_eng[ename]:
                o = ops[i]
                need = {}
                for j in o.deps:
                    p = ops[j]
                    if need.get(p.sem, 0) < p.val:
                        need[p.sem] = p.val
                if o.dma and o.slot_prev is not None:
                    p = ops[o.slot_prev]
                    if need.get(p.sem, 0) < p.val:
                        need[p.sem] = p.val
                for s, v in need.items():
                    if waited.get(s, 0) >= v:
                        continue
                    eng.wait_ge(semh(s), v)
                    waited[s] = v
                if o.fn is None:
                    continue
                ins = o.fn(eng)
                if o.signal:
                    ins.then_inc(semh(o.sem), 16 if o.dma else 1)

        with nc.Block() as block:
            @block.tensor
            def _(e):
                emit_stream("pe", e)

            @block.scalar
            def _(e):
                emit_stream("act", e)

            @block.vector
            def _(e):
                emit_stream("dve", e)

            @block.gpsimd
            def _(e):
                emit_stream("pool", e)

            @block.sync
            def _(e):
                emit_stream("sp", e)


class Arena:
    def __init__(self, nc, nbytes):
        self.t = nc.alloc_sbuf_tensor("arena", [128, nbytes // 4], F32)
        self.off = 0
        self.cap = nbytes
        self.hi = 0

    def alloc(self, shape, dtype=F32):
        esz = 2 if dtype == BF16 else 4
        n = 1
        for s in shape[1:]:
            n *= s
        nb = (n * esz + 31) // 32 * 32
        o = self.off
        self.off += nb
        self.hi = max(self.hi, self.off)
        assert self.off <= self.cap, ("arena overflow", self.off, self.cap)
        v = self.t[:, o // 4:(o + nb) // 4]
        if dtype != F32:
            v = v.bitcast(dtype)
        v = v[:, 0:n]
        if len(shape) == 3:
            v = v.rearrange("p (a b) -> p a b", a=shape[1])
        elif len(shape) == 4:
            v = v.rearrange("p (a b c) -> p a b c", a=shape[1], b=shape[2])
        if shape[0] != 128:
            v = v[0:shape[0]]
        return v


class _Stop(Exception):
    pass


STOP = int(os.environ.get("KSTOP", "9"))
NCORES = int(os.environ.get("KCORES", "8"))


def build_program():
    st = {}
    try:
        _body(st)
    except _Stop:
        pass
    S = st["S"]
    B = st["B"]
    S.op("sp", None, reads=[B("out%d" % i) for i in range(16)] + [B("dbgo"), B("dbgo2"), B("dbgo3")] + [B("dbgx%d" % i) for i in range(16)] + [B("dbgy%d" % i) for i in range(16)])
    S.emit()
    print("sched stats", S.stats)
    return st["nc"], list(st["dbg_out"].keys())


def _body(st):
    nc = bass.Bass("TRN2", target_bir_lowering=False)
    S = Sched(nc)
    bufs = {}
    st["nc"] = nc
    st["S"] = S

    def B(name):
        b = bufs.get(name)
        if b is None:
            b = bufs[name] = Buf(name)
        return b

    def din(name, shape, dt=F32):
        return nc.dram_tensor(name, list(shape), dt, kind="ExternalInput").ap()

    d_cwin = din("cwin", [2, 128, 8, 258])
    d_xwin = din("xwin", [NGRP, 128, 8, 514])
    d_xown = din("xown", [2048, 1024])
    d_wmod = din("wmod", [12, 128, 8, 512])
    d_bmod = din("bmod", [12, 2, 512])
    d_cvec = din("cvec", [128, 16])
    d_win = din("win", [8, 128, 2832])
    d_ktaps = din("ktaps", [128, 22 * 12])
    d_qtaps = din("qtaps", [128, 2 * 12])
    d_cb = din("convb", [128, 8])
    d_flags = din("flags", [128, 44])
    d_gmask = din("gmask", [128, 48])
    d_bg = din("bg", [128, 16])
    d_mhw = din("mhw", [128, 512])
    d_nw = din("nw", [128, 2])
    d_wout = din("wout", [8, 128, 1024])
    d_wqT = din("wqT", [16, 128, 1024])
    d_keysT = din("keysT", [128, 16 * 128])
    d_lnp = din("lnp", [128, 4 * 1024])
    d_pu = din("pu", [16384, 1024])
    d_pv = din("pv", [16384, 1024])
    d_cst = din("cst", [128, 9 * 128])
    d_iota = din("iota16", [128, 256])
    d_rope = din("rope", [16, 2, 128, 512])
    d_out = nc.dram_tensor("out", [2048, 1024], F32, kind="ExternalOutput").ap()
    d_pub = nc.dram_tensor("pub", [16384, 1024], BF16).ap()
    d_pvb = nc.dram_tensor("pvb", [16384, 1024], BF16).ap()
    dbg_out = {}
    st["B"] = B
    st["dbg_out"] = dbg_out

    def dbg(name, shape, dt=F32):
        if name in DBG.split(","):
            dbg_out[name] = nc.dram_tensor("dbg_" + name, list(shape), dt, kind="ExternalOutput").ap()
            return dbg_out[name]
        return None

    AR = Arena(nc, 212800)
    PS = [nc.alloc_psum_tensor("ps%d" % i, [128, 512], F32) for i in range(8)]

    CST = AR.alloc([128, 9 * 128])
    ident = CST[:, 0:128]
    ones = CST[:, 128:256]
    tri = CST[:, 256:384]
    mask_sr = CST[:, 384:512]
    mask_rs = CST[:, 512:640]
    jmat = CST[:, 640:768]
    blk64 = CST[:, 768:896]
    rTm = CST[:, 896:1024]
    sel0 = CST[:, 1024:1152]
    CSTB = AR.alloc([128, 2 * 128], BF16)
    identb = CSTB[:, 0:128]
    jb = CSTB[:, 128:256]
    COLS = AR.alloc([128, 48, 2])
    SC1P = AR.alloc([128, 8, 2])
    MIX = AR.alloc([128, 16, 1024], BF16)
    MHW = AR.alloc([128, 512])
    KTAPS = AR.alloc([128, 22 * 12])
    QTAPS = AR.alloc([128, 24])
    CONVB = AR.alloc([128, 8])
    FLAGS = AR.alloc([128, 44])
    GMASK = AR.alloc([128, 48])
    BG = AR.alloc([128, 16])
    NW = AR.alloc([128, 2])
    MST = [AR.alloc([128, 4]) for _ in range(2)]
    CT = [AR.alloc([128, 4, 129]) for _ in range(2)]
    CTB = [AR.alloc([128, 4, 129], BF16) for _ in range(2)]
    SMALL = AR.alloc([128, 512])
    smo = [0]

    def small(n):
        o = smo[0]
        smo[0] += n
        assert smo[0] <= 512
        return SMALL[:, o:o + n]

    bC = B("cst")
    S.dma(lambda e: e.dma_start(out=CST, in_=d_cst), writes=[bC])
    S.act(lambda e: e.activation(out=identb, in_=ident, func=AF.Copy), reads=[bC], writes=[B("cstb")])
    S.act(lambda e: e.activation(out=jb, in_=jmat, func=AF.Copy), reads=[bC], writes=[B("cstb")])
    bCB = B("cstb")
    for (t, d, n) in ((MHW, d_mhw, "mhw"), (KTAPS, d_ktaps, "ktaps"), (QTAPS, d_qtaps, "qtaps"), (CONVB, d_cb, "convb"),
                      (FLAGS, d_flags, "flags"), (GMASK, d_gmask, "gmask"), (BG, d_bg, "bg"), (NW, d_nw, "nw")):
        S.dma(lambda e, t=t, d=d: e.dma_start(out=t, in_=d), writes=[B(n)], q="pool")
    for d in range(2):
        S.pool(lambda e, d=d: e.memset(MST[d], 0.0), writes=[B("m%d" % d)])
        S.pool(lambda e, d=d: e.memset(CT[d], 0.0), writes=[B("ct%d" % d)])
        S.pool(lambda e, d=d: e.memset(CTB[d], 0.0), writes=[B("ctb%d" % d)])

    mark_persist = AR.off
    for (src_, dst_, nm) in ((d_pu, d_pub, "pub"), (d_pv, d_pvb, "pvb")):
        for r in range(0, 16384, 2048):
            S.dma(lambda e, src_=src_, dst_=dst_, r=r: e.dma_start(out=dst_[r:r + 2048, :], in_=src_[r:r + 2048, :]), writes=[B("%s%d" % (nm, r))], q="pool")

    CV = AR.alloc([128, 16])
    SIL = AR.alloc([128, 8, 2])
    WM = [AR.alloc([128, 8, 512]) for _ in range(2)]
    BM = [AR.alloc([2, 512]) for _ in range(2)]
    MROW = [AR.alloc([2, 512]) for _ in range(2)]
    S.dma(lambda e: e.dma_start(out=CV, in_=d_cvec), writes=[B("cv")])
    S.act(lambda e: e.activation(out=SIL.rearrange("p a b -> p (a b)"), in_=CV, func=AF.Silu), reads=[B("cv")], writes=[B("sil")])
    colps = PS[1][:, 0:96]
    for g in range(12):
        wm = WM[g % 2]
        bw = B("wm%d" % (g % 2))
        S.dma(lambda e, wm=wm, g=g: e.dma_start(out=wm, in_=d_wmod[g]), writes=[bw])
        S.dma(lambda e, g=g: e.dma_start(out=BM[g % 2], in_=d_bmod[g]), writes=[B("bm%d" % (g % 2))], q="pool")
        for kc in range(8):
            S.pe(lambda e, wm=wm, kc=kc: e.matmul(PS[0][0:2, :], lhsT=SIL[:, kc, :], rhs=wm[:, kc, :], start=(kc == 0), stop=(kc == 7)),
                 reads=[B("sil"), bw], writes=[B("ps0")])
        mr = MROW[g % 2]
        S.dve(lambda e, g=g, mr=mr: e.tensor_tensor(out=mr, in0=PS[0][0:2, :], in1=BM[g % 2], op=ALU.add),
              reads=[B("ps0"), B("bm%d" % (g % 2))], writes=[B("mrow%d" % (g % 2))])
        for fc in range(4):
            idx = g * 4 + fc
            S.pe(lambda e, mr=mr, fc=fc, idx=idx: e.matmul(colps[:, idx * 2:idx * 2 + 2], lhsT=mr[:, fc * 128:(fc + 1) * 128], rhs=ident[0:2, 0:2],
                                                           start=True, stop=True),
                 reads=[B("mrow%d" % (g % 2)), bC], writes=[B("ps1")])
    S.dve(lambda e: e.tensor_copy(out=COLS.rearrange("p a b -> p (a b)"), in_=colps), reads=[B("ps1")], writes=[B("cols")])
    S.dve(lambda e: e.tensor_scalar(out=SC1P, in0=COLS[:, 8:16, :], scalar1=1.0, scalar2=None, op0=ALU.add), reads=[B("cols")], writes=[B("sc1p")])
    dd = dbg("cols", [128, 96])
    if dd is not None:
        S.dma(lambda e, dd=dd: e.dma_start(out=dd, in_=COLS.rearrange("p a b -> p (a b)")), reads=[B("cols")], writes=[B("dbgo")])

    if STOP == 0:
        raise _Stop()
    AR.off = mark_persist
    S.barrier()

    WB = AR.alloc([128, 8, 2832], BF16)
    QAT = AR.alloc([128, 4, 2048], BF16)
    KAT = AR.alloc([128, 66 * 128], BF16)
    VA = AR.alloc([128, 66, 2, 65], BF16)
    XW = AR.alloc([128, 8, 514])
    HT = AR.alloc([128, 8, 514], BF16)
    KT = AR.alloc([128, 4, 512], BF16)
    QT = AR.alloc([128, 4, 512], BF16)
    ZB = [AR.alloc([128, 514]) for _ in range(2)]
    TB = [AR.alloc([128, 512]) for _ in range(2)]
    SQ = AR.alloc([128, 512])
    SD = AR.alloc([128, 512])
    KN = AR.alloc([128, 512])
    ROPE = AR.alloc([128, 2, 512])
    KTOK2 = [AR.alloc([128, 4, 128], BF16) for _ in range(2)]
    VAUG2 = [AR.alloc([128, 4, 129], BF16) for _ in range(2)]
    VW = [[AR.alloc([128, 129], BF16) for _ in range(2)] for _ in range(2)]
    DA = AR.alloc([128, 128])
    DM = AR.alloc([128, 128])
    DTT = AR.alloc([128, 128])
    PT = AR.alloc([128, 128], BF16)
    TT1 = AR.alloc([128, 129])
    TOT = AR.alloc([128, 129])
    HF = AR.alloc([128, 512], BF16)
    HN = AR.alloc([128, 512])
    OSG = AR.alloc([128, 512])
    GP2 = [AR.alloc([128, 16]) for _ in range(2)]
    WST = [QAT.rearrange("p a b -> p (a b)").bitcast(F32)[:, 0:2832], KAT.bitcast(F32)[:, 0:2832]]
    print("phase1 arena bytes", AR.off)

    for par in range(2):
        S.pool(lambda e: e.memset(VAUG2[par][:, :, 128:129], 1.0), writes=[B("vaug%d" % par)])
    S.pool(lambda e: e.memset(VA[:, :, :, 64:65], 1.0), writes=[B("va")])

    for kc in range(8):
        st = WST[kc % 2]
        bs = B("qat" if kc % 2 == 0 else "kat")
        S.dma(lambda e, st=st, kc=kc: e.dma_start(out=st, in_=d_win[kc]), writes=[bs])
        if kc % 2 == 0:
            S.act(lambda e, st=st, kc=kc: e.activation(out=WB[:, kc, :], in_=st, func=AF.Copy), reads=[bs], writes=[B("wb")])
        else:
            S.pool(lambda e, st=st, kc=kc: e.tensor_copy(out=WB[:, kc, :], in_=st), reads=[bs], writes=[B("wb")])

    C_KM, C_QM, C_KA, C_QA, C_VM, C_VA, C_G, C_O = 0, 512, 1024, 1152, 1664, 2176, 2304, 2320

    def mk_small():
        return dict(ef=small(4), nlf=small(4), li=small(4), a=small(4), amx=small(1), d4=small(4), ml=small(4), dm=small(4),
                    dec=small(4), aw=small(4), w=small(4), cm=small(1), mv=small(1), nmv=small(1), dwi=small(1), wi=small(1),
                    dn0=small(1), nrm=small(1), ad=small(1), dn=small(1), rd=small(1))
    SM = [mk_small(), mk_small()]
    S1 = small(4)
    S2 = small(4)
    MEAN = small(4)
    MSQ = small(4)
    VAR = small(4)
    RSTD = small(4)

    def fm_project(c0, N, halo, psb):
        for kc in range(8):
            S.pe(lambda e, kc=kc: e.matmul(PS[psb][:, 0:N], lhsT=WB[:, kc, c0:c0 + 128], rhs=HT[:, kc, 1:N + 1], start=(kc == 0), stop=(kc == 7)),
                 reads=[B("wb"), B("ht")], writes=[B("ps%d" % psb)])
        if halo:
            for kc in range(8):
                S.pe(lambda e, kc=kc: e.matmul(PS[2][:, 0:2], lhsT=WB[:, kc, c0:c0 + 128], rhs=HT[:, kc, 0:N + 2:N + 1], start=(kc == 0), stop=(kc == 7)),
                     reads=[B("wb"), B("ht")], writes=[B("ps2")])

    def conv_block(N, psb, zi, taps, cbias, fl, dst, qscale):
        Z = ZB[zi]
        T = TB[zi]
        bz = B("z%d" % zi)
        bt = B("t%d" % zi)
        S.act(lambda e: e.activation(out=Z[:, 1:N + 1], in_=PS[psb][:, 0:N], func=AF.Copy), reads=[B("ps%d" % psb)], writes=[bz])
        S.dve(lambda e: e.tensor_tensor(out=Z[:, 0:N + 2:N + 1], in0=PS[2][:, 0:2], in1=fl, op=ALU.mult), reads=[B("ps2"), B("flags")], writes=[bz])
        S.dve(lambda e: e.tensor_scalar(out=T[:, 0:N], in0=Z[:, 0:N], scalar1=taps[:, 0:1], scalar2=None, op0=ALU.mult),
              reads=[bz, B("ktaps"), B("qtaps")], writes=[bt])
        S.dve(lambda e: e.scalar_tensor_tensor(out=T[:, 0:N], in0=Z[:, 1:N + 1], scalar=taps[:, 1:2], in1=T[:, 0:N], op0=ALU.mult, op1=ALU.add),
              reads=[bz, bt], writes=[bt])
        S.dve(lambda e: e.scalar_tensor_tensor(out=T[:, 0:N], in0=Z[:, 2:N + 2], scalar=taps[:, 2:3], in1=T[:, 0:N], op0=ALU.mult, op1=ALU.add),
              reads=[bz, bt], writes=[bt])
        if qscale:
            S.act(lambda e: e.activation(out=T[:, 0:N], in_=T[:, 0:N], func=AF.Silu, bias=cbias), reads=[bt, B("convb")], writes=[bt])
            S.pool(lambda e: e.tensor_scalar(out=dst, in0=T[:, 0:N], scalar1=128.0 ** -0.5, scalar2=None, op0=ALU.mult), reads=[bt], writes=[B("qt")])
        else:
            S.act(lambda e: e.activation(out=dst, in_=T[:, 0:N], func=AF.Silu, bias=cbias), reads=[bt, B("convb")], writes=[B("kt")])

    def normrope(N, psb, nwcol, rope, dst, bdst):
        ps = PS[psb][:, 0:N]
        bp = B("ps%d" % psb)
        S.act(lambda e: e.activation(out=SQ[:, 0:N], in_=ps, func=AF.Square), reads=[bp], writes=[B("sq")])
        S.pe(lambda e: e.matmul(PS[6][:, 0:N], lhsT=blk64, rhs=SQ[:, 0:N], start=True, stop=True), reads=[bC, B("sq")], writes=[B("ps6")])
        S.act(lambda e: e.activation(out=SD[:, 0:N], in_=PS[6][:, 0:N], func=AF.Sqrt, scale=1.0 / 64, bias=RMS_EPS), reads=[B("ps6")], writes=[B("sd")])
        S.dve(lambda e: e.reciprocal(out=SD[:, 0:N], in_=SD[:, 0:N]), reads=[B("sd")], writes=[B("sd")])
        S.dve(lambda e: e.scalar_tensor_tensor(out=KN[:, 0:N], in0=ps, scalar=NW[:, nwcol:nwcol + 1], in1=SD[:, 0:N], op0=ALU.mult, op1=ALU.mult),
              reads=[bp, B("nw"), B("sd")], writes=[B("kn")])
        if rope:
            S.pe(lambda e: e.matmul(PS[5][:, 0:N], lhsT=rTm, rhs=KN[:, 0:N], start=True, stop=True), reads=[bC, B("kn")], writes=[B("ps5")])
            S.pool(lambda e: e.tensor_tensor(out=SQ[:, 0:N], in0=KN[:, 0:N], in1=ROPE[:, 0, 0:N], op=ALU.mult), reads=[B("kn"), B("rope")], writes=[B("sq")])
            S.dve(lambda e: e.tensor_tensor(out=KN[:, 0:N], in0=PS[5][:, 0:N], in1=ROPE[:, 1, 0:N], op=ALU.mult), reads=[B("ps5"), B("rope"), B("kn")], writes=[B("kn")])
            S.dve(lambda e: e.tensor_tensor(out=dst, in0=KN[:, 0:N], in1=SQ[:, 0:N], op=ALU.add), reads=[B("kn"), B("sq")], writes=[bdst])
        else:
            S.act(lambda e: e.activation(out=dst, in_=KN[:, 0:N], func=AF.Copy), reads=[B("kn")], writes=[bdst])

    def chunk_state(d, masked_g, t, par):
        GP = GP2[par]
        bgp = B("gp%d" % par)
        sm = SM[d]
        ic = 0 if d == 0 else 8
        fc = ic + 4
        bn = lambda n: B("sm%d_%s" % (d, n))
        bm = B("m%d" % d)
        S.act(lambda e: e.activation(out=sm["ef"], in_=GP[:, fc:fc + 4], func=AF.Exp, scale=-1.0), reads=[bgp], writes=[bn("ef")])
        S.act(lambda e: e.activation(out=sm["nlf"], in_=sm["ef"], func=AF.Ln, bias=1.0), reads=[bn("ef")], writes=[bn("nlf")])
        if masked_g is not None:
            kcol = GMASK[:, masked_g * 4 + 2 * d:masked_g * 4 + 2 * d + 1]
            acol = GMASK[:, masked_g * 4 + 2 * d + 1:masked_g * 4 + 2 * d + 2]
            S.dve(lambda e: e.tensor_scalar(out=sm["nlf"], in0=sm["nlf"], scalar1=kcol, scalar2=None, op0=ALU.mult), reads=[bn("nlf"), B("gmask")], writes=[bn("nlf")])
            S.dve(lambda e: e.tensor_scalar(out=sm["li"], in0=GP[:, ic:ic + 4], scalar1=kcol, scalar2=acol, op0=ALU.mult, op1=ALU.add),
                  reads=[bgp, B("gmask")], writes=[bn("li")])
        else:
            S.dve(lambda e: e.tensor_copy(out=sm["li"], in_=GP[:, ic:ic + 4]), reads=[bgp], writes=[bn("li")])
        o = 16 + d * 32
        nbps = PS[2][:, o:o + 4]
        nBps = PS[2][:, o + 4:o + 8]
        amBps = PS[2][:, o + 8:o + 12]
        aTps = PS[2][0:4, 128 + d * 128:256 + d * 128]
        bps = B("ps2s%d" % d)
        S.pe(lambda e: e.matmul(nbps, lhsT=tri, rhs=sm["nlf"], start=True, stop=True), reads=[bC, bn("nlf")], writes=[bps])
        S.pe(lambda e: e.matmul(nBps, lhsT=ones, rhs=sm["nlf"], start=True, stop=True), reads=[bC, bn("nlf")], writes=[bps])
        S.dve(lambda e: e.tensor_tensor(out=sm["a"], in0=nbps, in1=sm["li"], op=ALU.add), reads=[bps, bn("li")], writes=[bn("a")])
        S.pe(lambda e: e.matmul(aTps, lhsT=sm["a"], rhs=ident, start=True, stop=True), reads=[bC, bn("a")], writes=[B("ps2t%d" % d)])
        S.dve(lambda e: e.tensor_reduce(out=sm["amx"][0:4], in_=aTps, axis=AX.X, op=ALU.max), reads=[B("ps2t%d" % d)], writes=[bn("amx")])
        S.dve(lambda e: e.tensor_scalar(out=sm["d4"][0:4], in0=ident[0:4, 0:4], scalar1=sm["amx"][0:4], scalar2=None, op0=ALU.mult),
              reads=[bC, bn("amx")], writes=[bn("d4")])
        S.pe(lambda e: e.matmul(amBps, lhsT=ones[0:4, :], rhs=sm["d4"][0:4], start=True, stop=True), reads=[bC, bn("d4")], writes=[bps])
        S.dve(lambda e: e.tensor_tensor(out=sm["ml"], in0=amBps, in1=MST[d], op=ALU.max), reads=[bps, bm], writes=[bn("ml")])
        S.dve(lambda e: e.tensor_tensor(out=sm["dm"], in0=MST[d], in1=sm["ml"], op=ALU.subtract), reads=[bm, bn("ml")], writes=[bn("dm")])
        S.act(lambda e: e.activation(out=sm["dec"], in_=sm["dm"], func=AF.Exp), reads=[bn("dm")], writes=[bn("dec")])
        S.dve(lambda e: e.tensor_tensor(out=sm["aw"], in0=sm["a"], in1=sm["ml"], op=ALU.subtract), reads=[bn("a"), bn("ml")], writes=[bn("aw")])
        S.act(lambda e: e.activation(out=sm["w"], in_=sm["aw"], func=AF.Exp), reads=[bn("aw")], writes=[bn("w")])
        return nbps, nBps, bps

    def chunk_update(d, nBps, bps, par):
        VAUG = VAUG2[par]
        KTOK = KTOK2[par]
        bva = B("vaug%d" % par)
        bkt = B("ktok%d" % par)
        sm = SM[d]
        bn = lambda n: B("sm%d_%s" % (d, n))
        for h in range(4):
            vw = VW[d][h % 2]
            bvw = B("vw%d_%d" % (d, h % 2))
            S.dve(lambda e, h=h, vw=vw: e.tensor_scalar(out=vw, in0=VAUG[:, h, :], scalar1=sm["w"][:, h:h + 1], scalar2=None, op0=ALU.mult),
                  reads=[bva, bn("w")], writes=[bvw])
            up = PS[7][:, 256:385] if d == 0 else PS[1][:, 0:129]
            bup = B("ps7u") if d == 0 else B("ps1")
            S.pe(lambda e, h=h, vw=vw, up=up: e.matmul(up, lhsT=KTOK[:, h, :], rhs=vw, start=True, stop=True), reads=[bkt, bvw], writes=[bup])
            S.dve(lambda e, h=h, up=up: e.scalar_tensor_tensor(out=CT[d][:, h, :], in0=CT[d][:, h, :], scalar=sm["dec"][:, h:h + 1], in1=up,
                                                               op0=ALU.mult, op1=ALU.add),
                  reads=[B("ct%d" % d), bn("dec"), bup], writes=[B("ct%d" % d)])
        S.pool(lambda e: e.tensor_copy(out=CTB[d], in_=CT[d]), reads=[B("ct%d" % d)], writes=[B("ctb%d" % d)])
        S.dve(lambda e: e.tensor_tensor(out=MST[d], in0=sm["ml"], in1=nBps, op=ALU.subtract), reads=[bn("ml"), bps], writes=[B("m%d" % d)])

    def chunk_full(d, t, nbps, bps, hdst, bh, par):
        VAUG = VAUG2[par]
        bva = B("vaug%d" % par)
        sm = SM[d]
        bn = lambda n: B("sm%d_%s" % (d, n))
        bm = B("m%d" % d)
        cs = slice(t * 128, (t + 1) * 128)
        for h in range(4):
            S.dve(lambda e, h=h: e.tensor_scalar(out=DA, in0=ident, scalar1=sm["a"][:, h:h + 1], scalar2=None, op0=ALU.mult), reads=[bC, bn("a")], writes=[B("da")])
            S.pe(lambda e: e.matmul(PS[7][:, 0:128], lhsT=ones, rhs=DA, start=True, stop=False), reads=[bC, B("da")], writes=[B("ps7e")])
            S.pe(lambda e: e.matmul(PS[7][:, 0:128], lhsT=ident, rhs=mask_sr, start=False, stop=True), reads=[bC], writes=[B("ps7e")])
            S.dve(lambda e: e.tensor_reduce(out=sm["cm"], in_=PS[7][:, 0:128], axis=AX.X, op=ALU.max), reads=[B("ps7e")], writes=[bn("cm")])
            S.dve(lambda e, h=h: e.tensor_tensor(out=sm["mv"], in0=sm["cm"], in1=MST[d][:, h:h + 1], op=ALU.max), reads=[bn("cm"), bm], writes=[bn("mv")])
            S.dve(lambda e: e.tensor_scalar(out=sm["nmv"], in0=sm["mv"], scalar1=-1.0, scalar2=None, op0=ALU.mult), reads=[bn("mv")], writes=[bn("nmv")])
            S.dve(lambda e: e.tensor_scalar(out=DM, in0=ident, scalar1=sm["nmv"], scalar2=None, op0=ALU.mult), reads=[bC, bn("nmv")], writes=[B("dmm")])
            S.pe(lambda e: e.matmul(PS[7][:, 128:256], lhsT=ones, rhs=DM, start=True, stop=False), reads=[bC, B("dmm")], writes=[B("ps7x")])
            S.pe(lambda e: e.matmul(PS[7][:, 128:256], lhsT=ident, rhs=mask_rs, start=False, stop=True), reads=[bC], writes=[B("ps7x")])
            S.act(lambda e, h=h: e.activation(out=DTT, in_=PS[7][:, 128:256], func=AF.Exp, bias=sm["a"][:, h:h + 1]), reads=[B("ps7x"), bn("a")], writes=[B("dtt")])
            S.pe(lambda e, h=h: e.matmul(PS[4][:, 0:128], lhsT=KT[:, h, cs], rhs=QT[:, h, cs], start=True, stop=True), reads=[B("kt"), B("qt")], writes=[B("ps4s")])
            S.dve(lambda e: e.tensor_tensor(out=PT, in0=PS[4][:, 0:128], in1=DTT, op=ALU.mult), reads=[B("ps4s"), B("dtt")], writes=[B("pt")])
            S.pe(lambda e, h=h: e.matmul(PS[4][:, 128:257], lhsT=PT, rhs=VAUG[:, h, :], start=True, stop=True), reads=[B("pt"), bva], writes=[B("ps4n")])
            S.pe(lambda e, h=h: e.matmul(PS[4][:, 257:386], lhsT=QT[:, h, cs], rhs=CTB[d][:, h, :], start=True, stop=True), reads=[B("qt"), B("ctb%d" % d)], writes=[B("ps4i")])
            S.dve(lambda e, h=h: e.tensor_tensor(out=sm["dwi"], in0=MST[d][:, h:h + 1], in1=sm["mv"], op=ALU.subtract), reads=[bm, bn("mv")], writes=[bn("dwi")])
            S.act(lambda e: e.activation(out=sm["wi"], in_=sm["dwi"], func=AF.Exp), reads=[bn("dwi")], writes=[bn("wi")])
            S.act(lambda e: e.activation(out=TT1, in_=PS[4][:, 257:386], func=AF.Identity, scale=sm["wi"]), reads=[B("ps4i"), bn("wi")], writes=[B("tt1")])
            S.dve(lambda e: e.tensor_tensor(out=TOT, in0=PS[4][:, 128:257], in1=TT1, op=ALU.add), reads=[B("ps4n"), B("tt1")], writes=[B("tot")])
            S.dve(lambda e, h=h: e.tensor_tensor(out=sm["dn0"], in0=nbps[:, h:h + 1], in1=sm["mv"], op=ALU.subtract), reads=[bps, bn("mv")], writes=[bn("dn0")])
            S.act(lambda e: e.activation(out=sm["nrm"], in_=sm["dn0"], func=AF.Exp), reads=[bn("dn0")], writes=[bn("nrm")])
            S.act(lambda e: e.activation(out=sm["ad"], in_=TOT[:, 128:129], func=AF.Abs), reads=[B("tot")], writes=[bn("ad")])
            S.dve(lambda e: e.tensor_tensor(out=sm["dn"], in0=sm["ad"], in1=sm["nrm"], op=ALU.max), reads=[bn("ad"), bn("nrm")], writes=[bn("dn")])
            S.dve(lambda e: e.reciprocal(out=sm["rd"], in_=sm["dn"]), reads=[bn("dn")], writes=[bn("rd")])
            S.act(lambda e, h=h: e.activation(out=hdst[:, h * 128:(h + 1) * 128], in_=TOT[:, 0:128], func=AF.Identity, scale=sm["rd"]),
                  reads=[B("tot"), bn("rd")], writes=[bh])

    steps = [("ctxF", 0, 256, 0, 0), ("ctxB", 1, 256, 1, None)]
    for g in range(12):
        steps.append(("oth", g, 512, 2 + g, 2 + 4 * g))
    for g in range(4):
        steps.append(("ownB", 12 + g, 512, 14 + g, None))
    for g in range(4):
        steps.append(("ownF", 16 + g, 512, 18 + g, 50 + 4 * g))

    KG = int(os.environ.get("KG", "99"))
    KSUB = int(os.environ.get("KSUB", "99"))
    if KG == 0:
        raise _Stop()
    for gstep, (kind, src, N, tg, ktb) in enumerate(steps):
        if gstep >= KG:
            raise _Stop()
        last = gstep == KG - 1
        isctx = kind.startswith("ctx")
        mc = 1 if isctx else 0
        if isctx:
            S.dma(lambda e, src=src: e.dma_start(out=XW[:, :, 0:258], in_=d_cwin[src]), writes=[B("xw")])
        else:
            S.dma(lambda e, src=src: e.dma_start(out=XW, in_=d_xwin[src]), writes=[B("xw")])
        for kc in range(8):
            if kc % 2 == 0:
                S.act(lambda e, kc=kc: e.activation(out=HT[:, kc, 0:N + 2], in_=XW[:, kc, 0:N + 2], func=AF.Identity,
                                                    bias=COLS[:, kc, mc:mc + 1], scale=SC1P[:, kc, mc:mc + 1]),
                      reads=[B("xw"), B("cols"), B("sc1p")], writes=[B("ht")])
            else:
                S.pool(lambda e, kc=kc: e.tensor_scalar(out=HT[:, kc, 0:N + 2], in0=XW[:, kc, 0:N + 2], scalar1=SC1P[:, kc, mc:mc + 1],
                                                        scalar2=COLS[:, kc, mc:mc + 1], op0=ALU.mult, op1=ALU.add),
                       reads=[B("xw"), B("cols"), B("sc1p")], writes=[B("ht")])
        if last and KSUB == 0:
            raise _Stop()
        fl = FLAGS[:, tg * 2:tg * 2 + 2]
        for hb in range(4):
            fm_project(C_KM + hb * 128, N, True, hb % 2)
            conv_block(N, hb % 2, hb % 2, KTAPS[:, (tg * 4 + hb) * 3:(tg * 4 + hb) * 3 + 3], CONVB[:, 4 + hb:5 + hb], fl, KT[:, hb, 0:N], False)
        if kind in ("ownB", "ownF"):
            qd = 1 if kind == "ownB" else 0
            for hb in range(4):
                fm_project(C_QM + hb * 128, N, True, hb % 2)
                conv_block(N, hb % 2, hb % 2, QTAPS[:, (qd * 4 + hb) * 3:(qd * 4 + hb) * 3 + 3], CONVB[:, hb:hb + 1], fl, QT[:, hb, 0:N], True)
        if last and KSUB == 1:
            raise _Stop()
        if ktb is not None:
            if not isctx:
                ri = src if kind == "oth" else 12 + (src - 16)
                S.dma(lambda e, ri=ri: e.dma_start(out=ROPE, in_=d_rope[ri].rearrange("a p n -> p a n")), writes=[B("rope")])
            fm_project(C_KA, N, False, 0)
            normrope(N, 0, 1, not isctx, KAT[:, ktb * 128:ktb * 128 + N], B("kat"))
            if kind == "ownF":
                gi = src - 16
                for mcq in range(4):
                    fm_project(C_QA + mcq * 128, N, False, 1)
                    normrope(N, 1, 0, True, QAT[:, mcq, gi * 512:(gi + 1) * 512], B("qat"))
        if last and KSUB == 2:
            raise _Stop()
        for t in range(N // 128):
            par = t % 2
            VAUG = VAUG2[par]
            KTOK = KTOK2[par]
            GP = GP2[par]
            ts = slice(1 + t * 128, 1 + (t + 1) * 128)
            for kc in range(8):
                S.pe(lambda e, kc=kc, ts=ts: e.matmul(PS[3][:, 0:512], lhsT=HT[:, kc, ts], rhs=WB[:, kc, C_VM:C_VM + 512], start=(kc == 0), stop=(kc == 7)),
                     reads=[B("ht"), B("wb")], writes=[B("ps3")])
            for kc in range(8):
                S.pe(lambda e, kc=kc, ts=ts: e.matmul(PS[6][:, 0:144], lhsT=HT[:, kc, ts], rhs=WB[:, kc, C_VA:C_VA + 144], start=(kc == 0), stop=(kc == 7)),
                     reads=[B("ht"), B("wb")], writes=[B("ps6")])
            S.act(lambda e: e.activation(out=VAUG[:, :, 0:128], in_=PS[3][:, 0:512].rearrange("p (a b) -> p a b", a=4), func=AF.Copy),
                  reads=[B("ps3")], writes=[B("vaug%d" % par)])
            if ktb is not None:
                S.act(lambda e, kt_=ktb + t: e.activation(out=VA[:, kt_, :, 0:64], in_=PS[6][:, 0:128].rearrange("p (a b) -> p a b", a=2), func=AF.Copy),
                      reads=[B("ps6")], writes=[B("va")])
            S.dve(lambda e: e.tensor_tensor(out=GP, in0=PS[6][:, 128:144], in1=BG, op=ALU.add), reads=[B("ps6"), B("bg")], writes=[B("gp%d" % par)])
            for h in range(4):
                S.pe(lambda e, h=h, t=t: e.matmul(PS[5][:, h * 128:(h + 1) * 128], lhsT=KT[:, h, t * 128:(t + 1) * 128], rhs=identb, start=True, stop=True),
                     reads=[B("kt"), bCB], writes=[B("ps5")])
            S.act(lambda e: e.activation(out=KTOK.rearrange("p a b -> p (a b)"), in_=PS[5][:, 0:512], func=AF.Copy), reads=[B("ps5")], writes=[B("ktok%d" % par)])
            if kind == "ownF":
                for kc in range(8):
                    S.pe(lambda e, kc=kc, ts=ts: e.matmul(PS[3][:, 0:512], lhsT=HT[:, kc, ts], rhs=WB[:, kc, C_O:C_O + 512], start=(kc == 0), stop=(kc == 7)),
                         reads=[B("ht"), B("wb")], writes=[B("ps3")])
                S.act(lambda e: e.activation(out=OSG, in_=PS[3][:, 0:512], func=AF.Sigmoid), reads=[B("ps3")], writes=[B("osg")])
            if last and KSUB == 3:
                raise _Stop()
            dirs = {"ctxF": [0], "ctxB": [1], "oth": [0, 1], "ownB": [1], "ownF": [0]}[kind]
            if kind == "oth":
                S.fork()
                lists = []
                for d in dirs:
                    nbps, nBps, bps = chunk_state(d, src, t, par)
                    chunk_update(d, nBps, bps, par)
                    lists.append(S.take())
                S.join(lists)
                dirs = []
            for d in dirs:
                nbps, nBps, bps = chunk_state(d, src if kind == "oth" else None, t, par)
                if kind == "ownB":
                    cb = (src - 12) * 4 + t
                    chunk_full(d, t, nbps, bps, MIX[:, cb, 512:1024], B("mixb%d" % cb), par)
                elif kind == "ownF":
                    chunk_full(d, t, nbps, bps, HF, B("hf"), par)
                chunk_update(d, nBps, bps, par)
            if last and KSUB == 4:
                raise _Stop()
            if kind == "ownF":
                oc = (src - 16) * 4 + t
                hm = PS[3][:, 0:512]
                S.pe(lambda e: e.matmul(hm, lhsT=identb, rhs=HF, start=True, stop=False), reads=[bCB, B("hf")], writes=[B("ps3")])
                S.pe(lambda e, oc=oc: e.matmul(hm, lhsT=jb, rhs=MIX[:, 15 - oc, 512:1024], start=False, stop=True), reads=[bCB, B("mixb%d" % (15 - oc))], writes=[B("ps3")])
                hm3 = hm.rearrange("p (a b) -> p a b", a=4)
                S.dve(lambda e: e.tensor_reduce(out=S1, in_=hm3, axis=AX.X, op=ALU.add), reads=[B("ps3")], writes=[B("s1")])
                S.act(lambda e: e.activation(out=HN, in_=hm, func=AF.Square), reads=[B("ps3")], writes=[B("hn")])
                S.dve(lambda e: e.tensor_reduce(out=S2, in_=HN.rearrange("p (a b) -> p a b", a=4), axis=AX.X, op=ALU.add), reads=[B("hn")], writes=[B("s2")])
                if last and KSUB == 5:
                    raise _Stop()
                S.dve(lambda e: e.tensor_scalar(out=MEAN, in0=S1, scalar1=1.0 / 128, scalar2=None, op0=ALU.mult), reads=[B("s1")], writes=[B("mean")])
                S.dve(lambda e: e.tensor_tensor(out=MSQ, in0=MEAN, in1=MEAN, op=ALU.mult), reads=[B("mean")], writes=[B("msq")])
                S.dve(lambda e: e.scalar_tensor_tensor(out=VAR, in0=S2, scalar=1.0 / 128, in1=MSQ, op0=ALU.mult, op1=ALU.subtract), reads=[B("s2"), B("msq")], writes=[B("var")])
                S.act(lambda e: e.activation(out=RSTD, in_=VAR, func=AF.Sqrt, bias=LN_EPS), reads=[B("var")], writes=[B("rstd")])
                S.dve(lambda e: e.reciprocal(out=RSTD, in_=RSTD), reads=[B("rstd")], writes=[B("rstd")])
                if last and KSUB == 6:
                    raise _Stop()
                for h in range(4):
                    S.dve(lambda e, h=h: e.tensor_scalar(out=HN[:, h * 128:(h + 1) * 128], in0=hm[:, h * 128:(h + 1) * 128], scalar1=MEAN[:, h:h + 1],
                                                         scalar2=RSTD[:, h:h + 1], op0=ALU.subtract, op1=ALU.mult),
                          reads=[B("ps3"), B("mean"), B("rstd"), B("s2")], writes=[B("hn")])
                S.pool(lambda e: e.tensor_tensor(out=HN, in0=HN, in1=MHW, op=ALU.mult), reads=[B("hn"), B("mhw")], writes=[B("hn")])
                S.dve(lambda e, oc=oc: e.tensor_tensor(out=MIX[:, oc, 0:512], in0=HN, in1=OSG, op=ALU.mult), reads=[B("hn"), B("osg")], writes=[B("mixa%d" % oc)])

    dd = dbg("mixa", [128, 16, 512], BF16)
    if dd is not None:
        S.dma(lambda e, dd=dd: e.dma_start(out=dd, in_=MIX[:, :, 0:512]), reads=[B("mixa%d" % i) for i in range(16)], writes=[B("dbgo3")])

    if STOP == 1:
        raise _Stop()
    PB = [SQ.bitcast(BF16)[:, 0:512], SD.bitcast(BF16)[:, 0:512]]
    RDEN = [AR.alloc([128, 4]) for _ in range(2)]
    OTS = [HN, OSG]
    combo = 0
    for qg in range(4):
        for mcq in range(4):
            for half in range(2):
                hq = half * 4 + mcq
                r0 = half * 64
                ob = 4 + 2 * (combo % 2)
                eb = ob + 1
                def s_mm(kt):
                    sb = kt % 2
                    S.pe(lambda e: e.matmul(PS[sb][:, 0:512], lhsT=KAT[r0:r0 + 64, kt * 128:(kt + 1) * 128],
                                            rhs=QAT[r0:r0 + 64, mcq, qg * 512:(qg + 1) * 512], start=True, stop=True),
                         reads=[B("kat"), B("qat")], writes=[B("ps%d" % sb)])
                s_mm(0)
                for kt in range(66):
                    sb = kt % 2
                    if kt + 1 < 66:
                        s_mm(kt + 1)
                    S.act(lambda e: e.activation(out=PB[sb], in_=PS[sb][:, 0:512], func=AF.Exp, scale=0.125), reads=[B("ps%d" % sb)], writes=[B(("sq", "sd")[sb])])
                    S.pe(lambda e: e.matmul(PS[ob][0:65, 0:512], lhsT=VA[:, kt, half, :], rhs=PB[sb], start=(kt == 0), stop=(kt == 65)),
                         reads=[B(("sq", "sd")[sb]), B("va")], writes=[B("ps%d" % ob)])
                ots = OTS[combo % 2]
                bo = B(("hn", "osg")[combo % 2])
                S.act(lambda e: e.activation(out=ots[0:65, :], in_=PS[ob][0:65, 0:512], func=AF.Copy), reads=[B("ps%d" % ob)], writes=[bo])
                for qt in range(4):
                    S.pe(lambda e: e.matmul(PS[eb][:, qt * 65:(qt + 1) * 65], lhsT=ots[0:65, qt * 128:(qt + 1) * 128], rhs=ident[0:65, 0:65], start=True, stop=True),
                         reads=[bo, bC], writes=[B("ps%d" % eb)])
                rd = RDEN[combo % 2]
                brd = B("rden%d" % (combo % 2))
                S.dve(lambda e: e.reciprocal(out=rd, in_=PS[eb][:, 64:260:65]), reads=[B("ps%d" % eb)], writes=[brd])
                for qt in range(4):
                    oc = qg * 4 + qt
                    S.act(lambda e: e.activation(out=MIX[:, oc, 512 + hq * 64:512 + (hq + 1) * 64], in_=PS[eb][:, qt * 65:qt * 65 + 64], func=AF.Identity,
                                                 scale=rd[:, qt:qt + 1]),
                          reads=[B("ps%d" % eb), brd], writes=[B("att%d" % oc), B("mixb%d" % oc)])
                combo += 1

    dd = dbg("att", [128, 16, 512], BF16)
    if dd is not None:
        S.dma(lambda e, dd=dd: e.dma_start(out=dd, in_=MIX[:, :, 512:1024]), reads=[B("att%d" % i) for i in range(16)], writes=[B("dbgo2")])

    if STOP == 2:
        raise _Stop()
    AR.off = mark_persist
    S.barrier()

    WKB = AR.alloc([128, 8, 2048], BF16)
    WOB = AR.alloc([128, 8, 1024], BF16)
    GB = AR.alloc([128, 4, 1024])
    LNP = AR.alloc([128, 4, 1024])
    NSLOT = 8
    UGB = [AR.alloc([128, 1024], BF16) for _ in range(NSLOT)]
    XT = AR.alloc([128, 1024])
    R1 = AR.alloc([128, 1024])
    X1S = [AR.alloc([128, 1024]) for _ in range(2)]
    X1 = X1S[0]
    OUTT = AR.alloc([128, 1024])
    XM = AR.alloc([128, 1024])
    SC = AR.alloc([128, 2048])
    MIXT = AR.alloc([128, 8, 128], BF16)
    XMT = AR.alloc([128, 8, 128], BF16)
    TP1 = AR.alloc([128, 16, 16])
    IP1 = AR.alloc([128, 16, 16], U32)
    IP1F = AR.alloc([128, 16, 16])
    TMP = AR.alloc([128, 256])
    CAND = AR.alloc([128, 256])
    TP2 = AR.alloc([128, 8, 16])
    PP2 = AR.alloc([128, 8, 16], U32)
    PJ = AR.alloc([128, 2, 128], U32)
    PJF = AR.alloc([128, 2, 128])
    OH = AR.alloc([128, 2048])
    IDXF = AR.alloc([128, 2, 128])
    EIF = AR.alloc([128, 128])
    EIS = [AR.alloc([128, 128], I32) for _ in range(2)]
    GEX = AR.alloc([128, 8, 16])
    GSUM = AR.alloc([128, 8])
    DOT = AR.alloc([128, 128])
    COEF = AR.alloc([128, 128])
    DOT2 = AR.alloc([128, 128])
    IOTA = AR.alloc([128, 256])
    JUNK = AR.alloc([128, 1024])
    ST = AR.alloc([128, 8])
    print("phase3 arena bytes", AR.off)

    S.dma(lambda e: e.dma_start(out=LNP.rearrange("p a b -> p (a b)"), in_=d_lnp), writes=[B("lnp")])
    S.dma(lambda e: e.dma_start(out=IOTA, in_=d_iota), writes=[B("iota")], q="pool")
    for kc in range(8):
        st = (X1, XM)[kc % 2]
        bs = B(("x1_0", "xm")[kc % 2])
        S.dma(lambda e, st=st, kc=kc: e.dma_start(out=st, in_=d_wout[kc]), writes=[bs])
        S.act(lambda e, st=st, kc=kc: e.activation(out=WOB[:, kc, :], in_=st, func=AF.Copy), reads=[bs], writes=[B("wob")])
    KEYB = OH.bitcast(BF16)[:, 0:2048]
    S.dma(lambda e: e.dma_start(out=SC, in_=d_keysT), writes=[B("sc")])
    S.act(lambda e: e.activation(out=KEYB, in_=SC, func=AF.Copy), reads=[B("sc")], writes=[B("oh")])
    WQB = [XT.bitcast(BF16)[:, 0:1024], R1.bitcast(BF16)[:, 0:1024]]
    for blk in range(16):
        st = (X1, XM)[blk % 2]
        bs = B(("x1_0", "xm")[blk % 2])
        S.dma(lambda e, st=st, blk=blk: e.dma_start(out=st, in_=d_wqT[blk]), writes=[bs])
        S.act(lambda e, st=st, blk=blk: e.activation(out=WQB[blk % 2], in_=st, func=AF.Copy), reads=[bs], writes=[B(("xt", "r1")[blk % 2])])
        for kc in range(8):
            S.pe(lambda e, blk=blk, kc=kc: e.matmul(PS[kc % 2][:, 0:128], lhsT=WQB[blk % 2][:, kc * 128:(kc + 1) * 128], rhs=KEYB[:, blk * 128:(blk + 1) * 128],
                                                    start=True, stop=True),
                 reads=[B(("xt", "r1")[blk % 2]), B("oh")], writes=[B("ps%d" % (kc % 2))])
            if kc % 2 == 0:
                S.dve(lambda e, blk=blk, kc=kc: e.tensor_copy(out=WKB[:, kc, blk * 128:(blk + 1) * 128], in_=PS[kc % 2][:, 0:128]), reads=[B("ps%d" % (kc % 2))], writes=[B("wkb")])
            else:
                S.act(lambda e, blk=blk, kc=kc: e.activation(out=WKB[:, kc, blk * 128:(blk + 1) * 128], in_=PS[kc % 2][:, 0:128], func=AF.Copy),
                      reads=[B("ps%d" % (kc % 2))], writes=[B("wkb")])
    for vi, c0 in enumerate((16, 24, 32, 40)):
        for kc in range(8):
            S.dve(lambda e, c0=c0, kc=kc: e.tensor_scalar(out=JUNK[:, 0:128], in0=ident, scalar1=COLS[:, c0 + kc, 0:1], scalar2=None, op0=ALU.mult),
                  reads=[bC, B("cols")], writes=[B("junk")])
            S.pe(lambda e, kc=kc: e.matmul(PS[2 + kc % 2][:, 0:128], lhsT=ones, rhs=JUNK[:, 0:128], start=True, stop=True), reads=[bC, B("junk")], writes=[B("ps%d" % (2 + kc % 2))])
            if vi == 2:
                S.act(lambda e, vi=vi, kc=kc: e.activation(out=GB[:, vi, kc * 128:(kc + 1) * 128], in_=PS[2 + kc % 2][:, 0:128], func=AF.Identity, bias=1.0),
                      reads=[B("ps%d" % (2 + kc % 2))], writes=[B("gb")])
            else:
                S.act(lambda e, vi=vi, kc=kc: e.activation(out=GB[:, vi, kc * 128:(kc + 1) * 128], in_=PS[2 + kc % 2][:, 0:128], func=AF.Copy),
                      reads=[B("ps%d" % (2 + kc % 2))], writes=[B("gb")])

    def layer_norm(src, dst, gi, bi, bsrc, bdst):
        S.act(lambda e: e.activation(out=JUNK, in_=src, func=AF.Copy, accum_out=ST[:, 0:1]), reads=[bsrc], writes=[B("junk"), B("st")])
        S.act(lambda e: e.activation(out=JUNK, in_=src, func=AF.Square, accum_out=ST[:, 1:2]), reads=[bsrc], writes=[B("junk"), B("st")])
        S.dve(lambda e: e.tensor_scalar(out=ST[:, 2:3], in0=ST[:, 0:1], scalar1=1.0 / 1024, scalar2=None, op0=ALU.mult), reads=[B("st")], writes=[B("st")])
        S.dve(lambda e: e.tensor_tensor(out=ST[:, 3:4], in0=ST[:, 2:3], in1=ST[:, 2:3], op=ALU.mult), reads=[B("st")], writes=[B("st")])
        S.dve(lambda e: e.scalar_tensor_tensor(out=ST[:, 4:5], in0=ST[:, 1:2], scalar=1.0 / 1024, in1=ST[:, 3:4], op0=ALU.mult, op1=ALU.subtract), reads=[B("st")], writes=[B("st")])
        S.act(lambda e: e.activation(out=ST[:, 5:6], in_=ST[:, 4:5], func=AF.Sqrt, bias=LN_EPS), reads=[B("st")], writes=[B("st")])
        S.dve(lambda e: e.reciprocal(out=ST[:, 6:7], in_=ST[:, 5:6]), reads=[B("st")], writes=[B("st")])
        S.dve(lambda e: e.tensor_scalar(out=dst, in0=src, scalar1=ST[:, 2:3], scalar2=ST[:, 6:7], op0=ALU.subtract, op1=ALU.mult), reads=[bsrc, B("st")], writes=[bdst])
        S.pool(lambda e: e.tensor_tensor(out=dst, in0=dst, in1=LNP[:, gi, :], op=ALU.mult), reads=[bdst, B("lnp")], writes=[bdst])
        S.pool(lambda e: e.tensor_tensor(out=dst, in0=dst, in1=LNP[:, bi, :], op=ALU.add), reads=[bdst, B("lnp")], writes=[bdst])

    dd_x1 = dbg("x1", [2048, 1024])
    dd_y = dbg("y", [2048, 1024])
    gcount = [0]
    allpub = [B("pub%d" % r) for r in range(0, 16384, 2048)]
    allpvb = [B("pvb%d" % r) for r in range(0, 16384, 2048)]
    XMP = [PS[0], PS[1]]
    YP = [PS[2], PS[3]]
    def p3_front(tt):
        X1 = X1S[tt % 2]
        EI = EIS[tt % 2]
        S.dma(lambda e, tt=tt: e.dma_start(out=XT, in_=d_xown[tt * 128:(tt + 1) * 128, :]), writes=[B("xt")])
        for kc in range(8):
            S.pe(lambda e, kc=kc, tt=tt: e.matmul(PS[6 + kc // 4][:, (kc % 4) * 128:(kc % 4 + 1) * 128], lhsT=MIX[:, tt, kc * 128:(kc + 1) * 128], rhs=identb,
                                                  start=True, stop=True),
                 reads=[B("mixa%d" % tt), B("att%d" % tt), bCB], writes=[B("ps%d" % (6 + kc // 4))])
        for hf in range(2):
            S.act(lambda e, hf=hf: e.activation(out=MIXT[:, hf * 4:(hf + 1) * 4, :].rearrange("p a b -> p (a b)"), in_=PS[6 + hf][:, 0:512], func=AF.Copy),
                  reads=[B("ps%d" % (6 + hf))], writes=[B("mixt")])
        for hf in range(2):
            for kc in range(8):
                S.pe(lambda e, hf=hf, kc=kc: e.matmul(PS[4 + hf][:, 0:512], lhsT=MIXT[:, kc, :], rhs=WOB[:, kc, hf * 512:(hf + 1) * 512], start=(kc == 0), stop=(kc == 7)),
                     reads=[B("mixt"), B("wob")], writes=[B("ps%d" % (4 + hf))])
        for hf in range(2):
            S.dve(lambda e, hf=hf: e.tensor_tensor(out=R1[:, hf * 512:(hf + 1) * 512], in0=PS[4 + hf][:, 0:512], in1=GB[:, 0, hf * 512:(hf + 1) * 512], op=ALU.mult),
                  reads=[B("ps%d" % (4 + hf)), B("gb")], writes=[B("r1")])
        S.dve(lambda e: e.scalar_tensor_tensor(out=R1, in0=XT, scalar=ALPHA, in1=R1, op0=ALU.mult, op1=ALU.add), reads=[B("xt"), B("r1")], writes=[B("r1")])
        layer_norm(R1, X1, 0, 1, B("r1"), B("x1_%d" % (tt % 2)))
        if dd_x1 is not None:
            S.dma(lambda e, tt=tt: e.dma_start(out=dd_x1[tt * 128:(tt + 1) * 128, :], in_=X1), reads=[B("x1_%d" % (tt % 2))], writes=[B("dbgx%d" % tt)])
        S.dve(lambda e: e.tensor_tensor(out=XM, in0=X1, in1=GB[:, 2, :], op=ALU.mult), reads=[B("x1_%d" % (tt % 2)), B("gb")], writes=[B("xm")])
        S.dve(lambda e: e.tensor_tensor(out=XM, in0=XM, in1=GB[:, 1, :], op=ALU.add), reads=[B("xm"), B("gb")], writes=[B("xm")])
        for hf in range(2):
            S.act(lambda e, hf=hf: e.activation(out=XMP[hf][:, 0:512], in_=XM[:, hf * 512:(hf + 1) * 512], func=AF.Copy), reads=[B("xm")], writes=[B("ps%d" % hf)])
        for kc in range(8):
            S.pe(lambda e, kc=kc: e.matmul(PS[6 + kc // 4][:, (kc % 4) * 128:(kc % 4 + 1) * 128], lhsT=XM[:, kc * 128:(kc + 1) * 128], rhs=ident, start=True, stop=True),
                 reads=[B("xm"), bC], writes=[B("ps%d" % (6 + kc // 4))])
        for hf in range(2):
            S.act(lambda e, hf=hf: e.activation(out=XMT[:, hf * 4:(hf + 1) * 4, :].rearrange("p a b -> p (a b)"), in_=PS[6 + hf][:, 0:512], func=AF.Copy),
                  reads=[B("ps%d" % (6 + hf))], writes=[B("xmt")])
        for nb in range(4):
            for kc in range(8):
                S.pe(lambda e, nb=nb, kc=kc: e.matmul(PS[4 + nb][:, 0:512], lhsT=XMT[:, kc, :], rhs=WKB[:, kc, nb * 512:(nb + 1) * 512], start=(kc == 0), stop=(kc == 7)),
                     reads=[B("xmt"), B("wkb")], writes=[B("ps%d" % (4 + nb))])
            S.act(lambda e, nb=nb: e.activation(out=SC[:, nb * 512:(nb + 1) * 512], in_=PS[4 + nb][:, 0:512], func=AF.Copy), reads=[B("ps%d" % (4 + nb))], writes=[B("sc")])
        for blk in range(16):
            sc = SC[:, blk * 128:(blk + 1) * 128]
            S.dve(lambda e, blk=blk, sc=sc: e.max(out=TP1[:, blk, 0:8], in_=sc), reads=[B("sc")], writes=[B("tp1")])
            S.dve(lambda e, blk=blk, sc=sc: e.max_index(out=IP1[:, blk, 0:8], in_max=TP1[:, blk, 0:8], in_values=sc), reads=[B("sc"), B("tp1")], writes=[B("ip1")])
            S.dve(lambda e, blk=blk, sc=sc: e.match_replace(out=TMP[:, 0:128], in_to_replace=TP1[:, blk, 0:8], in_values=sc, imm_value=-1e30),
                  reads=[B("sc"), B("tp1")], writes=[B("tmp")])
            S.dve(lambda e, blk=blk: e.max(out=TP1[:, blk, 8:16], in_=TMP[:, 0:128]), reads=[B("tmp")], writes=[B("tp1")])
            S.dve(lambda e, blk=blk: e.max_index(out=IP1[:, blk, 8:16], in_max=TP1[:, blk, 8:16], in_values=TMP[:, 0:128]), reads=[B("tmp"), B("tp1")], writes=[B("ip1")])
        S.dve(lambda e: e.tensor_copy(out=IP1F, in_=IP1), reads=[B("ip1")], writes=[B("ip1f")])
        for h in range(8):
            S.dve(lambda e, h=h: e.tensor_tensor(out=CAND.rearrange("p (a b) -> p a b", a=16), in0=TP1[:, 2 * h, :].unsqueeze(2).to_broadcast([128, 16, 16]),
                                                 in1=TP1[:, 2 * h + 1, :].unsqueeze(1).to_broadcast([128, 16, 16]), op=ALU.add),
                  reads=[B("tp1")], writes=[B("cand")])
            S.dve(lambda e, h=h: e.max(out=TP2[:, h, 0:8], in_=CAND), reads=[B("cand")], writes=[B("tp2")])
            S.dve(lambda e, h=h: e.max_index(out=PP2[:, h, 0:8], in_max=TP2[:, h, 0:8], in_values=CAND), reads=[B("cand"), B("tp2")], writes=[B("pp2")])
            S.dve(lambda e, h=h: e.match_replace(out=TMP, in_to_replace=TP2[:, h, 0:8], in_values=CAND, imm_value=-1e30), reads=[B("cand"), B("tp2")], writes=[B("tmp")])
            S.dve(lambda e, h=h: e.max(out=TP2[:, h, 8:16], in_=TMP), reads=[B("tmp")], writes=[B("tp2")])
            S.dve(lambda e, h=h: e.max_index(out=PP2[:, h, 8:16], in_max=TP2[:, h, 8:16], in_values=TMP), reads=[B("tmp"), B("tp2")], writes=[B("pp2")])
        pp2f = PP2.rearrange("p a b -> p (a b)")
        S.dve(lambda e: e.tensor_single_scalar(out=PJ[:, 0, :], in_=pp2f, scalar=4, op=ALU.logical_shift_right), reads=[B("pp2")], writes=[B("pj")])
        S.dve(lambda e: e.tensor_single_scalar(out=PJ[:, 1, :], in_=pp2f, scalar=15, op=ALU.bitwise_and), reads=[B("pp2")], writes=[B("pj")])
        S.dve(lambda e: e.tensor_copy(out=PJF, in_=PJ), reads=[B("pj")], writes=[B("pjf")])
        for p in range(2):
            for h in range(8):
                oh = OH[:, h * 256:(h + 1) * 256].rearrange("p (a b) -> p a b", a=16)
                S.dve(lambda e, p=p, h=h, oh=oh: e.tensor_tensor(out=oh, in0=IOTA.rearrange("p (a b) -> p a b", a=16),
                                                                 in1=PJF[:, p, h * 16:(h + 1) * 16].unsqueeze(2).to_broadcast([128, 16, 16]), op=ALU.is_equal),
                      reads=[B("iota"), B("pjf")], writes=[B("oh")])
                S.dve(lambda e, p=p, h=h, oh=oh: e.tensor_tensor(out=oh, in0=oh, in1=IP1F[:, 2 * h + p, :].unsqueeze(1).to_broadcast([128, 16, 16]), op=ALU.mult),
                      reads=[B("oh"), B("ip1f")], writes=[B("oh")])
            S.dve(lambda e, p=p: e.tensor_reduce(out=IDXF[:, p, :], in_=OH.rearrange("p (a b) -> p a b", a=128), axis=AX.X, op=ALU.add), reads=[B("oh")], writes=[B("idxf")])
        S.dve(lambda e: e.scalar_tensor_tensor(out=EIF, in0=IDXF[:, 0, :], scalar=128.0, in1=IDXF[:, 1, :], op0=ALU.mult, op1=ALU.add), reads=[B("idxf")], writes=[B("eif")])
        S.dve(lambda e: e.tensor_copy(out=EI, in_=EIF), reads=[B("eif")], writes=[B("ei_%d" % (tt % 2))])
        S.dve(lambda e: e.tensor_tensor(out=GEX, in0=TP2, in1=TP2[:, :, 0:1].to_broadcast([128, 8, 16]), op=ALU.subtract), reads=[B("tp2")], writes=[B("gex")])
        S.act(lambda e: e.activation(out=GEX, in_=GEX, func=AF.Exp), reads=[B("gex")], writes=[B("gex")])
        S.dve(lambda e: e.tensor_reduce(out=GSUM, in_=GEX, axis=AX.X, op=ALU.add), reads=[B("gex")], writes=[B("gsum")])
        S.dve(lambda e: e.reciprocal(out=GSUM, in_=GSUM), reads=[B("gsum")], writes=[B("gsum")])
        S.dve(lambda e: e.tensor_tensor(out=GEX, in0=GEX, in1=GSUM.unsqueeze(2).to_broadcast([128, 8, 16]), op=ALU.mult), reads=[B("gex"), B("gsum")], writes=[B("gex")])
    def p3_u(tt):
        X1 = X1S[tt % 2]
        EI = EIS[tt % 2]
        for sl in range(128):
            ug = UGB[gcount[0] % NSLOT]
            bu = B("ugb%d" % (gcount[0] % NSLOT))
            gcount[0] += 1
            S.dma(lambda e, ug=ug, sl=sl: e.indirect_dma_start(out=ug, out_offset=None, in_=d_pub,
                                                                in_offset=bass.IndirectOffsetOnAxis(ap=EI[:, sl:sl + 1], axis=0)),
                  reads=[B("ei_%d" % (tt % 2))] + allpub, writes=[bu], q="pool")
            for hf in range(2):
                S.dve(lambda e, ug=ug, hf=hf, sl=sl: e.scalar_tensor_tensor(out=JUNK[:, hf * 512:(hf + 1) * 512], in0=ug[:, hf * 512:(hf + 1) * 512], scalar=1.0,
                                                                            in1=XMP[hf][:, 0:512], op0=ALU.mult, op1=ALU.mult,
                                                                            accum_out=DOT[:, sl:sl + 1] if hf == 0 else DOT2[:, sl:sl + 1]),
                      reads=[bu, B("ps%d" % hf)], writes=[B("junk"), B("dot")])
        S.dve(lambda e: e.tensor_tensor(out=DOT, in0=DOT, in1=DOT2, op=ALU.add), reads=[B("dot")], writes=[B("dot")])
        S.act(lambda e: e.activation(out=DOT, in_=DOT, func=AF.Gelu), reads=[B("dot")], writes=[B("dot")])
        S.dve(lambda e: e.tensor_tensor(out=COEF, in0=DOT, in1=GEX.rearrange("p a b -> p (a b)"), op=ALU.mult), reads=[B("dot"), B("gex")], writes=[B("coef")])
    def p3_v(tt):
        X1 = X1S[tt % 2]
        EI = EIS[tt % 2]
        for hf in range(2):
            S.dve(lambda e, hf=hf: e.memset(YP[hf][:, 0:512], 0.0), writes=[B("ps%d" % (2 + hf))])
        for sl in range(128):
            ug = UGB[gcount[0] % NSLOT]
            bu = B("ugb%d" % (gcount[0] % NSLOT))
            gcount[0] += 1
            S.dma(lambda e, ug=ug, sl=sl: e.indirect_dma_start(out=ug, out_offset=None, in_=d_pvb,
                                                                in_offset=bass.IndirectOffsetOnAxis(ap=EI[:, sl:sl + 1], axis=0)),
                  reads=[B("ei_%d" % (tt % 2))] + allpvb, writes=[bu], q="pool")
            for hf in range(2):
                S.dve(lambda e, ug=ug, hf=hf, sl=sl: e.scalar_tensor_tensor(out=YP[hf][:, 0:512], in0=ug[:, hf * 512:(hf + 1) * 512], scalar=COEF[:, sl:sl + 1],
                                                                            in1=YP[hf][:, 0:512], op0=ALU.mult, op1=ALU.add),
                      reads=[bu, B("coef"), B("ps%d" % (2 + hf))], writes=[B("ps%d" % (2 + hf))])
    def p3_back(tt):
        X1 = X1S[tt % 2]
        EI = EIS[tt % 2]
        if dd_y is not None:
            for hf in range(2):
                S.act(lambda e, hf=hf: e.activation(out=JUNK[:, hf * 512:(hf + 1) * 512], in_=YP[hf][:, 0:512], func=AF.Copy), reads=[B("ps%d" % (2 + hf))], writes=[B("junk")])
            S.dma(lambda e, tt=tt: e.dma_start(out=dd_y[tt * 128:(tt + 1) * 128, :], in_=JUNK), reads=[B("junk")], writes=[B("dbgy%d" % tt)])
        for hf in range(2):
            S.dve(lambda e, hf=hf: e.tensor_tensor(out=R1[:, hf * 512:(hf + 1) * 512], in0=YP[hf][:, 0:512], in1=GB[:, 3, hf * 512:(hf + 1) * 512], op=ALU.mult),
                  reads=[B("ps%d" % (2 + hf)), B("gb")], writes=[B("r1")])
        S.dve(lambda e: e.scalar_tensor_tensor(out=R1, in0=X1, scalar=ALPHA, in1=R1, op0=ALU.mult, op1=ALU.add), reads=[B("x1_%d" % (tt % 2)), B("r1")], writes=[B("r1")])
        layer_norm(R1, OUTT, 2, 3, B("r1"), B("outt"))
        S.dma(lambda e, tt=tt: e.dma_start(out=d_out[tt * 128:(tt + 1) * 128, :], in_=OUTT), reads=[B("outt")], writes=[B("out%d" % tt)])


    p3_front(0)
    for tt in range(16):
        p3_u(tt)
        S.fork()
        lists = []
        if tt + 1 < 16:
            p3_front(tt + 1)
            lists.append(S.take())
        p3_v(tt)
        lists.append(S.take())
        S.join(lists)
        p3_back(tt)
def _win(xb, lo, n, rev):
    L = xb.shape[0]
    w = np.zeros((n + 2, 1024), np.float32)
    a, b = lo - 1, lo + n + 1
    sa, sb = max(a, 0), min(b, L)
    w[sa - a:sb - a] = xb[sa:sb]
    if rev:
        w = w[::-1]
    return np.ascontiguousarray(w.reshape(n + 2, 8, 128).transpose(2, 1, 0))


def _rope_tables(tok):
    half = 16
    freq = (10000.0 ** (-np.arange(half, dtype=np.float32) / half)).astype(np.float32)
    row = (tok // 64).astype(np.float32)
    col = (tok % 64).astype(np.float32)
    cos = np.zeros((64, len(tok)), np.float32)
    sin = np.zeros((64, len(tok)), np.float32)
    for dd in range(64):
        pos = row if dd < 32 else col
        ang = pos * freq[dd % 16]
        cos[dd] = np.cos(ang)
        sin[dd] = np.sin(ang)
    return np.concatenate([cos, cos], 0), np.concatenate([sin, sin], 0)


_CACHE = {}


def kernel(x, c, ctx, c_ctx, w_mod, b_mod, w_in, conv_w, conv_b, b_gates, mh_norm_w, q_norm_w, k_norm_w, w_out,
           ln1_g, ln1_b, peer_wq, peer_keys, peer_u, peer_v, ln2_g, ln2_b):
    f32 = np.float32
    x = np.asarray(x, f32); c = np.asarray(c, f32); ctx = np.asarray(ctx, f32); c_ctx = np.asarray(c_ctx, f32)
    w_mod = np.asarray(w_mod, f32)[0]; b_mod = np.asarray(b_mod, f32)[0]; w_in = np.asarray(w_in, f32)[0]
    conv_w = np.asarray(conv_w, f32)[0]; conv_b = np.asarray(conv_b, f32)[0]; b_gates = np.asarray(b_gates, f32)[0]
    mh_norm_w = np.asarray(mh_norm_w, f32)[0]; q_norm_w = np.asarray(q_norm_w, f32)[0]; k_norm_w = np.asarray(k_norm_w, f32)[0]
    w_out = np.asarray(w_out, f32)[0]; ln1_g = np.asarray(ln1_g, f32)[0]; ln1_b = np.asarray(ln1_b, f32)[0]
    peer_wq = np.asarray(peer_wq, f32)[0]; peer_keys = np.asarray(peer_keys, f32)[0]
    peer_u = np.asarray(peer_u, f32)[0]; peer_v = np.asarray(peer_v, f32)[0]
    ln2_g = np.asarray(ln2_g, f32)[0]; ln2_b = np.asarray(ln2_b, f32)[0]

    if "nc" not in _CACHE:
        _CACHE["nc"] = build_program()
    nc, dbg_names = _CACHE["nc"]

    rep = lambda v: np.ascontiguousarray(np.broadcast_to(np.asarray(v, f32).reshape(1, -1), (128, np.asarray(v).size)))
    wmod_l = np.ascontiguousarray(w_mod.reshape(8, 128, 12, 512).transpose(2, 1, 0, 3))
    bmod_l = np.ascontiguousarray(np.broadcast_to(b_mod.reshape(12, 1, 512), (12, 2, 512)))
    qa_cols = []
    for mc in range(4):
        qa_cols += list(range(2064 + mc * 64, 2064 + (mc + 1) * 64)) + list(range(2064 + (4 + mc) * 64, 2064 + (5 + mc) * 64))
    perm = (list(range(512, 1024)) + list(range(0, 512)) + list(range(2576, 2704)) + qa_cols + list(range(1024, 1536))
            + list(range(2704, 2832)) + list(range(2048, 2064)) + list(range(1536, 2048)))
    win_l = np.ascontiguousarray(w_in[:, perm].reshape(8, 128, 2832))
    convb_l = np.ascontiguousarray(conv_b.reshape(8, 128).T)
    cw = conv_w.reshape(3, 8, 128)
    taps_nat = np.ascontiguousarray(cw.transpose(2, 1, 0))
    taps_rev = np.ascontiguousarray(taps_nat[:, :, ::-1])
    qtaps_l = np.ascontiguousarray(np.stack([taps_nat[:, 0:4], taps_rev[:, 0:4]], 1).reshape(128, 24))
    bg_l = rep(b_gates)
    mhw_l = rep(mh_norm_w)
    nw_l = np.ascontiguousarray(np.stack([np.tile(q_norm_w, 2), np.tile(k_norm_w, 2)], 1))
    wout_l = np.ascontiguousarray(w_out.reshape(8, 128, 1024))
    wqT_l = np.ascontiguousarray(peer_wq.T.reshape(16, 128, 1024))
    keysT_l = np.ascontiguousarray(peer_keys.reshape(16, 128, 128).transpose(2, 0, 1).reshape(128, 2048))
    lnp_l = np.ascontiguousarray(np.concatenate([rep(ln1_g), rep(ln1_b), rep(ln2_g), rep(ln2_b)], 1))
    ii = np.arange(128)
    ident = np.eye(128, dtype=f32)
    tri = (ii[:, None] <= ii[None, :]).astype(f32)
    mask_sr = np.where(ii[None, :] <= ii[:, None], 0.0, NEG).astype(f32)
    mask_rs = np.where(ii[:, None] <= ii[None, :], 0.0, NEG).astype(f32)
    jm = ident[::-1].copy()
    blk64 = (ii[:, None] // 64 == ii[None, :] // 64).astype(f32)
    R = np.zeros((128, 128), f32)
    for i in range(128):
        if (i % 32) < 16:
            R[i, i + 16] = -1.0
        else:
            R[i, i - 16] = 1.0
    sel0 = np.zeros((128, 128), f32)
    sel0[0, :] = 1.0
    cst_l = np.ascontiguousarray(np.concatenate([ident, np.ones((128, 128), f32), tri, mask_sr, mask_rs, jm, blk64, R.T.copy(), sel0], 1))
    iota_l = np.ascontiguousarray(np.broadcast_to(np.tile(np.arange(16, dtype=f32), 16).reshape(1, 256), (128, 256)))

    in_maps = []
    for core in range(NCORES):
        b, j = divmod(core, 4)
        xb = x[b]
        cwin = np.stack([_win(ctx[b], 0, 256, False), _win(ctx[b], 0, 256, True)], 0)
        oth = [(G, False) for G in range(0, 4 * j)] + [(G, True) for G in range(15, 4 * j + 3, -1)]
        ownB = [(G, True) for G in range(4 * j + 3, 4 * j - 1, -1)]
        ownF = [(G, False) for G in range(4 * j, 4 * j + 4)]
        srcs = oth + ownB + ownF
        assert len(oth) == 12 and len(srcs) == NGRP
        xwin = np.stack([_win(xb, 512 * G, 512, rv) for (G, rv) in srcs], 0)
        ktaps = np.zeros((128, 22, 4, 3), f32)
        flags = np.zeros((128, 22, 2), f32)
        ktaps[:, 0] = taps_nat[:, 4:8]
        ktaps[:, 1] = taps_rev[:, 4:8]
        for gi, (G, rv) in enumerate(srcs):
            ktaps[:, 2 + gi] = (taps_rev if rv else taps_nat)[:, 4:8]
            fl = [0.0 if G == 0 else 1.0, 0.0 if G == 15 else 1.0]
            flags[:, 2 + gi] = fl[::-1] if rv else fl
        gmask = np.zeros((128, 12, 4), f32)
        for gi, (G, rv) in enumerate(oth):
            fwd_real = not rv
            gmask[:, gi] = [1.0, 0.0, 0.0, NEG] if fwd_real else [0.0, NEG, 1.0, 0.0]
        rope = np.zeros((16, 2, 128, 512), f32)
        for ri, (G, rv) in enumerate(oth + ownF):
            tok = np.arange(512 * G, 512 * G + 512)
            if rv:
                tok = tok[::-1]
            cs, sn = _rope_tables(tok)
            rope[ri, 0] = cs
            rope[ri, 1] = sn
        cvec = np.ascontiguousarray(np.stack([c[b].reshape(8, 128).T, c_ctx.reshape(8, 128).T], 2).reshape(128, 16))
        in_maps.append(dict(
            cwin=cwin, xwin=xwin, xown=np.ascontiguousarray(xb[2048 * j:2048 * (j + 1)]), wmod=wmod_l, bmod=bmod_l, cvec=cvec,
            win=win_l, ktaps=np.ascontiguousarray(ktaps.reshape(128, 264)), qtaps=qtaps_l, convb=convb_l,
            flags=np.ascontiguousarray(flags.reshape(128, 44)), gmask=np.ascontiguousarray(gmask.reshape(128, 48)), bg=bg_l, mhw=mhw_l, nw=nw_l,
            wout=wout_l, wqT=wqT_l, keysT=keysT_l, lnp=lnp_l, pu=peer_u, pv=peer_v, cst=cst_l, iota16=iota_l, rope=rope))
    res = run_bass_kernel_spmd(nc, in_maps[:NCORES], core_ids=list(range(NCORES)))
    out = np.zeros((2, 8192, 1024), f32)
    for core in range(NCORES):
        b, j = divmod(core, 4)
        out[b, 2048 * j:2048 * (j + 1)] = res.results[core]["out"]
    if dbg_names:
        _CACHE["dbg"] = [{n: res.results[core]["dbg_" + n] for n in dbg_names} for core in range(NCORES)]
    return out
```

```python
import os
import types
import numpy as np
import concourse.bass as bass
import concourse.mybir as mybir
from concourse.bass_utils import run_bass_kernel_spmd

F32 = mybir.dt.float32
BF16 = mybir.dt.bfloat16
I32 = mybir.dt.int32
U32 = mybir.dt.uint32
ALU = mybir.AluOpType
AF = mybir.ActivationFunctionType
AX = mybir.AxisListType

NEG = -30000.0
LN_EPS = 1e-5
RMS_EPS = 1e-6
ALPHA = 2.0 ** 0.25
NGRP = 20
DBG = os.environ.get("KDBG", "")


class Buf:
    __slots__ = ("name", "lw", "rd")

    def __init__(self, name):
        self.name = name
        self.lw = None
        self.rd = []


class Op:
    __slots__ = ("eng", "fn", "reads", "writes", "dma", "deps", "signal", "sem", "val", "slot_prev", "bar")

    def __init__(self, eng, fn, reads, writes, dma):
        self.eng = eng
        self.fn = fn
        self.reads = reads
        self.writes = writes
        self.dma = dma
        self.deps = set()
        self.signal = False
        self.sem = None
        self.val = 0
        self.slot_prev = None
        self.bar = 0


def _freeze(fn):
    if fn is None or fn.__closure__ is None:
        return fn
    cells = []
    for c in fn.__closure__:
        try:
            cells.append(types.CellType(c.cell_contents))
        except ValueError:
            cells.append(c)
    return types.FunctionType(fn.__code__, fn.__globals__, fn.__name__, fn.__defaults__, tuple(cells))


class Sched:
    ENGS = ("pe", "act", "dve", "pool", "sp")

    def __init__(self, nc, n_dma_sems=32):
        self.nc = nc
        self.ops = []
        self.n_dma_sems = n_dma_sems
        self.nbar = 0
        self.bank = {}

    def op(self, eng, fn, reads=(), writes=(), dma=False):
        reads = [b for b in reads if b is not None]
        writes = [b for b in writes if b is not None]
        banks = set()
        for b in reads + writes:
            n = b.name
            if n.startswith("pso") and n[3:4].isdigit():
                banks.add(4 + int(n[3]))
            elif n.startswith("ps") and n[2:3].isdigit():
                banks.add(int(n[2]))
        for k in sorted(banks):
            if k not in self.bank:
                self.bank[k] = Buf("BANK%d" % k)
            writes.append(self.bank[k])
        o = Op(eng, _freeze(fn), reads, writes, dma)
        o.bar = self.nbar
        self.ops.append(o)
        return o

    def pe(self, fn, reads=(), writes=()):
        return self.op("pe", fn, reads, writes)

    def act(self, fn, reads=(), writes=()):
        return self.op("act", fn, reads, writes)

    def dve(self, fn, reads=(), writes=()):
        return self.op("dve", fn, reads, writes)

    def pool(self, fn, reads=(), writes=()):
        return self.op("pool", fn, reads, writes)

    def dma(self, fn, reads=(), writes=(), q="sp"):
        return self.op(q, fn, reads, writes, dma=True)

    def barrier(self):
        self.nbar += 1

    def fork(self):
        self._saved = self.ops
        self.ops = []

    def take(self):
        lst = self.ops
        self.ops = []
        return lst

    def join(self, lists):
        merged = []
        n = max(len(l) for l in lists)
        for i in range(n):
            for l in lists:
                if i < len(l):
                    merged.append(l[i])
        self.ops = self._saved + merged

    def _resolve(self):
        ops = self.ops
        last_eng = {}
        dma_all = []
        bar_deps = {}
        seen_bar = {e: 0 for e in self.ENGS}
        cur_bar = 0
        for i, o in enumerate(ops):
            if o.bar != cur_bar:
                cur_bar = o.bar
                bar_deps[cur_bar] = list(last_eng.values()) + list(dma_all)
            deps = set()
            raw = set()
            for b in o.reads:
                if b.lw is not None:
                    deps.add(b.lw)
                    raw.add(b.lw)
            for b in o.writes:
                if b.lw is not None:
                    deps.add(b.lw)
                for r in b.rd:
                    deps.add(r)
            for b in o.reads:
                b.rd.append(i)
            for b in o.writes:
                b.lw = i
                b.rd = []
            deps.discard(i)
            for j in deps:
                p = ops[j]
                if (not p.dma) and (not o.dma) and p.eng == o.eng:
                    if o.eng == "pe" or j not in raw:
                        continue
                o.deps.add(j)
                p.signal = True
            if seen_bar[o.eng] != cur_bar:
                seen_bar[o.eng] = cur_bar
                for j in bar_deps[cur_bar]:
                    p = ops[j]
                    if (not p.dma) and (not o.dma) and p.eng == o.eng:
                        continue
                    o.deps.add(j)
                    p.signal = True
            if o.dma:
                dma_all.append(i)
            elif o.fn is not None:
                last_eng[o.eng] = i

    def emit(self):
        nc = self.nc
        self._resolve()
        ops = self.ops
        esem = {e: nc.alloc_semaphore("s_" + e) for e in self.ENGS}
        nds = self.n_dma_sems
        dsem = [nc.alloc_semaphore("s_dma%d" % k) for k in range(2 * nds)]
        ecount = {e: 0 for e in self.ENGS}
        dcount = [0] * (2 * nds)
        dlast = [None] * (2 * nds)
        nd = 0
        ndq = {"sp": 0, "pool": 0, "act": 0}
        for i, o in enumerate(ops):
            if o.dma:
                base = nds if o.eng == "pool" else 0
                k = base + ndq[o.eng] % nds
                ndq[o.eng] += 1
                nd += 1
                o.signal = True
                o.sem = ("d", k)
                dcount[k] += 16
                o.val = dcount[k]
                o.slot_prev = dlast[k]
                dlast[k] = i
            elif o.signal:
                ecount[o.eng] += 1
                o.sem = ("e", o.eng)
                o.val = ecount[o.eng]
        self.stats = dict(ecount)
        self.stats["ndma"] = nd
        self.stats["nops"] = len(ops)

        def semh(s):
            return esem[s[1]] if s[0] == "e" else dsem[s[1]]

        per_eng = {e: [] for e in self.ENGS}
        for i, o in enumerate(ops):
            per_eng[o.eng].append(i)

        def emit_stream(ename, eng):
            waited = {}
            for i in per_eng[ename]:
                o = ops[i]
                need = {}
                for j in o.deps:
                    p = ops[j]
                    if need.get(p.sem, 0) < p.val:
                        need[p.sem] = p.val
                if o.dma and o.slot_prev is not None:
                    p = ops[o.slot_prev]
                    if need.get(p.sem, 0) < p.val:
                        need[p.sem] = p.val
                for s, v in need.items():
                    if waited.get(s, 0) >= v:
                        continue
                    eng.wait_ge(semh(s), v)
                    waited[s] = v
                if o.fn is None:
                    continue
                ins = o.fn(eng)
                if o.signal:
                    ins.then_inc(semh(o.sem), 16 if o.dma else 1)

        with nc.Block() as block:
            @block.tensor
            def _(e):
                emit_stream("pe", e)

            @block.scalar
            def _(e):
                emit_stream("act", e)

            @block.vector
            def _(e):
                emit_stream("dve", e)

            @block.gpsimd
            def _(e):
                emit_stream("pool", e)

            @block.sync
            def _(e):
                emit_stream("sp", e)


class Arena:
    def __init__(self, nc, nbytes):
        self.t = nc.alloc_sbuf_tensor("arena", [128, nbytes // 4], F32)
        self.off = 0
        self.cap = nbytes
        self.hi = 0

    def alloc(self, shape, dtype=F32):
        esz = 2 if dtype == BF16 else 4
        n = 1
        for s in shape[1:]:
            n *= s
        nb = (n * esz + 31) // 32 * 32
        o = self.off
        self.off += nb
        self.hi = max(self.hi, self.off)
        assert self.off <= self.cap, ("arena overflow", self.off, self.cap)
        v = self.t[:, o // 4:(o + nb) // 4]
        if dtype != F32:
            v = v.bitcast(dtype)
        v = v[:, 0:n]
        if len(shape) == 3:
            v = v.rearrange("p (a b) -> p a b", a=shape[1])
        elif len(shape) == 4:
            v = v.rearrange("p (a b c) -> p a b c", a=shape[1], b=shape[2])
        if shape[0] != 128:
            v = v[0:shape[0]]
        return v


class _Stop(Exception):
    pass


STOP = int(os.environ.get("KSTOP", "9"))
NCORES = int(os.environ.get("KCORES", "8"))


def build_program():
    st = {}
    try:
        _body(st)
    except _Stop:
        pass
    S = st["S"]
    B = st["B"]
    S.op("sp", None, reads=[B("out%d" % i) for i in range(16)] + [B("dbgo"), B("dbgo2"), B("dbgo3")] + [B("dbgx%d" % i) for i in range(16)] + [B("dbgy%d" % i) for i in range(16)])
    S.emit()
    print("sched stats", S.stats)
    return st["nc"], list(st["dbg_out"].keys())


def _body(st):
    nc = bass.Bass("TRN2", target_bir_lowering=False)
    S = Sched(nc)
    bufs = {}
    st["nc"] = nc
    st["S"] = S

    def B(name):
        b = bufs.get(name)
        if b is None:
            b = bufs[name] = Buf(name)
        return b

    def din(name, shape, dt=F32):
        return nc.dram_tensor(name, list(shape), dt, kind="ExternalInput").ap()

    d_cwin = din("cwin", [2, 128, 8, 258])
    d_xwin = din("xwin", [NGRP, 128, 8, 514])
    d_xown = din("xown", [2048, 1024])
    d_wmod = din("wmod", [12, 128, 8, 512])
    d_bmod = din("bmod", [12, 2, 512])
    d_cvec = din("cvec", [128, 16])
    d_win = din("win", [8, 128, 2832])
    d_ktaps = din("ktaps", [128, 22 * 12])
    d_qtaps = din("qtaps", [128, 2 * 12])
    d_cb = din("convb", [128, 8])
    d_flags = din("flags", [128, 44])
    d_gmask = din("gmask", [128, 48])
    d_bg = din("bg", [128, 16])
    d_mhw = din("mhw", [128, 512])
    d_nw = din("nw", [128, 2])
    d_wout = din("wout", [8, 128, 1024])
    d_wqT = din("wqT", [16, 128, 1024])
    d_keysT = din("keysT", [128, 16 * 128])
    d_lnp = din("lnp", [128, 4 * 1024])
    d_pu = din("pu", [16384, 1024])
    d_pv = din("pv", [16384, 1024])
    d_cst = din("cst", [128, 9 * 128])
    d_iota = din("iota16", [128, 256])
    d_rope = din("rope", [16, 2, 128, 512])
    d_out = nc.dram_tensor("out", [2048, 1024], F32, kind="ExternalOutput").ap()
    d_pub = nc.dram_tensor("pub", [16384, 1024], BF16).ap()
    d_pvb = nc.dram_tensor("pvb", [16384, 1024], BF16).ap()
    dbg_out = {}
    st["B"] = B
    st["dbg_out"] = dbg_out

    def dbg(name, shape, dt=F32):
        if name in DBG.split(","):
            dbg_out[name] = nc.dram_tensor("dbg_" + name, list(shape), dt, kind="ExternalOutput").ap()
            return dbg_out[name]
        return None

    AR = Arena(nc, 212800)
    PS = [nc.alloc_psum_tensor("ps%d" % i, [128, 512], F32) for i in range(8)]

    CST = AR.alloc([128, 9 * 128])
    ident = CST[:, 0:128]
    ones = CST[:, 128:256]
    tri = CST[:, 256:384]
    mask_sr = CST[:, 384:512]
    mask_rs = CST[:, 512:640]
    jmat = CST[:, 640:768]
    blk64 = CST[:, 768:896]
    rTm = CST[:, 896:1024]
    sel0 = CST[:, 1024:1152]
    CSTB = AR.alloc([128, 2 * 128], BF16)
    identb = CSTB[:, 0:128]
    jb = CSTB[:, 128:256]
    COLS = AR.alloc([128, 48, 2])
    SC1P = AR.alloc([128, 8, 2])
    MIX = AR.alloc([128, 16, 1024], BF16)
    MHW = AR.alloc([128, 512])
    KTAPS = AR.alloc([128, 22 * 12])
    QTAPS = AR.alloc([128, 24])
    CONVB = AR.alloc([128, 8])
    FLAGS = AR.alloc([128, 44])
    GMASK = AR.alloc([128, 48])
    BG = AR.alloc([128, 16])
    NW = AR.alloc([128, 2])
    MST = [AR.alloc([128, 4]) for _ in range(2)]
    CT = [AR.alloc([128, 4, 129]) for _ in range(2)]
    CTB = [AR.alloc([128, 4, 129], BF16) for _ in range(2)]
    SMALL = AR.alloc([128, 512])
    smo = [0]

    def small(n):
        o = smo[0]
        smo[0] += n
        assert smo[0] <= 512
        return SMALL[:, o:o + n]

    bC = B("cst")
    S.dma(lambda e: e.dma_start(out=CST, in_=d_cst), writes=[bC])
    S.act(lambda e: e.activation(out=identb, in_=ident, func=AF.Copy), reads=[bC], writes=[B("cstb")])
    S.act(lambda e: e.activation(out=jb, in_=jmat, func=AF.Copy), reads=[bC], writes=[B("cstb")])
    bCB = B("cstb")
    for (t, d, n) in ((MHW, d_mhw, "mhw"), (KTAPS, d_ktaps, "ktaps"), (QTAPS, d_qtaps, "qtaps"), (CONVB, d_cb, "convb"),
                      (FLAGS, d_flags, "flags"), (GMASK, d_gmask, "gmask"), (BG, d_bg, "bg"), (NW, d_nw, "nw")):
        S.dma(lambda e, t=t, d=d: e.dma_start(out=t, in_=d), writes=[B(n)], q="pool")
    for d in range(2):
        S.pool(lambda e, d=d: e.memset(MST[d], 0.0), writes=[B("m%d" % d)])
        S.pool(lambda e, d=d: e.memset(CT[d], 0.0), writes=[B("ct%d" % d)])
        S.pool(lambda e, d=d: e.memset(CTB[d], 0.0), writes=[B("ctb%d" % d)])

    mark_persist = AR.off

    CV = AR.alloc([128, 16])
    SIL = AR.alloc([128, 8, 2])
    WM = [AR.alloc([128, 8, 512]) for _ in range(2)]
    BM = [AR.alloc([2, 512]) for _ in range(2)]
    MROW = [AR.alloc([2, 512]) for _ in range(2)]
    S.dma(lambda e: e.dma_start(out=CV, in_=d_cvec), writes=[B("cv")])
    S.act(lambda e: e.activation(out=SIL.rearrange("p a b -> p (a b)"), in_=CV, func=AF.Silu), reads=[B("cv")], writes=[B("sil")])
    colps = PS[1][:, 0:96]
    for g in range(12):
        wm = WM[g % 2]
        bw = B("wm%d" % (g % 2))
        S.dma(lambda e, wm=wm, g=g: e.dma_start(out=wm, in_=d_wmod[g]), writes=[bw])
        S.dma(lambda e, g=g: e.dma_start(out=BM[g % 2], in_=d_bmod[g]), writes=[B("bm%d" % (g % 2))], q="pool")
        for kc in range(8):
            S.pe(lambda e, wm=wm, kc=kc: e.matmul(PS[0][0:2, :], lhsT=SIL[:, kc, :], rhs=wm[:, kc, :], start=(kc == 0), stop=(kc == 7)),
                 reads=[B("sil"), bw], writes=[B("ps0")])
        mr = MROW[g % 2]
        S.dve(lambda e, g=g, mr=mr: e.tensor_tensor(out=mr, in0=PS[0][0:2, :], in1=BM[g % 2], op=ALU.add),
              reads=[B("ps0"), B("bm%d" % (g % 2))], writes=[B("mrow%d" % (g % 2))])
        for fc in range(4):
            idx = g * 4 + fc
            S.pe(lambda e, mr=mr, fc=fc, idx=idx: e.matmul(colps[:, idx * 2:idx * 2 + 2], lhsT=mr[:, fc * 128:(fc + 1) * 128], rhs=ident[0:2, 0:2],
                                                           start=True, stop=True),
                 reads=[B("mrow%d" % (g % 2)), bC], writes=[B("ps1")])
    S.dve(lambda e: e.tensor_copy(out=COLS.rearrange("p a b -> p (a b)"), in_=colps), reads=[B("ps1")], writes=[B("cols")])
    S.dve(lambda e: e.tensor_scalar(out=SC1P, in0=COLS[:, 8:16, :], scalar1=1.0, scalar2=None, op0=ALU.add), reads=[B("cols")], writes=[B("sc1p")])
    dd = dbg("cols", [128, 96])
    if dd is not None:
        S.dma(lambda e, dd=dd: e.dma_start(out=dd, in_=COLS.rearrange("p a b -> p (a b)")), reads=[B("cols")], writes=[B("dbgo")])

    if STOP == 0:
        raise _Stop()
    AR.off = mark_persist
    S.barrier()

    WB = AR.alloc([128, 8, 2832], BF16)
    QAT = AR.alloc([128, 4, 2048], BF16)
    KAT = AR.alloc([128, 66 * 128], BF16)
    VA = AR.alloc([128, 66, 2, 65], BF16)
    XW = AR.alloc([128, 8, 514])
    HT = AR.alloc([128, 8, 514], BF16)
    KT = AR.alloc([128, 4, 512], BF16)
    QT = AR.alloc([128, 4, 512], BF16)
    ZB = [AR.alloc([128, 514]) for _ in range(2)]
    TB = [AR.alloc([128, 512]) for _ in range(2)]
    SQ = AR.alloc([128, 512])
    SD = AR.alloc([128, 512])
    KN = AR.alloc([128, 512])
    ROPE = AR.alloc([128, 2, 512])
    KTOK2 = [AR.alloc([128, 4, 128], BF16) for _ in range(2)]
    VAUG2 = [AR.alloc([128, 4, 129], BF16) for _ in range(2)]
    VW = [[AR.alloc([128, 129], BF16) for _ in range(2)] for _ in range(2)]
    DA = AR.alloc([128, 128])
    DM = AR.alloc([128, 128])
    DTT = AR.alloc([128, 128])
    PT = AR.alloc([128, 128], BF16)
    TT1 = AR.alloc([128, 129])
    TOT = AR.alloc([128, 129])
    HF = AR.alloc([128, 512], BF16)
    HN = AR.alloc([128, 512])
    OSG = AR.alloc([128, 512])
    GP2 = [AR.alloc([128, 16]) for _ in range(2)]
    WST = [QAT.rearrange("p a b -> p (a b)").bitcast(F32)[:, 0:2832], KAT.bitcast(F32)[:, 0:2832]]
    print("phase1 arena bytes", AR.off)

    for par in range(2):
        S.pool(lambda e: e.memset(VAUG2[par][:, :, 128:129], 1.0), writes=[B("vaug%d" % par)])
    S.pool(lambda e: e.memset(VA[:, :, :, 64:65], 1.0), writes=[B("va")])

    for kc in range(8):
        st = WST[kc % 2]
        bs = B("qat" if kc % 2 == 0 else "kat")
        S.dma(lambda e, st=st, kc=kc: e.dma_start(out=st, in_=d_win[kc]), writes=[bs])
        if kc % 2 == 0:
            S.act(lambda e, st=st, kc=kc: e.activation(out=WB[:, kc, :], in_=st, func=AF.Copy), reads=[bs], writes=[B("wb")])
        else:
            S.pool(lambda e, st=st, kc=kc: e.tensor_copy(out=WB[:, kc, :], in_=st), reads=[bs], writes=[B("wb")])

    C_KM, C_QM, C_KA, C_QA, C_VM, C_VA, C_G, C_O = 0, 512, 1024, 1152, 1664, 2176, 2304, 2320

    def mk_small():
        return dict(ef=small(4), nlf=small(4), li=small(4), a=small(4), amx=small(1), d4=small(4), ml=small(4), dm=small(4),
                    dec=small(4), aw=small(4), w=small(4), cm=small(1), mv=small(1), nmv=small(1), dwi=small(1), wi=small(1),
                    dn0=small(1), nrm=small(1), ad=small(1), dn=small(1), rd=small(1))
    SM = [mk_small(), mk_small()]
    S1 = small(4)
    S2 = small(4)
    MEAN = small(4)
    MSQ = small(4)
    VAR = small(4)
    RSTD = small(4)

    def fm_project(c0, N, halo, psb):
        for kc in range(8):
            S.pe(lambda e, kc=kc: e.matmul(PS[psb][:, 0:N], lhsT=WB[:, kc, c0:c0 + 128], rhs=HT[:, kc, 1:N + 1], start=(kc == 0), stop=(kc == 7)),
                 reads=[B("wb"), B("ht")], writes=[B("ps%d" % psb)])
        if halo:
            for kc in range(8):
                S.pe(lambda e, kc=kc: e.matmul(PS[2][:, 0:2], lhsT=WB[:, kc, c0:c0 + 128], rhs=HT[:, kc, 0:N + 2:N + 1], start=(kc == 0), stop=(kc == 7)),
                     reads=[B("wb"), B("ht")], writes=[B("ps2")])

    def conv_block(N, psb, zi, taps, cbias, fl, dst, qscale):
        Z = ZB[zi]
        T = TB[zi]
        bz = B("z%d" % zi)
        bt = B("t%d" % zi)
        S.act(lambda e: e.activation(out=Z[:, 1:N + 1], in_=PS[psb][:, 0:N], func=AF.Copy), reads=[B("ps%d" % psb)], writes=[bz])
        S.dve(lambda e: e.tensor_tensor(out=Z[:, 0:N + 2:N + 1], in0=PS[2][:, 0:2], in1=fl, op=ALU.mult), reads=[B("ps2"), B("flags")], writes=[bz])
        S.dve(lambda e: e.tensor_scalar(out=T[:, 0:N], in0=Z[:, 0:N], scalar1=taps[:, 0:1], scalar2=None, op0=ALU.mult),
              reads=[bz, B("ktaps"), B("qtaps")], writes=[bt])
        S.dve(lambda e: e.scalar_tensor_tensor(out=T[:, 0:N], in0=Z[:, 1:N + 1], scalar=taps[:, 1:2], in1=T[:, 0:N], op0=ALU.mult, op1=ALU.add),
              reads=[bz, bt], writes=[bt])
        S.dve(lambda e: e.scalar_tensor_tensor(out=T[:, 0:N], in0=Z[:, 2:N + 2], scalar=taps[:, 2:3], in1=T[:, 0:N], op0=ALU.mult, op1=ALU.add),
              reads=[bz, bt], writes=[bt])
        if qscale:
            S.act(lambda e: e.activation(out=T[:, 0:N], in_=T[:, 0:N], func=AF.Silu, bias=cbias), reads=[bt, B("convb")], writes=[bt])
            S.pool(lambda e: e.tensor_scalar(out=dst, in0=T[:, 0:N], scalar1=128.0 ** -0.5, scalar2=None, op0=ALU.mult), reads=[bt], writes=[B("qt")])
        else:
            S.act(lambda e: e.activation(out=dst, in_=T[:, 0:N], func=AF.Silu, bias=cbias), reads=[bt, B("convb")], writes=[B("kt")])

    def normrope(N, psb, nwcol, rope, dst, bdst):
        ps = PS[psb][:, 0:N]
        bp = B("ps%d" % psb)
        S.act(lambda e: e.activation(out=SQ[:, 0:N], in_=ps, func=AF.Square), reads=[bp], writes=[B("sq")])
        S.pe(lambda e: e.matmul(PS[6][:, 0:N], lhsT=blk64, rhs=SQ[:, 0:N], start=True, stop=True), reads=[bC, B("sq")], writes=[B("ps6")])
        S.act(lambda e: e.activation(out=SD[:, 0:N], in_=PS[6][:, 0:N], func=AF.Sqrt, scale=1.0 / 64, bias=RMS_EPS), reads=[B("ps6")], writes=[B("sd")])
        S.dve(lambda e: e.reciprocal(out=SD[:, 0:N], in_=SD[:, 0:N]), reads=[B("sd")], writes=[B("sd")])
        S.dve(lambda e: e.scalar_tensor_tensor(out=KN[:, 0:N], in0=ps, scalar=NW[:, nwcol:nwcol + 1], in1=SD[:, 0:N], op0=ALU.mult, op1=ALU.mult),
              reads=[bp, B("nw"), B("sd")], writes=[B("kn")])
        if rope:
            S.pe(lambda e: e.matmul(PS[5][:, 0:N], lhsT=rTm, rhs=KN[:, 0:N], start=True, stop=True), reads=[bC, B("kn")], writes=[B("ps5")])
            S.pool(lambda e: e.tensor_tensor(out=SQ[:, 0:N], in0=KN[:, 0:N], in1=ROPE[:, 0, 0:N], op=ALU.mult), reads=[B("kn"), B("rope")], writes=[B("sq")])
            S.dve(lambda e: e.tensor_tensor(out=KN[:, 0:N], in0=PS[5][:, 0:N], in1=ROPE[:, 1, 0:N], op=ALU.mult), reads=[B("ps5"), B("rope"), B("kn")], writes=[B("kn")])
            S.dve(lambda e: e.tensor_tensor(out=dst, in0=KN[:, 0:N], in1=SQ[:, 0:N], op=ALU.add), reads=[B("kn"), B("sq")], writes=[bdst])
        else:
            S.act(lambda e: e.activation(out=dst, in_=KN[:, 0:N], func=AF.Copy), reads=[B("kn")], writes=[bdst])

    def chunk_state(d, masked_g, t, par):
        GP = GP2[par]
        bgp = B("gp%d" % par)
        sm = SM[d]
        ic = 0 if d == 0 else 8
        fc = ic + 4
        bn = lambda n: B("sm%d_%s" % (d, n))
        bm = B("m%d" % d)
        S.act(lambda e: e.activation(out=sm["ef"], in_=GP[:, fc:fc + 4], func=AF.Exp, scale=-1.0), reads=[bgp], writes=[bn("ef")])
        S.act(lambda e: e.activation(out=sm["nlf"], in_=sm["ef"], func=AF.Ln, bias=1.0), reads=[bn("ef")], writes=[bn("nlf")])
        if masked_g is not None:
            kcol = GMASK[:, masked_g * 4 + 2 * d:masked_g * 4 + 2 * d + 1]
            acol = GMASK[:, masked_g * 4 + 2 * d + 1:masked_g * 4 + 2 * d + 2]
            S.dve(lambda e: e.tensor_scalar(out=sm["nlf"], in0=sm["nlf"], scalar1=kcol, scalar2=None, op0=ALU.mult), reads=[bn("nlf"), B("gmask")], writes=[bn("nlf")])
            S.dve(lambda e: e.tensor_scalar(out=sm["li"], in0=GP[:, ic:ic + 4], scalar1=kcol, scalar2=acol, op0=ALU.mult, op1=ALU.add),
                  reads=[bgp, B("gmask")], writes=[bn("li")])
        else:
            S.dve(lambda e: e.tensor_copy(out=sm["li"], in_=GP[:, ic:ic + 4]), reads=[bgp], writes=[bn("li")])
        o = 16 + d * 32
        nbps = PS[2][:, o:o + 4]
        nBps = PS[2][:, o + 4:o + 8]
        amBps = PS[2][:, o + 8:o + 12]
        aTps = PS[2][0:4, 128 + d * 128:256 + d * 128]
        bps = B("ps2s%d" % d)
        S.pe(lambda e: e.matmul(nbps, lhsT=tri, rhs=sm["nlf"], start=True, stop=True), reads=[bC, bn("nlf")], writes=[bps])
        S.pe(lambda e: e.matmul(nBps, lhsT=ones, rhs=sm["nlf"], start=True, stop=True), reads=[bC, bn("nlf")], writes=[bps])
        S.dve(lambda e: e.tensor_tensor(out=sm["a"], in0=nbps, in1=sm["li"], op=ALU.add), reads=[bps, bn("li")], writes=[bn("a")])
        S.pe(lambda e: e.matmul(aTps, lhsT=sm["a"], rhs=ident, start=True, stop=True), reads=[bC, bn("a")], writes=[B("ps2t%d" % d)])
        S.dve(lambda e: e.tensor_reduce(out=sm["amx"][0:4], in_=aTps, axis=AX.X, op=ALU.max), reads=[B("ps2t%d" % d)], writes=[bn("amx")])
        S.dve(lambda e: e.tensor_scalar(out=sm["d4"][0:4], in0=ident[0:4, 0:4], scalar1=sm["amx"][0:4], scalar2=None, op0=ALU.mult),
              reads=[bC, bn("amx")], writes=[bn("d4")])
        S.pe(lambda e: e.matmul(amBps, lhsT=ones[0:4, :], rhs=sm["d4"][0:4], start=True, stop=True), reads=[bC, bn("d4")], writes=[bps])
        S.dve(lambda e: e.tensor_tensor(out=sm["ml"], in0=amBps, in1=MST[d], op=ALU.max), reads=[bps, bm], writes=[bn("ml")])
        S.dve(lambda e: e.tensor_tensor(out=sm["dm"], in0=MST[d], in1=sm["ml"], op=ALU.subtract), reads=[bm, bn("ml")], writes=[bn("dm")])
        S.act(lambda e: e.activation(out=sm["dec"], in_=sm["dm"], func=AF.Exp), reads=[bn("dm")], writes=[bn("dec")])
        S.dve(lambda e: e.tensor_tensor(out=sm["aw"], in0=sm["a"], in1=sm["ml"], op=ALU.subtract), reads=[bn("a"), bn("ml")], writes=[bn("aw")])
        S.act(lambda e: e.activation(out=sm["w"], in_=sm["aw"], func=AF.Exp), reads=[bn("aw")], writes=[bn("w")])
        return nbps, nBps, bps

    def chunk_update(d, nBps, bps, par):
        VAUG = VAUG2[par]
        KTOK = KTOK2[par]
        bva = B("vaug%d" % par)
        bkt = B("ktok%d" % par)
        sm = SM[d]
        bn = lambda n: B("sm%d_%s" % (d, n))
        for h in range(4):
            vw = VW[d][h % 2]
            bvw = B("vw%d_%d" % (d, h % 2))
            S.dve(lambda e, h=h, vw=vw: e.tensor_scalar(out=vw, in0=VAUG[:, h, :], scalar1=sm["w"][:, h:h + 1], scalar2=None, op0=ALU.mult),
                  reads=[bva, bn("w")], writes=[bvw])
            up = PS[7][:, 256:385] if d == 0 else PS[1][:, 0:129]
            bup = B("ps7u") if d == 0 else B("ps1")
            S.pe(lambda e, h=h, vw=vw, up=up: e.matmul(up, lhsT=KTOK[:, h, :], rhs=vw, start=True, stop=True), reads=[bkt, bvw], writes=[bup])
            S.dve(lambda e, h=h, up=up: e.scalar_tensor_tensor(out=CT[d][:, h, :], in0=CT[d][:, h, :], scalar=sm["dec"][:, h:h + 1], in1=up,
                                                               op0=ALU.mult, op1=ALU.add),
                  reads=[B("ct%d" % d), bn("dec"), bup], writes=[B("ct%d" % d)])
        S.pool(lambda e: e.tensor_copy(out=CTB[d], in_=CT[d]), reads=[B("ct%d" % d)], writes=[B("ctb%d" % d)])
        S.dve(lambda e: e.tensor_tensor(out=MST[d], in0=sm["ml"], in1=nBps, op=ALU.subtract), reads=[bn("ml"), bps], writes=[B("m%d" % d)])

    def chunk_full(d, t, nbps, bps, hdst, bh, par):
        VAUG = VAUG2[par]
        bva = B("vaug%d" % par)
        sm = SM[d]
        bn = lambda n: B("sm%d_%s" % (d, n))
        bm = B("m%d" % d)
        cs = slice(t * 128, (t + 1) * 128)
        for h in range(4):
            S.dve(lambda e, h=h: e.tensor_scalar(out=DA, in0=ident, scalar1=sm["a"][:, h:h + 1], scalar2=None, op0=ALU.mult), reads=[bC, bn("a")], writes=[B("da")])
            S.pe(lambda e: e.matmul(PS[7][:, 0:128], lhsT=ones, rhs=DA, start=True, stop=False), reads=[bC, B("da")], writes=[B("ps7e")])
            S.pe(lambda e: e.matmul(PS[7][:, 0:128], lhsT=ident, rhs=mask_sr, start=False, stop=True), reads=[bC], writes=[B("ps7e")])
            S.dve(lambda e: e.tensor_reduce(out=sm["cm"], in_=PS[7][:, 0:128], axis=AX.X, op=ALU.max), reads=[B("ps7e")], writes=[bn("cm")])
            S.dve(lambda e, h=h: e.tensor_tensor(out=sm["mv"], in0=sm["cm"], in1=MST[d][:, h:h + 1], op=ALU.max), reads=[bn("cm"), bm], writes=[bn("mv")])
            S.dve(lambda e: e.tensor_scalar(out=sm["nmv"], in0=sm["mv"], scalar1=-1.0, scalar2=None, op0=ALU.mult), reads=[bn("mv")], writes=[bn("nmv")])
            S.dve(lambda e: e.tensor_scalar(out=DM, in0=ident, scalar1=sm["nmv"], scalar2=None, op0=ALU.mult), reads=[bC, bn("nmv")], writes=[B("dmm")])
            S.pe(lambda e: e.matmul(PS[7][:, 128:256], lhsT=ones, rhs=DM, start=True, stop=False), reads=[bC, B("dmm")], writes=[B("ps7x")])
            S.pe(lambda e: e.matmul(PS[7][:, 128:256], lhsT=ident, rhs=mask_rs, start=False, stop=True), reads=[bC], writes=[B("ps7x")])
            S.act(lambda e, h=h: e.activation(out=DTT, in_=PS[7][:, 128:256], func=AF.Exp, bias=sm["a"][:, h:h + 1]), reads=[B("ps7x"), bn("a")], writes=[B("dtt")])
            S.pe(lambda e, h=h: e.matmul(PS[4][:, 0:128], lhsT=KT[:, h, cs], rhs=QT[:, h, cs], start=True, stop=True), reads=[B("kt"), B("qt")], writes=[B("ps4s")])
            S.dve(lambda e: e.tensor_tensor(out=PT, in0=PS[4][:, 0:128], in1=DTT, op=ALU.mult), reads=[B("ps4s"), B("dtt")], writes=[B("pt")])
            S.pe(lambda e, h=h: e.matmul(PS[4][:, 128:257], lhsT=PT, rhs=VAUG[:, h, :], start=True, stop=True), reads=[B("pt"), bva], writes=[B("ps4n")])
            S.pe(lambda e, h=h: e.matmul(PS[4][:, 257:386], lhsT=QT[:, h, cs], rhs=CTB[d][:, h, :], start=True, stop=True), reads=[B("qt"), B("ctb%d" % d)], writes=[B("ps4i")])
            S.dve(lambda e, h=h: e.tensor_tensor(out=sm["dwi"], in0=MST[d][:, h:h + 1], in1=sm["mv"], op=ALU.subtract), reads=[bm, bn("mv")], writes=[bn("dwi")])
            S.act(lambda e: e.activation(out=sm["wi"], in_=sm["dwi"], func=AF.Exp), reads=[bn("dwi")], writes=[bn("wi")])
            S.act(lambda e: e.activation(out=TT1, in_=PS[4][:, 257:386], func=AF.Identity, scale=sm["wi"]), reads=[B("ps4i"), bn("wi")], writes=[B("tt1")])
            S.dve(lambda e: e.tensor_tensor(out=TOT, in0=PS[4][:, 128:257], in1=TT1, op=ALU.add), reads=[B("ps4n"), B("tt1")], writes=[B("tot")])
            S.dve(lambda e, h=h: e.tensor_tensor(out=sm["dn0"], in0=nbps[:, h:h + 1], in1=sm["mv"], op=ALU.subtract), reads=[bps, bn("mv")], writes=[bn("dn0")])
            S.act(lambda e: e.activation(out=sm["nrm"], in_=sm["dn0"], func=AF.Exp), reads=[bn("dn0")], writes=[bn("nrm")])
            S.act(lambda e: e.activation(out=sm["ad"], in_=TOT[:, 128:129], func=AF.Abs), reads=[B("tot")], writes=[bn("ad")])
            S.dve(lambda e: e.tensor_tensor(out=sm["dn"], in0=sm["ad"], in1=sm["nrm"], op=ALU.max), reads=[bn("ad"), bn("nrm")], writes=[bn("dn")])
            S.dve(lambda e: e.reciprocal(out=sm["rd"], in_=sm["dn"]), reads=[bn("dn")], writes=[bn("rd")])
            S.act(lambda e, h=h: e.activation(out=hdst[:, h * 128:(h + 1) * 128], in_=TOT[:, 0:128], func=AF.Identity, scale=sm["rd"]),
                  reads=[B("tot"), bn("rd")], writes=[bh])

    steps = [("ctxF", 0, 256, 0, 0), ("ctxB", 1, 256, 1, None)]
    for g in range(12):
        steps.append(("oth", g, 512, 2 + g, 2 + 4 * g))
    for g in range(4):
        steps.append(("ownB", 12 + g, 512, 14 + g, None))
    for g in range(4):
        steps.append(("ownF", 16 + g, 512, 18 + g, 50 + 4 * g))

    KG = int(os.environ.get("KG", "99"))
    KSUB = int(os.environ.get("KSUB", "99"))
    if KG == 0:
        raise _Stop()
    for gstep, (kind, src, N, tg, ktb) in enumerate(steps):
        if gstep >= KG:
            raise _Stop()
        last = gstep == KG - 1
        isctx = kind.startswith("ctx")
        mc = 1 if isctx else 0
        if isctx:
            S.dma(lambda e, src=src: e.dma_start(out=XW[:, :, 0:258], in_=d_cwin[src]), writes=[B("xw")])
        else:
            S.dma(lambda e, src=src: e.dma_start(out=XW, in_=d_xwin[src]), writes=[B("xw")])
        for kc in range(8):
            if kc % 2 == 0:
                S.act(lambda e, kc=kc: e.activation(out=HT[:, kc, 0:N + 2], in_=XW[:, kc, 0:N + 2], func=AF.Identity,
                                                    bias=COLS[:, kc, mc:mc + 1], scale=SC1P[:, kc, mc:mc + 1]),
                      reads=[B("xw"), B("cols"), B("sc1p")], writes=[B("ht")])
            else:
                S.pool(lambda e, kc=kc: e.tensor_scalar(out=HT[:, kc, 0:N + 2], in0=XW[:, kc, 0:N + 2], scalar1=SC1P[:, kc, mc:mc + 1],
                                                        scalar2=COLS[:, kc, mc:mc + 1], op0=ALU.mult, op1=ALU.add),
                       reads=[B("xw"), B("cols"), B("sc1p")], writes=[B("ht")])
        if last and KSUB == 0:
            raise _Stop()
        fl = FLAGS[:, tg * 2:tg * 2 + 2]
        for hb in range(4):
            fm_project(C_KM + hb * 128, N, True, hb % 2)
            conv_block(N, hb % 2, hb % 2, KTAPS[:, (tg * 4 + hb) * 3:(tg * 4 + hb) * 3 + 3], CONVB[:, 4 + hb:5 + hb], fl, KT[:, hb, 0:N], False)
        if kind in ("ownB", "ownF"):
            qd = 1 if kind == "ownB" else 0
            for hb in range(4):
                fm_project(C_QM + hb * 128, N, True, hb % 2)
                conv_block(N, hb % 2, hb % 2, QTAPS[:, (qd * 4 + hb) * 3:(qd * 4 + hb) * 3 + 3], CONVB[:, hb:hb + 1], fl, QT[:, hb, 0:N], True)
        if last and KSUB == 1:
            raise _Stop()
        if ktb is not None:
            if not isctx:
                ri = src if kind == "oth" else 12 + (src - 16)
                S.dma(lambda e, ri=ri: e.dma_start(out=ROPE, in_=d_rope[ri].rearrange("a p n -> p a n")), writes=[B("rope")])
            fm_project(C_KA, N, False, 0)
            normrope(N, 0, 1, not isctx, KAT[:, ktb * 128:ktb * 128 + N], B("kat"))
            if kind == "ownF":
                gi = src - 16
                for mcq in range(4):
                    fm_project(C_QA + mcq * 128, N, False, 1)
                    normrope(N, 1, 0, True, QAT[:, mcq, gi * 512:(gi + 1) * 512], B("qat"))
        if last and KSUB == 2:
            raise _Stop()
        for t in range(N // 128):
            par = t % 2
            VAUG = VAUG2[par]
            KTOK = KTOK2[par]
            GP = GP2[par]
            ts = slice(1 + t * 128, 1 + (t + 1) * 128)
            for kc in range(8):
                S.pe(lambda e, kc=kc, ts=ts: e.matmul(PS[3][:, 0:512], lhsT=HT[:, kc, ts], rhs=WB[:, kc, C_VM:C_VM + 512], start=(kc == 0), stop=(kc == 7)),
                     reads=[B("ht"), B("wb")], writes=[B("ps3")])
            for kc in range(8):
                S.pe(lambda e, kc=kc, ts=ts: e.matmul(PS[6][:, 0:144], lhsT=HT[:, kc, ts], rhs=WB[:, kc, C_VA:C_VA + 144], start=(kc == 0), stop=(kc == 7)),
                     reads=[B("ht"), B("wb")], writes=[B("ps6")])
            S.act(lambda e: e.activation(out=VAUG[:, :, 0:128], in_=PS[3][:, 0:512].rearrange("p (a b) -> p a b", a=4), func=AF.Copy),
                  reads=[B("ps3")], writes=[B("vaug%d" % par)])
            if ktb is not None:
                S.act(lambda e, kt_=ktb + t: e.activation(out=VA[:, kt_, :, 0:64], in_=PS[6][:, 0:128].rearrange("p (a b) -> p a b", a=2), func=AF.Copy),
                      reads=[B("ps6")], writes=[B("va")])
            S.dve(lambda e: e.tensor_tensor(out=GP, in0=PS[6][:, 128:144], in1=BG, op=ALU.add), reads=[B("ps6"), B("bg")], writes=[B("gp%d" % par)])
            for h in range(4):
                S.pe(lambda e, h=h, t=t: e.matmul(PS[5][:, h * 128:(h + 1) * 128], lhsT=KT[:, h, t * 128:(t + 1) * 128], rhs=identb, start=True, stop=True),
                     reads=[B("kt"), bCB], writes=[B("ps5")])
            S.act(lambda e: e.activation(out=KTOK.rearrange("p a b -> p (a b)"), in_=PS[5][:, 0:512], func=AF.Copy), reads=[B("ps5")], writes=[B("ktok%d" % par)])
            if kind == "ownF":
                for kc in range(8):
                    S.pe(lambda e, kc=kc, ts=ts: e.matmul(PS[3][:, 0:512], lhsT=HT[:, kc, ts], rhs=WB[:, kc, C_O:C_O + 512], start=(kc == 0), stop=(kc == 7)),
                         reads=[B("ht"), B("wb")], writes=[B("ps3")])
                S.act(lambda e: e.activation(out=OSG, in_=PS[3][:, 0:512], func=AF.Sigmoid), reads=[B("ps3")], writes=[B("osg")])
            if last and KSUB == 3:
                raise _Stop()
            dirs = {"ctxF": [0], "ctxB": [1], "oth": [0, 1], "ownB": [1], "ownF": [0]}[kind]
            if kind == "oth":
                S.fork()
                lists = []
                for d in dirs:
                    nbps, nBps, bps = chunk_state(d, src, t, par)
                    chunk_update(d, nBps, bps, par)
                    lists.append(S.take())
                S.join(lists)
                dirs = []
            for d in dirs:
                nbps, nBps, bps = chunk_state(d, src if kind == "oth" else None, t, par)
                if kind == "ownB":
                    cb = (src - 12) * 4 + t
                    chunk_full(d, t, nbps, bps, MIX[:, cb, 512:1024], B("mixb%d" % cb), par)
                elif kind == "ownF":
                    chunk_full(d, t, nbps, bps, HF, B("hf"), par)
                chunk_update(d, nBps, bps, par)
            if last and KSUB == 4:
                raise _Stop()
            if kind == "ownF":
                oc = (src - 16) * 4 + t
                hm = PS[3][:, 0:512]
                S.pe(lambda e: e.matmul(hm, lhsT=identb, rhs=HF, start=True, stop=False), reads=[bCB, B("hf")], writes=[B("ps3")])
                S.pe(lambda e, oc=oc: e.matmul(hm, lhsT=jb, rhs=MIX[:, 15 - oc, 512:1024], start=False, stop=True), reads=[bCB, B("mixb%d" % (15 - oc))], writes=[B("ps3")])
                hm3 = hm.rearrange("p (a b) -> p a b", a=4)
                S.dve(lambda e: e.tensor_reduce(out=S1, in_=hm3, axis=AX.X, op=ALU.add), reads=[B("ps3")], writes=[B("s1")])
                S.act(lambda e: e.activation(out=HN, in_=hm, func=AF.Square), reads=[B("ps3")], writes=[B("hn")])
                S.dve(lambda e: e.tensor_reduce(out=S2, in_=HN.rearrange("p (a b) -> p a b", a=4), axis=AX.X, op=ALU.add), reads=[B("hn")], writes=[B("s2")])
                if last and KSUB == 5:
                    raise _Stop()
                S.dve(lambda e: e.tensor_scalar(out=MEAN, in0=S1, scalar1=1.0 / 128, scalar2=None, op0=ALU.mult), reads=[B("s1")], writes=[B("mean")])
                S.dve(lambda e: e.tensor_tensor(out=MSQ, in0=MEAN, in1=MEAN, op=ALU.mult), reads=[B("mean")], writes=[B("msq")])
                S.dve(lambda e: e.scalar_tensor_tensor(out=VAR, in0=S2, scalar=1.0 / 128, in1=MSQ, op0=ALU.mult, op1=ALU.subtract), reads=[B("s2"), B("msq")], writes=[B("var")])
                S.act(lambda e: e.activation(out=RSTD, in_=VAR, func=AF.Sqrt, bias=LN_EPS), reads=[B("var")], writes=[B("rstd")])
                S.dve(lambda e: e.reciprocal(out=RSTD, in_=RSTD), reads=[B("rstd")], writes=[B("rstd")])
                if last and KSUB == 6:
                    raise _Stop()
                for h in range(4):
                    S.dve(lambda e, h=h: e.tensor_scalar(out=HN[:, h * 128:(h + 1) * 128], in0=hm[:, h * 128:(h + 1) * 128], scalar1=MEAN[:, h:h + 1],
                                                         scalar2=RSTD[:, h:h + 1], op0=ALU.subtract, op1=ALU.mult),
                          reads=[B("ps3"), B("mean"), B("rstd"), B("s2")], writes=[B("hn")])
                S.pool(lambda e: e.tensor_tensor(out=HN, in0=HN, in1=MHW, op=ALU.mult), reads=[B("hn"), B("mhw")], writes=[B("hn")])
                S.dve(lambda e, oc=oc: e.tensor_tensor(out=MIX[:, oc, 0:512], in0=HN, in1=OSG, op=ALU.mult), reads=[B("hn"), B("osg")], writes=[B("mixa%d" % oc)])

    dd = dbg("mixa", [128, 16, 512], BF16)
    if dd is not None:
        S.dma(lambda e, dd=dd: e.dma_start(out=dd, in_=MIX[:, :, 0:512]), reads=[B("mixa%d" % i) for i in range(16)], writes=[B("dbgo3")])

    if STOP == 1:
        raise _Stop()
    for (src_, dst_, nm) in ((d_pu, d_pub, "pub"), (d_pv, d_pvb, "pvb")):
        for r in range(0, 16384, 2048):
            S.dma(lambda e, src_=src_, dst_=dst_, r=r: e.dma_start(out=dst_[r:r + 2048, :], in_=src_[r:r + 2048, :]), writes=[B("%s%d" % (nm, r))], q="pool")
    PB = [SQ.bitcast(BF16)[:, 0:512], SD.bitcast(BF16)[:, 0:512]]
    RDEN = [AR.alloc([128, 4]) for _ in range(2)]
    OTS = [HN, OSG]
    combo = 0
    for qg in range(4):
        for mcq in range(4):
            for half in range(2):
                hq = half * 4 + mcq
                r0 = half * 64
                ob = 4 + 2 * (combo % 2)
                eb = ob + 1
                def s_mm(kt):
                    sb = kt % 2
                    S.pe(lambda e: e.matmul(PS[sb][:, 0:512], lhsT=KAT[r0:r0 + 64, kt * 128:(kt + 1) * 128],
                                            rhs=QAT[r0:r0 + 64, mcq, qg * 512:(qg + 1) * 512], start=True, stop=True),
                         reads=[B("kat"), B("qat")], writes=[B("ps%d" % sb)])
                s_mm(0)
                for kt in range(66):
                    sb = kt % 2
                    if kt + 1 < 66:
                        s_mm(kt + 1)
                    S.act(lambda e: e.activation(out=PB[sb], in_=PS[sb][:, 0:512], func=AF.Exp, scale=0.125), reads=[B("ps%d" % sb)], writes=[B(("sq", "sd")[sb])])
                    S.pe(lambda e: e.matmul(PS[ob][0:65, 0:512], lhsT=VA[:, kt, half, :], rhs=PB[sb], start=(kt == 0), stop=(kt == 65)),
                         reads=[B(("sq", "sd")[sb]), B("va")], writes=[B("ps%d" % ob)])
                ots = OTS[combo % 2]
                bo = B(("hn", "osg")[combo % 2])
                S.act(lambda e: e.activation(out=ots[0:65, :], in_=PS[ob][0:65, 0:512], func=AF.Copy), reads=[B("ps%d" % ob)], writes=[bo])
                for qt in range(4):
                    S.pe(lambda e: e.matmul(PS[eb][:, qt * 65:(qt + 1) * 65], lhsT=ots[0:65, qt * 128:(qt + 1) * 128], rhs=ident[0:65, 0:65], start=True, stop=True),
                         reads=[bo, bC], writes=[B("ps%d" % eb)])
                rd = RDEN[combo % 2]
                brd = B("rden%d" % (combo % 2))
                S.dve(lambda e: e.reciprocal(out=rd, in_=PS[eb][:, 64:260:65]), reads=[B("ps%d" % eb)], writes=[brd])
                for qt in range(4):
                    oc = qg * 4 + qt
                    S.act(lambda e: e.activation(out=MIX[:, oc, 512 + hq * 64:512 + (hq + 1) * 64], in_=PS[eb][:, qt * 65:qt * 65 + 64], func=AF.Identity,
                                                 scale=rd[:, qt:qt + 1]),
                          reads=[B("ps%d" % eb), brd], writes=[B("att%d" % oc), B("mixb%d" % oc)])
                combo += 1

    dd = dbg("att", [128, 16, 512], BF16)
    if dd is not None:
        S.dma(lambda e, dd=dd: e.dma_start(out=dd, in_=MIX[:, :, 512:1024]), reads=[B("att%d" % i) for i in range(16)], writes=[B("dbgo2")])

    if STOP == 2:
        raise _Stop()
    AR.off = mark_persist
    S.barrier()

    WKB = AR.alloc([128, 8, 2048], BF16)
    WOB = AR.alloc([128, 8, 1024], BF16)
    GB = AR.alloc([128, 4, 1024])
    LNP = AR.alloc([128, 4, 1024])
    NSLOT = 8
    UGB = [AR.alloc([128, 1024], BF16) for _ in range(NSLOT)]
    XT = AR.alloc([128, 1024])
    R1 = AR.alloc([128, 1024])
    X1S = [AR.alloc([128, 1024]) for _ in range(2)]
    X1 = X1S[0]
    OUTT = AR.alloc([128, 1024])
    XM = AR.alloc([128, 1024])
    SC = AR.alloc([128, 2048])
    MIXT = AR.alloc([128, 8, 128], BF16)
    XMT = AR.alloc([128, 8, 128], BF16)
    TP1 = AR.alloc([128, 16, 16])
    IP1 = AR.alloc([128, 16, 16], U32)
    IP1F = AR.alloc([128, 16, 16])
    TMP = AR.alloc([128, 256])
    CAND = AR.alloc([128, 256])
    TP2 = AR.alloc([128, 8, 16])
    PP2 = AR.alloc([128, 8, 16], U32)
    PJ = AR.alloc([128, 2, 128], U32)
    PJF = AR.alloc([128, 2, 128])
    OH = AR.alloc([128, 2048])
    IDXF = AR.alloc([128, 2, 128])
    EIF = AR.alloc([128, 128])
    EIS = [AR.alloc([128, 128], I32) for _ in range(2)]
    GEX = AR.alloc([128, 8, 16])
    GSUM = AR.alloc([128, 8])
    DOT = AR.alloc([128, 128])
    COEF = AR.alloc([128, 128])
    DOT2 = AR.alloc([128, 128])
    IOTA = AR.alloc([128, 256])
    JUNK = AR.alloc([128, 1024])
    ST = AR.alloc([128, 8])
    print("phase3 arena bytes", AR.off)

    S.dma(lambda e: e.dma_start(out=LNP.rearrange("p a b -> p (a b)"), in_=d_lnp), writes=[B("lnp")])
    S.dma(lambda e: e.dma_start(out=IOTA, in_=d_iota), writes=[B("iota")], q="pool")
    for kc in range(8):
        st = (X1, XM)[kc % 2]
        bs = B(("x1_0", "xm")[kc % 2])
        S.dma(lambda e, st=st, kc=kc: e.dma_start(out=st, in_=d_wout[kc]), writes=[bs])
        S.act(lambda e, st=st, kc=kc: e.activation(out=WOB[:, kc, :], in_=st, func=AF.Copy), reads=[bs], writes=[B("wob")])
    KEYB = OH.bitcast(BF16)[:, 0:2048]
    S.dma(lambda e: e.dma_start(out=SC, in_=d_keysT), writes=[B("sc")])
    S.act(lambda e: e.activation(out=KEYB, in_=SC, func=AF.Copy), reads=[B("sc")], writes=[B("oh")])
    WQB = [XT.bitcast(BF16)[:, 0:1024], R1.bitcast(BF16)[:, 0:1024]]
    for blk in range(16):
        st = (X1, XM)[blk % 2]
        bs = B(("x1_0", "xm")[blk % 2])
        S.dma(lambda e, st=st, blk=blk: e.dma_start(out=st, in_=d_wqT[blk]), writes=[bs])
        S.act(lambda e, st=st, blk=blk: e.activation(out=WQB[blk % 2], in_=st, func=AF.Copy), reads=[bs], writes=[B(("xt", "r1")[blk % 2])])
        for kc in range(8):
            S.pe(lambda e, blk=blk, kc=kc: e.matmul(PS[kc % 2][:, 0:128], lhsT=WQB[blk % 2][:, kc * 128:(kc + 1) * 128], rhs=KEYB[:, blk * 128:(blk + 1) * 128],
                                                    start=True, stop=True),
                 reads=[B(("xt", "r1")[blk % 2]), B("oh")], writes=[B("ps%d" % (kc % 2))])
            if kc % 2 == 0:
                S.dve(lambda e, blk=blk, kc=kc: e.tensor_copy(out=WKB[:, kc, blk * 128:(blk + 1) * 128], in_=PS[kc % 2][:, 0:128]), reads=[B("ps%d" % (kc % 2))], writes=[B("wkb")])
            else:
                S.act(lambda e, blk=blk, kc=kc: e.activation(out=WKB[:, kc, blk * 128:(blk + 1) * 128], in_=PS[kc % 2][:, 0:128], func=AF.Copy),
                      reads=[B("ps%d" % (kc % 2))], writes=[B("wkb")])
    for vi, c0 in enumerate((16, 24, 32, 40)):
        for kc in range(8):
            S.dve(lambda e, c0=c0, kc=kc: e.tensor_scalar(out=JUNK[:, 0:128], in0=ident, scalar1=COLS[:, c0 + kc, 0:1], scalar2=None, op0=ALU.mult),
                  reads=[bC, B("cols")], writes=[B("junk")])
            S.pe(lambda e, kc=kc: e.matmul(PS[2 + kc % 2][:, 0:128], lhsT=ones, rhs=JUNK[:, 0:128], start=True, stop=True), reads=[bC, B("junk")], writes=[B("ps%d" % (2 + kc % 2))])
            if vi == 2:
                S.act(lambda e, vi=vi, kc=kc: e.activation(out=GB[:, vi, kc * 128:(kc + 1) * 128], in_=PS[2 + kc % 2][:, 0:128], func=AF.Identity, bias=1.0),
                      reads=[B("ps%d" % (2 + kc % 2))], writes=[B("gb")])
            else:
                S.act(lambda e, vi=vi, kc=kc: e.activation(out=GB[:, vi, kc * 128:(kc + 1) * 128], in_=PS[2 + kc % 2][:, 0:128], func=AF.Copy),
                      reads=[B("ps%d" % (2 + kc % 2))], writes=[B("gb")])

    def layer_norm(src, dst, gi, bi, bsrc, bdst):
        S.act(lambda e: e.activation(out=JUNK, in_=src, func=AF.Copy, accum_out=ST[:, 0:1]), reads=[bsrc], writes=[B("junk"), B("st")])
        S.act(lambda e: e.activation(out=JUNK, in_=src, func=AF.Square, accum_out=ST[:, 1:2]), reads=[bsrc], writes=[B("junk"), B("st")])
        S.dve(lambda e: e.tensor_scalar(out=ST[:, 2:3], in0=ST[:, 0:1], scalar1=1.0 / 1024, scalar2=None, op0=ALU.mult), reads=[B("st")], writes=[B("st")])
        S.dve(lambda e: e.tensor_tensor(out=ST[:, 3:4], in0=ST[:, 2:3], in1=ST[:, 2:3], op=ALU.mult), reads=[B("st")], writes=[B("st")])
        S.dve(lambda e: e.scalar_tensor_tensor(out=ST[:, 4:5], in0=ST[:, 1:2], scalar=1.0 / 1024, in1=ST[:, 3:4], op0=ALU.mult, op1=ALU.subtract), reads=[B("st")], writes=[B("st")])
        S.act(lambda e: e.activation(out=ST[:, 5:6], in_=ST[:, 4:5], func=AF.Sqrt, bias=LN_EPS), reads=[B("st")], writes=[B("st")])
        S.dve(lambda e: e.reciprocal(out=ST[:, 6:7], in_=ST[:, 5:6]), reads=[B("st")], writes=[B("st")])
        S.dve(lambda e: e.tensor_scalar(out=dst, in0=src, scalar1=ST[:, 2:3], scalar2=ST[:, 6:7], op0=ALU.subtract, op1=ALU.mult), reads=[bsrc, B("st")], writes=[bdst])
        S.pool(lambda e: e.tensor_tensor(out=dst, in0=dst, in1=LNP[:, gi, :], op=ALU.mult), reads=[bdst, B("lnp")], writes=[bdst])
        S.pool(lambda e: e.tensor_tensor(out=dst, in0=dst, in1=LNP[:, bi, :], op=ALU.add), reads=[bdst, B("lnp")], writes=[bdst])

    dd_x1 = dbg("x1", [2048, 1024])
    dd_y = dbg("y", [2048, 1024])
    gcount = [0]
    allpub = [B("pub%d" % r) for r in range(0, 16384, 2048)]
    allpvb = [B("pvb%d" % r) for r in range(0, 16384, 2048)]
    XMP = [PS[0], PS[1]]
    YP = [PS[2], PS[3]]
    def p3_front(tt):
        X1 = X1S[tt % 2]
        EI = EIS[tt % 2]
        S.dma(lambda e, tt=tt: e.dma_start(out=XT, in_=d_xown[tt * 128:(tt + 1) * 128, :]), writes=[B("xt")])
        for kc in range(8):
            S.pe(lambda e, kc=kc, tt=tt: e.matmul(PS[6 + kc // 4][:, (kc % 4) * 128:(kc % 4 + 1) * 128], lhsT=MIX[:, tt, kc * 128:(kc + 1) * 128], rhs=identb,
                                                  start=True, stop=True),
                 reads=[B("mixa%d" % tt), B("att%d" % tt), bCB], writes=[B("ps%d" % (6 + kc // 4))])
        for hf in range(2):
            S.act(lambda e, hf=hf: e.activation(out=MIXT[:, hf * 4:(hf + 1) * 4, :].rearrange("p a b -> p (a b)"), in_=PS[6 + hf][:, 0:512], func=AF.Copy),
                  reads=[B("ps%d" % (6 + hf))], writes=[B("mixt")])
        for hf in range(2):
            for kc in range(8):
                S.pe(lambda e, hf=hf, kc=kc: e.matmul(PS[4 + hf][:, 0:512], lhsT=MIXT[:, kc, :], rhs=WOB[:, kc, hf * 512:(hf + 1) * 512], start=(kc == 0), stop=(kc == 7)),
                     reads=[B("mixt"), B("wob")], writes=[B("ps%d" % (4 + hf))])
        for hf in range(2):
            S.dve(lambda e, hf=hf: e.tensor_tensor(out=R1[:, hf * 512:(hf + 1) * 512], in0=PS[4 + hf][:, 0:512], in1=GB[:, 0, hf * 512:(hf + 1) * 512], op=ALU.mult),
                  reads=[B("ps%d" % (4 + hf)), B("gb")], writes=[B("r1")])
        S.dve(lambda e: e.scalar_tensor_tensor(out=R1, in0=XT, scalar=ALPHA, in1=R1, op0=ALU.mult, op1=ALU.add), reads=[B("xt"), B("r1")], writes=[B("r1")])
        layer_norm(R1, X1, 0, 1, B("r1"), B("x1_%d" % (tt % 2)))
        if dd_x1 is not None:
            S.dma(lambda e, tt=tt: e.dma_start(out=dd_x1[tt * 128:(tt + 1) * 128, :], in_=X1), reads=[B("x1_%d" % (tt % 2))], writes=[B("dbgx%d" % tt)])
        S.dve(lambda e: e.tensor_tensor(out=XM, in0=X1, in1=GB[:, 2, :], op=ALU.mult), reads=[B("x1_%d" % (tt % 2)), B("gb")], writes=[B("xm")])
        S.dve(lambda e: e.tensor_tensor(out=XM, in0=XM, in1=GB[:, 1, :], op=ALU.add), reads=[B("xm"), B("gb")], writes=[B("xm")])
        for hf in range(2):
            S.act(lambda e, hf=hf: e.activation(out=XMP[hf][:, 0:512], in_=XM[:, hf * 512:(hf + 1) * 512], func=AF.Copy), reads=[B("xm")], writes=[B("ps%d" % hf)])
        for kc in range(8):
            S.pe(lambda e, kc=kc: e.matmul(PS[6 + kc // 4][:, (kc % 4) * 128:(kc % 4 + 1) * 128], lhsT=XM[:, kc * 128:(kc + 1) * 128], rhs=ident, start=True, stop=True),
                 reads=[B("xm"), bC], writes=[B("ps%d" % (6 + kc // 4))])
        for hf in range(2):
            S.act(lambda e, hf=hf: e.activation(out=XMT[:, hf * 4:(hf + 1) * 4, :].rearrange("p a b -> p (a b)"), in_=PS[6 + hf][:, 0:512], func=AF.Copy),
                  reads=[B("ps%d" % (6 + hf))], writes=[B("xmt")])
        for nb in range(4):
            for kc in range(8):
                S.pe(lambda e, nb=nb, kc=kc: e.matmul(PS[4 + nb][:, 0:512], lhsT=XMT[:, kc, :], rhs=WKB[:, kc, nb * 512:(nb + 1) * 512], start=(kc == 0), stop=(kc == 7)),
                     reads=[B("xmt"), B("wkb")], writes=[B("ps%d" % (4 + nb))])
            S.act(lambda e, nb=nb: e.activation(out=SC[:, nb * 512:(nb + 1) * 512], in_=PS[4 + nb][:, 0:512], func=AF.Copy), reads=[B("ps%d" % (4 + nb))], writes=[B("sc")])
        for blk in range(16):
            sc = SC[:, blk * 128:(blk + 1) * 128]
            S.dve(lambda e, blk=blk, sc=sc: e.max(out=TP1[:, blk, 0:8], in_=sc), reads=[B("sc")], writes=[B("tp1")])
            S.dve(lambda e, blk=blk, sc=sc: e.max_index(out=IP1[:, blk, 0:8], in_max=TP1[:, blk, 0:8], in_values=sc), reads=[B("sc"), B("tp1")], writes=[B("ip1")])
            S.dve(lambda e, blk=blk, sc=sc: e.match_replace(out=TMP[:, 0:128], in_to_replace=TP1[:, blk, 0:8], in_values=sc, imm_value=-1e30),
                  reads=[B("sc"), B("tp1")], writes=[B("tmp")])
            S.dve(lambda e, blk=blk: e.max(out=TP1[:, blk, 8:16], in_=TMP[:, 0:128]), reads=[B("tmp")], writes=[B("tp1")])
            S.dve(lambda e, blk=blk: e.max_index(out=IP1[:, blk, 8:16], in_max=TP1[:, blk, 8:16], in_values=TMP[:, 0:128]), reads=[B("tmp"), B("tp1")], writes=[B("ip1")])
        S.dve(lambda e: e.tensor_copy(out=IP1F, in_=IP1), reads=[B("ip1")], writes=[B("ip1f")])
        for h in range(8):
            S.dve(lambda e, h=h: e.tensor_tensor(out=CAND.rearrange("p (a b) -> p a b", a=16), in0=TP1[:, 2 * h, :].unsqueeze(2).to_broadcast([128, 16, 16]),
                                                 in1=TP1[:, 2 * h + 1, :].unsqueeze(1).to_broadcast([128, 16, 16]), op=ALU.add),
                  reads=[B("tp1")], writes=[B("cand")])
            S.dve(lambda e, h=h: e.max(out=TP2[:, h, 0:8], in_=CAND), reads=[B("cand")], writes=[B("tp2")])
            S.dve(lambda e, h=h: e.max_index(out=PP2[:, h, 0:8], in_max=TP2[:, h, 0:8], in_values=CAND), reads=[B("cand"), B("tp2")], writes=[B("pp2")])
            S.dve(lambda e, h=h: e.match_replace(out=TMP, in_to_replace=TP2[:, h, 0:8], in_values=CAND, imm_value=-1e30), reads=[B("cand"), B("tp2")], writes=[B("tmp")])
            S.dve(lambda e, h=h: e.max(out=TP2[:, h, 8:16], in_=TMP), reads=[B("tmp")], writes=[B("tp2")])
            S.dve(lambda e, h=h: e.max_index(out=PP2[:, h, 8:16], in_max=TP2[:, h, 8:16], in_values=TMP), reads=[B("tmp"), B("tp2")], writes=[B("pp2")])
        pp2f = PP2.rearrange("p a b -> p (a b)")
        S.dve(lambda e: e.tensor_single_scalar(out=PJ[:, 0, :], in_=pp2f, scalar=4, op=ALU.logical_shift_right), reads=[B("pp2")], writes=[B("pj")])
        S.dve(lambda e: e.tensor_single_scalar(out=PJ[:, 1, :], in_=pp2f, scalar=15, op=ALU.bitwise_and), reads=[B("pp2")], writes=[B("pj")])
        S.dve(lambda e: e.tensor_copy(out=PJF, in_=PJ), reads=[B("pj")], writes=[B("pjf")])
        for p in range(2):
            for h in range(8):
                oh = OH[:, h * 256:(h + 1) * 256].rearrange("p (a b) -> p a b", a=16)
                S.dve(lambda e, p=p, h=h, oh=oh: e.tensor_tensor(out=oh, in0=IOTA.rearrange("p (a b) -> p a b", a=16),
                                                                 in1=PJF[:, p, h * 16:(h + 1) * 16].unsqueeze(2).to_broadcast([128, 16, 16]), op=ALU.is_equal),
                      reads=[B("iota"), B("pjf")], writes=[B("oh")])
                S.dve(lambda e, p=p, h=h, oh=oh: e.tensor_tensor(out=oh, in0=oh, in1=IP1F[:, 2 * h + p, :].unsqueeze(1).to_broadcast([128, 16, 16]), op=ALU.mult),
                      reads=[B("oh"), B("ip1f")], writes=[B("oh")])
            S.dve(lambda e, p=p: e.tensor_reduce(out=IDXF[:, p, :], in_=OH.rearrange("p (a b) -> p a b", a=128), axis=AX.X, op=ALU.add), reads=[B("oh")], writes=[B("idxf")])
        S.dve(lambda e: e.scalar_tensor_tensor(out=EIF, in0=IDXF[:, 0, :], scalar=128.0, in1=IDXF[:, 1, :], op0=ALU.mult, op1=ALU.add), reads=[B("idxf")], writes=[B("eif")])
        S.dve(lambda e: e.tensor_copy(out=EI, in_=EIF), reads=[B("eif")], writes=[B("ei_%d" % (tt % 2))])
        S.dve(lambda e: e.tensor_tensor(out=GEX, in0=TP2, in1=TP2[:, :, 0:1].to_broadcast([128, 8, 16]), op=ALU.subtract), reads=[B("tp2")], writes=[B("gex")])
        S.act(lambda e: e.activation(out=GEX, in_=GEX, func=AF.Exp), reads=[B("gex")], writes=[B("gex")])
        S.dve(lambda e: e.tensor_reduce(out=GSUM, in_=GEX, axis=AX.X, op=ALU.add), reads=[B("gex")], writes=[B("gsum")])
        S.dve(lambda e: e.reciprocal(out=GSUM, in_=GSUM), reads=[B("gsum")], writes=[B("gsum")])
        S.dve(lambda e: e.tensor_tensor(out=GEX, in0=GEX, in1=GSUM.unsqueeze(2).to_broadcast([128, 8, 16]), op=ALU.mult), reads=[B("gex"), B("gsum")], writes=[B("gex")])
    def p3_u(tt):
        X1 = X1S[tt % 2]
        EI = EIS[tt % 2]
        for sl in range(128):
            ug = UGB[gcount[0] % NSLOT]
            bu = B("ugb%d" % (gcount[0] % NSLOT))
            gcount[0] += 1
            S.dma(lambda e, ug=ug, sl=sl: e.indirect_dma_start(out=ug, out_offset=None, in_=d_pub,
                                                                in_offset=bass.IndirectOffsetOnAxis(ap=EI[:, sl:sl + 1], axis=0)),
                  reads=[B("ei_%d" % (tt % 2))] + allpub, writes=[bu], q="pool")
            for hf in range(2):
                S.dve(lambda e, ug=ug, hf=hf, sl=sl: e.scalar_tensor_tensor(out=JUNK[:, hf * 512:(hf + 1) * 512], in0=ug[:, hf * 512:(hf + 1) * 512], scalar=1.0,
                                                                            in1=XMP[hf][:, 0:512], op0=ALU.mult, op1=ALU.mult,
                                                                            accum_out=DOT[:, sl:sl + 1] if hf == 0 else DOT2[:, sl:sl + 1]),
                      reads=[bu, B("ps%d" % hf)], writes=[B("junk"), B("dot")])
        S.dve(lambda e: e.tensor_tensor(out=DOT, in0=DOT, in1=DOT2, op=ALU.add), reads=[B("dot")], writes=[B("dot")])
        S.act(lambda e: e.activation(out=DOT, in_=DOT, func=AF.Gelu), reads=[B("dot")], writes=[B("dot")])
        S.dve(lambda e: e.tensor_tensor(out=COEF, in0=DOT, in1=GEX.rearrange("p a b -> p (a b)"), op=ALU.mult), reads=[B("dot"), B("gex")], writes=[B("coef")])
    def p3_v(tt):
        X1 = X1S[tt % 2]
        EI = EIS[tt % 2]
        for hf in range(2):
            S.dve(lambda e, hf=hf: e.memset(YP[hf][:, 0:512], 0.0), writes=[B("ps%d" % (2 + hf))])
        for sl in range(128):
            ug = UGB[gcount[0] % NSLOT]
            bu = B("ugb%d" % (gcount[0] % NSLOT))
            gcount[0] += 1
            S.dma(lambda e, ug=ug, sl=sl: e.indirect_dma_start(out=ug, out_offset=None, in_=d_pvb,
                                                                in_offset=bass.IndirectOffsetOnAxis(ap=EI[:, sl:sl + 1], axis=0)),
                  reads=[B("ei_%d" % (tt % 2))] + allpvb, writes=[bu], q="pool")
            for hf in range(2):
                S.dve(lambda e, ug=ug, hf=hf, sl=sl: e.scalar_tensor_tensor(out=YP[hf][:, 0:512], in0=ug[:, hf * 512:(hf + 1) * 512], scalar=COEF[:, sl:sl + 1],
                                                                            in1=YP[hf][:, 0:512], op0=ALU.mult, op1=ALU.add),
                      reads=[bu, B("coef"), B("ps%d" % (2 + hf))], writes=[B("ps%d" % (2 + hf))])
    def p3_back(tt):
        X1 = X1S[tt % 2]
        EI = EIS[tt % 2]
        if dd_y is not None:
            for hf in range(2):
                S.act(lambda e, hf=hf: e.activation(out=JUNK[:, hf * 512:(hf + 1) * 512], in_=YP[hf][:, 0:512], func=AF.Copy), reads=[B("ps%d" % (2 + hf))], writes=[B("junk")])
            S.dma(lambda e, tt=tt: e.dma_start(out=dd_y[tt * 128:(tt + 1) * 128, :], in_=JUNK), reads=[B("junk")], writes=[B("dbgy%d" % tt)])
        for hf in range(2):
            S.dve(lambda e, hf=hf: e.tensor_tensor(out=R1[:, hf * 512:(hf + 1) * 512], in0=YP[hf][:, 0:512], in1=GB[:, 3, hf * 512:(hf + 1) * 512], op=ALU.mult),
                  reads=[B("ps%d" % (2 + hf)), B("gb")], writes=[B("r1")])
        S.dve(lambda e: e.scalar_tensor_tensor(out=R1, in0=X1, scalar=ALPHA, in1=R1, op0=ALU.mult, op1=ALU.add), reads=[B("x1_%d" % (tt % 2)), B("r1")], writes=[B("r1")])
        layer_norm(R1, OUTT, 2, 3, B("r1"), B("outt"))
        S.dma(lambda e, tt=tt: e.dma_start(out=d_out[tt * 128:(tt + 1) * 128, :], in_=OUTT), reads=[B("outt")], writes=[B("out%d" % tt)])


    p3_front(0)
    for tt in range(16):
        p3_u(tt)
        S.fork()
        lists = []
        if tt + 1 < 16:
            p3_front(tt + 1)
            lists.append(S.take())
        p3_v(tt)
        lists.append(S.take())
        S.join(lists)
        p3_back(tt)
def _win(xb, lo, n, rev):
    L = xb.shape[0]
    w = np.zeros((n + 2, 1024), np.float32)
    a, b = lo - 1, lo + n + 1
    sa, sb = max(a, 0), min(b, L)
    w[sa - a:sb - a] = xb[sa:sb]
    if rev:
        w = w[::-1]
    return np.ascontiguousarray(w.reshape(n + 2, 8, 128).transpose(2, 1, 0))


def _rope_tables(tok):
    half = 16
    freq = (10000.0 ** (-np.arange(half, dtype=np.float32) / half)).astype(np.float32)
    row = (tok // 64).astype(np.float32)
    col = (tok % 64).astype(np.float32)
    cos = np.zeros((64, len(tok)), np.float32)
    sin = np.zeros((64, len(tok)), np.float32)
    for dd in range(64):
        pos = row if dd < 32 else col
        ang = pos * freq[dd % 16]
        cos[dd] = np.cos(ang)
        sin[dd] = np.sin(ang)
    return np.concatenate([cos, cos], 0), np.concatenate([sin, sin], 0)


_CACHE = {}


def kernel(x, c, ctx, c_ctx, w_mod, b_mod, w_in, conv_w, conv_b, b_gates, mh_norm_w, q_norm_w, k_norm_w, w_out,
           ln1_g, ln1_b, peer_wq, peer_keys, peer_u, peer_v, ln2_g, ln2_b):
    f32 = np.float32
    x = np.asarray(x, f32); c = np.asarray(c, f32); ctx = np.asarray(ctx, f32); c_ctx = np.asarray(c_ctx, f32)
    w_mod = np.asarray(w_mod, f32)[0]; b_mod = np.asarray(b_mod, f32)[0]; w_in = np.asarray(w_in, f32)[0]
    conv_w = np.asarray(conv_w, f32)[0]; conv_b = np.asarray(conv_b, f32)[0]; b_gates = np.asarray(b_gates, f32)[0]
    mh_norm_w = np.asarray(mh_norm_w, f32)[0]; q_norm_w = np.asarray(q_norm_w, f32)[0]; k_norm_w = np.asarray(k_norm_w, f32)[0]
    w_out = np.asarray(w_out, f32)[0]; ln1_g = np.asarray(ln1_g, f32)[0]; ln1_b = np.asarray(ln1_b, f32)[0]
    peer_wq = np.asarray(peer_wq, f32)[0]; peer_keys = np.asarray(peer_keys, f32)[0]
    peer_u = np.asarray(peer_u, f32)[0]; peer_v = np.asarray(peer_v, f32)[0]
    ln2_g = np.asarray(ln2_g, f32)[0]; ln2_b = np.asarray(ln2_b, f32)[0]

    if "nc" not in _CACHE:
        _CACHE["nc"] = build_program()
    nc, dbg_names = _CACHE["nc"]

    rep = lambda v: np.ascontiguousarray(np.broadcast_to(np.asarray(v, f32).reshape(1, -1), (128, np.asarray(v).size)))
    wmod_l = np.ascontiguousarray(w_mod.reshape(8, 128, 12, 512).transpose(2, 1, 0, 3))
    bmod_l = np.ascontiguousarray(np.broadcast_to(b_mod.reshape(12, 1, 512), (12, 2, 512)))
    qa_cols = []
    for mc in range(4):
        qa_cols += list(range(2064 + mc * 64, 2064 + (mc + 1) * 64)) + list(range(2064 + (4 + mc) * 64, 2064 + (5 + mc) * 64))
    perm = (list(range(512, 1024)) + list(range(0, 512)) + list(range(2576, 2704)) + qa_cols + list(range(1024, 1536))
            + list(range(2704, 2832)) + list(range(2048, 2064)) + list(range(1536, 2048)))
    win_l = np.ascontiguousarray(w_in[:, perm].reshape(8, 128, 2832))
    convb_l = np.ascontiguousarray(conv_b.reshape(8, 128).T)
    cw = conv_w.reshape(3, 8, 128)
    taps_nat = np.ascontiguousarray(cw.transpose(2, 1, 0))
    taps_rev = np.ascontiguousarray(taps_nat[:, :, ::-1])
    qtaps_l = np.ascontiguousarray(np.stack([taps_nat[:, 0:4], taps_rev[:, 0:4]], 1).reshape(128, 24))
    bg_l = rep(b_gates)
    mhw_l = rep(mh_norm_w)
    nw_l = np.ascontiguousarray(np.stack([np.tile(q_norm_w, 2), np.tile(k_norm_w, 2)], 1))
    wout_l = np.ascontiguousarray(w_out.reshape(8, 128, 1024))
    wqT_l = np.ascontiguousarray(peer_wq.T.reshape(16, 128, 1024))
    keysT_l = np.ascontiguousarray(peer_keys.reshape(16, 128, 128).transpose(2, 0, 1).reshape(128, 2048))
    lnp_l = np.ascontiguousarray(np.concatenate([rep(ln1_g), rep(ln1_b), rep(ln2_g), rep(ln2_b)], 1))
    ii = np.arange(128)
    ident = np.eye(128, dtype=f32)
    tri = (ii[:, None] <= ii[None, :]).astype(f32)
    mask_sr = np.where(ii[None, :] <= ii[:, None], 0.0, NEG).astype(f32)
    mask_rs = np.where(ii[:, None] <= ii[None, :], 0.0, NEG).astype(f32)
    jm = ident[::-1].copy()
    blk64 = (ii[:, None] // 64 == ii[None, :] // 64).astype(f32)
    R = np.zeros((128, 128), f32)
    for i in range(128):
        if (i % 32) < 16:
            R[i, i + 16] = -1.0
        else:
            R[i, i - 16] = 1.0
    sel0 = np.zeros((128, 128), f32)
    sel0[0, :] = 1.0
    cst_l = np.ascontiguousarray(np.concatenate([ident, np.ones((128, 128), f32), tri, mask_sr, mask_rs, jm, blk64, R.T.copy(), sel0], 1))
    iota_l = np.ascontiguousarray(np.broadcast_to(np.tile(np.arange(16, dtype=f32), 16).reshape(1, 256), (128, 256)))

    in_maps = []
    for core in range(NCORES):
        b, j = divmod(core, 4)
        xb = x[b]
        cwin = np.stack([_win(ctx[b], 0, 256, False), _win(ctx[b], 0, 256, True)], 0)
        oth = [(G, False) for G in range(0, 4 * j)] + [(G, True) for G in range(15, 4 * j + 3, -1)]
        ownB = [(G, True) for G in range(4 * j + 3, 4 * j - 1, -1)]
        ownF = [(G, False) for G in range(4 * j, 4 * j + 4)]
        srcs = oth + ownB + ownF
        assert len(oth) == 12 and len(srcs) == NGRP
        xwin = np.stack([_win(xb, 512 * G, 512, rv) for (G, rv) in srcs], 0)
        ktaps = np.zeros((128, 22, 4, 3), f32)
        flags = np.zeros((128, 22, 2), f32)
        ktaps[:, 0] = taps_nat[:, 4:8]
        ktaps[:, 1] = taps_rev[:, 4:8]
        for gi, (G, rv) in enumerate(srcs):
            ktaps[:, 2 + gi] = (taps_rev if rv else taps_nat)[:, 4:8]
            fl = [0.0 if G == 0 else 1.0, 0.0 if G == 15 else 1.0]
            flags[:, 2 + gi] = fl[::-1] if rv else fl
        gmask = np.zeros((128, 12, 4), f32)
        for gi, (G, rv) in enumerate(oth):
            fwd_real = not rv
            gmask[:, gi] = [1.0, 0.0, 0.0, NEG] if fwd_real else [0.0, NEG, 1.0, 0.0]
        rope = np.zeros((16, 2, 128, 512), f32)
        for ri, (G, rv) in enumerate(oth + ownF):
            tok = np.arange(512 * G, 512 * G + 512)
            if rv:
                tok = tok[::-1]
            cs, sn = _rope_tables(tok)
            rope[ri, 0] = cs
            rope[ri, 1] = sn
        cvec = np.ascontiguousarray(np.stack([c[b].reshape(8, 128).T, c_ctx.reshape(8, 128).T], 2).reshape(128, 16))
        in_maps.append(dict(
            cwin=cwin, xwin=xwin, xown=np.ascontiguousarray(xb[2048 * j:2048 * (j + 1)]), wmod=wmod_l, bmod=bmod_l, cvec=cvec,
            win=win_l, ktaps=np.ascontiguousarray(ktaps.reshape(128, 264)), qtaps=qtaps_l, convb=convb_l,
            flags=np.ascontiguousarray(flags.reshape(128, 44)), gmask=np.ascontiguousarray(gmask.reshape(128, 48)), bg=bg_l, mhw=mhw_l, nw=nw_l,
            wout=wout_l, wqT=wqT_l, keysT=keysT_l, lnp=lnp_l, pu=peer_u, pv=peer_v, cst=cst_l, iota16=iota_l, rope=rope))
    res = run_bass_kernel_spmd(nc, in_maps[:NCORES], core_ids=list(range(NCORES)))
    out = np.zeros((2, 8192, 1024), f32)
    for core in range(NCORES):
        b, j = divmod(core, 4)
        out[b, 2048 * j:2048 * (j + 1)] = res.results[core]["out"]
    if dbg_names:
        _CACHE["dbg"] = [{n: res.results[core]["dbg_" + n] for n in dbg_names} for core in range(NCORES)]
    return out
```
